# Optimizing a Trainium2 kernel written in Bass

```python
import jax
import jax.numpy as jnp
from jax import lax
import numpy as np

D_MODEL = 1024
BATCH = 16
SEQ = 2048
DEPTH = 4

GRID_W = 64
CTX_LEN = 256
D_A = D_MODEL // 2
D_B = D_MODEL // 2
HEAD_B = 64
H_B = D_B // HEAD_B
R_W = 64
R_A = 64
R_G = 128
N_GROUPS = 4
EXP_PER_GROUP = 8
N_EXPERTS = N_GROUPS * EXP_PER_GROUP
TOP_K = 2
D_FF_EXPERT = D_MODEL // 2
MOE_BLOCK = 128
NORM_EPS = 1e-6
GN_EPS = 64e-5
RW_COLS = 3 * D_B + 2 * R_W + 2 * R_A + R_G
RW_START = 3 * D_A
N_IN = RW_START + RW_COLS + 2 * D_MODEL
IN_SPLITS = (D_A, 2 * D_A, RW_START, RW_START + RW_COLS, RW_START + RW_COLS + D_MODEL)
RW_SPLITS = (D_B, 2 * D_B, 3 * D_B, 3 * D_B + R_W, 3 * D_B + 2 * R_W,
             3 * D_B + 2 * R_W + R_A, 3 * D_B + 2 * R_W + 2 * R_A)

kernel_name = "hybrid_conv_rwkv7_hmoe_diffusion_trunk"


def _rmsnorm(x, g):
    xf = x.astype(jnp.float32)
    y = xf * lax.rsqrt(jnp.mean(xf * xf, axis=-1, keepdims=True) + NORM_EPS)
    return y.astype(x.dtype) * g


def _dwconv3(u, w, axis):
    pad = [(0, 0)] * u.ndim
    pad[axis] = (1, 1)
    up = jnp.pad(u, pad)
    n = u.shape[axis]
    sl = lambda i: lax.slice_in_dim(up, i, i + n, axis=axis)
    return sl(0) * w[0] + sl(1) * w[1] + sl(2) * w[2]


def _conv_grid(u, w):
    bsz, s, ch = u.shape
    rows = s // GRID_W
    g = u.reshape(bsz, rows, GRID_W, ch)
    half = ch // 2
    horiz = _dwconv3(g[..., :half], w[:, :half], axis=2)
    vert = _dwconv3(g[..., half:], w[:, half:], axis=1)
    return jnp.concatenate([horiz, vert], axis=-1).reshape(bsz, s, ch)


def _token_shift(z, mu):
    zp = jnp.pad(z, ((0, 0), (1, 1), (0, 0)))
    return z + mu * (0.5 * (zp[:, :-2] + zp[:, 2:]) - z)


def _rwkv_prep(zrw, lp):
    bsz, t, _ = zrw.shape
    hd = lambda u: u.astype(jnp.float32).reshape(bsz, t, H_B, HEAD_B)
    r, k, v, zwf, zwb, zaf, zab, zg = jnp.split(zrw, RW_SPLITS, axis=-1)
    kk = hd(k * lp["k_k"])
    kk = kk * lax.rsqrt(jnp.sum(kk * kk, axis=-1, keepdims=True) + 1e-12)
    k_a = lp["k_a"].astype(jnp.float32).reshape(H_B, HEAD_B)
    kh = hd(k)
    dirs = []
    for d, (zw, za) in enumerate(((zwf, zaf), (zwb, zab))):
        wl = -jax.nn.softplus(-hd(lp["w0"][d] + jnp.tanh(zw) @ lp["w_up"][d])) - 0.5
        a = jax.nn.sigmoid(hd(lp["a0"][d] + za @ lp["a_up"][d]))
        kd = kh * (1.0 + (a - 1.0) * k_a)
        dirs.append((jnp.exp(-jnp.exp(wl)), kd, kk * a))
    return hd(r), hd(v), kk, dirs, zg


def _wkv_scan(s0, r, decay, k, v, kk, b, reverse):
    def step(s, inp):
        w_t, k_t, v_t, kk_t, b_t = inp[:5]
        sa = jnp.einsum("bhvk,bhk->bhv", s, kk_t)
        s = (s * w_t[:, :, None, :] - sa[..., None] * b_t[:, :, None, :]
             + v_t[..., None] * k_t[:, :, None, :])
        if r is None:
            return s, None
        return s, jnp.einsum("bhvk,bhk->bhv", s, inp[5])
    seqs = (decay, k, v, kk, b) + (() if r is None else (r,))
    xs = tuple(jnp.swapaxes(u, 0, 1) for u in seqs)
    s_fin, ys = lax.scan(step, s0, xs, reverse=reverse)
    return s_fin, (None if r is None else jnp.swapaxes(ys, 0, 1))


def _rwkv_readout(y, r, v, kds, zg, lp):
    bsz, t = y.shape[:2]
    mu = jnp.mean(y, axis=-1, keepdims=True)
    var = jnp.mean(jnp.square(y - mu), axis=-1, keepdims=True)
    yn = ((y - mu) * lax.rsqrt(var + GN_EPS)).reshape(bsz, t, D_B) * lp["lnx_g"] + lp["lnx_b"]
    rk = lp["r_k"].astype(jnp.float32)
    bonus = sum(jnp.sum(r * kd * rk, axis=-1, keepdims=True) for kd in kds) * v
    g = jax.nn.sigmoid(zg) @ lp["g_up"]
    out = ((yn + bonus.reshape(bsz, t, D_B)) * g).astype(zg.dtype)
    return out @ lp["w_b_out"]


def _token_mixer(h_lat, h_ctx, lp, with_ctx_out):
    bsz = h_lat.shape[0]
    bg, cg, ha, rw, ga, gb = jnp.split(h_lat @ lp["w_in"], IN_SPLITS, axis=-1)
    if with_ctx_out:
        bgc, cgc, hac, rwc, gac, gbc = jnp.split(h_ctx @ lp["w_in"], IN_SPLITS, axis=-1)
    else:
        rwc = h_ctx @ lp["w_in"][:, RW_START:RW_START + RW_COLS]
    r_l, v_l, kk_l, dirs_l, zg_l = _rwkv_prep(_token_shift(rw, lp["shift_mu"]), lp)
    r_c, v_c, kk_c, dirs_c, zg_c = _rwkv_prep(_token_shift(rwc, lp["shift_mu"]), lp)
    s0 = jnp.zeros((bsz, H_B, HEAD_B, HEAD_B), jnp.float32)
    y_l = 0.0
    y_c = 0.0
    for d in range(2):
        dec_c, k_c, b_c = dirs_c[d]
        s_c, yc_d = _wkv_scan(s0, r_c if with_ctx_out else None, dec_c, k_c, v_c, kk_c, b_c, d == 1)
        dec_l, k_l, b_l = dirs_l[d]
        _, yl_d = _wkv_scan(s_c, r_l, dec_l, k_l, v_l, kk_l, b_l, d == 1)
        y_l = y_l + yl_d
        if with_ctx_out:
            y_c = y_c + yc_d
    ya = (bg * _conv_grid(cg * ha, lp["conv_w"])) @ lp["w_a_out"]
    yb = _rwkv_readout(y_l, r_l, v_l, [dl[1] for dl in dirs_l], zg_l, lp)
    y_lat = (jax.nn.sigmoid(ga) * ya + jax.nn.sigmoid(gb) * yb) @ lp["w_o"]
    if not with_ctx_out:
        return y_lat, None
    yac = (bgc * _dwconv3(cgc * hac, lp["conv_w"], axis=1)) @ lp["w_a_out"]
    ybc = _rwkv_readout(y_c, r_c, v_c, [dc[1] for dc in dirs_c], zg_c, lp)
    y_ctx = (jax.nn.sigmoid(gac) * yac + jax.nn.sigmoid(gbc) * ybc) @ lp["w_o"]
    return y_lat, y_ctx


def _hier_moe(h, lp):
    n, dm = h.shape
    lg = (h @ lp["router_g"] + lp["router_g_b"]).astype(jnp.float32)
    p_g = jax.nn.softmax(lg, axis=-1)
    g_sel = jnp.argmax(lg, axis=-1).astype(jnp.int32)
    le = (h @ lp["router_e"] + lp["router_e_b"]).astype(jnp.float32).reshape(n, N_GROUPS, EXP_PER_GROUP)
    le = jnp.take_along_axis(le, jnp.broadcast_to(g_sel[:, None, None], (n, 1, EXP_PER_GROUP)), axis=1)[:, 0]
    top_p, top_i = lax.top_k(jax.nn.softmax(le, axis=-1), TOP_K)
    gate = jnp.max(p_g, axis=-1, keepdims=True) * top_p / jnp.sum(top_p, axis=-1, keepdims=True)
    expert = (g_sel[:, None] * EXP_PER_GROUP + top_i).reshape(-1).astype(jnp.int32)
    m = n * TOP_K
    token = jnp.repeat(jnp.arange(n, dtype=jnp.int32), TOP_K)
    order = jnp.argsort(expert)
    e_sorted = expert[order]
    counts = jnp.bincount(expert, length=N_EXPERTS)
    padded = (counts + MOE_BLOCK - 1) // MOE_BLOCK * MOE_BLOCK
    pad_end = jnp.cumsum(padded)
    dest = (pad_end - padded)[e_sorted] + jnp.arange(m) - (jnp.cumsum(counts) - counts)[e_sorted]
    n_blocks = -(-m // MOE_BLOCK) + N_EXPERTS
    slot_tok = jnp.full((n_blocks * MOE_BLOCK,), n, jnp.int32).at[dest].set(token[order])
    slot_gate = jnp.zeros((n_blocks * MOE_BLOCK,), jnp.float32).at[dest].set(gate.reshape(-1)[order])
    block_exp = jnp.minimum(
        jnp.searchsorted(pad_end, jnp.arange(n_blocks) * MOE_BLOCK, side="right"), N_EXPERTS - 1)
    h_pad = jnp.concatenate([h, jnp.zeros((1, dm), h.dtype)], axis=0)

    def expert_block(args):
        tok, e = args
        xb = h_pad[tok]
        hid = jax.nn.silu(xb @ lp["exp_w1"][e]) * (xb @ lp["exp_w3"][e])
        return hid @ lp["exp_w2"][e]

    yb = lax.map(expert_block, (slot_tok.reshape(n_blocks, MOE_BLOCK), block_exp))
    yb = yb.reshape(-1, dm) * slot_gate[:, None].astype(h.dtype)
    return jnp.zeros((n + 1, dm), h.dtype).at[slot_tok].add(yb)[:n]


def setup_inputs(seed: int = 0) -> dict:
    key = jax.random.key(seed)
    ks = iter(jax.random.split(key, 40))
    nrm = lambda shape, s: jax.random.normal(next(ks), shape, jnp.float32) * s
    L, D = DEPTH, D_MODEL
    return {
        "x": nrm((BATCH, SEQ, D), 1.0),
        "c": nrm((BATCH, D), 1.0),
        "ctx": nrm((BATCH, CTX_LEN, D), 1.0),
        "c_ctx": nrm((D,), 1.0),
        "w_mod": nrm((L, D, 6 * D), 0.5 * D ** -0.5),
        "b_mod": nrm((L, 6 * D), 0.02),
        "norm1_g": 1.0 + nrm((L, D), 0.02),
        "norm2_g": 1.0 + nrm((L, D), 0.02),
        "w_in": nrm((L, D, N_IN), D ** -0.5),
        "shift_mu": jax.random.uniform(next(ks), (L, RW_COLS), jnp.float32),
        "conv_w": nrm((L, 3, D_A), 3 ** -0.5),
        "w_up": nrm((L, 2, R_W, D_B), 0.1 * R_W ** -0.5),
        "w0": jax.random.uniform(next(ks), (L, 2, D_B), jnp.float32, -6.5, -1.5),
        "a_up": nrm((L, 2, R_A, D_B), 0.5 * R_A ** -0.5),
        "a0": nrm((L, 2, D_B), 0.1),
        "g_up": nrm((L, R_G, D_B), R_G ** -0.5),
        "k_k": 0.85 + nrm((L, D_B), 0.02),
        "k_a": 1.0 + nrm((L, D_B), 0.02),
        "r_k": nrm((L, H_B, HEAD_B), 0.1),
        "lnx_g": 1.0 + nrm((L, D_B), 0.02),
        "lnx_b": nrm((L, D_B), 0.02),
        "w_a_out": nrm((L, D_A, D), D_A ** -0.5),
        "w_b_out": nrm((L, D_B, D), D_B ** -0.5),
        "w_o": nrm((L, D, D), D ** -0.5),
        "router_g": nrm((L, D, N_GROUPS), D ** -0.5),
        "router_g_b": nrm((L, N_GROUPS), 0.01),
        "router_e": nrm((L, D, N_EXPERTS), D ** -0.5),
        "router_e_b": nrm((L, N_EXPERTS), 0.01),
        "exp_w1": nrm((L, N_EXPERTS, D, D_FF_EXPERT), D ** -0.5),
        "exp_w3": nrm((L, N_EXPERTS, D, D_FF_EXPERT), D ** -0.5),
        "exp_w2": nrm((L, N_EXPERTS, D_FF_EXPERT, D), D_FF_EXPERT ** -0.5),
        "final_g": 1.0 + nrm((D,), 0.02),
    }


def reference(x, c, ctx, c_ctx, w_mod, b_mod, norm1_g, norm2_g, w_in, shift_mu, conv_w,
              w_up, w0, a_up, a0, g_up, k_k, k_a, r_k, lnx_g, lnx_b, w_a_out, w_b_out, w_o,
              router_g, router_g_b, router_e, router_e_b, exp_w1, exp_w3, exp_w2, final_g):
    dt = x.dtype
    dm = x.shape[-1]
    xc = ctx
    s_lat = jax.nn.silu(c)
    s_ctx = jax.nn.silu(c_ctx)
    for layer in range(DEPTH):
        last = layer == DEPTH - 1
        lp = {
            "w_in": w_in[layer], "shift_mu": shift_mu[layer], "conv_w": conv_w[layer],
            "w_up": w_up[layer], "w0": w0[layer], "a_up": a_up[layer], "a0": a0[layer],
            "g_up": g_up[layer], "k_k": k_k[layer], "k_a": k_a[layer], "r_k": r_k[layer],
            "lnx_g": lnx_g[layer], "lnx_b": lnx_b[layer], "w_a_out": w_a_out[layer],
            "w_b_out": w_b_out[layer], "w_o": w_o[layer],
            "router_g": router_g[layer], "router_g_b": router_g_b[layer],
            "router_e": router_e[layer], "router_e_b": router_e_b[layer],
            "exp_w1": exp_w1[layer], "exp_w3": exp_w3[layer], "exp_w2": exp_w2[layer],
        }
        sh1, sc1, g1, sh2, sc2, g2 = [m[:, None, :] for m in jnp.split(s_lat @ w_mod[layer] + b_mod[layer], 6, axis=-1)]
        ch1, cs1, cg1, ch2, cs2, cg2 = jnp.split(s_ctx @ w_mod[layer] + b_mod[layer], 6, axis=-1)
        h_lat = _rmsnorm(x, norm1_g[layer]) * (1.0 + sc1) + sh1
        h_ctx = _rmsnorm(xc, norm1_g[layer]) * (1.0 + cs1) + ch1
        y_lat, y_ctx = _token_mixer(h_lat, h_ctx, lp, not last)
        x = x + (g1 * y_lat).astype(dt)
        h2_lat = _rmsnorm(x, norm2_g[layer]) * (1.0 + sc2) + sh2
        if last:
            y2 = _hier_moe(h2_lat.reshape(-1, dm), lp).reshape(x.shape)
            x = x + (g2 * y2).astype(dt)
        else:
            xc = xc + (cg1 * y_ctx).astype(dt)
            h2_ctx = _rmsnorm(xc, norm2_g[layer]) * (1.0 + cs2) + ch2
            nc = h2_ctx.shape[0] * h2_ctx.shape[1]
            y2 = _hier_moe(jnp.concatenate([h2_ctx.reshape(-1, dm), h2_lat.reshape(-1, dm)], axis=0), lp)
            xc = xc + (cg2 * y2[:nc].reshape(xc.shape)).astype(dt)
            x = x + (g2 * y2[nc:].reshape(x.shape)).astype(dt)
    return _rmsnorm(x, final_g)
```

```python
import contextlib
import numpy as np
import concourse.bass as bass
import concourse.mybir as mybir
from concourse.bass_utils import run_bass_kernel_spmd

F32 = mybir.dt.float32
BF16 = mybir.dt.bfloat16
I32 = mybir.dt.int32
AF = mybir.ActivationFunctionType
ALU = mybir.AluOpType
AX = mybir.AxisListType

D = 1024
DEPTH = 4
NB = 2
TCTX = 256
TLAT = 2048
T = TCTX + TLAT
NT = T // 128
NIN = 5504
RW0 = 1536
NEXP = 32
DFF = 512
CAP = 640
NSL = CAP // 128
CH = 128
NCH = T // CH
EPS = 1e-6
GN_EPS = 64e-5
TT = [(0, 512), (512, 512), (1024, 512), (1536, 512), (2048, 256)]


class Tl:
    def __init__(self, ap, name=""):
        self.ap = ap
        self.name = name
        self.lw = {}
        self.rd = {}

    def __getitem__(self, k):
        return Vw(self, self.ap[k])

    def re(self, s, **kw):
        return Vw(self, self.ap.rearrange(s, **kw))

    def bc(self, shape):
        return Vw(self, self.ap.to_broadcast(list(shape)))

    def pb(self, n):
        return Vw(self, self.ap.partition_broadcast(n))


class Vw:
    def __init__(self, t, ap):
        self.t = t
        self.ap = ap

    def __getitem__(self, k):
        return Vw(self.t, self.ap[k])

    def re(self, s, **kw):
        return Vw(self.t, self.ap.rearrange(s, **kw))

    def bc(self, shape):
        return Vw(self.t, self.ap.to_broadcast(list(shape)))

    def pb(self, n):
        return Vw(self.t, self.ap.partition_broadcast(n))


def A(x):
    return x.ap if isinstance(x, (Tl, Vw)) else x


def TT_(x):
    if isinstance(x, Tl):
        return x
    if isinstance(x, Vw):
        return x.t
    return None


class P:
    KD = 8

    def __init__(self, nc):
        self.nc = nc
        self.es = contextlib.ExitStack()
        self.eng = {"pe": nc.tensor, "act": nc.scalar, "dve": nc.vector, "pool": nc.gpsimd, "sp": nc.sync}
        self.sem = {}
        self.cur = {}
        for e in ("pe", "act", "dve", "pool"):
            self.sem[e] = self.es.enter_context(nc.semaphore("s_" + e))
            self.cur[e] = 0
        self.dcount = {}
        for q in ("sp", "pool", "act"):
            self.dcount[q] = 0
            for s in range(self.KD):
                k = ("d", q, s)
                self.sem[k] = self.es.enter_context(nc.semaphore("d_%s_%d" % (q, s)))
                self.cur[k] = 0
        self.waited = {e: {} for e in self.eng}
        self.nins = 0

    def sb(self, stack, name, shape, dt=F32):
        self.uid = getattr(self, "uid", 0) + 1
        name = "%s_%d" % (name, self.uid)
        h = stack.enter_context(self.nc.sbuf_tensor(name, list(shape), dt))
        return Tl(h[:], name)

    def ps(self, stack, name, shape, dt=F32):
        self.uid = getattr(self, "uid", 0) + 1
        name = "%s_%d" % (name, self.uid)
        h = stack.enter_context(self.nc.psum_tensor(name, list(shape), dt))
        return Tl(h[:], name)

    def dram(self, name, shape, dt=F32, kind="Internal"):
        h = self.nc.dram_tensor(name, list(shape), dt, kind=kind)
        return Tl(h.ap(), name)

    def _deps(self, eng, reads, writes, acc=False):
        need = {}

        def add(tok):
            if tok is not None:
                k, v = tok
                if need.get(k, 0) < v:
                    need[k] = v
        for x in reads:
            t = TT_(x)
            if t is not None:
                for k, v in t.lw.items():
                    add((k, v))
        for x in writes:
            t = TT_(x)
            if t is not None:
                if not acc:
                    for k, v in t.lw.items():
                        add((k, v))
                for k, v in t.rd.items():
                    add((k, v))
        out = []
        w = self.waited[eng]
        for k, v in need.items():
            if k == eng and eng == "pe":
                continue
            if w.get(k, 0) >= v:
                continue
            w[k] = v
            out.append((k, v))
        return out

    def _commit(self, tok, reads, writes, acc=False):
        k, v = tok
        for x in reads:
            t = TT_(x)
            if t is not None and t.rd.get(k, 0) < v:
                t.rd[k] = v
        for x in writes:
            t = TT_(x)
            if t is not None:
                if acc:
                    if t.lw.get(k, 0) < v:
                        t.lw[k] = v
                else:
                    t.lw = {k: v}
                    t.rd = {}

    def op(self, eng, fn, reads, writes):
        e = self.eng[eng]
        for k, v in self._deps(eng, reads, writes):
            e.wait_ge(self.sem[k], v)
        ins = fn(e)
        self.cur[eng] += 1
        ins.then_inc(self.sem[eng], 1)
        self._commit((eng, self.cur[eng]), reads, writes)
        self.nins += 1

    def dma(self, out, in_, q="sp", acc=False, fn=None, extra_reads=(), **kw):
        e = self.eng[q]
        j = self.dcount[q]
        self.dcount[q] = j + 1
        slot, gen = j % self.KD, j // self.KD
        key = ("d", q, slot)
        rds = [in_] + list(extra_reads)
        deps = self._deps(q, rds, [out], acc=acc)
        if gen > 0 and self.waited[q].get(key, 0) < 16 * gen:
            self.waited[q][key] = 16 * gen
            deps.append((key, 16 * gen))
        for k, v in deps:
            e.wait_ge(self.sem[k], v)
        if fn is None:
            ins = e.dma_start(out=A(out), in_=A(in_), **kw)
        else:
            ins = fn(e)
        ins.then_inc(self.sem[key], 16)
        self.cur[key] = 16 * (gen + 1)
        self._commit((key, 16 * (gen + 1)), rds, [out], acc=acc)
        self.nins += 1

    def barrier(self, engines=None):
        for en, e in self.eng.items():
            if engines is not None and en not in engines:
                continue
            w = self.waited[en]
            for k, v in self.cur.items():
                if v == 0 or (k == en and en == "pe"):
                    continue
                if w.get(k, 0) >= v:
                    continue
                w[k] = v
                e.wait_ge(self.sem[k], v)

    def mm(self, out, lhsT, rhs, start=True, stop=True):
        self.op("pe", lambda e: e.matmul(A(out), A(lhsT), A(rhs), start=start, stop=stop), [lhsT, rhs], [out])

    def tr(self, out, in_, ident):
        self.op("pe", lambda e: e.transpose(A(out), A(in_), A(ident)), [in_, ident], [out])

    def act(self, out, in_, func, bias=None, scale=None, accum=None, extra_reads=()):
        kw = {}
        rd = [in_] + list(extra_reads)
        if bias is not None:
            kw["bias"] = A(bias)
            rd.append(bias)
        if scale is not None:
            kw["scale"] = A(scale)
            rd.append(scale)
        wr = [out]
        if accum is not None:
            kw["accum_out"] = A(accum)
            wr.append(accum)
        self.op("act", lambda e: e.activation(out=A(out), in_=A(in_), func=func, **kw), rd, wr)

    def tt(self, out, in0, in1, op, eng="dve"):
        self.op(eng, lambda e: e.tensor_tensor(out=A(out), in0=A(in0), in1=A(in1), op=op), [in0, in1], [out])

    def ts(self, out, in0, s1, op0, s2=None, op1=None, eng="dve", accum=None):
        rd = [in0, s1, s2]
        kw = {}
        if op1 is not None:
            kw["op1"] = op1
        wr = [out]
        if accum is not None:
            kw["accum_out"] = A(accum)
            wr.append(accum)
        self.op(eng, lambda e: e.tensor_scalar(out=A(out), in0=A(in0), scalar1=A(s1), scalar2=A(s2), op0=op0, **kw), rd, wr)

    def stt(self, out, in0, scalar, in1, op0, op1, eng="dve"):
        self.op(eng, lambda e: e.scalar_tensor_tensor(out=A(out), in0=A(in0), scalar=A(scalar), in1=A(in1), op0=op0, op1=op1),
                [in0, scalar, in1], [out])

    def cp(self, out, in_, eng="dve"):
        if eng == "act":
            self.op("act", lambda e: e.copy(out=A(out), in_=A(in_)), [in_], [out])
        else:
            self.op(eng, lambda e: e.tensor_copy(out=A(out), in_=A(in_)), [in_], [out])

    def recip(self, out, in_):
        self.op("dve", lambda e: e.reciprocal(out=A(out), in_=A(in_)), [in_], [out])

    def memset(self, out, val, eng="dve"):
        self.op(eng, lambda e: e.memset(A(out), val), [], [out])

    def red(self, out, in_, op, axis=AX.X, eng="dve"):
        self.op(eng, lambda e: e.tensor_reduce(out=A(out), in_=A(in_), axis=axis, op=op), [in_], [out])


class G:
    pass


SH1, SC1, G1, SH2, SC2, G2 = [i * D for i in range(6)]
C_MU, C_CONV = 0, 15
C_KK, C_KA, C_RK, C_W0, C_A0, C_LG, C_LB = 0, 8, 16, 24, 40, 56, 64
C2_KK, C2_KA, C2_RK, C2_W0, C2_A0, C2_LG, C2_LB = 27, 31, 35, 39, 47, 55, 59
NP128 = 63


def stage_params(p, g):
    nc = p.nc
    with contextlib.ExitStack() as st:
        crow = p.sb(st, "crow", [3, D])
        p.dma(crow, g.c3)
        srow = p.sb(st, "srow", [3, D])
        p.act(srow, crow, AF.Silu)
        sT = p.sb(st, "sT", [128, 8, 3])
        pst = p.ps(st, "pst", [128, 8, 4])
        for c in range(8):
            p.tr(pst[:, c, 0:3], srow[:, c * 128:(c + 1) * 128], g.ident[0:3, 0:3])
        p.cp(sT, pst[:, :, 0:3])
        wst = [p.sb(st, "wst%d" % i, [128, 8, 512]) for i in range(2)]
        bm = p.sb(st, "bm", [3, 6 * D])
        mrow = p.sb(st, "mrow", [3, 6 * D])
        pm = [p.ps(st, "pm%d" % i, [3, 512]) for i in range(2)]
        k = 0
        for l in range(DEPTH):
            p.dma(bm, g.b_mod[l].pb(3))
            for cg in range(12):
                ws = wst[k % 2]
                p.dma(ws, g.w_mod[l].re("(c q) n -> q c n", q=128)[:, :, cg * 512:(cg + 1) * 512],
                      q=("sp" if k % 2 == 0 else "act"))
                pp = pm[k % 2]
                for kc in range(8):
                    p.mm(pp, sT[:, kc, :], ws[:, kc, :], start=(kc == 0), stop=(kc == 7))
                p.tt(mrow[:, cg * 512:(cg + 1) * 512], pp, bm[:, cg * 512:(cg + 1) * 512], ALU.add)
                k += 1
            p.dma(g.MODR[l], mrow, q="pool")
        pr128 = p.sb(st, "pr128", [NP128, 128])
        pr64 = p.sb(st, "pr64", [72, 64])
        pp128 = p.ps(st, "pp128", [128, 64])
        pp64 = p.ps(st, "pp64", [64, 72])
        for l in range(DEPTH):
            p.dma(pr128[0:15, :], g.shift_mu[l].re("(c q) -> c q", q=128))
            p.dma(pr128[15:27, :], g.conv_w[l].re("j (c q) -> (j c) q", q=128))
            p.dma(pr64[C_KK:C_KK + 8, :], g.k_k[l].re("(h k) -> h k", k=64))
            p.dma(pr64[C_KA:C_KA + 8, :], g.k_a[l].re("(h k) -> h k", k=64))
            p.dma(pr64[C_RK:C_RK + 8, :], g.r_k[l])
            p.dma(pr64[C_W0:C_W0 + 16, :], g.w0[l].re("d (h k) -> (d h) k", k=64))
            p.dma(pr64[C_A0:C_A0 + 16, :], g.a0[l].re("d (h k) -> (d h) k", k=64))
            p.dma(pr64[C_LG:C_LG + 8, :], g.lnx_g[l].re("(h k) -> h k", k=64))
            p.dma(pr64[C_LB:C_LB + 8, :], g.lnx_b[l].re("(h k) -> h k", k=64))
            p.dma(pr128[C2_KK:C2_KK + 4, :], g.k_k[l].re("(c q) -> c q", q=128))
            p.dma(pr128[C2_KA:C2_KA + 4, :], g.k_a[l].re("(c q) -> c q", q=128))
            p.dma(pr128[C2_RK:C2_RK + 4, :], g.r_k[l].re("(c a) k -> c (a k)", a=2))
            p.dma(pr128[C2_W0:C2_W0 + 8, :], g.w0[l].re("d (c q) -> (d c) q", q=128))
            p.dma(pr128[C2_A0:C2_A0 + 8, :], g.a0[l].re("d (c q) -> (d c) q", q=128))
            p.dma(pr128[C2_LG:C2_LG + 4, :], g.lnx_g[l].re("(c q) -> c q", q=128))
            p.dma(pr128[C2_LB:C2_LB + 4, :], g.lnx_b[l].re("(c q) -> c q", q=128))
            p.tr(pp128[:, 0:NP128], pr128, g.ident[0:NP128, 0:NP128])
            p.cp(g.P128T[:, l, :], pp128[:, 0:NP128])
            p.tr(pp64, pr64, g.ident[0:72, 0:72])
            p.cp(g.P64T[:, l, :], pp64)
        p.ts(g.OMM, g.P128T[:, :, C_MU:C_MU + 15], -1.0, ALU.mult, 1.0, ALU.add)
        p.ts(g.HMU, g.P128T[:, :, C_MU:C_MU + 15], 0.5, ALU.mult)
        p.ts(g.OMKA, g.P128T[:, :, C2_KA:C2_KA + 4], -1.0, ALU.mult, 1.0, ALU.add)
    p.barrier()


def stage_norm(p, g, src, l, b, second, hT, hTf=None):
    SHc, SCc = (SH2, SC2) if second else (SH1, SC1)
    ng = g.norm2_g if second else g.norm1_g
    with contextlib.ExitStack() as st:
        gt = p.sb(st, "gt", [128, D])
        p.dma(gt, ng[l].pb(128))
        Ab, Bb = [], []
        for kind, row in ((0, 2), (1, b)):
            sc = p.sb(st, "scb%d" % kind, [128, D])
            p.dma(sc, g.MODR[l, row, SCc:SCc + D].pb(128))
            a = p.sb(st, "Ab%d" % kind, [128, D])
            p.stt(a, sc, 1.0, gt, ALU.add, ALU.mult)
            bb = p.sb(st, "Bb%d" % kind, [128, D])
            p.dma(bb, g.MODR[l, row, SHc:SHc + D].pb(128))
            Ab.append(a)
            Bb.append(bb)
        xts = [p.sb(st, "xt%d" % i, [128, D]) for i in range(2)]
        sqs = [p.sb(st, "sq%d" % i, [128, D]) for i in range(2)]
        hns = [p.sb(st, "hn%d" % i, [128, D]) for i in range(2)]
        hbs = [p.sb(st, "hb%d" % i, [128, D], BF16) for i in range(2)]
        sss = [p.sb(st, "ss%d" % i, [128, 2]) for i in range(2)]
        ptr = [p.ps(st, "ptr%d" % i, [128, 8, 128], BF16) for i in range(2)]
        for i in range(NT):
            kind = 0 if i < 2 else 1
            xt, sq, hn, hb, ss, pt = xts[i % 2], sqs[i % 2], hns[i % 2], hbs[i % 2], sss[i % 2], ptr[i % 2]
            p.dma(xt, src[i * 128:(i + 1) * 128, :], q=("sp" if i % 2 == 0 else "act"))
            p.act(sq, xt, AF.Square)
            p.red(ss[:, 0:1], sq, ALU.add)
            p.ts(ss[:, 1:2], ss[:, 0:1], 1.0 / D, ALU.mult, EPS, ALU.add)
            p.act(ss[:, 1:2], ss[:, 1:2], AF.Sqrt)
            p.recip(ss[:, 1:2], ss[:, 1:2])
            p.stt(hn, xt, ss[:, 1:2], Ab[kind], ALU.mult, ALU.mult)
            p.tt(hb, hn, Bb[kind], ALU.add, eng="pool")
            if second:
                p.dma(g.H2T[i * 128:(i + 1) * 128, :], hb, q="pool", acc=True)
            for c in range(8):
                p.tr(pt[:, c, :], hb[:, c * 128:(c + 1) * 128], g.identb)
            p.cp(hT[:, :, i * 128:(i + 1) * 128], pt, eng="act")


def stage_proj(p, g, hT, l, b):
    groups = [(c0, min(512, NIN - c0)) for c0 in range(0, NIN, 512)]
    with contextlib.ExitStack() as st:
        wst = [p.sb(st, "pwst%d" % i, [128, 8, 512]) for i in range(2)]
        wbf = [p.sb(st, "pwbf%d" % i, [128, 8, 512], BF16) for i in range(2)]
        ot = [p.sb(st, "pot%d" % i, [128, T]) for i in range(2)]
        zt = [p.sb(st, "pzt%d" % i, [128, T]) for i in range(2)]
        pss = [p.ps(st, "pps%d" % i, [128, 512]) for i in range(4)]
        k = 0
        kk = 0
        for gi, (c0, w) in enumerate(groups):
            ws, wb = wst[gi % 2], wbf[gi % 2]
            p.dma(ws[:, :, :w], g.w_in[l].re("(c q) n -> q c n", q=128)[:, :, c0:c0 + w],
                  q=("sp" if gi % 2 == 0 else "act"))
            p.cp(wb[:, :, :w], ws[:, :, :w], eng="pool")
            for cc in range(w // 128):
                col = c0 + cc * 128
                chunk = col // 128
                o = ot[k % 2]
                for (t0, tn) in TT:
                    ps = pss[kk % 4]
                    for kc in range(8):
                        p.mm(ps[:, :tn], wb[:, kc, cc * 128:(cc + 1) * 128], hT[:, kc, t0:t0 + tn],
                             start=(kc == 0), stop=(kc == 7))
                    if chunk >= 27:
                        p.act(o[:, t0:t0 + tn], ps[:, :tn], AF.Sigmoid)
                    elif kk % 2 == 0:
                        p.cp(o[:, t0:t0 + tn], ps[:, :tn], eng="act")
                    else:
                        p.cp(o[:, t0:t0 + tn], ps[:, :tn], eng="dve")
                    kk += 1
                if 12 <= chunk < 27:
                    j = chunk - 12
                    z = zt[k % 2]
                    om = g.OMM[:, l, j:j + 1]
                    hm = g.HMU[:, l, j:j + 1]
                    p.ts(z, o, om, ALU.mult)
                    for (a0, a1) in ((0, TCTX), (TCTX, T)):
                        p.stt(z[:, a0 + 1:a1], o[:, a0:a1 - 1], hm, z[:, a0 + 1:a1], ALU.mult, ALU.add)
                        p.stt(z[:, a0:a1 - 1], o[:, a0 + 1:a1], hm, z[:, a0:a1 - 1], ALU.mult, ALU.add)
                    o = z
                p.dma(g.ZF[col:col + 128, :], o, q="pool", acc=True)
                k += 1


WEIGHTS = [
    ("w_mod", [DEPTH, D, 6 * D]), ("b_mod", [DEPTH, 6 * D]), ("norm1_g", [DEPTH, D]), ("norm2_g", [DEPTH, D]),
    ("w_in", [DEPTH, D, NIN]), ("shift_mu", [DEPTH, 1920]), ("conv_w", [DEPTH, 3, 512]),
    ("w_up", [DEPTH, 2, 64, 512]), ("w0", [DEPTH, 2, 512]), ("a_up", [DEPTH, 2, 64, 512]), ("a0", [DEPTH, 2, 512]),
    ("g_up", [DEPTH, 128, 512]), ("k_k", [DEPTH, 512]), ("k_a", [DEPTH, 512]), ("r_k", [DEPTH, 8, 64]),
    ("lnx_g", [DEPTH, 512]), ("lnx_b", [DEPTH, 512]), ("w_a_out", [DEPTH, 512, D]), ("w_b_out", [DEPTH, 512, D]),
    ("w_o", [DEPTH, D, D]), ("router_g", [DEPTH, D, 4]), ("router_g_b", [DEPTH, 4]),
    ("router_e", [DEPTH, D, NEXP]), ("router_e_b", [DEPTH, NEXP]),
    ("exp_w1", [DEPTH, NEXP, D, DFF]), ("exp_w3", [DEPTH, NEXP, D, DFF]), ("exp_w2", [DEPTH, NEXP, DFF, D]),
    ("final_g", [D]),
]


def build(stages="all", dbg=()):
    nc = bass.Bass("TRN2", target_bir_lowering=False)
    p = P(nc)
    g = G()
    g.p = p

    def inp(name, shape):
        return Tl(nc.dram_tensor(name, list(shape), F32, kind="ExternalInput").ap(), name)
    g.xin = inp("xin", [NB, T, D])
    g.c3 = inp("c3", [3, D])
    g.idin = inp("idin", [128, 128])
    g.mskin = inp("mskin", [128, 2, 4, 128])
    g.eoffin = inp("eoffin", [128, NEXP])
    for name, shape in WEIGHTS:
        setattr(g, name, inp(name, shape))
    g.out = Tl(nc.dram_tensor("out", [NB, TLAT, D], F32, kind="ExternalOutput").ap(), "out")
    g.MODR = p.dram("MODR", [DEPTH, 3, 6 * D])
    g.ZF = p.dram("ZF", [NIN, T])
    g.XS = [p.dram("XS%d" % b, [T, D]) for b in range(NB)]
    g.SCF = p.dram("SCF", [2, 512, NCH, 4, CH], BF16)
    g.SCT = p.dram("SCT", [2, NCH, CH, 2, 512], BF16)
    g.VTD = p.dram("VTD", [NCH, CH, 512], BF16)
    g.WCD = p.dram("WCD", [512, 2, NCH])
    g.RO = p.dram("RO", [2, 512, T])
    g.YD = p.dram("YD", [2, 512, T])
    g.H2T = p.dram("H2T", [T, D], BF16)
    g.XE = p.dram("XE", [NEXP * CAP + 1, D], BF16)
    g.YE = p.dram("YE", [NEXP * CAP + 1, D])
    g.YEZ = p.dram("YEZ", [1, D])
    dbg_out = {}
    for name, shape in dbg:
        dbg_out[name] = Tl(nc.dram_tensor("dbg_" + name, list(shape), F32, kind="ExternalOutput").ap(), name)
    g.dbg = dbg_out
    top = p.es
    g.ident = p.sb(top, "ident", [128, 128])
    g.identb = p.sb(top, "identb", [128, 128], BF16)
    p.dma(g.ident, g.idin)
    p.cp(g.identb, g.ident)
    g.P128T = p.sb(top, "P128T", [128, DEPTH, NP128])
    g.P64T = p.sb(top, "P64T", [64, DEPTH, 72])
    g.OMM = p.sb(top, "OMM", [128, DEPTH, 15])
    g.HMU = p.sb(top, "HMU", [128, DEPTH, 15])
    g.OMKA = p.sb(top, "OMKA", [128, DEPTH, 4])
    g.MASK4 = p.sb(top, "MASK4", [128, 2, 4, 128])
    p.dma(g.MASK4, g.mskin)
    g.ones64 = p.sb(top, "ones64", [64, 64])
    p.memset(g.ones64, 1.0)
    g.onesf = p.sb(top, "onesf", [128, 128])
    p.memset(g.onesf, 1.0)
    g.EOFF = p.sb(top, "EOFF", [128, NEXP])
    p.dma(g.EOFF, g.eoffin)
    with contextlib.ExitStack() as stz:
        zt = p.sb(stz, "zrow", [1, D])
        p.memset(zt, 0.0)
        p.dma(g.YE[NEXP * CAP:NEXP * CAP + 1, :], zt, q="pool")
        p.barrier()
    g.ones2 = p.sb(top, "ones2", [128, 128])
    p.memset(g.ones2, 0.0)
    p.memset(g.ones2[0:64, 0:64], 1.0)
    p.memset(g.ones2[64:128, 64:128], 1.0)
    g.RM = p.sb(top, "RM", [128, TH])
    p.memset(g.RM, 1.0)
    p.memset(g.RM.re("k (c t) -> k c t", t=CH)[:, :, 0:1], 0.0)

    stage_params(p, g)
    nl = DEPTH if stages == "all" else stages[0]
    nb = NB if stages == "all" else stages[1]
    upto = "z" if stages == "all" else stages[2]
    for b in range(nb):
        for l in range(nl):
            src = g.xin[b] if l == 0 else g.XS[b]
            with contextlib.ExitStack() as st:
                hT = p.sb(st, "hT", [128, 8, T], BF16)
                stage_norm(p, g, src, l, b, False, hT)
                if upto == "A":
                    if "hT" in g.dbg:
                        with contextlib.ExitStack() as s2:
                            tmp = p.sb(s2, "dbgtmp", [128, 8, T])
                            p.cp(tmp, hT)
                            p.dma(g.dbg["hT"], tmp)
                            p.barrier()
                    p.barrier()
                    continue
                stage_proj(p, g, hT, l, b)
                p.barrier()
            if upto == "B":
                continue
            with contextlib.ExitStack() as st:
                CO = p.sb(st, "CO", [128, 4, T], BF16)
                stage_conv(p, g, l, b, CO)
                stage_prep(p, g, l, b)
                if upto == "D":
                    continue
                stage_scan(p, g, l, b)
                if upto == "E":
                    continue
                stage_mix(p, g, l, b, CO, src, g.XS[b])
            if upto == "G":
                continue
            with contextlib.ExitStack() as st:
                GW = p.sb(st, "GW", [128, NT, 2])
                DEST = p.sb(st, "DEST", [128, NT, 2], I32)
                with contextlib.ExitStack() as st2:
                    hT = p.sb(st2, "hT2", [128, 8, T], BF16)
                    stage_norm(p, g, g.XS[b], l, b, True, hT)
                    stage_router(p, g, l, hT, GW, DEST)
                stage_moe(p, g, l, b, GW, DEST)
        if upto == "z":
            stage_final(p, g, b)
    if "ZF" in g.dbg:
        p.barrier()
        with contextlib.ExitStack() as s2:
            tmp = p.sb(s2, "dbgtmp", [128, T])
            for c in range(NIN // 128):
                p.dma(tmp, g.ZF[c * 128:(c + 1) * 128, :])
                p.dma(g.dbg["ZF"][c * 128:(c + 1) * 128, :], tmp)
            p.barrier()
    for nm, t in (("YD", g.YD.re("d n t -> (d n) t")), ("RO", g.RO.re("q n t -> (q n) t")), ("XS0", g.XS[0])):
        if nm in g.dbg:
            with contextlib.ExitStack() as s2:
                n, w = t.ap.shape
                tmp = p.sb(s2, "dbgtmp3", [128, w])
                for c in range(n // 128):
                    p.dma(tmp, t[c * 128:(c + 1) * 128, :])
                    p.dma(g.dbg[nm][c * 128:(c + 1) * 128, :], tmp)
                p.barrier()
    if "MODR" in g.dbg:
        with contextlib.ExitStack() as s2:
            tmp = p.sb(s2, "dbgtmp2", [12, 6 * D])
            p.dma(tmp, g.MODR.re("l r n -> (l r) n"))
            p.dma(g.dbg["MODR"], tmp)
            p.barrier()
    p.barrier()
    p.es.close()
    return nc, p


def make_in_maps(inputs, ncores=8):
    idm = np.eye(128, dtype=np.float32)
    ii = np.arange(128)
    us = (ii[None, :] > ii[:, None]).astype(np.float32)
    ui = (ii[None, :] >= ii[:, None]).astype(np.float32)
    msk = np.ascontiguousarray(np.stack([np.stack([us, ui, us, ui], 0), np.stack([us.T, ui.T, us.T, ui.T], 0)], 0).transpose(2, 0, 1, 3))
    eoff = np.ascontiguousarray(np.broadcast_to((np.arange(NEXP, dtype=np.float32) * CAP)[None, :], (128, NEXP)))
    maps = []
    for core in range(ncores):
        b0 = core * NB
        xin = np.concatenate([inputs["ctx"][b0:b0 + NB], inputs["x"][b0:b0 + NB]], axis=1)
        c3 = np.concatenate([inputs["c"][b0:b0 + NB], inputs["c_ctx"][None, :]], axis=0)
        m = {"xin": np.ascontiguousarray(xin, dtype=np.float32), "c3": np.ascontiguousarray(c3, dtype=np.float32), "idin": idm,
             "mskin": msk, "eoffin": eoff}
        for name, _ in WEIGHTS:
            m[name] = np.ascontiguousarray(inputs[name], dtype=np.float32)
        maps.append(m)
    return maps


def kernel(**inputs):
    inputs = {k: np.asarray(v) for k, v in inputs.items()}
    nc, p = build("all")
    maps = make_in_maps(inputs, 8)
    res = run_bass_kernel_spmd(nc, maps, core_ids=list(range(8)))
    return np.concatenate([r["out"] for r in res.results], axis=0).astype(np.float32)


def stage_conv(p, g, l, b, CO):
    with contextlib.ExitStack() as st:
        bgt = [p.sb(st, "cbg%d" % i, [128, T]) for i in range(2)]
        cgt = [p.sb(st, "ccg%d" % i, [128, T]) for i in range(2)]
        hat = [p.sb(st, "cha%d" % i, [128, T]) for i in range(2)]
        ut = [p.sb(st, "cu%d" % i, [128, T]) for i in range(2)]
        ott = [p.sb(st, "co%d" % i, [128, T]) for i in range(2)]
        for j in range(4):
            bg, cg, ha, u, o = bgt[j % 2], cgt[j % 2], hat[j % 2], ut[j % 2], ott[j % 2]
            p.dma(bg, g.ZF[j * 128:(j + 1) * 128, :], q="sp")
            p.dma(cg, g.ZF[512 + j * 128:512 + (j + 1) * 128, :], q="act")
            p.dma(ha, g.ZF[1024 + j * 128:1024 + (j + 1) * 128, :], q="sp")
            w0 = g.P128T[:, l, C_CONV + 0 * 4 + j:C_CONV + 0 * 4 + j + 1]
            w1 = g.P128T[:, l, C_CONV + 1 * 4 + j:C_CONV + 1 * 4 + j + 1]
            w2 = g.P128T[:, l, C_CONV + 2 * 4 + j:C_CONV + 2 * 4 + j + 1]
            p.tt(u, cg, ha, ALU.mult, eng="pool")
            p.ts(o, u, w1, ALU.mult)
            p.stt(o[:, 1:TCTX], u[:, 0:TCTX - 1], w0, o[:, 1:TCTX], ALU.mult, ALU.add)
            p.stt(o[:, 0:TCTX - 1], u[:, 1:TCTX], w2, o[:, 0:TCTX - 1], ALU.mult, ALU.add)
            if j < 2:
                ug = u[:, TCTX:T].re("q (r w) -> q r w", w=64)
                og = o[:, TCTX:T].re("q (r w) -> q r w", w=64)
                p.stt(og[:, :, 1:64], ug[:, :, 0:63], w0, og[:, :, 1:64], ALU.mult, ALU.add)
                p.stt(og[:, :, 0:63], ug[:, :, 1:64], w2, og[:, :, 0:63], ALU.mult, ALU.add)
            else:
                p.stt(o[:, TCTX + 64:T], u[:, TCTX:T - 64], w0, o[:, TCTX + 64:T], ALU.mult, ALU.add)
                p.stt(o[:, TCTX:T - 64], u[:, TCTX + 64:T], w2, o[:, TCTX:T - 64], ALU.mult, ALU.add)
            p.tt(CO[:, j, :], o, bg, ALU.mult, eng="pool")
    p.barrier()


TH = 1152
THT = [(0, 512), (512, 512), (1024, 128)]
NCH2 = TH // CH


def stage_prep(p, g, l, b):
    with contextlib.ExitStack() as st:
        tmpf = p.sb(st, "dtmpf", [128, T])
        tzw = [p.sb(st, "tzw%d" % d, [64, T], BF16) for d in range(2)]
        zab = [p.sb(st, "zab%d" % d, [64, T], BF16) for d in range(2)]
        sgz = p.sb(st, "sgz", [128, T], BF16)
        for d in range(2):
            p.dma(tmpf[0:64, :], g.ZF[RW0 + 1536 + d * 64:RW0 + 1536 + (d + 1) * 64, :])
            p.act(tzw[d], tmpf[0:64, :], AF.Tanh)
            p.dma(tmpf[0:64, :], g.ZF[RW0 + 1664 + d * 64:RW0 + 1664 + (d + 1) * 64, :])
            p.cp(zab[d], tmpf[0:64, :], eng="act")
        p.dma(tmpf, g.ZF[RW0 + 1792:RW0 + 1920, :])
        p.act(sgz, tmpf, AF.Sigmoid)
        wupf = p.sb(st, "wupf", [64, 2, 512])
        aupf = p.sb(st, "aupf", [64, 2, 512])
        gupf = p.sb(st, "gupf", [128, 512])
        wupb = p.sb(st, "wupb", [64, 2, 512], BF16)
        aupb = p.sb(st, "aupb", [64, 2, 512], BF16)
        gupb = p.sb(st, "gupb", [128, 512], BF16)
        p.dma(wupf, g.w_up[l].re("d r n -> r d n"))
        p.dma(aupf, g.a_up[l].re("d r n -> r d n"))
        p.dma(gupf, g.g_up[l])
        p.cp(wupb, wupf)
        p.cp(aupb, aupf)
        p.cp(gupb, gupf)
        rt = p.sb(st, "d_r", [128, TH])
        kt = p.sb(st, "d_k", [128, TH])
        vt = p.sb(st, "d_v", [128, TH])
        kk = p.sb(st, "d_kk", [128, TH])
        X = [p.sb(st, "d_x%d" % i, [128, TH]) for i in range(8)]
        ob = {n: p.sb(st, "d_ob_" + n, [128, TH], BF16) for n in ("kk", "r", "kh", "bh", "kp", "bp", "v")}
        tst = [p.sb(st, "d_tst%d" % i, [128, NCH2, 128], BF16) for i in range(3)]
        wc = p.sb(st, "d_wc", [128, NCH2])
        ps = [p.ps(st, "d_ps%d" % i, [128, 512]) for i in range(3)]
        pst = [p.ps(st, "d_pst%d" % i, [128, 16, 128], BF16) for i in range(2)]
        npst = 0
        nps = 0

        def pk(col):
            return g.P128T[:, l, col:col + 1]

        def tposed(src_b, dst_dram, q, k):
            nonlocal npst
            pt = pst[npst % 2]
            npst += 1
            stg = tst[k]
            for c in range(NCH2):
                p.tr(pt[:, c, :], src_b[:, c * CH:(c + 1) * CH], g.identb)
            p.cp(stg, pt[:, 0:NCH2, :], eng=("act" if k % 2 else "dve"))
            p.dma(dst_dram, stg, q=q, acc=True)

        for pr in range(4):
            r0 = pr * 128
            for hf in range(2):
                tb = hf * TH
                cb = hf * NCH2
                p.dma(rt, g.ZF[RW0 + r0:RW0 + r0 + 128, tb:tb + TH], q="sp")
                p.dma(kt, g.ZF[RW0 + 512 + r0:RW0 + 512 + r0 + 128, tb:tb + TH], q="act")
                p.dma(vt, g.ZF[RW0 + 1024 + r0:RW0 + 1024 + r0 + 128, tb:tb + TH], q="sp")
                p.ts(X[0], kt, pk(C2_KK + pr), ALU.mult, eng="pool")
                p.act(X[1], X[0], AF.Square)
                for (t0, tn) in THT:
                    pp = ps[nps % 3]
                    nps += 1
                    p.mm(pp[:, :tn], g.ones2, X[1][:, t0:t0 + tn])
                    p.ts(X[2][:, t0:t0 + tn], pp[:, :tn], 1e-12, ALU.add)
                p.act(X[2], X[2], AF.Sqrt)
                p.recip(X[2], X[2])
                p.tt(kk, X[0], X[2], ALU.mult)
                p.cp(ob["v"], vt, eng="pool")
                tposed(ob["v"], g.VTD[cb:cb + NCH2, :, r0:r0 + 128].re("c t n -> t c n"), "pool", 0)
                for d in range(2):
                    lw, a, kd, bb, L = X[0], X[1], X[2], X[3], X[4]
                    for (t0, tn) in THT:
                        pp = ps[nps % 3]
                        nps += 1
                        p.mm(pp[:, :tn], wupb[:, d, r0:r0 + 128], tzw[d][:, tb + t0:tb + t0 + tn])
                        p.act(lw[:, t0:t0 + tn], pp[:, :tn], AF.Sigmoid, bias=pk(C2_W0 + d * 4 + pr))
                        pp = ps[nps % 3]
                        nps += 1
                        p.mm(pp[:, :tn], aupb[:, d, r0:r0 + 128], zab[d][:, tb + t0:tb + t0 + tn])
                        p.act(a[:, t0:t0 + tn], pp[:, :tn], AF.Sigmoid, bias=pk(C2_A0 + d * 4 + pr))
                    p.ts(lw, lw, -0.6065306597126334, ALU.mult, eng="pool")
                    p.ts(kd, a, pk(C2_KA + pr), ALU.mult, g.OMKA[:, l, pr:pr + 1], ALU.add)
                    p.tt(kd, kd, kt, ALU.mult)
                    p.tt(bb, kk, a, ALU.mult, eng="pool")
                    if d == 0:
                        p.cp(X[7], kd, eng="pool")
                    else:
                        p.tt(X[7], X[7], kd, ALU.add, eng="pool")
                    p.op("dve", lambda e: e.tensor_tensor_scan(out=A(L), data0=A(g.RM), data1=A(lw), initial=0.0,
                                                               op0=ALU.mult, op1=ALU.add), [g.RM, lw], [L])
                    if d == 1:
                        Lp = X[1]
                        p.tt(Lp, lw, L, ALU.subtract)
                        p.tt(Lp.re("k (c t) -> k c t", t=CH), Lp.re("k (c t) -> k c t", t=CH),
                             L.re("k (c t) -> k c t", t=CH)[:, :, CH - 1:CH].bc([128, NCH2, CH]), ALU.add)
                        end = 0
                    else:
                        Lp = L
                        end = CH - 1
                    E = X[5]
                    p.act(E, Lp, AF.Exp)
                    p.tt(ob["r"], rt, E, ALU.mult)
                    p.act(wc.re("k (c o) -> k c o", o=1), Lp.re("k (c t) -> k c t", t=CH)[:, :, end:end + 1], AF.Exp)
                    E2 = X[6]
                    p.act(E2, Lp, AF.Exp, scale=-1.0)
                    p.tt(kd, kd, E2, ALU.mult)
                    p.tt(bb, bb, E2, ALU.mult, eng="pool")
                    p.tt(lw, Lp, lw, ALU.subtract, eng="pool")
                    p.act(lw, lw, AF.Exp)
                    p.tt(ob["kk"], kk, lw, ALU.mult)
                    p.cp(ob["kh"], kd, eng="pool")
                    p.cp(ob["bh"], bb, eng="act")
                    wcb = wc.re("k (c o) -> k c o", o=1).bc([128, NCH2, CH])
                    p.tt(ob["kp"].re("k (c t) -> k c t", t=CH), kd.re("k (c t) -> k c t", t=CH), wcb, ALU.mult)
                    p.tt(ob["bp"].re("k (c t) -> k c t", t=CH), bb.re("k (c t) -> k c t", t=CH), wcb, ALU.mult, eng="pool")
                    for qi, n in enumerate(("kk", "r", "kh", "bh")):
                        p.dma(g.SCF[d, r0:r0 + 128, cb:cb + NCH2, qi, :], ob[n].re("k (c t) -> k c t", t=CH), q="pool", acc=True)
                    tposed(ob["kp"], g.SCT[d, cb:cb + NCH2, :, 0, r0:r0 + 128].re("c t n -> t c n"), "pool", 1)
                    tposed(ob["bp"], g.SCT[d, cb:cb + NCH2, :, 1, r0:r0 + 128].re("c t n -> t c n"), "pool", 2)
                    p.dma(g.WCD[r0:r0 + 128, d, cb:cb + NCH2], wc, q="pool", acc=True)
                p.ts(X[0], rt, pk(C2_RK + pr), ALU.mult, eng="pool")
                p.tt(X[0], X[0], X[7], ALU.mult)
                for (t0, tn) in THT:
                    pp = ps[nps % 3]
                    nps += 1
                    p.mm(pp[:, :tn], g.ones2, X[0][:, t0:t0 + tn])
                    p.tt(X[1][:, t0:t0 + tn], pp[:, :tn], vt[:, t0:t0 + tn], ALU.mult)
                    pp = ps[nps % 3]
                    nps += 1
                    p.mm(pp[:, :tn], gupb[:, r0:r0 + 128], sgz[:, tb + t0:tb + t0 + tn])
                    p.cp(X[2][:, t0:t0 + tn], pp[:, :tn], eng="act")
                p.dma(g.RO[0, r0:r0 + 128, tb:tb + TH], X[1], q="pool", acc=True)
                p.dma(g.RO[1, r0:r0 + 128, tb:tb + TH], X[2], q="pool", acc=True)
    p.barrier()


ORDER_B = [1, 0] + list(range(NCH - 1, 1, -1))


def stage_scan(p, g, l, b):
    with contextlib.ExitStack() as st:
        B = [p.ps(st, "e_b%d" % i, [128, 512]) for i in range(8)]

        def bv(i, n, w, parts=128):
            return B[i].re("s (a t) -> s a t", t=w)[0:parts, 0:n, :]
        WC = p.sb(st, "e_wc", [64, 2, 8, NCH])
        p.dma(WC, g.WCD.re("(h k) d c -> k d h c", k=64))
        ST = p.sb(st, "e_st", [64, 16, 64])
        STb = p.sb(st, "e_stb", [64, 16, 64], BF16)
        p.memset(ST, 0.0)
        p.memset(STb, 0.0, eng="pool")
        FQ = [p.sb(st, "e_fq%d" % i, [64, 2, 8, 4, 128], BF16) for i in range(2)]
        TQ = [p.sb(st, "e_tq%d" % i, [128, 2, 2, 8, 64], BF16) for i in range(2)]
        VT = [p.sb(st, "e_vt%d" % i, [128, 2, 8, 64], BF16) for i in range(2)]
        AMb = p.sb(st, "e_am", [128, 16, 4, 128], BF16)
        Xs = [[p.sb(st, "e_x%d_%d" % (i, q), [128, 4, 128]) for q in range(4)] for i in range(2)]
        XTs = [[p.sb(st, "e_xt%d_%d" % (i, q), [128, 4, 128]) for q in range(4)] for i in range(2)]
        Ps = [[p.sb(st, "e_p%d_%d" % (i, q), [128, 4, 128]) for q in range(4)] for i in range(2)]
        RT = p.sb(st, "e_rt", [128, 16, 64])
        nUT = p.sb(st, "e_nut", [128, 16, 64], BF16)
        YS = [p.sb(st, "e_ys%d" % i, [64, 16, 128]) for i in range(2)]
        idb = g.ident.re("s (o t) -> s o t", o=1).bc([128, 4, 128])
        for j in range(NCH):
            cd = (j, ORDER_B[j])
            fq, tq, vt, ys = FQ[j % 2], TQ[j % 2], VT[j % 2], YS[j % 2]
            for d in range(2):
                c = cd[d]
                p.dma(fq[:, d], g.SCF[d, :, c].re("(h k) q t -> k h q t", k=64), q="sp")
                p.dma(tq[:, d], g.SCT[d, c].re("t q (h k) -> t q h k", k=64), q="act")
                p.dma(vt[:, d], g.VTD[c].re("t (h k) -> t h k", k=64), q="sp")
            for ci in range(16):
                d, h = divmod(ci, 8)
                pa = B[6 + ci % 2]
                rhs = fq[:, d, h, 0:2, :]
                p.mm(pa.re("s (q t) -> s q t", t=128)[:, 0:2, :], fq[:, d, h, 2, :], rhs)
                p.mm(pa.re("s (q t) -> s q t", t=128)[:, 2:4, :], fq[:, d, h, 3, :], rhs)
                pn = bv(5, 4, 128)
                p.mm(pn[:, ci % 4, :], fq[:, d, h, 0, :], fq[:, d, h, 3, :])
                p.tt(AMb[:, ci], pa.re("s (q t) -> s q t", t=128), g.MASK4[:, d], ALU.mult)
                p.tt(Xs[0][ci // 4][:, ci % 4, :], pa[:, 256:384], g.MASK4[:, d, 0, :], ALU.mult)
                if ci % 4 == 3:
                    p.tt(XTs[0][ci // 4], pn, g.MASK4[:, 1 - d, 0:1, :].bc([128, 4, 128]), ALU.mult)
            for gq in range(4):
                p.tt(Ps[0][gq], idb, Xs[0][gq], ALU.subtract, eng="pool")
            cur = 0
            for lev in range(6):
                last = lev == 5
                for gq in range(4):
                    X, XT = Xs[cur][gq], XTs[cur][gq]
                    bs = 3 * (gq % 2)
                    for q in range(4):
                        if not last:
                            p.mm(bv(bs, 4, 128)[:, q, :], XT[:, q, :], X[:, q, :])
                        p.mm(bv(bs + 1, 4, 128)[:, q, :], X[:, q, :], XT[:, q, :])
                    if not last:
                        p.cp(Xs[1 - cur][gq], bv(bs, 4, 128), eng="act")
                    p.cp(XTs[1 - cur][gq], bv(bs + 1, 4, 128), eng="dve")
                for gq in range(4):
                    bs = 3 * (gq % 2)
                    for q in range(4):
                        p.mm(bv(bs + 2, 4, 128)[:, q, :], XTs[1 - cur][gq][:, q, :], Ps[cur][gq][:, q, :])
                    p.tt(Ps[1 - cur][gq], bv(bs + 2, 4, 128), Ps[cur][gq], ALU.add, eng=("dve" if gq % 2 == 0 else "dve"))
                cur = 1 - cur
            Pf = Ps[cur]
            for ci in range(16):
                d, h = divmod(ci, 8)
                pr = bv(6 + ci // 8, 8, 64)[:, ci % 8, :]
                p.mm(pr, fq[:, d, h, 0, :], STb[:, ci, :], start=True, stop=False)
                p.mm(pr, AMb[:, ci, 0, :], vt[:, d, h, :], start=False, stop=True)
            p.cp(RT[:, 0:8, :], bv(6, 8, 64), eng="act")
            p.cp(RT[:, 8:16, :], bv(7, 8, 64), eng="dve")
            for ci in range(16):
                pr = bv(6 + ci // 8, 8, 64)[:, ci % 8, :]
                p.mm(pr, Pf[ci // 4][:, ci % 4, :], RT[:, ci, :])
            p.ts(nUT[:, 0:8, :], bv(6, 8, 64), -1.0, ALU.mult)
            p.op("act", lambda e: e.mul(out=A(nUT[:, 8:16, :]), in_=A(bv(7, 8, 64)), mul=-1.0), [B[7]], [nUT])
            for q4 in range(4):
                for q in range(4):
                    ci = 4 * q4 + q
                    d, h = divmod(ci, 8)
                    pv = bv(q4 % 2, 4, 128, 64)[:, q, :]
                    p.mm(pv, STb[:, ci, :], fq[:, d, h, 1, :], start=True, stop=False)
                    p.mm(pv, vt[:, d, h, :], AMb[:, ci, 1, :], start=False, stop=False)
                    p.mm(pv, nUT[:, ci, :], AMb[:, ci, 3, :], start=False, stop=True)
                p.cp(ys[:, 4 * q4:4 * q4 + 4, :], bv(q4 % 2, 4, 128, 64), eng=("act" if q4 % 2 else "dve"))
            for d in range(2):
                c = cd[d]
                p.dma(g.YD[d, :, c * CH:(c + 1) * CH].re("(h k) t -> k h t", k=64), ys[:, d * 8:(d + 1) * 8, :], q="pool", acc=True)
            for ci in range(16):
                d, h = divmod(ci, 8)
                pv = bv(2 + ci // 8, 8, 64, 64)[:, ci % 8, :]
                p.mm(pv, tq[:, d, 0, h, :], vt[:, d, h, :], start=True, stop=False)
                p.mm(pv, tq[:, d, 1, h, :], nUT[:, ci, :], start=False, stop=True)
            for d in range(2):
                wcv = WC[:, d, :, cd[d]:cd[d] + 1].bc([64, 8, 64])
                p.tt(ST[:, d * 8:(d + 1) * 8, :], ST[:, d * 8:(d + 1) * 8, :], wcv, ALU.mult, eng="pool")
                p.tt(ST[:, d * 8:(d + 1) * 8, :], ST[:, d * 8:(d + 1) * 8, :], bv(2 + d, 8, 64, 64), ALU.add)
            p.cp(STb, ST, eng="act")
    p.barrier()


def stage_mix(p, g, l, b, CO, src, last_dst):
    with contextlib.ExitStack() as st:
        wa = p.sb(st, "m_wa", [128, 4, 1024], BF16)
        wb = p.sb(st, "m_wb", [64, 8, 1024], BF16)
        wo = p.sb(st, "m_wo", [128, 8, 1024], BF16)
        G1b = []
        for kind, row in ((0, 2), (1, b)):
            t = p.sb(st, "m_g1b%d" % kind, [128, D])
            p.dma(t, g.MODR[l, row, G1:G1 + D].pb(128), q="act")
            G1b.append(t)
        with contextlib.ExitStack() as st2:
            stg = p.sb(st2, "m_stg", [128, 8, 1024])
            p.dma(stg[:, 0:4, :], g.w_a_out[l].re("(c q) n -> q c n", q=128))
            p.cp(wa, stg[:, 0:4, :], eng="pool")
            p.dma(stg[0:64, :, :], g.w_b_out[l].re("(h k) n -> k h n", k=64))
            p.cp(wb, stg[0:64, :, :], eng="pool")
            p.dma(stg, g.w_o[l].re("(c q) n -> q c n", q=128))
            p.cp(wo, stg, eng="pool")
            p.barrier()
        y0 = p.sb(st, "m_y0", [64, 8, 512])
        y1 = p.sb(st, "m_y1", [64, 8, 512])
        aux = p.sb(st, "m_aux", [64, 8, 512])
        ybin = p.sb(st, "m_ybin", [64, 8, 512], BF16)
        sgat = [p.sb(st, "m_sga%d" % i, [128, 512]) for i in range(2)]
        sgbt = [p.sb(st, "m_sgb%d" % i, [128, 512]) for i in range(2)]
        m1 = [p.sb(st, "m_m1%d" % i, [128, 512]) for i in range(1)] * 2
        m2 = [p.sb(st, "m_m2%d" % i, [128, 512]) for i in range(1)] * 2
        mrg = p.sb(st, "m_mrg", [128, 8, 512], BF16)
        xt = [p.sb(st, "m_xt%d" % i, [128, D]) for i in range(2)]
        xo = [p.sb(st, "m_xo%d" % i, [128, D]) for i in range(2)]
        B = [p.ps(st, "m_b%d" % i, [128, 512]) for i in range(8)]
        nb = 0
        nx = 0
        for (t0, tn) in TT:
            for d, yt in ((0, y0), (1, y1)):
                p.dma(yt[:, :, :tn], g.YD[d, :, t0:t0 + tn].re("(h k) t -> k h t", k=64), q=("sp" if d == 0 else "act"))
            p.tt(y0[:, :, :tn], y0[:, :, :tn], y1[:, :, :tn], ALU.add, eng="pool")
            for h in range(8):
                pm = B[nb % 8]
                nb += 1
                p.mm(pm[0:64, :tn], g.ones64, y0[:, h, :tn])
                p.stt(y1[:, h, :tn], pm[0:64, :tn], -1.0 / 64, y0[:, h, :tn], ALU.mult, ALU.add)
            p.act(y0[:, :, :tn], y1[:, :, :tn], AF.Square)
            for h in range(8):
                pm = B[nb % 8]
                nb += 1
                p.mm(pm[0:64, :tn], g.ones64, y0[:, h, :tn])
                p.ts(y0[:, h, :tn], pm[0:64, :tn], 1.0 / 64, ALU.mult, GN_EPS, ALU.add)
            p.act(y0[:, :, :tn], y0[:, :, :tn], AF.Sqrt)
            p.recip(y0[:, :, :tn], y0[:, :, :tn])
            p.tt(y1[:, :, :tn], y1[:, :, :tn], y0[:, :, :tn], ALU.mult, eng="pool")
            for h in range(8):
                p.ts(y1[:, h, :tn], y1[:, h, :tn], g.P64T[:, l, C_LG + h:C_LG + h + 1], ALU.mult,
                     g.P64T[:, l, C_LB + h:C_LB + h + 1], ALU.add)
            p.dma(aux[:, :, :tn], g.RO[0, :, t0:t0 + tn].re("(h k) t -> k h t", k=64), q="sp")
            p.tt(y1[:, :, :tn], y1[:, :, :tn], aux[:, :, :tn], ALU.add, eng="pool")
            p.dma(aux[:, :, :tn], g.RO[1, :, t0:t0 + tn].re("(h k) t -> k h t", k=64), q="sp")
            p.tt(ybin[:, :, :tn], y1[:, :, :tn], aux[:, :, :tn], ALU.mult)
            for cc in range(8):
                sga, sgb = sgat[cc % 2], sgbt[cc % 2]
                p.dma(sga[:, :tn], g.ZF[3456 + cc * 128:3456 + (cc + 1) * 128, t0:t0 + tn], q="sp")
                p.dma(sgb[:, :tn], g.ZF[4480 + cc * 128:4480 + (cc + 1) * 128, t0:t0 + tn], q="act")
                pa = B[nb % 8]
                nb += 1
                for jj in range(4):
                    p.mm(pa[:, :tn], wa[:, jj, cc * 128:(cc + 1) * 128], CO[:, jj, t0:t0 + tn], start=(jj == 0), stop=(jj == 3))
                pb = B[nb % 8]
                nb += 1
                for h in range(8):
                    p.mm(pb[:, :tn], wb[:, h, cc * 128:(cc + 1) * 128], ybin[:, h, :tn], start=(h == 0), stop=(h == 7))
                p.tt(m1[cc % 2][:, :tn], pa[:, :tn], sga[:, :tn], ALU.mult)
                p.tt(m2[cc % 2][:, :tn], pb[:, :tn], sgb[:, :tn], ALU.mult)
                p.tt(mrg[:, cc, :tn], m1[cc % 2][:, :tn], m2[cc % 2][:, :tn], ALU.add, eng="pool")
            for sub in range(tn // 128):
                tok = t0 + sub * 128
                kind = 0 if tok < TCTX else 1
                x, o = xt[nx % 2], xo[nx % 2]
                nx += 1
                p.dma(x, src[tok:tok + 128, :], q="sp")
                for hc in range(2):
                    po = B[nb % 8]
                    nb += 1
                    for cc in range(8):
                        p.mm(po, mrg[:, cc, sub * 128:(sub + 1) * 128], wo[:, cc, hc * 512:(hc + 1) * 512],
                             start=(cc == 0), stop=(cc == 7))
                    p.tt(o[:, hc * 512:(hc + 1) * 512], po, G1b[kind][:, hc * 512:(hc + 1) * 512], ALU.mult)
                p.tt(o, o, x, ALU.add, eng="pool")
                p.dma(last_dst[tok:tok + 128, :], o, q="pool", acc=True)
    p.barrier()


def stage_router(p, g, l, hT2, GW, DEST):
    with contextlib.ExitStack() as st:
        rwf = p.sb(st, "r_wf", [128, 8, 36])
        rwb = p.sb(st, "r_wb", [128, 8, 36], BF16)
        p.dma(rwf[:, :, 0:4], g.router_g[l].re("(c q) n -> q c n", q=128))
        p.dma(rwf[:, :, 4:36], g.router_e[l].re("(c q) n -> q c n", q=128))
        p.cp(rwb, rwf)
        RB = p.sb(st, "r_rb", [128, 36])
        p.dma(RB[:, 0:4], g.router_g_b[l].pb(128))
        p.dma(RB[:, 4:36], g.router_e_b[l].pb(128))
        LG = p.sb(st, "r_lg", [128, NT, 36])
        pl = [p.ps(st, "r_pl%d" % i, [128, 36]) for i in range(2)]
        for i in range(NT):
            pp = pl[i % 2]
            for c in range(8):
                p.mm(pp, hT2[:, c, i * 128:(i + 1) * 128], rwb[:, c, :], start=(c == 0), stop=(c == 7))
            p.cp(LG[:, i, :], pp, eng=("act" if i % 2 else "dve"))
        p.tt(LG, LG, RB.re("q (o e) -> q o e", o=1).bc([128, NT, 36]), ALU.add)
        lg = LG[:, :, 0:4]
        le = LG[:, :, 4:36].re("q i (g e) -> q i g e", e=8)
        mg = p.sb(st, "r_mg", [128, NT])
        oh = p.sb(st, "r_oh", [128, NT, 4])
        eg = p.sb(st, "r_eg", [128, NT, 4])
        pg = p.sb(st, "r_pg", [128, NT])
        tmp = p.sb(st, "r_tmp", [128, NT, 4, 8])
        les = p.sb(st, "r_les", [128, NT, 8])
        les2 = p.sb(st, "r_les2", [128, NT, 8])
        m1 = p.sb(st, "r_m1", [128, NT])
        m2 = p.sb(st, "r_m2", [128, NT])
        k1 = p.sb(st, "r_k1", [128, NT, 8])
        k2 = p.sb(st, "r_k2", [128, NT, 8])
        ex = p.sb(st, "r_ex", [128, NT, 8])

        def b3(t, n):
            return t.re("q (i o) -> q i o", o=1).bc([128, NT, n])
        p.red(mg, lg, ALU.max)
        p.tt(oh, lg, b3(mg, 4), ALU.is_equal)
        p.tt(eg, lg, b3(mg, 4), ALU.subtract)
        p.act(eg, eg, AF.Exp)
        p.red(pg, eg, ALU.add)
        p.recip(pg, pg)
        p.tt(tmp, le, oh.re("q i (g o) -> q i g o", o=1).bc([128, NT, 4, 8]), ALU.mult)
        p.red(les, tmp.re("q i g e -> q i e g"), ALU.add)
        p.red(m1, les, ALU.max)
        p.tt(k1, les, b3(m1, 8), ALU.is_equal)
        p.stt(les2, k1, -1e30, les, ALU.mult, ALU.add)
        p.red(m2, les2, ALU.max)
        p.tt(k2, les2, b3(m2, 8), ALU.is_equal)
        p.tt(m2, m2, m1, ALU.subtract)
        p.act(m2, m2, AF.Exp)
        p.ts(m2, m2, 1.0, ALU.add)
        p.recip(m2, m2)
        p.tt(GW[:, :, 0], m2, pg, ALU.mult)
        p.tt(GW[:, :, 1], pg, GW[:, :, 0], ALU.subtract)
        ohb = oh.re("q i (g o) -> q i g o", o=1).bc([128, NT, 4, 8])
        M1 = p.sb(st, "r_M1", [128, NT, 4, 8])
        M2 = p.sb(st, "r_M2", [128, NT, 4, 8])
        MM = p.sb(st, "r_MM", [128, NT, 4, 8])
        p.tt(M1, ohb, k1.re("q i (o e) -> q i o e", o=1).bc([128, NT, 4, 8]), ALU.mult)
        p.tt(M2, ohb, k2.re("q i (o e) -> q i o e", o=1).bc([128, NT, 4, 8]), ALU.mult)
        p.tt(MM, M1, M2, ALU.add)
        MMf = MM.re("q i g e -> q (i g e)")
        WI = p.sb(st, "r_WI", [128, NT, 32])
        TOT = p.sb(st, "r_TOT", [128, NT, 32])
        pw = [p.ps(st, "r_pw%d" % i, [128, 512]) for i in range(4)]
        NF = NT * 32
        for (c0, cn, k) in ((0, 512, 0), (512, NF - 512, 1)):
            p.mm(pw[k][:, :cn], g.MASK4[:, 0, 0, :], MMf[:, c0:c0 + cn])
            p.cp(WI.re("q i e -> q (i e)")[:, c0:c0 + cn], pw[k][:, :cn], eng="act")
            p.mm(pw[2 + k][:, :cn], g.onesf, MMf[:, c0:c0 + cn])
            p.cp(TOT.re("q i e -> q (i e)")[:, c0:c0 + cn], pw[2 + k][:, :cn], eng="dve")
        BASE = p.sb(st, "r_BASE", [128, NT, 32])
        p.memset(BASE[:, 0, :], 0.0)
        for i in range(1, NT):
            p.tt(BASE[:, i, :], BASE[:, i - 1, :], TOT[:, i - 1, :], ALU.add)
        p.tt(WI, WI, BASE, ALU.add)
        p.ts(TOT, WI, CAP - 0.5, ALU.is_ge)
        p.tt(WI, WI, g.EOFF.re("q (o e) -> q o e", o=1).bc([128, NT, 32]), ALU.add)
        p.ts(BASE, TOT, -1.0, ALU.mult, 1.0, ALU.add)
        p.tt(WI, WI, BASE, ALU.mult)
        p.stt(WI, TOT, float(NEXP * CAP), WI, ALU.mult, ALU.add)
        DF = p.sb(st, "r_DF", [128, NT, 2])
        for k, Mk in ((0, M1), (1, M2)):
            p.tt(Mk.re("q i g e -> q i (g e)"), Mk.re("q i g e -> q i (g e)"), WI, ALU.mult)
            p.red(DF[:, :, k], Mk.re("q i g e -> q i (g e)"), ALU.add)
        p.cp(DEST, DF)
    p.barrier()


def stage_moe(p, g, l, b, GW, DEST):
    NR = NEXP * CAP
    IOA = bass.IndirectOffsetOnAxis
    with contextlib.ExitStack() as st:
        G2b = []
        for kind, row in ((0, 2), (1, b)):
            t = p.sb(st, "e_g2b%d" % kind, [128, D])
            p.dma(t, g.MODR[l, row, G2:G2 + D].pb(128), q="act")
            G2b.append(t)
        with contextlib.ExitStack() as st2:
            hbt = [p.sb(st2, "e_hbt%d" % i, [128, D], BF16) for i in range(2)]
            for i in range(NT):
                hb = hbt[i % 2]
                p.dma(hb, g.H2T[i * 128:(i + 1) * 128, :], q="sp")
                for k in range(2):
                    idx = DEST[:, i, k:k + 1]
                    p.dma(g.XE, hb, q="pool", acc=True, extra_reads=[DEST],
                          fn=lambda e, hb=hb, idx=idx: e.indirect_dma_start(
                              out=A(g.XE), out_offset=IOA(ap=A(idx), axis=0), in_=A(hb), in_offset=None))
            stg = [p.sb(st2, "e_stg%d" % i, [128, 8, 512]) for i in range(2)]
            w1b = [p.sb(st2, "e_w1b%d" % i, [128, 8, 512], BF16) for i in range(2)]
            w3b = [p.sb(st2, "e_w3b%d" % i, [128, 8, 512], BF16) for i in range(2)]
            w2b = [p.sb(st2, "e_w2b%d" % i, [128, 4, 1024], BF16) for i in range(2)]
            xe = [p.sb(st2, "e_xe%d" % i, [128, NSL, D], BF16) for i in range(2)]
            hTe = p.sb(st2, "e_hTe", [128, 8, CAP], BF16)
            sl = [p.sb(st2, "e_sl%d" % i, [128, 512]) for i in range(2)]
            hid = p.sb(st2, "e_hid", [128, 4, CAP], BF16)
            ye = [p.sb(st2, "e_ye%d" % i, [128, NSL, D]) for i in range(2)]
            B = [p.ps(st2, "e_pb%d" % i, [128, 512]) for i in range(6)]
            pt = [p.ps(st2, "e_pt%d" % i, [128, 8, 128], BF16) for i in range(2)]
            nb = 0
            ns = 0
            nsl = 0
            npt = 0
            CT = [(0, 512), (512, CAP - 512)] if CAP > 512 else [(0, CAP)]
            for e in range(NEXP):
                k = e % 2
                for wsrc, wdst, ceng in ((g.exp_w1, w1b[k], "dve"), (g.exp_w3, w3b[k], "act")):
                    sg_ = stg[ns % 2]
                    p.dma(sg_, wsrc[l, e].re("(c q) n -> q c n", q=128), q=("sp" if ns % 2 == 0 else "act"))
                    p.cp(wdst, sg_, eng=ceng)
                    ns += 1
                sg_ = stg[ns % 2]
                sv = sg_.re("q c n -> q (c n)").re("q (c n) -> q c n", n=1024)
                p.dma(sv, g.exp_w2[l, e].re("(c q) n -> q c n", q=128), q=("sp" if ns % 2 == 0 else "act"))
                p.cp(w2b[k], sv, eng="pool")
                ns += 1
                x_ = xe[k]
                p.dma(x_, g.XE[e * CAP:(e + 1) * CAP, :].re("(j q) n -> q j n", q=128), q="sp")
                for j in range(NSL):
                    ptt = pt[npt % 2]
                    npt += 1
                    for c in range(8):
                        p.tr(ptt[:, c, :], x_[:, j, c * 128:(c + 1) * 128], g.identb)
                    p.cp(hTe[:, :, j * 128:(j + 1) * 128], ptt, eng=("act" if j % 2 else "dve"))
                for ff in range(4):
                    for (t0, tn) in CT:
                        p1 = B[nb % 6]
                        p3 = B[(nb + 1) % 6]
                        nb += 2
                        for kc in range(8):
                            p.mm(p1[:, :tn], w1b[k][:, kc, ff * 128:(ff + 1) * 128], hTe[:, kc, t0:t0 + tn],
                                 start=(kc == 0), stop=(kc == 7))
                        for kc in range(8):
                            p.mm(p3[:, :tn], w3b[k][:, kc, ff * 128:(ff + 1) * 128], hTe[:, kc, t0:t0 + tn],
                                 start=(kc == 0), stop=(kc == 7))
                        s_ = sl[nsl % 2]
                        nsl += 1
                        p.act(s_[:, :tn], p1[:, :tn], AF.Silu)
                        p.tt(hid[:, ff, t0:t0 + tn], s_[:, :tn], p3[:, :tn], ALU.mult)
                y_ = ye[k]
                for j in range(NSL):
                    for hc in range(2):
                        po = B[nb % 6]
                        nb += 1
                        for ff in range(4):
                            p.mm(po, hid[:, ff, j * 128:(j + 1) * 128], w2b[k][:, ff, hc * 512:(hc + 1) * 512],
                                 start=(ff == 0), stop=(ff == 3))
                        p.cp(y_[:, j, hc * 512:(hc + 1) * 512], po, eng=("act" if (j + hc) % 2 else "dve"))
                p.dma(g.YE[e * CAP:(e + 1) * CAP, :].re("(j q) n -> q j n", q=128), y_, q="pool", acc=True)
            p.barrier()
        ya = [p.sb(st, "e_ya%d" % i, [128, D]) for i in range(2)]
        yb = [p.sb(st, "e_yb%d" % i, [128, D]) for i in range(2)]
        xt = [p.sb(st, "e_xt%d" % i, [128, D]) for i in range(2)]
        for i in range(NT):
            tok = i * 128
            kind = 0 if i < 2 else 1
            a_, b_, x_ = ya[i % 2], yb[i % 2], xt[i % 2]
            p.dma(x_, g.XS[b][tok:tok + 128, :], q="sp")
            for k, dst in ((0, a_), (1, b_)):
                p.memset(dst, 0.0, eng="pool")
                idx = DEST[:, i, k:k + 1]
                p.dma(dst, g.YE, q="pool", extra_reads=[DEST],
                      fn=lambda e, dst=dst, idx=idx: e.indirect_dma_start(
                          out=A(dst), out_offset=None, in_=A(g.YE), in_offset=IOA(ap=A(idx), axis=0)))
            p.ts(a_, a_, GW[:, i, 0:1], ALU.mult)
            p.stt(a_, b_, GW[:, i, 1:2], a_, ALU.mult, ALU.add)
            p.tt(a_, a_, G2b[kind], ALU.mult, eng="pool")
            p.tt(a_, a_, x_, ALU.add, eng="pool")
            p.dma(g.XS[b][tok:tok + 128, :], a_, q="pool", acc=True)
    p.barrier()


def stage_final(p, g, b):
    with contextlib.ExitStack() as st:
        gt = p.sb(st, "f_gt", [128, D])
        p.dma(gt, g.final_g.pb(128))
        xts = [p.sb(st, "f_xt%d" % i, [128, D]) for i in range(2)]
        sqs = [p.sb(st, "f_sq%d" % i, [128, D]) for i in range(2)]
        sss = [p.sb(st, "f_ss%d" % i, [128, 2]) for i in range(2)]
        for i in range(TLAT // 128):
            xt, sq, ss = xts[i % 2], sqs[i % 2], sss[i % 2]
            p.dma(xt, g.XS[b][TCTX + i * 128:TCTX + (i + 1) * 128, :], q=("sp" if i % 2 == 0 else "act"))
            p.act(sq, xt, AF.Square)
            p.red(ss[:, 0:1], sq, ALU.add)
            p.ts(ss[:, 1:2], ss[:, 0:1], 1.0 / D, ALU.mult, EPS, ALU.add)
            p.act(ss[:, 1:2], ss[:, 1:2], AF.Sqrt)
            p.recip(ss[:, 1:2], ss[:, 1:2])
            p.stt(sq, xt, ss[:, 1:2], gt, ALU.mult, ALU.mult)
            p.dma(g.out[b, i * 128:(i + 1) * 128, :], sq, q="pool", acc=True)
    p.barrier()
```

```python
import contextlib
import numpy as np
import concourse.bass as bass
import concourse.mybir as mybir
from concourse.bass_utils import run_bass_kernel_spmd

F32 = mybir.dt.float32
BF16 = mybir.dt.bfloat16
I32 = mybir.dt.int32
AF = mybir.ActivationFunctionType
ALU = mybir.AluOpType
AX = mybir.AxisListType

D = 1024
DEPTH = 4
NB = 2
TCTX = 256
TLAT = 2048
T = TCTX + TLAT
NT = T // 128
NIN = 5504
RW0 = 1536
NEXP = 32
DFF = 512
CAP = 640
NSL = CAP // 128
CH = 128
NCH = T // CH
EPS = 1e-6
GN_EPS = 64e-5
TT = [(0, 512), (512, 512), (1024, 512), (1536, 512), (2048, 256)]


class Tl:
    def __init__(self, ap, name=""):
        self.ap = ap
        self.name = name
        self.lw = {}
        self.rd = {}

    def __getitem__(self, k):
        return Vw(self, self.ap[k])

    def re(self, s, **kw):
        return Vw(self, self.ap.rearrange(s, **kw))

    def bc(self, shape):
        return Vw(self, self.ap.to_broadcast(list(shape)))

    def pb(self, n):
        return Vw(self, self.ap.partition_broadcast(n))


class Vw:
    def __init__(self, t, ap):
        self.t = t
        self.ap = ap

    def __getitem__(self, k):
        return Vw(self.t, self.ap[k])

    def re(self, s, **kw):
        return Vw(self.t, self.ap.rearrange(s, **kw))

    def bc(self, shape):
        return Vw(self.t, self.ap.to_broadcast(list(shape)))

    def pb(self, n):
        return Vw(self.t, self.ap.partition_broadcast(n))


def A(x):
    return x.ap if isinstance(x, (Tl, Vw)) else x


def TT_(x):
    if isinstance(x, Tl):
        return x
    if isinstance(x, Vw):
        return x.t
    return None


class P:
    KD = 8

    def __init__(self, nc):
        self.nc = nc
        self.es = contextlib.ExitStack()
        self.eng = {"pe": nc.tensor, "act": nc.scalar, "dve": nc.vector, "pool": nc.gpsimd, "sp": nc.sync}
        self.sem = {}
        self.cur = {}
        for e in ("pe", "act", "dve", "pool"):
            self.sem[e] = self.es.enter_context(nc.semaphore("s_" + e))
            self.cur[e] = 0
        self.dcount = {}
        for q in ("sp", "pool", "act"):
            self.dcount[q] = 0
            for s in range(self.KD):
                k = ("d", q, s)
                self.sem[k] = self.es.enter_context(nc.semaphore("d_%s_%d" % (q, s)))
                self.cur[k] = 0
        self.waited = {e: {} for e in self.eng}
        self.nins = 0

    def sb(self, stack, name, shape, dt=F32):
        self.uid = getattr(self, "uid", 0) + 1
        name = "%s_%d" % (name, self.uid)
        h = stack.enter_context(self.nc.sbuf_tensor(name, list(shape), dt))
        return Tl(h[:], name)

    def ps(self, stack, name, shape, dt=F32):
        self.uid = getattr(self, "uid", 0) + 1
        name = "%s_%d" % (name, self.uid)
        h = stack.enter_context(self.nc.psum_tensor(name, list(shape), dt))
        return Tl(h[:], name)

    def dram(self, name, shape, dt=F32, kind="Internal"):
        h = self.nc.dram_tensor(name, list(shape), dt, kind=kind)
        return Tl(h.ap(), name)

    def _deps(self, eng, reads, writes, acc=False):
        need = {}

        def add(tok):
            if tok is not None:
                k, v = tok
                if need.get(k, 0) < v:
                    need[k] = v
        for x in reads:
            t = TT_(x)
            if t is not None:
                for k, v in t.lw.items():
                    add((k, v))
        for x in writes:
            t = TT_(x)
            if t is not None:
                if not acc:
                    for k, v in t.lw.items():
                        add((k, v))
                for k, v in t.rd.items():
                    add((k, v))
        out = []
        w = self.waited[eng]
        for k, v in need.items():
            if k == eng and eng == "pe":
                continue
            if w.get(k, 0) >= v:
                continue
            w[k] = v
            out.append((k, v))
        return out

    def _commit(self, tok, reads, writes, acc=False):
        k, v = tok
        for x in reads:
            t = TT_(x)
            if t is not None and t.rd.get(k, 0) < v:
                t.rd[k] = v
        for x in writes:
            t = TT_(x)
            if t is not None:
                if acc:
                    if t.lw.get(k, 0) < v:
                        t.lw[k] = v
                else:
                    t.lw = {k: v}
                    t.rd = {}

    def op(self, eng, fn, reads, writes):
        e = self.eng[eng]
        for k, v in self._deps(eng, reads, writes):
            e.wait_ge(self.sem[k], v)
        ins = fn(e)
        self.cur[eng] += 1
        ins.then_inc(self.sem[eng], 1)
        self._commit((eng, self.cur[eng]), reads, writes)
        self.nins += 1

    def dma(self, out, in_, q="sp", acc=False, fn=None, extra_reads=(), **kw):
        e = self.eng[q]
        j = self.dcount[q]
        self.dcount[q] = j + 1
        slot, gen = j % self.KD, j // self.KD
        key = ("d", q, slot)
        rds = [in_] + list(extra_reads)
        deps = self._deps(q, rds, [out], acc=acc)
        if gen > 0 and self.waited[q].get(key, 0) < 16 * gen:
            self.waited[q][key] = 16 * gen
            deps.append((key, 16 * gen))
        for k, v in deps:
            e.wait_ge(self.sem[k], v)
        if fn is None:
            ins = e.dma_start(out=A(out), in_=A(in_), **kw)
        else:
            ins = fn(e)
        ins.then_inc(self.sem[key], 16)
        self.cur[key] = 16 * (gen + 1)
        self._commit((key, 16 * (gen + 1)), rds, [out], acc=acc)
        self.nins += 1

    def barrier(self, engines=None):
        for en, e in self.eng.items():
            if engines is not None and en not in engines:
                continue
            w = self.waited[en]
            for k, v in self.cur.items():
                if v == 0 or (k == en and en == "pe"):
                    continue
                if w.get(k, 0) >= v:
                    continue
                w[k] = v
                e.wait_ge(self.sem[k], v)

    def mm(self, out, lhsT, rhs, start=True, stop=True):
        self.op("pe", lambda e: e.matmul(A(out), A(lhsT), A(rhs), start=start, stop=stop), [lhsT, rhs], [out])

    def tr(self, out, in_, ident):
        self.op("pe", lambda e: e.transpose(A(out), A(in_), A(ident)), [in_, ident], [out])

    def act(self, out, in_, func, bias=None, scale=None, accum=None, extra_reads=()):
        kw = {}
        rd = [in_] + list(extra_reads)
        if bias is not None:
            kw["bias"] = A(bias)
            rd.append(bias)
        if scale is not None:
            kw["scale"] = A(scale)
            rd.append(scale)
        wr = [out]
        if accum is not None:
            kw["accum_out"] = A(accum)
            wr.append(accum)
        self.op("act", lambda e: e.activation(out=A(out), in_=A(in_), func=func, **kw), rd, wr)

    def tt(self, out, in0, in1, op, eng="dve"):
        self.op(eng, lambda e: e.tensor_tensor(out=A(out), in0=A(in0), in1=A(in1), op=op), [in0, in1], [out])

    def ts(self, out, in0, s1, op0, s2=None, op1=None, eng="dve", accum=None):
        rd = [in0, s1, s2]
        kw = {}
        if op1 is not None:
            kw["op1"] = op1
        wr = [out]
        if accum is not None:
            kw["accum_out"] = A(accum)
            wr.append(accum)
        self.op(eng, lambda e: e.tensor_scalar(out=A(out), in0=A(in0), scalar1=A(s1), scalar2=A(s2), op0=op0, **kw), rd, wr)

    def stt(self, out, in0, scalar, in1, op0, op1, eng="dve"):
        self.op(eng, lambda e: e.scalar_tensor_tensor(out=A(out), in0=A(in0), scalar=A(scalar), in1=A(in1), op0=op0, op1=op1),
                [in0, scalar, in1], [out])

    def cp(self, out, in_, eng="dve"):
        if eng == "act":
            self.op("act", lambda e: e.copy(out=A(out), in_=A(in_)), [in_], [out])
        else:
            self.op(eng, lambda e: e.tensor_copy(out=A(out), in_=A(in_)), [in_], [out])

    def recip(self, out, in_):
        self.op("dve", lambda e: e.reciprocal(out=A(out), in_=A(in_)), [in_], [out])

    def memset(self, out, val, eng="dve"):
        self.op(eng, lambda e: e.memset(A(out), val), [], [out])

    def red(self, out, in_, op, axis=AX.X, eng="dve"):
        self.op(eng, lambda e: e.tensor_reduce(out=A(out), in_=A(in_), axis=axis, op=op), [in_], [out])


class G:
    pass


SH1, SC1, G1, SH2, SC2, G2 = [i * D for i in range(6)]
C_MU, C_CONV = 0, 15
C_KK, C_KA, C_RK, C_W0, C_A0, C_LG, C_LB = 0, 8, 16, 24, 40, 56, 64
C2_KK, C2_KA, C2_RK, C2_W0, C2_A0, C2_LG, C2_LB = 27, 31, 35, 39, 47, 55, 59
NP128 = 63


def stage_params(p, g):
    nc = p.nc
    with contextlib.ExitStack() as st:
        crow = p.sb(st, "crow", [3, D])
        p.dma(crow, g.c3)
        srow = p.sb(st, "srow", [3, D])
        p.act(srow, crow, AF.Silu)
        sT = p.sb(st, "sT", [128, 8, 3])
        pst = p.ps(st, "pst", [128, 8, 4])
        for c in range(8):
            p.tr(pst[:, c, 0:3], srow[:, c * 128:(c + 1) * 128], g.ident[0:3, 0:3])
        p.cp(sT, pst[:, :, 0:3])
        wst = [p.sb(st, "wst%d" % i, [128, 8, 512]) for i in range(2)]
        bm = p.sb(st, "bm", [3, 6 * D])
        mrow = p.sb(st, "mrow", [3, 6 * D])
        pm = [p.ps(st, "pm%d" % i, [3, 512]) for i in range(2)]
        k = 0
        for l in range(DEPTH):
            p.dma(bm, g.b_mod[l].pb(3))
            for cg in range(12):
                ws = wst[k % 2]
                p.dma(ws, g.w_mod[l].re("(c q) n -> q c n", q=128)[:, :, cg * 512:(cg + 1) * 512],
                      q=("sp" if k % 2 == 0 else "act"))
                pp = pm[k % 2]
                for kc in range(8):
                    p.mm(pp, sT[:, kc, :], ws[:, kc, :], start=(kc == 0), stop=(kc == 7))
                p.tt(mrow[:, cg * 512:(cg + 1) * 512], pp, bm[:, cg * 512:(cg + 1) * 512], ALU.add)
                k += 1
            p.dma(g.MODR[l], mrow, q="pool")
        pr128 = p.sb(st, "pr128", [NP128, 128])
        pr64 = p.sb(st, "pr64", [72, 64])
        pp128 = p.ps(st, "pp128", [128, 64])
        pp64 = p.ps(st, "pp64", [64, 72])
        for l in range(DEPTH):
            p.dma(pr128[0:15, :], g.shift_mu[l].re("(c q) -> c q", q=128))
            p.dma(pr128[15:27, :], g.conv_w[l].re("j (c q) -> (j c) q", q=128))
            p.dma(pr64[C_KK:C_KK + 8, :], g.k_k[l].re("(h k) -> h k", k=64))
            p.dma(pr64[C_KA:C_KA + 8, :], g.k_a[l].re("(h k) -> h k", k=64))
            p.dma(pr64[C_RK:C_RK + 8, :], g.r_k[l])
            p.dma(pr64[C_W0:C_W0 + 16, :], g.w0[l].re("d (h k) -> (d h) k", k=64))
            p.dma(pr64[C_A0:C_A0 + 16, :], g.a0[l].re("d (h k) -> (d h) k", k=64))
            p.dma(pr64[C_LG:C_LG + 8, :], g.lnx_g[l].re("(h k) -> h k", k=64))
            p.dma(pr64[C_LB:C_LB + 8, :], g.lnx_b[l].re("(h k) -> h k", k=64))
            p.dma(pr128[C2_KK:C2_KK + 4, :], g.k_k[l].re("(c q) -> c q", q=128))
            p.dma(pr128[C2_KA:C2_KA + 4, :], g.k_a[l].re("(c q) -> c q", q=128))
            p.dma(pr128[C2_RK:C2_RK + 4, :], g.r_k[l].re("(c a) k -> c (a k)", a=2))
            p.dma(pr128[C2_W0:C2_W0 + 8, :], g.w0[l].re("d (c q) -> (d c) q", q=128))
            p.dma(pr128[C2_A0:C2_A0 + 8, :], g.a0[l].re("d (c q) -> (d c) q", q=128))
            p.dma(pr128[C2_LG:C2_LG + 4, :], g.lnx_g[l].re("(c q) -> c q", q=128))
            p.dma(pr128[C2_LB:C2_LB + 4, :], g.lnx_b[l].re("(c q) -> c q", q=128))
            p.tr(pp128[:, 0:NP128], pr128, g.ident[0:NP128, 0:NP128])
            p.cp(g.P128T[:, l, :], pp128[:, 0:NP128])
            p.tr(pp64, pr64, g.ident[0:72, 0:72])
            p.cp(g.P64T[:, l, :], pp64)
        p.ts(g.OMM, g.P128T[:, :, C_MU:C_MU + 15], -1.0, ALU.mult, 1.0, ALU.add)
        p.ts(g.HMU, g.P128T[:, :, C_MU:C_MU + 15], 0.5, ALU.mult)
        p.ts(g.OMKA, g.P128T[:, :, C2_KA:C2_KA + 4], -1.0, ALU.mult, 1.0, ALU.add)
    p.barrier()


def stage_norm(p, g, src, l, b, second, hT, hTf=None):
    SHc, SCc = (SH2, SC2) if second else (SH1, SC1)
    ng = g.norm2_g if second else g.norm1_g
    with contextlib.ExitStack() as st:
        gt = p.sb(st, "gt", [128, D])
        p.dma(gt, ng[l].pb(128))
        Ab, Bb = [], []
        for kind, row in ((0, 2), (1, b)):
            sc = p.sb(st, "scb%d" % kind, [128, D])
            p.dma(sc, g.MODR[l, row, SCc:SCc + D].pb(128))
            a = p.sb(st, "Ab%d" % kind, [128, D])
            p.stt(a, sc, 1.0, gt, ALU.add, ALU.mult)
            bb = p.sb(st, "Bb%d" % kind, [128, D])
            p.dma(bb, g.MODR[l, row, SHc:SHc + D].pb(128))
            Ab.append(a)
            Bb.append(bb)
        xts = [p.sb(st, "xt%d" % i, [128, D]) for i in range(2)]
        sqs = [p.sb(st, "sq%d" % i, [128, D]) for i in range(2)]
        hns = [p.sb(st, "hn%d" % i, [128, D]) for i in range(2)]
        hbs = [p.sb(st, "hb%d" % i, [128, D], BF16) for i in range(2)]
        sss = [p.sb(st, "ss%d" % i, [128, 2]) for i in range(2)]
        ptr = [p.ps(st, "ptr%d" % i, [128, 8, 128], BF16) for i in range(2)]
        for i in range(NT):
            kind = 0 if i < 2 else 1
            xt, sq, hn, hb, ss, pt = xts[i % 2], sqs[i % 2], hns[i % 2], hbs[i % 2], sss[i % 2], ptr[i % 2]
            p.dma(xt, src[i * 128:(i + 1) * 128, :], q=("sp" if i % 2 == 0 else "act"))
            p.act(sq, xt, AF.Square)
            p.red(ss[:, 0:1], sq, ALU.add)
            p.ts(ss[:, 1:2], ss[:, 0:1], 1.0 / D, ALU.mult, EPS, ALU.add)
            p.act(ss[:, 1:2], ss[:, 1:2], AF.Sqrt)
            p.recip(ss[:, 1:2], ss[:, 1:2])
            p.stt(hn, xt, ss[:, 1:2], Ab[kind], ALU.mult, ALU.mult)
            p.tt(hb, hn, Bb[kind], ALU.add, eng="pool")
            if second:
                p.dma(g.H2T[i * 128:(i + 1) * 128, :], hb, q="pool", acc=True)
            for c in range(8):
                p.tr(pt[:, c, :], hb[:, c * 128:(c + 1) * 128], g.identb)
            p.cp(hT[:, :, i * 128:(i + 1) * 128], pt, eng="act")


def stage_proj(p, g, hT, l, b):
    groups = [(c0, min(512, NIN - c0)) for c0 in range(0, NIN, 512)]
    with contextlib.ExitStack() as st:
        wbf = [p.sb(st, "pwbf%d" % i, [128, 8, 512], BF16) for i in range(2)]

        def loadg(gi):
            c0, w = groups[gi]
            p.dma(wbf[gi % 2][:, :, :w], g.w_in[l].re("(c q) n -> q c n", q=128)[:, :, c0:c0 + w], q="pool")
        loadg(0)
        ot = [p.sb(st, "pot%d" % i, [128, T]) for i in range(2)]
        zt = [p.sb(st, "pzt%d" % i, [128, T]) for i in range(2)]
        pss = [p.ps(st, "pps%d" % i, [128, 512]) for i in range(4)]
        k = 0
        kk = 0
        for gi, (c0, w) in enumerate(groups):
            wb = wbf[gi % 2]
            if gi + 1 < len(groups):
                loadg(gi + 1)
            for cc in range(w // 128):
                col = c0 + cc * 128
                chunk = col // 128
                o = ot[k % 2]
                for (t0, tn) in TT:
                    ps = pss[kk % 4]
                    for kc in range(8):
                        p.mm(ps[:, :tn], wb[:, kc, cc * 128:(cc + 1) * 128], hT[:, kc, t0:t0 + tn],
                             start=(kc == 0), stop=(kc == 7))
                    if chunk >= 27:
                        p.act(o[:, t0:t0 + tn], ps[:, :tn], AF.Sigmoid)
                    elif kk % 2 == 0:
                        p.cp(o[:, t0:t0 + tn], ps[:, :tn], eng="act")
                    else:
                        p.cp(o[:, t0:t0 + tn], ps[:, :tn], eng="dve")
                    kk += 1
                if 12 <= chunk < 27:
                    j = chunk - 12
                    z = zt[k % 2]
                    om = g.OMM[:, l, j:j + 1]
                    hm = g.HMU[:, l, j:j + 1]
                    p.ts(z, o, om, ALU.mult)
                    for (a0, a1) in ((0, TCTX), (TCTX, T)):
                        p.stt(z[:, a0 + 1:a1], o[:, a0:a1 - 1], hm, z[:, a0 + 1:a1], ALU.mult, ALU.add)
                        p.stt(z[:, a0:a1 - 1], o[:, a0 + 1:a1], hm, z[:, a0:a1 - 1], ALU.mult, ALU.add)
                    o = z
                p.dma(g.ZF[col:col + 128, :], o, q="sp", acc=True)
                k += 1


WEIGHTS = [
    ("w_mod", [DEPTH, D, 6 * D]), ("b_mod", [DEPTH, 6 * D]), ("norm1_g", [DEPTH, D]), ("norm2_g", [DEPTH, D]),
    ("w_in", [DEPTH, D, NIN]), ("shift_mu", [DEPTH, 1920]), ("conv_w", [DEPTH, 3, 512]),
    ("w_up", [DEPTH, 2, 64, 512]), ("w0", [DEPTH, 2, 512]), ("a_up", [DEPTH, 2, 64, 512]), ("a0", [DEPTH, 2, 512]),
    ("g_up", [DEPTH, 128, 512]), ("k_k", [DEPTH, 512]), ("k_a", [DEPTH, 512]), ("r_k", [DEPTH, 8, 64]),
    ("lnx_g", [DEPTH, 512]), ("lnx_b", [DEPTH, 512]), ("w_a_out", [DEPTH, 512, D]), ("w_b_out", [DEPTH, 512, D]),
    ("w_o", [DEPTH, D, D]), ("router_g", [DEPTH, D, 4]), ("router_g_b", [DEPTH, 4]),
    ("router_e", [DEPTH, D, NEXP]), ("router_e_b", [DEPTH, NEXP]),
    ("exp_w1", [DEPTH, NEXP, D, DFF]), ("exp_w3", [DEPTH, NEXP, D, DFF]), ("exp_w2", [DEPTH, NEXP, DFF, D]),
    ("final_g", [D]),
]


def build(stages="all", dbg=()):
    nc = bass.Bass("TRN2", target_bir_lowering=False)
    p = P(nc)
    g = G()
    g.p = p

    def inp(name, shape):
        return Tl(nc.dram_tensor(name, list(shape), F32, kind="ExternalInput").ap(), name)
    g.xin = inp("xin", [NB, T, D])
    g.c3 = inp("c3", [3, D])
    g.idin = inp("idin", [128, 128])
    g.mskin = inp("mskin", [128, 2, 4, 128])
    g.eoffin = inp("eoffin", [128, NEXP])
    for name, shape in WEIGHTS:
        setattr(g, name, inp(name, shape))
    g.out = Tl(nc.dram_tensor("out", [NB, TLAT, D], F32, kind="ExternalOutput").ap(), "out")
    g.MODR = p.dram("MODR", [DEPTH, 3, 6 * D])
    g.ZF = p.dram("ZF", [NIN, T])
    g.XS = [p.dram("XS%d" % b, [T, D]) for b in range(NB)]
    g.SCF = p.dram("SCF", [2, 512, NCH, 4, CH], BF16)
    g.SCT = p.dram("SCT", [2, NCH, CH, 2, 512], BF16)
    g.VTD = p.dram("VTD", [NCH, CH, 512], BF16)
    g.WCD = p.dram("WCD", [512, 2, NCH])
    g.RO = p.dram("RO", [2, 512, T])
    g.YD = p.dram("YD", [2, 512, T])
    g.H2T = p.dram("H2T", [T, D], BF16)
    g.XE = p.dram("XE", [NEXP * CAP + 1, D], BF16)
    g.YE = p.dram("YE", [NEXP * CAP + 1, D])
    g.YEZ = p.dram("YEZ", [1, D])
    dbg_out = {}
    for name, shape in dbg:
        dbg_out[name] = Tl(nc.dram_tensor("dbg_" + name, list(shape), F32, kind="ExternalOutput").ap(), name)
    g.dbg = dbg_out
    top = p.es
    g.ident = p.sb(top, "ident", [128, 128])
    g.identb = p.sb(top, "identb", [128, 128], BF16)
    p.dma(g.ident, g.idin)
    p.cp(g.identb, g.ident)
    g.P128T = p.sb(top, "P128T", [128, DEPTH, NP128])
    g.P64T = p.sb(top, "P64T", [64, DEPTH, 72])
    g.OMM = p.sb(top, "OMM", [128, DEPTH, 15])
    g.HMU = p.sb(top, "HMU", [128, DEPTH, 15])
    g.OMKA = p.sb(top, "OMKA", [128, DEPTH, 4])
    g.MASK4 = p.sb(top, "MASK4", [128, 2, 4, 128])
    p.dma(g.MASK4, g.mskin)
    g.ones64 = p.sb(top, "ones64", [64, 64])
    p.memset(g.ones64, 1.0)
    g.onesf = p.sb(top, "onesf", [128, 128])
    p.memset(g.onesf, 1.0)
    g.EOFF = p.sb(top, "EOFF", [128, NEXP])
    p.dma(g.EOFF, g.eoffin)
    with contextlib.ExitStack() as stz:
        zt = p.sb(stz, "zrow", [1, D])
        p.memset(zt, 0.0)
        p.dma(g.YE[NEXP * CAP:NEXP * CAP + 1, :], zt, q="pool")
        p.barrier()
    g.ones2 = p.sb(top, "ones2", [128, 128])
    p.memset(g.ones2, 0.0)
    p.memset(g.ones2[0:64, 0:64], 1.0)
    p.memset(g.ones2[64:128, 64:128], 1.0)
    g.RM = p.sb(top, "RM", [128, TH])
    p.memset(g.RM, 1.0)
    p.memset(g.RM.re("k (c t) -> k c t", t=CH)[:, :, 0:1], 0.0)

    stage_params(p, g)
    nl = DEPTH if stages == "all" else stages[0]
    nb = NB if stages == "all" else stages[1]
    upto = "z" if stages == "all" else stages[2]
    for b in range(nb):
        for l in range(nl):
            src = g.xin[b] if l == 0 else g.XS[b]
            with contextlib.ExitStack() as st:
                hT = p.sb(st, "hT", [128, 8, T], BF16)
                stage_norm(p, g, src, l, b, False, hT)
                if upto == "A":
                    if "hT" in g.dbg:
                        with contextlib.ExitStack() as s2:
                            tmp = p.sb(s2, "dbgtmp", [128, 8, T])
                            p.cp(tmp, hT)
                            p.dma(g.dbg["hT"], tmp)
                            p.barrier()
                    p.barrier()
                    continue
                stage_proj(p, g, hT, l, b)
                p.barrier()
            if upto == "B":
                continue
            with contextlib.ExitStack() as st:
                CO = p.sb(st, "CO", [128, 4, T], BF16)
                stage_conv(p, g, l, b, CO)
                stage_prep(p, g, l, b)
                if upto == "D":
                    continue
                stage_scan(p, g, l, b)
                if upto == "E":
                    continue
                stage_mix(p, g, l, b, CO, src, g.XS[b])
            if upto == "G":
                continue
            with contextlib.ExitStack() as st:
                GW = p.sb(st, "GW", [128, NT, 2])
                DEST = p.sb(st, "DEST", [128, NT, 2], I32)
                with contextlib.ExitStack() as st2:
                    hT = p.sb(st2, "hT2", [128, 8, T], BF16)
                    stage_norm(p, g, g.XS[b], l, b, True, hT)
                    stage_router(p, g, l, hT, GW, DEST)
                stage_moe(p, g, l, b, GW, DEST)
        if upto == "z":
            stage_final(p, g, b)
    if "ZF" in g.dbg:
        p.barrier()
        with contextlib.ExitStack() as s2:
            tmp = p.sb(s2, "dbgtmp", [128, T])
            for c in range(NIN // 128):
                p.dma(tmp, g.ZF[c * 128:(c + 1) * 128, :])
                p.dma(g.dbg["ZF"][c * 128:(c + 1) * 128, :], tmp)
            p.barrier()
    for nm, t in (("YD", g.YD.re("d n t -> (d n) t")), ("RO", g.RO.re("q n t -> (q n) t")), ("XS0", g.XS[0])):
        if nm in g.dbg:
            with contextlib.ExitStack() as s2:
                n, w = t.ap.shape
                tmp = p.sb(s2, "dbgtmp3", [128, w])
                for c in range(n // 128):
                    p.dma(tmp, t[c * 128:(c + 1) * 128, :])
                    p.dma(g.dbg[nm][c * 128:(c + 1) * 128, :], tmp)
                p.barrier()
    if "MODR" in g.dbg:
        with contextlib.ExitStack() as s2:
            tmp = p.sb(s2, "dbgtmp2", [12, 6 * D])
            p.dma(tmp, g.MODR.re("l r n -> (l r) n"))
            p.dma(g.dbg["MODR"], tmp)
            p.barrier()
    p.barrier()
    p.es.close()
    return nc, p


def make_in_maps(inputs, ncores=8):
    idm = np.eye(128, dtype=np.float32)
    ii = np.arange(128)
    us = (ii[None, :] > ii[:, None]).astype(np.float32)
    ui = (ii[None, :] >= ii[:, None]).astype(np.float32)
    msk = np.ascontiguousarray(np.stack([np.stack([us, ui, us, ui], 0), np.stack([us.T, ui.T, us.T, ui.T], 0)], 0).transpose(2, 0, 1, 3))
    eoff = np.ascontiguousarray(np.broadcast_to((np.arange(NEXP, dtype=np.float32) * CAP)[None, :], (128, NEXP)))
    maps = []
    for core in range(ncores):
        b0 = core * NB
        xin = np.concatenate([inputs["ctx"][b0:b0 + NB], inputs["x"][b0:b0 + NB]], axis=1)
        c3 = np.concatenate([inputs["c"][b0:b0 + NB], inputs["c_ctx"][None, :]], axis=0)
        m = {"xin": np.ascontiguousarray(xin, dtype=np.float32), "c3": np.ascontiguousarray(c3, dtype=np.float32), "idin": idm,
             "mskin": msk, "eoffin": eoff}
        for name, _ in WEIGHTS:
            m[name] = np.ascontiguousarray(inputs[name], dtype=np.float32)
        maps.append(m)
    return maps


def kernel(**inputs):
    inputs = {k: np.asarray(v) for k, v in inputs.items()}
    nc, p = build("all")
    maps = make_in_maps(inputs, 8)
    res = run_bass_kernel_spmd(nc, maps, core_ids=list(range(8)))
    return np.concatenate([r["out"] for r in res.results], axis=0).astype(np.float32)


def stage_conv(p, g, l, b, CO):
    with contextlib.ExitStack() as st:
        bgt = [p.sb(st, "cbg%d" % i, [128, T]) for i in range(2)]
        cgt = [p.sb(st, "ccg%d" % i, [128, T]) for i in range(2)]
        hat = [p.sb(st, "cha%d" % i, [128, T]) for i in range(2)]
        ut = [p.sb(st, "cu%d" % i, [128, T]) for i in range(2)]
        ott = [p.sb(st, "co%d" % i, [128, T]) for i in range(2)]
        for j in range(4):
            bg, cg, ha, u, o = bgt[j % 2], cgt[j % 2], hat[j % 2], ut[j % 2], ott[j % 2]
            p.dma(bg, g.ZF[j * 128:(j + 1) * 128, :], q="sp")
            p.dma(cg, g.ZF[512 + j * 128:512 + (j + 1) * 128, :], q="act")
            p.dma(ha, g.ZF[1024 + j * 128:1024 + (j + 1) * 128, :], q="sp")
            w0 = g.P128T[:, l, C_CONV + 0 * 4 + j:C_CONV + 0 * 4 + j + 1]
            w1 = g.P128T[:, l, C_CONV + 1 * 4 + j:C_CONV + 1 * 4 + j + 1]
            w2 = g.P128T[:, l, C_CONV + 2 * 4 + j:C_CONV + 2 * 4 + j + 1]
            p.tt(u, cg, ha, ALU.mult, eng="pool")
            p.ts(o, u, w1, ALU.mult)
            p.stt(o[:, 1:TCTX], u[:, 0:TCTX - 1], w0, o[:, 1:TCTX], ALU.mult, ALU.add)
            p.stt(o[:, 0:TCTX - 1], u[:, 1:TCTX], w2, o[:, 0:TCTX - 1], ALU.mult, ALU.add)
            if j < 2:
                ug = u[:, TCTX:T].re("q (r w) -> q r w", w=64)
                og = o[:, TCTX:T].re("q (r w) -> q r w", w=64)
                p.stt(og[:, :, 1:64], ug[:, :, 0:63], w0, og[:, :, 1:64], ALU.mult, ALU.add)
                p.stt(og[:, :, 0:63], ug[:, :, 1:64], w2, og[:, :, 0:63], ALU.mult, ALU.add)
            else:
                p.stt(o[:, TCTX + 64:T], u[:, TCTX:T - 64], w0, o[:, TCTX + 64:T], ALU.mult, ALU.add)
                p.stt(o[:, TCTX:T - 64], u[:, TCTX + 64:T], w2, o[:, TCTX:T - 64], ALU.mult, ALU.add)
            p.tt(CO[:, j, :], o, bg, ALU.mult, eng="pool")
    p.barrier()


TH = 1152
THT = [(0, 512), (512, 512), (1024, 128)]
NCH2 = TH // CH


def stage_prep(p, g, l, b):
    with contextlib.ExitStack() as st:
        tmpf = p.sb(st, "dtmpf", [128, T])
        tzw = [p.sb(st, "tzw%d" % d, [64, T], BF16) for d in range(2)]
        zab = [p.sb(st, "zab%d" % d, [64, T], BF16) for d in range(2)]
        sgz = p.sb(st, "sgz", [128, T], BF16)
        for d in range(2):
            p.dma(tmpf[0:64, :], g.ZF[RW0 + 1536 + d * 64:RW0 + 1536 + (d + 1) * 64, :])
            p.act(tzw[d], tmpf[0:64, :], AF.Tanh)
            p.dma(tmpf[0:64, :], g.ZF[RW0 + 1664 + d * 64:RW0 + 1664 + (d + 1) * 64, :])
            p.cp(zab[d], tmpf[0:64, :], eng="act")
        p.dma(tmpf, g.ZF[RW0 + 1792:RW0 + 1920, :])
        p.act(sgz, tmpf, AF.Sigmoid)
        wupf = p.sb(st, "wupf", [64, 2, 512])
        aupf = p.sb(st, "aupf", [64, 2, 512])
        gupf = p.sb(st, "gupf", [128, 512])
        wupb = p.sb(st, "wupb", [64, 2, 512], BF16)
        aupb = p.sb(st, "aupb", [64, 2, 512], BF16)
        gupb = p.sb(st, "gupb", [128, 512], BF16)
        p.dma(wupf, g.w_up[l].re("d r n -> r d n"))
        p.dma(aupf, g.a_up[l].re("d r n -> r d n"))
        p.dma(gupf, g.g_up[l])
        p.cp(wupb, wupf)
        p.cp(aupb, aupf)
        p.cp(gupb, gupf)
        rt = p.sb(st, "d_r", [128, TH])
        kt = p.sb(st, "d_k", [128, TH])
        vt = p.sb(st, "d_v", [128, TH])
        kk = p.sb(st, "d_kk", [128, TH])
        X = [p.sb(st, "d_x%d" % i, [128, TH]) for i in range(8)]
        ob = {n: p.sb(st, "d_ob_" + n, [128, TH], BF16) for n in ("kk", "r", "kh", "bh", "kp", "bp", "v")}
        tst = [p.sb(st, "d_tst%d" % i, [128, NCH2, 128], BF16) for i in range(3)]
        wc = p.sb(st, "d_wc", [128, NCH2])
        ps = [p.ps(st, "d_ps%d" % i, [128, 512]) for i in range(3)]
        pst = [p.ps(st, "d_pst%d" % i, [128, 16, 128], BF16) for i in range(2)]
        npst = 0
        nps = 0

        def pk(col):
            return g.P128T[:, l, col:col + 1]

        def tposed(src_b, dst_dram, q, k):
            nonlocal npst
            pt = pst[npst % 2]
            npst += 1
            stg = tst[k]
            for c in range(NCH2):
                p.tr(pt[:, c, :], src_b[:, c * CH:(c + 1) * CH], g.identb)
            p.cp(stg, pt[:, 0:NCH2, :], eng=("act" if k % 2 else "dve"))
            p.dma(dst_dram, stg, q=q, acc=True)

        for pr in range(4):
            r0 = pr * 128
            for hf in range(2):
                tb = hf * TH
                cb = hf * NCH2
                p.dma(rt, g.ZF[RW0 + r0:RW0 + r0 + 128, tb:tb + TH], q="sp")
                p.dma(kt, g.ZF[RW0 + 512 + r0:RW0 + 512 + r0 + 128, tb:tb + TH], q="act")
                p.dma(vt, g.ZF[RW0 + 1024 + r0:RW0 + 1024 + r0 + 128, tb:tb + TH], q="sp")
                p.ts(X[0], kt, pk(C2_KK + pr), ALU.mult, eng="pool")
                p.act(X[1], X[0], AF.Square)
                for (t0, tn) in THT:
                    pp = ps[nps % 3]
                    nps += 1
                    p.mm(pp[:, :tn], g.ones2, X[1][:, t0:t0 + tn])
                    p.ts(X[2][:, t0:t0 + tn], pp[:, :tn], 1e-12, ALU.add)
                p.act(X[2], X[2], AF.Sqrt)
                p.recip(X[2], X[2])
                p.tt(kk, X[0], X[2], ALU.mult)
                p.cp(ob["v"], vt, eng="pool")
                tposed(ob["v"], g.VTD[cb:cb + NCH2, :, r0:r0 + 128].re("c t n -> t c n"), "pool", 0)
                for d in range(2):
                    lw, a, kd, bb, L = X[0], X[1], X[2], X[3], X[4]
                    for (t0, tn) in THT:
                        pp = ps[nps % 3]
                        nps += 1
                        p.mm(pp[:, :tn], wupb[:, d, r0:r0 + 128], tzw[d][:, tb + t0:tb + t0 + tn])
                        p.act(lw[:, t0:t0 + tn], pp[:, :tn], AF.Sigmoid, bias=pk(C2_W0 + d * 4 + pr))
                        pp = ps[nps % 3]
                        nps += 1
                        p.mm(pp[:, :tn], aupb[:, d, r0:r0 + 128], zab[d][:, tb + t0:tb + t0 + tn])
                        p.act(a[:, t0:t0 + tn], pp[:, :tn], AF.Sigmoid, bias=pk(C2_A0 + d * 4 + pr))
                    p.ts(lw, lw, -0.6065306597126334, ALU.mult, eng="pool")
                    p.ts(kd, a, pk(C2_KA + pr), ALU.mult, g.OMKA[:, l, pr:pr + 1], ALU.add)
                    p.tt(kd, kd, kt, ALU.mult)
                    p.tt(bb, kk, a, ALU.mult, eng="pool")
                    if d == 0:
                        p.cp(X[7], kd, eng="pool")
                    else:
                        p.tt(X[7], X[7], kd, ALU.add, eng="pool")
                    p.op("dve", lambda e: e.tensor_tensor_scan(out=A(L), data0=A(g.RM), data1=A(lw), initial=0.0,
                                                               op0=ALU.mult, op1=ALU.add), [g.RM, lw], [L])
                    if d == 1:
                        Lp = X[1]
                        p.tt(Lp, lw, L, ALU.subtract)
                        p.tt(Lp.re("k (c t) -> k c t", t=CH), Lp.re("k (c t) -> k c t", t=CH),
                             L.re("k (c t) -> k c t", t=CH)[:, :, CH - 1:CH].bc([128, NCH2, CH]), ALU.add)
                        end = 0
                    else:
                        Lp = L
                        end = CH - 1
                    E = X[5]
                    p.act(E, Lp, AF.Exp)
                    p.tt(ob["r"], rt, E, ALU.mult)
                    p.act(wc.re("k (c o) -> k c o", o=1), Lp.re("k (c t) -> k c t", t=CH)[:, :, end:end + 1], AF.Exp)
                    E2 = X[6]
                    p.act(E2, Lp, AF.Exp, scale=-1.0)
                    p.tt(kd, kd, E2, ALU.mult)
                    p.tt(bb, bb, E2, ALU.mult, eng="pool")
                    p.tt(lw, Lp, lw, ALU.subtract, eng="pool")
                    p.act(lw, lw, AF.Exp)
                    p.tt(ob["kk"], kk, lw, ALU.mult)
                    p.cp(ob["kh"], kd, eng="pool")
                    p.cp(ob["bh"], bb, eng="act")
                    wcb = wc.re("k (c o) -> k c o", o=1).bc([128, NCH2, CH])
                    p.tt(ob["kp"].re("k (c t) -> k c t", t=CH), kd.re("k (c t) -> k c t", t=CH), wcb, ALU.mult)
                    p.tt(ob["bp"].re("k (c t) -> k c t", t=CH), bb.re("k (c t) -> k c t", t=CH), wcb, ALU.mult, eng="pool")
                    for qi, n in enumerate(("kk", "r", "kh", "bh")):
                        p.dma(g.SCF[d, r0:r0 + 128, cb:cb + NCH2, qi, :], ob[n].re("k (c t) -> k c t", t=CH), q="pool", acc=True)
                    tposed(ob["kp"], g.SCT[d, cb:cb + NCH2, :, 0, r0:r0 + 128].re("c t n -> t c n"), "pool", 1)
                    tposed(ob["bp"], g.SCT[d, cb:cb + NCH2, :, 1, r0:r0 + 128].re("c t n -> t c n"), "pool", 2)
                    p.dma(g.WCD[r0:r0 + 128, d, cb:cb + NCH2], wc, q="pool", acc=True)
                p.ts(X[0], rt, pk(C2_RK + pr), ALU.mult, eng="pool")
                p.tt(X[0], X[0], X[7], ALU.mult)
                for (t0, tn) in THT:
                    pp = ps[nps % 3]
                    nps += 1
                    p.mm(pp[:, :tn], g.ones2, X[0][:, t0:t0 + tn])
                    p.tt(X[1][:, t0:t0 + tn], pp[:, :tn], vt[:, t0:t0 + tn], ALU.mult)
                    pp = ps[nps % 3]
                    nps += 1
                    p.mm(pp[:, :tn], gupb[:, r0:r0 + 128], sgz[:, tb + t0:tb + t0 + tn])
                    p.cp(X[2][:, t0:t0 + tn], pp[:, :tn], eng="act")
                p.dma(g.RO[0, r0:r0 + 128, tb:tb + TH], X[1], q="pool", acc=True)
                p.dma(g.RO[1, r0:r0 + 128, tb:tb + TH], X[2], q="pool", acc=True)
    p.barrier()


ORDER_B = [1, 0] + list(range(NCH - 1, 1, -1))


def stage_scan(p, g, l, b):
    with contextlib.ExitStack() as st:
        B = [p.ps(st, "e_b%d" % i, [128, 512]) for i in range(8)]

        def bv(i, n, w, parts=128):
            return B[i].re("s (a t) -> s a t", t=w)[0:parts, 0:n, :]
        WC = p.sb(st, "e_wc", [64, 2, 8, NCH])
        p.dma(WC, g.WCD.re("(h k) d c -> k d h c", k=64))
        ST = p.sb(st, "e_st", [64, 16, 64])
        STb = p.sb(st, "e_stb", [64, 16, 64], BF16)
        p.memset(ST, 0.0)
        p.memset(STb, 0.0, eng="pool")
        FQ = [p.sb(st, "e_fq%d" % i, [64, 2, 8, 4, 128], BF16) for i in range(2)]
        TQ = [p.sb(st, "e_tq%d" % i, [128, 2, 2, 8, 64], BF16) for i in range(2)]
        VT = [p.sb(st, "e_vt%d" % i, [128, 2, 8, 64], BF16) for i in range(2)]
        AMs = [p.sb(st, "e_am%d" % i, [128, 16, 4, 128], BF16) for i in range(2)]
        Xs = [[p.sb(st, "e_x%d_%d" % (i, q), [128, 4, 128]) for q in range(4)] for i in range(2)]
        XTs = [[p.sb(st, "e_xt%d_%d" % (i, q), [128, 4, 128]) for q in range(4)] for i in range(2)]
        Ps = [[p.sb(st, "e_p%d_%d" % (i, q), [128, 4, 128]) for q in range(4)] for i in range(2)]
        RT = p.sb(st, "e_rt", [128, 16, 64])
        nUT = p.sb(st, "e_nut", [128, 16, 64], BF16)
        YS = [p.sb(st, "e_ys%d" % i, [64, 16, 128]) for i in range(2)]
        idb = g.ident.re("s (o t) -> s o t", o=1).bc([128, 4, 128])
        def loads(j):
            cd = (j, ORDER_B[j])
            fq, tq, vt = FQ[j % 2], TQ[j % 2], VT[j % 2]
            for d in range(2):
                c = cd[d]
                p.dma(fq[:, d], g.SCF[d, :, c].re("(h k) q t -> k h q t", k=64), q="sp")
                p.dma(tq[:, d], g.SCT[d, c].re("t q (h k) -> t q h k", k=64), q="act")
                p.dma(vt[:, d], g.VTD[c].re("t (h k) -> t h k", k=64), q="sp")

        def phase1(j):
            fq, AMb = FQ[j % 2], AMs[j % 2]
            for ci in range(16):
                d, h = divmod(ci, 8)
                pa = B[3 + ci % 2]
                rhs = fq[:, d, h, 0:2, :]
                p.mm(pa.re("s (q t) -> s q t", t=128)[:, 0:2, :], fq[:, d, h, 2, :], rhs)
                p.mm(pa.re("s (q t) -> s q t", t=128)[:, 2:4, :], fq[:, d, h, 3, :], rhs)
                pn = bv(5, 4, 128)
                p.mm(pn[:, ci % 4, :], fq[:, d, h, 0, :], fq[:, d, h, 3, :])
                p.tt(AMb[:, ci], pa.re("s (q t) -> s q t", t=128), g.MASK4[:, d], ALU.mult)
                p.tt(Xs[0][ci // 4][:, ci % 4, :], pa[:, 256:384], g.MASK4[:, d, 0, :], ALU.mult)
                if ci % 4 == 3:
                    p.tt(XTs[0][ci // 4], pn, g.MASK4[:, 1 - d, 0:1, :].bc([128, 4, 128]), ALU.mult)

        def phase2(j):
            for gq in range(4):
                p.tt(Ps[0][gq], idb, Xs[0][gq], ALU.subtract, eng="pool")
            cur = 0
            for lev in range(6):
                last = lev == 5
                for gq in range(4):
                    X, XT = Xs[cur][gq], XTs[cur][gq]
                    bs = 3 * (gq % 2)
                    for q in range(4):
                        if not last:
                            p.mm(bv(bs, 4, 128)[:, q, :], XT[:, q, :], X[:, q, :])
                        p.mm(bv(bs + 1, 4, 128)[:, q, :], X[:, q, :], XT[:, q, :])
                    if not last:
                        p.cp(Xs[1 - cur][gq], bv(bs, 4, 128), eng="act")
                    p.cp(XTs[1 - cur][gq], bv(bs + 1, 4, 128), eng="dve")
                for gq in range(4):
                    bs = 3 * (gq % 2)
                    for q in range(4):
                        p.mm(bv(bs + 2, 4, 128)[:, q, :], XTs[1 - cur][gq][:, q, :], Ps[cur][gq][:, q, :])
                    p.tt(Ps[1 - cur][gq], bv(bs + 2, 4, 128), Ps[cur][gq], ALU.add)
                cur = 1 - cur
            return Ps[cur]

        def phase3(j, Pf):
            cd = (j, ORDER_B[j])
            fq, tq, vt, ys, AMb = FQ[j % 2], TQ[j % 2], VT[j % 2], YS[j % 2], AMs[j % 2]
            for ci in range(16):
                d, h = divmod(ci, 8)
                pr = bv(6 + ci // 8, 8, 64)[:, ci % 8, :]
                p.mm(pr, fq[:, d, h, 0, :], STb[:, ci, :], start=True, stop=False)
                p.mm(pr, AMb[:, ci, 0, :], vt[:, d, h, :], start=False, stop=True)
            p.cp(RT[:, 0:8, :], bv(6, 8, 64), eng="act")
            p.cp(RT[:, 8:16, :], bv(7, 8, 64), eng="dve")
            for ci in range(16):
                pr = bv(6 + ci // 8, 8, 64)[:, ci % 8, :]
                p.mm(pr, Pf[ci // 4][:, ci % 4, :], RT[:, ci, :])
            p.ts(nUT[:, 0:8, :], bv(6, 8, 64), -1.0, ALU.mult)
            p.op("act", lambda e: e.mul(out=A(nUT[:, 8:16, :]), in_=A(bv(7, 8, 64)), mul=-1.0), [B[7]], [nUT])
            for q4 in range(4):
                for q in range(4):
                    ci = 4 * q4 + q
                    d, h = divmod(ci, 8)
                    pv = bv(q4 % 2, 4, 128, 64)[:, q, :]
                    p.mm(pv, STb[:, ci, :], fq[:, d, h, 1, :], start=True, stop=False)
                    p.mm(pv, vt[:, d, h, :], AMb[:, ci, 1, :], start=False, stop=False)
                    p.mm(pv, nUT[:, ci, :], AMb[:, ci, 3, :], start=False, stop=True)
                p.cp(ys[:, 4 * q4:4 * q4 + 4, :], bv(q4 % 2, 4, 128, 64), eng=("act" if q4 % 2 else "dve"))
            for d in range(2):
                c = cd[d]
                p.dma(g.YD[d, :, c * CH:(c + 1) * CH].re("(h k) t -> k h t", k=64), ys[:, d * 8:(d + 1) * 8, :], q="pool", acc=True)
            sb_ = (2, 0)
            for ci in range(16):
                d, h = divmod(ci, 8)
                pv = bv(sb_[d], 8, 64, 64)[:, ci % 8, :]
                p.mm(pv, tq[:, d, 0, h, :], vt[:, d, h, :], start=True, stop=False)
                p.mm(pv, tq[:, d, 1, h, :], nUT[:, ci, :], start=False, stop=True)
            for d in range(2):
                wcv = WC[:, d, :, cd[d]:cd[d] + 1].bc([64, 8, 64])
                p.tt(ST[:, d * 8:(d + 1) * 8, :], ST[:, d * 8:(d + 1) * 8, :], wcv, ALU.mult, eng="pool")
                p.tt(ST[:, d * 8:(d + 1) * 8, :], ST[:, d * 8:(d + 1) * 8, :], bv(sb_[d], 8, 64, 64), ALU.add)
            p.cp(STb, ST, eng="act")

        loads(0)
        phase1(0)
        for j in range(NCH):
            Pf = phase2(j)
            if j + 1 < NCH:
                loads(j + 1)
                phase1(j + 1)
            phase3(j, Pf)
    p.barrier()


def stage_mix(p, g, l, b, CO, src, last_dst):
    with contextlib.ExitStack() as st:
        wa = p.sb(st, "m_wa", [128, 4, 1024], BF16)
        wb = p.sb(st, "m_wb", [64, 8, 1024], BF16)
        wo = p.sb(st, "m_wo", [128, 8, 1024], BF16)
        G1b = []
        for kind, row in ((0, 2), (1, b)):
            t = p.sb(st, "m_g1b%d" % kind, [128, D])
            p.dma(t, g.MODR[l, row, G1:G1 + D].pb(128), q="act")
            G1b.append(t)
        p.dma(wa, g.w_a_out[l].re("(c q) n -> q c n", q=128), q="pool")
        p.dma(wb, g.w_b_out[l].re("(h k) n -> k h n", k=64), q="pool")
        p.dma(wo, g.w_o[l].re("(c q) n -> q c n", q=128), q="pool")
        y0 = p.sb(st, "m_y0", [64, 8, 512])
        y1 = p.sb(st, "m_y1", [64, 8, 512])
        aux = p.sb(st, "m_aux", [64, 8, 512])
        ybin = p.sb(st, "m_ybin", [64, 8, 512], BF16)
        sgat = [p.sb(st, "m_sga%d" % i, [128, 512]) for i in range(2)]
        sgbt = [p.sb(st, "m_sgb%d" % i, [128, 512]) for i in range(2)]
        m1 = [p.sb(st, "m_m1%d" % i, [128, 512]) for i in range(1)] * 2
        m2 = [p.sb(st, "m_m2%d" % i, [128, 512]) for i in range(1)] * 2
        mrg = p.sb(st, "m_mrg", [128, 8, 512], BF16)
        xt = [p.sb(st, "m_xt%d" % i, [128, D]) for i in range(2)]
        xo = [p.sb(st, "m_xo%d" % i, [128, D]) for i in range(2)]
        B = [p.ps(st, "m_b%d" % i, [128, 512]) for i in range(8)]
        nb = 0
        nx = 0
        for (t0, tn) in TT:
            for d, yt in ((0, y0), (1, y1)):
                p.dma(yt[:, :, :tn], g.YD[d, :, t0:t0 + tn].re("(h k) t -> k h t", k=64), q=("sp" if d == 0 else "act"))
            p.tt(y0[:, :, :tn], y0[:, :, :tn], y1[:, :, :tn], ALU.add, eng="pool")
            for h in range(8):
                pm = B[nb % 8]
                nb += 1
                p.mm(pm[0:64, :tn], g.ones64, y0[:, h, :tn])
                p.stt(y1[:, h, :tn], pm[0:64, :tn], -1.0 / 64, y0[:, h, :tn], ALU.mult, ALU.add)
            p.act(y0[:, :, :tn], y1[:, :, :tn], AF.Square)
            for h in range(8):
                pm = B[nb % 8]
                nb += 1
                p.mm(pm[0:64, :tn], g.ones64, y0[:, h, :tn])
                p.ts(y0[:, h, :tn], pm[0:64, :tn], 1.0 / 64, ALU.mult, GN_EPS, ALU.add)
            p.act(y0[:, :, :tn], y0[:, :, :tn], AF.Sqrt)
            p.recip(y0[:, :, :tn], y0[:, :, :tn])
            p.tt(y1[:, :, :tn], y1[:, :, :tn], y0[:, :, :tn], ALU.mult, eng="pool")
            for h in range(8):
                p.ts(y1[:, h, :tn], y1[:, h, :tn], g.P64T[:, l, C_LG + h:C_LG + h + 1], ALU.mult,
                     g.P64T[:, l, C_LB + h:C_LB + h + 1], ALU.add)
            p.dma(aux[:, :, :tn], g.RO[0, :, t0:t0 + tn].re("(h k) t -> k h t", k=64), q="sp")
            p.tt(y1[:, :, :tn], y1[:, :, :tn], aux[:, :, :tn], ALU.add, eng="pool")
            p.dma(aux[:, :, :tn], g.RO[1, :, t0:t0 + tn].re("(h k) t -> k h t", k=64), q="sp")
            p.tt(ybin[:, :, :tn], y1[:, :, :tn], aux[:, :, :tn], ALU.mult)
            for cc in range(8):
                sga, sgb = sgat[cc % 2], sgbt[cc % 2]
                p.dma(sga[:, :tn], g.ZF[3456 + cc * 128:3456 + (cc + 1) * 128, t0:t0 + tn], q="sp")
                p.dma(sgb[:, :tn], g.ZF[4480 + cc * 128:4480 + (cc + 1) * 128, t0:t0 + tn], q="act")
                pa = B[nb % 8]
                nb += 1
                for jj in range(4):
                    p.mm(pa[:, :tn], wa[:, jj, cc * 128:(cc + 1) * 128], CO[:, jj, t0:t0 + tn], start=(jj == 0), stop=(jj == 3))
                pb = B[nb % 8]
                nb += 1
                for h in range(8):
                    p.mm(pb[:, :tn], wb[:, h, cc * 128:(cc + 1) * 128], ybin[:, h, :tn], start=(h == 0), stop=(h == 7))
                p.tt(m1[cc % 2][:, :tn], pa[:, :tn], sga[:, :tn], ALU.mult)
                p.tt(m2[cc % 2][:, :tn], pb[:, :tn], sgb[:, :tn], ALU.mult)
                p.tt(mrg[:, cc, :tn], m1[cc % 2][:, :tn], m2[cc % 2][:, :tn], ALU.add, eng="pool")
            for sub in range(tn // 128):
                tok = t0 + sub * 128
                kind = 0 if tok < TCTX else 1
                x, o = xt[nx % 2], xo[nx % 2]
                nx += 1
                p.dma(x, src[tok:tok + 128, :], q="sp")
                for hc in range(2):
                    po = B[nb % 8]
                    nb += 1
                    for cc in range(8):
                        p.mm(po, mrg[:, cc, sub * 128:(sub + 1) * 128], wo[:, cc, hc * 512:(hc + 1) * 512],
                             start=(cc == 0), stop=(cc == 7))
                    p.tt(o[:, hc * 512:(hc + 1) * 512], po, G1b[kind][:, hc * 512:(hc + 1) * 512], ALU.mult)
                p.tt(o, o, x, ALU.add, eng="pool")
                p.dma(last_dst[tok:tok + 128, :], o, q="pool", acc=True)
    p.barrier()


def stage_router(p, g, l, hT2, GW, DEST):
    with contextlib.ExitStack() as st:
        rwf = p.sb(st, "r_wf", [128, 8, 36])
        rwb = p.sb(st, "r_wb", [128, 8, 36], BF16)
        p.dma(rwf[:, :, 0:4], g.router_g[l].re("(c q) n -> q c n", q=128))
        p.dma(rwf[:, :, 4:36], g.router_e[l].re("(c q) n -> q c n", q=128))
        p.cp(rwb, rwf)
        RB = p.sb(st, "r_rb", [128, 36])
        p.dma(RB[:, 0:4], g.router_g_b[l].pb(128))
        p.dma(RB[:, 4:36], g.router_e_b[l].pb(128))
        LG = p.sb(st, "r_lg", [128, NT, 36])
        pl = [p.ps(st, "r_pl%d" % i, [128, 36]) for i in range(2)]
        for i in range(NT):
            pp = pl[i % 2]
            for c in range(8):
                p.mm(pp, hT2[:, c, i * 128:(i + 1) * 128], rwb[:, c, :], start=(c == 0), stop=(c == 7))
            p.cp(LG[:, i, :], pp, eng=("act" if i % 2 else "dve"))
        p.tt(LG, LG, RB.re("q (o e) -> q o e", o=1).bc([128, NT, 36]), ALU.add)
        lg = LG[:, :, 0:4]
        le = LG[:, :, 4:36].re("q i (g e) -> q i g e", e=8)
        mg = p.sb(st, "r_mg", [128, NT])
        oh = p.sb(st, "r_oh", [128, NT, 4])
        eg = p.sb(st, "r_eg", [128, NT, 4])
        pg = p.sb(st, "r_pg", [128, NT])
        tmp = p.sb(st, "r_tmp", [128, NT, 4, 8])
        les = p.sb(st, "r_les", [128, NT, 8])
        les2 = p.sb(st, "r_les2", [128, NT, 8])
        m1 = p.sb(st, "r_m1", [128, NT])
        m2 = p.sb(st, "r_m2", [128, NT])
        k1 = p.sb(st, "r_k1", [128, NT, 8])
        k2 = p.sb(st, "r_k2", [128, NT, 8])
        ex = p.sb(st, "r_ex", [128, NT, 8])

        def b3(t, n):
            return t.re("q (i o) -> q i o", o=1).bc([128, NT, n])
        p.red(mg, lg, ALU.max)
        p.tt(oh, lg, b3(mg, 4), ALU.is_equal)
        p.tt(eg, lg, b3(mg, 4), ALU.subtract)
        p.act(eg, eg, AF.Exp)
        p.red(pg, eg, ALU.add)
        p.recip(pg, pg)
        p.tt(tmp, le, oh.re("q i (g o) -> q i g o", o=1).bc([128, NT, 4, 8]), ALU.mult)
        p.red(les, tmp.re("q i g e -> q i e g"), ALU.add)
        p.red(m1, les, ALU.max)
        p.tt(k1, les, b3(m1, 8), ALU.is_equal)
        p.stt(les2, k1, -1e30, les, ALU.mult, ALU.add)
        p.red(m2, les2, ALU.max)
        p.tt(k2, les2, b3(m2, 8), ALU.is_equal)
        p.tt(m2, m2, m1, ALU.subtract)
        p.act(m2, m2, AF.Exp)
        p.ts(m2, m2, 1.0, ALU.add)
        p.recip(m2, m2)
        p.tt(GW[:, :, 0], m2, pg, ALU.mult)
        p.tt(GW[:, :, 1], pg, GW[:, :, 0], ALU.subtract)
        ohb = oh.re("q i (g o) -> q i g o", o=1).bc([128, NT, 4, 8])
        M1 = p.sb(st, "r_M1", [128, NT, 4, 8])
        M2 = p.sb(st, "r_M2", [128, NT, 4, 8])
        MM = p.sb(st, "r_MM", [128, NT, 4, 8])
        p.tt(M1, ohb, k1.re("q i (o e) -> q i o e", o=1).bc([128, NT, 4, 8]), ALU.mult)
        p.tt(M2, ohb, k2.re("q i (o e) -> q i o e", o=1).bc([128, NT, 4, 8]), ALU.mult)
        p.tt(MM, M1, M2, ALU.add)
        MMf = MM.re("q i g e -> q (i g e)")
        WI = p.sb(st, "r_WI", [128, NT, 32])
        TOT = p.sb(st, "r_TOT", [128, NT, 32])
        pw = [p.ps(st, "r_pw%d" % i, [128, 512]) for i in range(4)]
        NF = NT * 32
        for (c0, cn, k) in ((0, 512, 0), (512, NF - 512, 1)):
            p.mm(pw[k][:, :cn], g.MASK4[:, 0, 0, :], MMf[:, c0:c0 + cn])
            p.cp(WI.re("q i e -> q (i e)")[:, c0:c0 + cn], pw[k][:, :cn], eng="act")
            p.mm(pw[2 + k][:, :cn], g.onesf, MMf[:, c0:c0 + cn])
            p.cp(TOT.re("q i e -> q (i e)")[:, c0:c0 + cn], pw[2 + k][:, :cn], eng="dve")
        BASE = p.sb(st, "r_BASE", [128, NT, 32])
        p.memset(BASE[:, 0, :], 0.0)
        for i in range(1, NT):
            p.tt(BASE[:, i, :], BASE[:, i - 1, :], TOT[:, i - 1, :], ALU.add)
        p.tt(WI, WI, BASE, ALU.add)
        p.ts(TOT, WI, CAP - 0.5, ALU.is_ge)
        p.tt(WI, WI, g.EOFF.re("q (o e) -> q o e", o=1).bc([128, NT, 32]), ALU.add)
        p.ts(BASE, TOT, -1.0, ALU.mult, 1.0, ALU.add)
        p.tt(WI, WI, BASE, ALU.mult)
        p.stt(WI, TOT, float(NEXP * CAP), WI, ALU.mult, ALU.add)
        DF = p.sb(st, "r_DF", [128, NT, 2])
        for k, Mk in ((0, M1), (1, M2)):
            p.tt(Mk.re("q i g e -> q i (g e)"), Mk.re("q i g e -> q i (g e)"), WI, ALU.mult)
            p.red(DF[:, :, k], Mk.re("q i g e -> q i (g e)"), ALU.add)
        p.cp(DEST, DF)
    p.barrier()


def stage_moe(p, g, l, b, GW, DEST):
    NR = NEXP * CAP
    IOA = bass.IndirectOffsetOnAxis
    with contextlib.ExitStack() as st:
        G2b = []
        for kind, row in ((0, 2), (1, b)):
            t = p.sb(st, "e_g2b%d" % kind, [128, D])
            p.dma(t, g.MODR[l, row, G2:G2 + D].pb(128), q="act")
            G2b.append(t)
        with contextlib.ExitStack() as st2:
            hbt = [p.sb(st2, "e_hbt%d" % i, [128, D], BF16) for i in range(2)]
            for i in range(NT):
                hb = hbt[i % 2]
                p.dma(hb, g.H2T[i * 128:(i + 1) * 128, :], q="sp")
                for k in range(2):
                    idx = DEST[:, i, k:k + 1]
                    p.dma(g.XE, hb, q="pool", acc=True, extra_reads=[DEST],
                          fn=lambda e, hb=hb, idx=idx: e.indirect_dma_start(
                              out=A(g.XE), out_offset=IOA(ap=A(idx), axis=0), in_=A(hb), in_offset=None))
            w1b = [p.sb(st2, "e_w1b%d" % i, [128, 8, 512], BF16) for i in range(2)]
            w3b = [p.sb(st2, "e_w3b%d" % i, [128, 8, 512], BF16) for i in range(2)]
            w2b = [p.sb(st2, "e_w2b%d" % i, [128, 4, 1024], BF16) for i in range(2)]
            xe = [p.sb(st2, "e_xe%d" % i, [128, NSL, D], BF16) for i in range(2)]
            hTe = p.sb(st2, "e_hTe", [128, 8, CAP], BF16)
            sl = [p.sb(st2, "e_sl%d" % i, [128, 512]) for i in range(2)]
            hid = p.sb(st2, "e_hid", [128, 4, CAP], BF16)
            ye = [p.sb(st2, "e_ye%d" % i, [128, NSL, D]) for i in range(2)]
            B = [p.ps(st2, "e_pb%d" % i, [128, 512]) for i in range(6)]
            pt = [p.ps(st2, "e_pt%d" % i, [128, 8, 128], BF16) for i in range(2)]
            nb = 0
            ns = 0
            nsl = 0
            npt = 0
            CT = [(0, 512), (512, CAP - 512)] if CAP > 512 else [(0, CAP)]
            def load_w(e):
                k = e % 2
                p.dma(w1b[k], g.exp_w1[l, e].re("(c q) n -> q c n", q=128), q="pool")
                p.dma(w3b[k], g.exp_w3[l, e].re("(c q) n -> q c n", q=128), q="pool")
                p.dma(w2b[k], g.exp_w2[l, e].re("(c q) n -> q c n", q=128), q="pool")
                p.dma(xe[k], g.XE[e * CAP:(e + 1) * CAP, :].re("(j q) n -> q j n", q=128), q="sp")
            load_w(0)
            for e in range(NEXP):
                k = e % 2
                if e + 1 < NEXP:
                    load_w(e + 1)
                x_ = xe[k]
                for j in range(NSL):
                    ptt = pt[npt % 2]
                    npt += 1
                    for c in range(8):
                        p.tr(ptt[:, c, :], x_[:, j, c * 128:(c + 1) * 128], g.identb)
                    p.cp(hTe[:, :, j * 128:(j + 1) * 128], ptt, eng=("act" if j % 2 else "dve"))
                for ff in range(4):
                    for (t0, tn) in CT:
                        p1 = B[nb % 6]
                        p3 = B[(nb + 1) % 6]
                        nb += 2
                        for kc in range(8):
                            p.mm(p1[:, :tn], w1b[k][:, kc, ff * 128:(ff + 1) * 128], hTe[:, kc, t0:t0 + tn],
                                 start=(kc == 0), stop=(kc == 7))
                        for kc in range(8):
                            p.mm(p3[:, :tn], w3b[k][:, kc, ff * 128:(ff + 1) * 128], hTe[:, kc, t0:t0 + tn],
                                 start=(kc == 0), stop=(kc == 7))
                        s_ = sl[nsl % 2]
                        nsl += 1
                        p.act(s_[:, :tn], p1[:, :tn], AF.Silu)
                        p.tt(hid[:, ff, t0:t0 + tn], s_[:, :tn], p3[:, :tn], ALU.mult)
                y_ = ye[k]
                for j in range(NSL):
                    for hc in range(2):
                        po = B[nb % 6]
                        nb += 1
                        for ff in range(4):
                            p.mm(po, hid[:, ff, j * 128:(j + 1) * 128], w2b[k][:, ff, hc * 512:(hc + 1) * 512],
                                 start=(ff == 0), stop=(ff == 3))
                        p.cp(y_[:, j, hc * 512:(hc + 1) * 512], po, eng=("act" if (j + hc) % 2 else "dve"))
                p.dma(g.YE[e * CAP:(e + 1) * CAP, :].re("(j q) n -> q j n", q=128), y_, q="sp", acc=True)
            p.barrier()
        ya = [p.sb(st, "e_ya%d" % i, [128, D]) for i in range(2)]
        yb = [p.sb(st, "e_yb%d" % i, [128, D]) for i in range(2)]
        xt = [p.sb(st, "e_xt%d" % i, [128, D]) for i in range(2)]
        for i in range(NT):
            tok = i * 128
            kind = 0 if i < 2 else 1
            a_, b_, x_ = ya[i % 2], yb[i % 2], xt[i % 2]
            p.dma(x_, g.XS[b][tok:tok + 128, :], q="sp")
            for k, dst in ((0, a_), (1, b_)):
                p.memset(dst, 0.0, eng="pool")
                idx = DEST[:, i, k:k + 1]
                p.dma(dst, g.YE, q="pool", extra_reads=[DEST],
                      fn=lambda e, dst=dst, idx=idx: e.indirect_dma_start(
                          out=A(dst), out_offset=None, in_=A(g.YE), in_offset=IOA(ap=A(idx), axis=0)))
            p.ts(a_, a_, GW[:, i, 0:1], ALU.mult)
            p.stt(a_, b_, GW[:, i, 1:2], a_, ALU.mult, ALU.add)
            p.tt(a_, a_, G2b[kind], ALU.mult)
            p.tt(a_, a_, x_, ALU.add)
            p.dma(g.XS[b][tok:tok + 128, :], a_, q="act", acc=True)
    p.barrier()


def stage_final(p, g, b):
    with contextlib.ExitStack() as st:
        gt = p.sb(st, "f_gt", [128, D])
        p.dma(gt, g.final_g.pb(128))
        xts = [p.sb(st, "f_xt%d" % i, [128, D]) for i in range(2)]
        sqs = [p.sb(st, "f_sq%d" % i, [128, D]) for i in range(2)]
        sss = [p.sb(st, "f_ss%d" % i, [128, 2]) for i in range(2)]
        for i in range(TLAT // 128):
            xt, sq, ss = xts[i % 2], sqs[i % 2], sss[i % 2]
            p.dma(xt, g.XS[b][TCTX + i * 128:TCTX + (i + 1) * 128, :], q=("sp" if i % 2 == 0 else "act"))
            p.act(sq, xt, AF.Square)
            p.red(ss[:, 0:1], sq, ALU.add)
            p.ts(ss[:, 1:2], ss[:, 0:1], 1.0 / D, ALU.mult, EPS, ALU.add)
            p.act(ss[:, 1:2], ss[:, 1:2], AF.Sqrt)
            p.recip(ss[:, 1:2], ss[:, 1:2])
            p.stt(sq, xt, ss[:, 1:2], gt, ALU.mult, ALU.mult)
            p.dma(g.out[b, i * 128:(i + 1) * 128, :], sq, q="pool", acc=True)
    p.barrier()
```

```python
import contextlib
import numpy as np
import concourse.bass as bass
import concourse.mybir as mybir
from concourse.bass_utils import run_bass_kernel_spmd

F32 = mybir.dt.float32
BF16 = mybir.dt.bfloat16
I32 = mybir.dt.int32
AF = mybir.ActivationFunctionType
ALU = mybir.AluOpType
AX = mybir.AxisListType

D = 1024
DEPTH = 4
NB = 2
TCTX = 256
TLAT = 2048
T = TCTX + TLAT
NT = T // 128
NIN = 5504
RW0 = 1536
NEXP = 32
DFF = 512
CAP = 640
NSL = CAP // 128
CH = 128
NCH = T // CH
EPS = 1e-6
GN_EPS = 64e-5
TT = [(0, 512), (512, 512), (1024, 512), (1536, 512), (2048, 256)]


class Tl:
    def __init__(self, ap, name=""):
        self.ap = ap
        self.name = name
        self.lw = {}
        self.rd = {}

    def __getitem__(self, k):
        return Vw(self, self.ap[k])

    def re(self, s, **kw):
        return Vw(self, self.ap.rearrange(s, **kw))

    def bc(self, shape):
        return Vw(self, self.ap.to_broadcast(list(shape)))

    def pb(self, n):
        return Vw(self, self.ap.partition_broadcast(n))


class Vw:
    def __init__(self, t, ap):
        self.t = t
        self.ap = ap

    def __getitem__(self, k):
        return Vw(self.t, self.ap[k])

    def re(self, s, **kw):
        return Vw(self.t, self.ap.rearrange(s, **kw))

    def bc(self, shape):
        return Vw(self.t, self.ap.to_broadcast(list(shape)))

    def pb(self, n):
        return Vw(self.t, self.ap.partition_broadcast(n))


def A(x):
    return x.ap if isinstance(x, (Tl, Vw)) else x


def TT_(x):
    if isinstance(x, Tl):
        return x
    if isinstance(x, Vw):
        return x.t
    return None


class P:
    KD = 8

    def __init__(self, nc):
        self.nc = nc
        self.es = contextlib.ExitStack()
        self.eng = {"pe": nc.tensor, "act": nc.scalar, "dve": nc.vector, "pool": nc.gpsimd, "sp": nc.sync}
        self.sem = {}
        self.cur = {}
        for e in ("pe", "act", "dve", "pool"):
            self.sem[e] = self.es.enter_context(nc.semaphore("s_" + e))
            self.cur[e] = 0
        self.dcount = {}
        for q in ("sp", "pool", "act"):
            self.dcount[q] = 0
            for s in range(self.KD):
                k = ("d", q, s)
                self.sem[k] = self.es.enter_context(nc.semaphore("d_%s_%d" % (q, s)))
                self.cur[k] = 0
        self.waited = {e: {} for e in self.eng}
        self.nins = 0

    def sb(self, stack, name, shape, dt=F32):
        self.uid = getattr(self, "uid", 0) + 1
        name = "%s_%d" % (name, self.uid)
        h = stack.enter_context(self.nc.sbuf_tensor(name, list(shape), dt))
        return Tl(h[:], name)

    def ps(self, stack, name, shape, dt=F32):
        self.uid = getattr(self, "uid", 0) + 1
        name = "%s_%d" % (name, self.uid)
        h = stack.enter_context(self.nc.psum_tensor(name, list(shape), dt))
        return Tl(h[:], name)

    def dram(self, name, shape, dt=F32, kind="Internal"):
        h = self.nc.dram_tensor(name, list(shape), dt, kind=kind)
        return Tl(h.ap(), name)

    def _deps(self, eng, reads, writes, acc=False):
        need = {}

        def add(tok):
            if tok is not None:
                k, v = tok
                if need.get(k, 0) < v:
                    need[k] = v
        for x in reads:
            t = TT_(x)
            if t is not None:
                for k, v in t.lw.items():
                    add((k, v))
        for x in writes:
            t = TT_(x)
            if t is not None:
                if not acc:
                    for k, v in t.lw.items():
                        add((k, v))
                for k, v in t.rd.items():
                    add((k, v))
        out = []
        w = self.waited[eng]
        for k, v in need.items():
            if k == eng and eng == "pe":
                continue
            if w.get(k, 0) >= v:
                continue
            w[k] = v
            out.append((k, v))
        return out

    def _commit(self, tok, reads, writes, acc=False):
        k, v = tok
        for x in reads:
            t = TT_(x)
            if t is not None and t.rd.get(k, 0) < v:
                t.rd[k] = v
        for x in writes:
            t = TT_(x)
            if t is not None:
                if acc:
                    if t.lw.get(k, 0) < v:
                        t.lw[k] = v
                else:
                    t.lw = {k: v}
                    t.rd = {}

    def op(self, eng, fn, reads, writes):
        e = self.eng[eng]
        for k, v in self._deps(eng, reads, writes):
            e.wait_ge(self.sem[k], v)
        ins = fn(e)
        self.cur[eng] += 1
        ins.then_inc(self.sem[eng], 1)
        self._commit((eng, self.cur[eng]), reads, writes)
        self.nins += 1

    def dma(self, out, in_, q="sp", acc=False, fn=None, extra_reads=(), **kw):
        e = self.eng[q]
        j = self.dcount[q]
        self.dcount[q] = j + 1
        slot, gen = j % self.KD, j // self.KD
        key = ("d", q, slot)
        rds = [in_] + list(extra_reads)
        deps = self._deps(q, rds, [out], acc=acc)
        if gen > 0 and self.waited[q].get(key, 0) < 16 * gen:
            self.waited[q][key] = 16 * gen
            deps.append((key, 16 * gen))
        for k, v in deps:
            e.wait_ge(self.sem[k], v)
        if fn is None:
            ins = e.dma_start(out=A(out), in_=A(in_), **kw)
        else:
            ins = fn(e)
        ins.then_inc(self.sem[key], 16)
        self.cur[key] = 16 * (gen + 1)
        self._commit((key, 16 * (gen + 1)), rds, [out], acc=acc)
        self.nins += 1

    def barrier(self, engines=None):
        for en, e in self.eng.items():
            if engines is not None and en not in engines:
                continue
            w = self.waited[en]
            for k, v in self.cur.items():
                if v == 0 or (k == en and en == "pe"):
                    continue
                if w.get(k, 0) >= v:
                    continue
                w[k] = v
                e.wait_ge(self.sem[k], v)

    def mm(self, out, lhsT, rhs, start=True, stop=True):
        self.op("pe", lambda e: e.matmul(A(out), A(lhsT), A(rhs), start=start, stop=stop), [lhsT, rhs], [out])

    def tr(self, out, in_, ident):
        self.op("pe", lambda e: e.transpose(A(out), A(in_), A(ident)), [in_, ident], [out])

    def act(self, out, in_, func, bias=None, scale=None, accum=None, extra_reads=()):
        kw = {}
        rd = [in_] + list(extra_reads)
        if bias is not None:
            kw["bias"] = A(bias)
            rd.append(bias)
        if scale is not None:
            kw["scale"] = A(scale)
            rd.append(scale)
        wr = [out]
        if accum is not None:
            kw["accum_out"] = A(accum)
            wr.append(accum)
        self.op("act", lambda e: e.activation(out=A(out), in_=A(in_), func=func, **kw), rd, wr)

    def tt(self, out, in0, in1, op, eng="dve"):
        self.op(eng, lambda e: e.tensor_tensor(out=A(out), in0=A(in0), in1=A(in1), op=op), [in0, in1], [out])

    def ts(self, out, in0, s1, op0, s2=None, op1=None, eng="dve", accum=None):
        rd = [in0, s1, s2]
        kw = {}
        if op1 is not None:
            kw["op1"] = op1
        wr = [out]
        if accum is not None:
            kw["accum_out"] = A(accum)
            wr.append(accum)
        self.op(eng, lambda e: e.tensor_scalar(out=A(out), in0=A(in0), scalar1=A(s1), scalar2=A(s2), op0=op0, **kw), rd, wr)

    def stt(self, out, in0, scalar, in1, op0, op1, eng="dve"):
        self.op(eng, lambda e: e.scalar_tensor_tensor(out=A(out), in0=A(in0), scalar=A(scalar), in1=A(in1), op0=op0, op1=op1),
                [in0, scalar, in1], [out])

    def cp(self, out, in_, eng="dve"):
        if eng == "act":
            self.op("act", lambda e: e.copy(out=A(out), in_=A(in_)), [in_], [out])
        else:
            self.op(eng, lambda e: e.tensor_copy(out=A(out), in_=A(in_)), [in_], [out])

    def recip(self, out, in_):
        self.op("dve", lambda e: e.reciprocal(out=A(out), in_=A(in_)), [in_], [out])

    def memset(self, out, val, eng="dve"):
        self.op(eng, lambda e: e.memset(A(out), val), [], [out])

    def red(self, out, in_, op, axis=AX.X, eng="dve"):
        self.op(eng, lambda e: e.tensor_reduce(out=A(out), in_=A(in_), axis=axis, op=op), [in_], [out])


class G:
    pass


SH1, SC1, G1, SH2, SC2, G2 = [i * D for i in range(6)]
C_MU, C_CONV = 0, 15
C_KK, C_KA, C_RK, C_W0, C_A0, C_LG, C_LB = 0, 8, 16, 24, 40, 56, 64
C2_KK, C2_KA, C2_RK, C2_W0, C2_A0, C2_LG, C2_LB = 27, 31, 35, 39, 47, 55, 59
NP128 = 63


def stage_params(p, g):
    nc = p.nc
    with contextlib.ExitStack() as st:
        crow = p.sb(st, "crow", [3, D])
        p.dma(crow, g.c3)
        srow = p.sb(st, "srow", [3, D])
        p.act(srow, crow, AF.Silu)
        sT = p.sb(st, "sT", [128, 8, 3])
        pst = p.ps(st, "pst", [128, 8, 4])
        for c in range(8):
            p.tr(pst[:, c, 0:3], srow[:, c * 128:(c + 1) * 128], g.ident[0:3, 0:3])
        p.cp(sT, pst[:, :, 0:3])
        wst = [p.sb(st, "wst%d" % i, [128, 8, 512]) for i in range(2)]
        bm = p.sb(st, "bm", [3, 6 * D])
        mrow = p.sb(st, "mrow", [3, 6 * D])
        pm = [p.ps(st, "pm%d" % i, [3, 512]) for i in range(2)]
        k = 0
        for l in range(DEPTH):
            p.dma(bm, g.b_mod[l].pb(3))
            for cg in range(12):
                ws = wst[k % 2]
                p.dma(ws, g.w_mod[l].re("(c q) n -> q c n", q=128)[:, :, cg * 512:(cg + 1) * 512],
                      q=("sp" if k % 2 == 0 else "act"))
                pp = pm[k % 2]
                for kc in range(8):
                    p.mm(pp, sT[:, kc, :], ws[:, kc, :], start=(kc == 0), stop=(kc == 7))
                p.tt(mrow[:, cg * 512:(cg + 1) * 512], pp, bm[:, cg * 512:(cg + 1) * 512], ALU.add)
                k += 1
            p.dma(g.MODR[l], mrow, q="pool")
        pr128 = p.sb(st, "pr128", [NP128, 128])
        pr64 = p.sb(st, "pr64", [72, 64])
        pp128 = p.ps(st, "pp128", [128, 64])
        pp64 = p.ps(st, "pp64", [64, 72])
        for l in range(DEPTH):
            p.dma(pr128[0:15, :], g.shift_mu[l].re("(c q) -> c q", q=128))
            p.dma(pr128[15:27, :], g.conv_w[l].re("j (c q) -> (j c) q", q=128))
            p.dma(pr64[C_KK:C_KK + 8, :], g.k_k[l].re("(h k) -> h k", k=64))
            p.dma(pr64[C_KA:C_KA + 8, :], g.k_a[l].re("(h k) -> h k", k=64))
            p.dma(pr64[C_RK:C_RK + 8, :], g.r_k[l])
            p.dma(pr64[C_W0:C_W0 + 16, :], g.w0[l].re("d (h k) -> (d h) k", k=64))
            p.dma(pr64[C_A0:C_A0 + 16, :], g.a0[l].re("d (h k) -> (d h) k", k=64))
            p.dma(pr64[C_LG:C_LG + 8, :], g.lnx_g[l].re("(h k) -> h k", k=64))
            p.dma(pr64[C_LB:C_LB + 8, :], g.lnx_b[l].re("(h k) -> h k", k=64))
            p.dma(pr128[C2_KK:C2_KK + 4, :], g.k_k[l].re("(c q) -> c q", q=128))
            p.dma(pr128[C2_KA:C2_KA + 4, :], g.k_a[l].re("(c q) -> c q", q=128))
            p.dma(pr128[C2_RK:C2_RK + 4, :], g.r_k[l].re("(c a) k -> c (a k)", a=2))
            p.dma(pr128[C2_W0:C2_W0 + 8, :], g.w0[l].re("d (c q) -> (d c) q", q=128))
            p.dma(pr128[C2_A0:C2_A0 + 8, :], g.a0[l].re("d (c q) -> (d c) q", q=128))
            p.dma(pr128[C2_LG:C2_LG + 4, :], g.lnx_g[l].re("(c q) -> c q", q=128))
            p.dma(pr128[C2_LB:C2_LB + 4, :], g.lnx_b[l].re("(c q) -> c q", q=128))
            p.tr(pp128[:, 0:NP128], pr128, g.ident[0:NP128, 0:NP128])
            p.cp(g.P128T[:, l, :], pp128[:, 0:NP128])
            p.tr(pp64, pr64, g.ident[0:72, 0:72])
            p.cp(g.P64T[:, l, :], pp64)
        p.ts(g.OMM, g.P128T[:, :, C_MU:C_MU + 15], -1.0, ALU.mult, 1.0, ALU.add)
        p.ts(g.HMU, g.P128T[:, :, C_MU:C_MU + 15], 0.5, ALU.mult)
        p.ts(g.OMKA, g.P128T[:, :, C2_KA:C2_KA + 4], -1.0, ALU.mult, 1.0, ALU.add)
    p.barrier()


def stage_norm(p, g, src, l, b, second, hT, hTf=None):
    SHc, SCc = (SH2, SC2) if second else (SH1, SC1)
    ng = g.norm2_g if second else g.norm1_g
    with contextlib.ExitStack() as st:
        gt = p.sb(st, "gt", [128, D])
        p.dma(gt, ng[l].pb(128))
        Ab, Bb = [], []
        for kind, row in ((0, 2), (1, b)):
            sc = p.sb(st, "scb%d" % kind, [128, D])
            p.dma(sc, g.MODR[l, row, SCc:SCc + D].pb(128))
            a = p.sb(st, "Ab%d" % kind, [128, D])
            p.stt(a, sc, 1.0, gt, ALU.add, ALU.mult)
            bb = p.sb(st, "Bb%d" % kind, [128, D])
            p.dma(bb, g.MODR[l, row, SHc:SHc + D].pb(128))
            Ab.append(a)
            Bb.append(bb)
        xall = [p.sb(st, "xa%d" % i, [128, D]) for i in range(NT)]
        sqs = [p.sb(st, "sq%d" % i, [128, D]) for i in range(2)]
        hns = [p.sb(st, "hn%d" % i, [128, D]) for i in range(2)]
        hbs = [p.sb(st, "hb%d" % i, [128, D], BF16) for i in range(2)]
        ssall = p.sb(st, "ssall", [128, NT])
        rs = p.sb(st, "rsall", [128, NT])
        ptr = [p.ps(st, "ptr%d" % i, [128, 8, 128], BF16) for i in range(2)]
        for i in range(NT):
            p.dma(xall[i], src[i * 128:(i + 1) * 128, :], q=("sp" if i % 2 == 0 else "act"))
            p.act(sqs[i % 2], xall[i], AF.Square)
            p.red(ssall[:, i:i + 1], sqs[i % 2], ALU.add)
        p.ts(rs, ssall, 1.0 / D, ALU.mult, EPS, ALU.add)
        p.act(rs, rs, AF.Sqrt)
        p.recip(rs, rs)
        for i in range(NT):
            kind = 0 if i < 2 else 1
            hn, hb, pt = hns[i % 2], hbs[i % 2], ptr[i % 2]
            p.stt(hn, xall[i], rs[:, i:i + 1], Ab[kind], ALU.mult, ALU.mult)
            p.tt(hb, hn, Bb[kind], ALU.add, eng="pool")
            if second:
                p.dma(g.H2T[i * 128:(i + 1) * 128, :], hb, q="pool", acc=True)
            for c in range(8):
                p.tr(pt[:, c, :], hb[:, c * 128:(c + 1) * 128], g.identb)
            p.cp(hT[:, :, i * 128:(i + 1) * 128], pt, eng="act")


def stage_proj(p, g, hT, l, b):
    groups = [(c0, min(512, NIN - c0)) for c0 in range(0, NIN, 512)]
    with contextlib.ExitStack() as st:
        wbf = [p.sb(st, "pwbf%d" % i, [128, 8, 512], BF16) for i in range(2)]

        def loadg(gi):
            c0, w = groups[gi]
            p.dma(wbf[gi % 2][:, :, :w], g.w_in[l].re("(c q) n -> q c n", q=128)[:, :, c0:c0 + w], q="pool")
        loadg(0)
        ot = [p.sb(st, "pot%d" % i, [128, T]) for i in range(2)]
        zt = [p.sb(st, "pzt%d" % i, [128, T]) for i in range(2)]
        pss = [p.ps(st, "pps%d" % i, [128, 512]) for i in range(4)]
        k = 0
        kk = 0
        for gi, (c0, w) in enumerate(groups):
            wb = wbf[gi % 2]
            if gi + 1 < len(groups):
                loadg(gi + 1)
            for cc in range(w // 128):
                col = c0 + cc * 128
                chunk = col // 128
                o = ot[k % 2]
                for (t0, tn) in TT:
                    ps = pss[kk % 4]
                    for kc in range(8):
                        p.mm(ps[:, :tn], wb[:, kc, cc * 128:(cc + 1) * 128], hT[:, kc, t0:t0 + tn],
                             start=(kc == 0), stop=(kc == 7))
                    if chunk >= 27:
                        p.act(o[:, t0:t0 + tn], ps[:, :tn], AF.Sigmoid)
                    elif kk % 2 == 0:
                        p.cp(o[:, t0:t0 + tn], ps[:, :tn], eng="act")
                    else:
                        p.cp(o[:, t0:t0 + tn], ps[:, :tn], eng="dve")
                    kk += 1
                if 12 <= chunk < 27:
                    j = chunk - 12
                    z = zt[k % 2]
                    om = g.OMM[:, l, j:j + 1]
                    hm = g.HMU[:, l, j:j + 1]
                    p.ts(z, o, om, ALU.mult)
                    for (a0, a1) in ((0, TCTX), (TCTX, T)):
                        p.stt(z[:, a0 + 1:a1], o[:, a0:a1 - 1], hm, z[:, a0 + 1:a1], ALU.mult, ALU.add)
                        p.stt(z[:, a0:a1 - 1], o[:, a0 + 1:a1], hm, z[:, a0:a1 - 1], ALU.mult, ALU.add)
                    o = z
                p.dma(g.ZF[col:col + 128, :], o, q="sp", acc=True)
                k += 1


WEIGHTS = [
    ("w_mod", [DEPTH, D, 6 * D]), ("b_mod", [DEPTH, 6 * D]), ("norm1_g", [DEPTH, D]), ("norm2_g", [DEPTH, D]),
    ("w_in", [DEPTH, D, NIN]), ("shift_mu", [DEPTH, 1920]), ("conv_w", [DEPTH, 3, 512]),
    ("w_up", [DEPTH, 2, 64, 512]), ("w0", [DEPTH, 2, 512]), ("a_up", [DEPTH, 2, 64, 512]), ("a0", [DEPTH, 2, 512]),
    ("g_up", [DEPTH, 128, 512]), ("k_k", [DEPTH, 512]), ("k_a", [DEPTH, 512]), ("r_k", [DEPTH, 8, 64]),
    ("lnx_g", [DEPTH, 512]), ("lnx_b", [DEPTH, 512]), ("w_a_out", [DEPTH, 512, D]), ("w_b_out", [DEPTH, 512, D]),
    ("w_o", [DEPTH, D, D]), ("router_g", [DEPTH, D, 4]), ("router_g_b", [DEPTH, 4]),
    ("router_e", [DEPTH, D, NEXP]), ("router_e_b", [DEPTH, NEXP]),
    ("exp_w1", [DEPTH, NEXP, D, DFF]), ("exp_w3", [DEPTH, NEXP, D, DFF]), ("exp_w2", [DEPTH, NEXP, DFF, D]),
    ("final_g", [D]),
]


def build(stages="all", dbg=()):
    nc = bass.Bass("TRN2", target_bir_lowering=False)
    p = P(nc)
    g = G()
    g.p = p

    def inp(name, shape):
        return Tl(nc.dram_tensor(name, list(shape), F32, kind="ExternalInput").ap(), name)
    g.xin = inp("xin", [NB, T, D])
    g.c3 = inp("c3", [3, D])
    g.idin = inp("idin", [128, 128])
    g.mskin = inp("mskin", [128, 2, 4, 128])
    g.eoffin = inp("eoffin", [128, NEXP])
    for name, shape in WEIGHTS:
        setattr(g, name, inp(name, shape))
    g.out = Tl(nc.dram_tensor("out", [NB, TLAT, D], F32, kind="ExternalOutput").ap(), "out")
    g.MODR = p.dram("MODR", [DEPTH, 3, 6 * D])
    g.ZF = p.dram("ZF", [NIN, T])
    g.XS = [p.dram("XS%d" % b, [T, D]) for b in range(NB)]
    g.SCF = p.dram("SCF", [2, 512, NCH, 4, CH], BF16)
    g.SCT = p.dram("SCT", [2, NCH, CH, 2, 512], BF16)
    g.VTD = p.dram("VTD", [NCH, CH, 512], BF16)
    g.WCD = p.dram("WCD", [512, 2, NCH])
    g.RO = p.dram("RO", [2, 512, T])
    g.YD = p.dram("YD", [2, 512, T])
    g.H2T = p.dram("H2T", [T, D], BF16)
    g.XE = p.dram("XE", [NEXP * CAP + 1, D], BF16)
    g.YE = p.dram("YE", [NEXP * CAP + 1, D])
    g.YEZ = p.dram("YEZ", [1, D])
    dbg_out = {}
    for name, shape in dbg:
        dbg_out[name] = Tl(nc.dram_tensor("dbg_" + name, list(shape), F32, kind="ExternalOutput").ap(), name)
    g.dbg = dbg_out
    top = p.es
    g.ident = p.sb(top, "ident", [128, 128])
    g.identb = p.sb(top, "identb", [128, 128], BF16)
    p.dma(g.ident, g.idin)
    p.cp(g.identb, g.ident)
    g.P128T = p.sb(top, "P128T", [128, DEPTH, NP128])
    g.P64T = p.sb(top, "P64T", [64, DEPTH, 72])
    g.OMM = p.sb(top, "OMM", [128, DEPTH, 15])
    g.HMU = p.sb(top, "HMU", [128, DEPTH, 15])
    g.OMKA = p.sb(top, "OMKA", [128, DEPTH, 4])
    g.MASK4 = p.sb(top, "MASK4", [128, 2, 4, 128])
    p.dma(g.MASK4, g.mskin)
    g.ones64 = p.sb(top, "ones64", [64, 64])
    p.memset(g.ones64, 1.0)
    g.onesf = p.sb(top, "onesf", [128, 128])
    p.memset(g.onesf, 1.0)
    g.EOFF = p.sb(top, "EOFF", [128, NEXP])
    p.dma(g.EOFF, g.eoffin)
    with contextlib.ExitStack() as stz:
        zt = p.sb(stz, "zrow", [1, D])
        p.memset(zt, 0.0)
        p.dma(g.YE[NEXP * CAP:NEXP * CAP + 1, :], zt, q="pool")
        p.barrier()
    g.ones2 = p.sb(top, "ones2", [128, 128])
    p.memset(g.ones2, 0.0)
    p.memset(g.ones2[0:64, 0:64], 1.0)
    p.memset(g.ones2[64:128, 64:128], 1.0)
    g.RM = p.sb(top, "RM", [128, TH])
    p.memset(g.RM, 1.0)
    p.memset(g.RM.re("k (c t) -> k c t", t=CH)[:, :, 0:1], 0.0)

    stage_params(p, g)
    nl = DEPTH if stages == "all" else stages[0]
    nb = NB if stages == "all" else stages[1]
    upto = "z" if stages == "all" else stages[2]
    for b in range(nb):
        for l in range(nl):
            src = g.xin[b] if l == 0 else g.XS[b]
            with contextlib.ExitStack() as st:
                hT = p.sb(st, "hT", [128, 8, T], BF16)
                stage_norm(p, g, src, l, b, False, hT)
                if upto == "A":
                    if "hT" in g.dbg:
                        with contextlib.ExitStack() as s2:
                            tmp = p.sb(s2, "dbgtmp", [128, 8, T])
                            p.cp(tmp, hT)
                            p.dma(g.dbg["hT"], tmp)
                            p.barrier()
                    p.barrier()
                    continue
                stage_proj(p, g, hT, l, b)
                p.barrier()
            if upto == "B":
                continue
            with contextlib.ExitStack() as st:
                CO = p.sb(st, "CO", [128, 4, T], BF16)
                stage_conv(p, g, l, b, CO)
                stage_prep(p, g, l, b)
                if upto == "D":
                    continue
                stage_scan(p, g, l, b)
                if upto == "E":
                    continue
                stage_mix(p, g, l, b, CO, src, g.XS[b])
            if upto == "G":
                continue
            with contextlib.ExitStack() as st:
                GW = p.sb(st, "GW", [128, NT, 2])
                DEST = p.sb(st, "DEST", [128, NT, 2], I32)
                with contextlib.ExitStack() as st2:
                    hT = p.sb(st2, "hT2", [128, 8, T], BF16)
                    stage_norm(p, g, g.XS[b], l, b, True, hT)
                    stage_router(p, g, l, hT, GW, DEST)
                stage_moe(p, g, l, b, GW, DEST)
        if upto == "z":
            stage_final(p, g, b)
    if "ZF" in g.dbg:
        p.barrier()
        with contextlib.ExitStack() as s2:
            tmp = p.sb(s2, "dbgtmp", [128, T])
            for c in range(NIN // 128):
                p.dma(tmp, g.ZF[c * 128:(c + 1) * 128, :])
                p.dma(g.dbg["ZF"][c * 128:(c + 1) * 128, :], tmp)
            p.barrier()
    for nm, t in (("YD", g.YD.re("d n t -> (d n) t")), ("RO", g.RO.re("q n t -> (q n) t")), ("XS0", g.XS[0])):
        if nm in g.dbg:
            with contextlib.ExitStack() as s2:
                n, w = t.ap.shape
                tmp = p.sb(s2, "dbgtmp3", [128, w])
                for c in range(n // 128):
                    p.dma(tmp, t[c * 128:(c + 1) * 128, :])
                    p.dma(g.dbg[nm][c * 128:(c + 1) * 128, :], tmp)
                p.barrier()
    if "MODR" in g.dbg:
        with contextlib.ExitStack() as s2:
            tmp = p.sb(s2, "dbgtmp2", [12, 6 * D])
            p.dma(tmp, g.MODR.re("l r n -> (l r) n"))
            p.dma(g.dbg["MODR"], tmp)
            p.barrier()
    p.barrier()
    p.es.close()
    return nc, p


def make_in_maps(inputs, ncores=8):
    idm = np.eye(128, dtype=np.float32)
    ii = np.arange(128)
    us = (ii[None, :] > ii[:, None]).astype(np.float32)
    ui = (ii[None, :] >= ii[:, None]).astype(np.float32)
    msk = np.ascontiguousarray(np.stack([np.stack([us, ui, us, ui], 0), np.stack([us.T, ui.T, us.T, ui.T], 0)], 0).transpose(2, 0, 1, 3))
    eoff = np.ascontiguousarray(np.broadcast_to((np.arange(NEXP, dtype=np.float32) * CAP)[None, :], (128, NEXP)))
    maps = []
    for core in range(ncores):
        b0 = core * NB
        xin = np.concatenate([inputs["ctx"][b0:b0 + NB], inputs["x"][b0:b0 + NB]], axis=1)
        c3 = np.concatenate([inputs["c"][b0:b0 + NB], inputs["c_ctx"][None, :]], axis=0)
        m = {"xin": np.ascontiguousarray(xin, dtype=np.float32), "c3": np.ascontiguousarray(c3, dtype=np.float32), "idin": idm,
             "mskin": msk, "eoffin": eoff}
        for name, _ in WEIGHTS:
            m[name] = np.ascontiguousarray(inputs[name], dtype=np.float32)
        maps.append(m)
    return maps


def kernel(**inputs):
    inputs = {k: np.asarray(v) for k, v in inputs.items()}
    nc, p = build("all")
    maps = make_in_maps(inputs, 8)
    res = run_bass_kernel_spmd(nc, maps, core_ids=list(range(8)))
    return np.concatenate([r["out"] for r in res.results], axis=0).astype(np.float32)


def stage_conv(p, g, l, b, CO):
    with contextlib.ExitStack() as st:
        bgt = [p.sb(st, "cbg%d" % i, [128, T]) for i in range(2)]
        cgt = [p.sb(st, "ccg%d" % i, [128, T]) for i in range(2)]
        hat = [p.sb(st, "cha%d" % i, [128, T]) for i in range(2)]
        ut = [p.sb(st, "cu%d" % i, [128, T]) for i in range(2)]
        ott = [p.sb(st, "co%d" % i, [128, T]) for i in range(2)]
        for j in range(4):
            bg, cg, ha, u, o = bgt[j % 2], cgt[j % 2], hat[j % 2], ut[j % 2], ott[j % 2]
            p.dma(bg, g.ZF[j * 128:(j + 1) * 128, :], q="sp")
            p.dma(cg, g.ZF[512 + j * 128:512 + (j + 1) * 128, :], q="act")
            p.dma(ha, g.ZF[1024 + j * 128:1024 + (j + 1) * 128, :], q="sp")
            w0 = g.P128T[:, l, C_CONV + 0 * 4 + j:C_CONV + 0 * 4 + j + 1]
            w1 = g.P128T[:, l, C_CONV + 1 * 4 + j:C_CONV + 1 * 4 + j + 1]
            w2 = g.P128T[:, l, C_CONV + 2 * 4 + j:C_CONV + 2 * 4 + j + 1]
            p.tt(u, cg, ha, ALU.mult, eng="pool")
            p.ts(o, u, w1, ALU.mult)
            p.stt(o[:, 1:TCTX], u[:, 0:TCTX - 1], w0, o[:, 1:TCTX], ALU.mult, ALU.add)
            p.stt(o[:, 0:TCTX - 1], u[:, 1:TCTX], w2, o[:, 0:TCTX - 1], ALU.mult, ALU.add)
            if j < 2:
                ug = u[:, TCTX:T].re("q (r w) -> q r w", w=64)
                og = o[:, TCTX:T].re("q (r w) -> q r w", w=64)
                p.stt(og[:, :, 1:64], ug[:, :, 0:63], w0, og[:, :, 1:64], ALU.mult, ALU.add)
                p.stt(og[:, :, 0:63], ug[:, :, 1:64], w2, og[:, :, 0:63], ALU.mult, ALU.add)
            else:
                p.stt(o[:, TCTX + 64:T], u[:, TCTX:T - 64], w0, o[:, TCTX + 64:T], ALU.mult, ALU.add)
                p.stt(o[:, TCTX:T - 64], u[:, TCTX + 64:T], w2, o[:, TCTX:T - 64], ALU.mult, ALU.add)
            p.tt(CO[:, j, :], o, bg, ALU.mult, eng="pool")
    p.barrier()


TH = 1152
THT = [(0, 512), (512, 512), (1024, 128)]
NCH2 = TH // CH


def stage_prep(p, g, l, b):
    with contextlib.ExitStack() as st:
        tmpf = p.sb(st, "dtmpf", [128, T])
        tzw = [p.sb(st, "tzw%d" % d, [64, T], BF16) for d in range(2)]
        zab = [p.sb(st, "zab%d" % d, [64, T], BF16) for d in range(2)]
        sgz = p.sb(st, "sgz", [128, T], BF16)
        for d in range(2):
            p.dma(tmpf[0:64, :], g.ZF[RW0 + 1536 + d * 64:RW0 + 1536 + (d + 1) * 64, :])
            p.act(tzw[d], tmpf[0:64, :], AF.Tanh)
            p.dma(tmpf[0:64, :], g.ZF[RW0 + 1664 + d * 64:RW0 + 1664 + (d + 1) * 64, :])
            p.cp(zab[d], tmpf[0:64, :], eng="act")
        p.dma(tmpf, g.ZF[RW0 + 1792:RW0 + 1920, :])
        p.act(sgz, tmpf, AF.Sigmoid)
        wupb = p.sb(st, "wupb", [64, 2, 512], BF16)
        aupb = p.sb(st, "aupb", [64, 2, 512], BF16)
        gupb = p.sb(st, "gupb", [128, 512], BF16)
        p.dma(wupb, g.w_up[l].re("d r n -> r d n"), q="pool")
        p.dma(aupb, g.a_up[l].re("d r n -> r d n"), q="pool")
        p.dma(gupb, g.g_up[l], q="pool")
        rts = [p.sb(st, "d_r%d" % i, [128, TH]) for i in range(2)]
        kts = [p.sb(st, "d_k%d" % i, [128, TH]) for i in range(2)]
        vts = [p.sb(st, "d_v%d" % i, [128, TH]) for i in range(2)]
        kks = [p.sb(st, "d_kk%d" % i, [128, TH]) for i in range(2)]
        Xss = [[p.sb(st, "d_x%d_%d" % (i, q), [128, TH]) for i in range(8)] for q in range(2)]
        ob = {n: p.sb(st, "d_ob_" + n, [128, TH], BF16) for n in ("kk", "r", "kh", "bh", "kp", "bp", "v")}
        tst = [p.sb(st, "d_tst%d" % i, [128, NCH2, 128], BF16) for i in range(3)]
        wc = p.sb(st, "d_wc", [128, NCH2])
        ps = [p.ps(st, "d_ps%d" % i, [128, 512]) for i in range(3)]
        pst = [p.ps(st, "d_pst%d" % i, [128, 16, 128], BF16) for i in range(2)]
        npst = 0
        nps = 0

        def pk(col):
            return g.P128T[:, l, col:col + 1]

        def tposed(src_b, dst_dram, q, k):
            nonlocal npst
            pt = pst[npst % 2]
            npst += 1
            stg = tst[k]
            for c in range(NCH2):
                p.tr(pt[:, c, :], src_b[:, c * CH:(c + 1) * CH], g.identb)
            p.cp(stg, pt[:, 0:NCH2, :], eng=("act" if k % 2 else "dve"))
            p.dma(dst_dram, stg, q=q, acc=True)

        for pr in range(4):
            r0 = pr * 128
            for hf in range(2):
                tb = hf * TH
                cb = hf * NCH2
                rt, kt, vt, kk, X = rts[hf], kts[hf], vts[hf], kks[hf], Xss[hf]
                p.dma(rt, g.ZF[RW0 + r0:RW0 + r0 + 128, tb:tb + TH], q="sp")
                p.dma(kt, g.ZF[RW0 + 512 + r0:RW0 + 512 + r0 + 128, tb:tb + TH], q="act")
                p.dma(vt, g.ZF[RW0 + 1024 + r0:RW0 + 1024 + r0 + 128, tb:tb + TH], q="sp")
                p.ts(X[0], kt, pk(C2_KK + pr), ALU.mult, eng="pool")
                p.act(X[1], X[0], AF.Square)
                for (t0, tn) in THT:
                    pp = ps[nps % 3]
                    nps += 1
                    p.mm(pp[:, :tn], g.ones2, X[1][:, t0:t0 + tn])
                    p.ts(X[2][:, t0:t0 + tn], pp[:, :tn], 1e-12, ALU.add)
                p.act(X[2], X[2], AF.Sqrt)
                p.recip(X[2], X[2])
                p.tt(kk, X[0], X[2], ALU.mult)
                p.cp(ob["v"], vt, eng="pool")
                tposed(ob["v"], g.VTD[cb:cb + NCH2, :, r0:r0 + 128].re("c t n -> t c n"), "pool", 0)
                for d in range(2):
                    lw, a, kd, bb, L = X[0], X[1], X[2], X[3], X[4]
                    for (t0, tn) in THT:
                        pp = ps[nps % 3]
                        nps += 1
                        p.mm(pp[:, :tn], wupb[:, d, r0:r0 + 128], tzw[d][:, tb + t0:tb + t0 + tn])
                        p.act(lw[:, t0:t0 + tn], pp[:, :tn], AF.Sigmoid, bias=pk(C2_W0 + d * 4 + pr))
                        pp = ps[nps % 3]
                        nps += 1
                        p.mm(pp[:, :tn], aupb[:, d, r0:r0 + 128], zab[d][:, tb + t0:tb + t0 + tn])
                        p.act(a[:, t0:t0 + tn], pp[:, :tn], AF.Sigmoid, bias=pk(C2_A0 + d * 4 + pr))
                    p.ts(lw, lw, -0.6065306597126334, ALU.mult, eng="pool")
                    p.ts(kd, a, pk(C2_KA + pr), ALU.mult, g.OMKA[:, l, pr:pr + 1], ALU.add)
                    p.tt(kd, kd, kt, ALU.mult)
                    p.tt(bb, kk, a, ALU.mult, eng="pool")
                    if d == 0:
                        p.cp(X[7], kd, eng="pool")
                    else:
                        p.tt(X[7], X[7], kd, ALU.add, eng="pool")
                    p.op("dve", lambda e: e.tensor_tensor_scan(out=A(L), data0=A(g.RM), data1=A(lw), initial=0.0,
                                                               op0=ALU.mult, op1=ALU.add), [g.RM, lw], [L])
                    if d == 1:
                        Lp = X[1]
                        p.tt(Lp, lw, L, ALU.subtract)
                        p.tt(Lp.re("k (c t) -> k c t", t=CH), Lp.re("k (c t) -> k c t", t=CH),
                             L.re("k (c t) -> k c t", t=CH)[:, :, CH - 1:CH].bc([128, NCH2, CH]), ALU.add)
                        end = 0
                    else:
                        Lp = L
                        end = CH - 1
                    E = X[5]
                    p.act(E, Lp, AF.Exp)
                    p.tt(ob["r"], rt, E, ALU.mult)
                    p.act(wc.re("k (c o) -> k c o", o=1), Lp.re("k (c t) -> k c t", t=CH)[:, :, end:end + 1], AF.Exp)
                    E2 = X[6]
                    p.act(E2, Lp, AF.Exp, scale=-1.0)
                    p.tt(kd, kd, E2, ALU.mult)
                    p.tt(bb, bb, E2, ALU.mult, eng="pool")
                    p.tt(lw, Lp, lw, ALU.subtract, eng="pool")
                    p.act(lw, lw, AF.Exp)
                    p.tt(ob["kk"], kk, lw, ALU.mult)
                    p.cp(ob["kh"], kd, eng="pool")
                    p.cp(ob["bh"], bb, eng="act")
                    wcb = wc.re("k (c o) -> k c o", o=1).bc([128, NCH2, CH])
                    p.tt(ob["kp"].re("k (c t) -> k c t", t=CH), kd.re("k (c t) -> k c t", t=CH), wcb, ALU.mult)
                    p.tt(ob["bp"].re("k (c t) -> k c t", t=CH), bb.re("k (c t) -> k c t", t=CH), wcb, ALU.mult, eng="pool")
                    for qi, n in enumerate(("kk", "r", "kh", "bh")):
                        p.dma(g.SCF[d, r0:r0 + 128, cb:cb + NCH2, qi, :], ob[n].re("k (c t) -> k c t", t=CH), q="pool", acc=True)
                    tposed(ob["kp"], g.SCT[d, cb:cb + NCH2, :, 0, r0:r0 + 128].re("c t n -> t c n"), "pool", 1)
                    tposed(ob["bp"], g.SCT[d, cb:cb + NCH2, :, 1, r0:r0 + 128].re("c t n -> t c n"), "pool", 2)
                    p.dma(g.WCD[r0:r0 + 128, d, cb:cb + NCH2], wc, q="pool", acc=True)
                p.ts(X[0], rt, pk(C2_RK + pr), ALU.mult, eng="pool")
                p.tt(X[0], X[0], X[7], ALU.mult)
                for (t0, tn) in THT:
                    pp = ps[nps % 3]
                    nps += 1
                    p.mm(pp[:, :tn], g.ones2, X[0][:, t0:t0 + tn])
                    p.tt(X[1][:, t0:t0 + tn], pp[:, :tn], vt[:, t0:t0 + tn], ALU.mult)
                    pp = ps[nps % 3]
                    nps += 1
                    p.mm(pp[:, :tn], gupb[:, r0:r0 + 128], sgz[:, tb + t0:tb + t0 + tn])
                    p.cp(X[2][:, t0:t0 + tn], pp[:, :tn], eng="act")
                p.dma(g.RO[0, r0:r0 + 128, tb:tb + TH], X[1], q="pool", acc=True)
                p.dma(g.RO[1, r0:r0 + 128, tb:tb + TH], X[2], q="pool", acc=True)
    p.barrier()


ORDER_B = [1, 0] + list(range(NCH - 1, 1, -1))


def stage_scan(p, g, l, b):
    with contextlib.ExitStack() as st:
        B = [p.ps(st, "e_b%d" % i, [128, 512]) for i in range(8)]

        def bv(i, n, w, parts=128):
            return B[i].re("s (a t) -> s a t", t=w)[0:parts, 0:n, :]
        WC = p.sb(st, "e_wc", [64, 2, 8, NCH])
        p.dma(WC, g.WCD.re("(h k) d c -> k d h c", k=64))
        ST = p.sb(st, "e_st", [64, 16, 64])
        STb = p.sb(st, "e_stb", [64, 16, 64], BF16)
        p.memset(ST, 0.0)
        p.memset(STb, 0.0, eng="pool")
        FQ = [p.sb(st, "e_fq%d" % i, [64, 2, 8, 4, 128], BF16) for i in range(2)]
        TQ = [p.sb(st, "e_tq%d" % i, [128, 2, 2, 8, 64], BF16) for i in range(2)]
        VT = [p.sb(st, "e_vt%d" % i, [128, 2, 8, 64], BF16) for i in range(2)]
        AMs = [p.sb(st, "e_am%d" % i, [128, 16, 4, 128], BF16) for i in range(2)]
        Xs = [[p.sb(st, "e_x%d_%d" % (i, q), [128, 4, 128]) for q in range(4)] for i in range(2)]
        XTs = [[p.sb(st, "e_xt%d_%d" % (i, q), [128, 4, 128]) for q in range(4)] for i in range(2)]
        Ps = [[p.sb(st, "e_p%d_%d" % (i, q), [128, 4, 128]) for q in range(4)] for i in range(2)]
        RT = p.sb(st, "e_rt", [128, 16, 64])
        PF = [[p.sb(st, "e_pf%d_%d" % (i, q), [128, 4, 128]) for q in range(4)] for i in range(2)]
        nUT = p.sb(st, "e_nut", [128, 16, 64], BF16)
        YS = [p.sb(st, "e_ys%d" % i, [64, 16, 128]) for i in range(2)]
        idb = g.ident.re("s (o t) -> s o t", o=1).bc([128, 4, 128])
        def loads(j):
            cd = (j, ORDER_B[j])
            fq, tq, vt = FQ[j % 2], TQ[j % 2], VT[j % 2]
            for d in range(2):
                c = cd[d]
                p.dma(fq[:, d], g.SCF[d, :, c].re("(h k) q t -> k h q t", k=64), q="sp")
                p.dma(tq[:, d], g.SCT[d, c].re("t q (h k) -> t q h k", k=64), q="act")
                p.dma(vt[:, d], g.VTD[c].re("t (h k) -> t h k", k=64), q="sp")

        def phase1(j):
            fq, AMb = FQ[j % 2], AMs[j % 2]
            for ci in range(16):
                d, h = divmod(ci, 8)
                pa = B[3 + ci % 2]
                rhs = fq[:, d, h, 0:2, :]
                p.mm(pa.re("s (q t) -> s q t", t=128)[:, 0:2, :], fq[:, d, h, 2, :], rhs)
                p.mm(pa.re("s (q t) -> s q t", t=128)[:, 2:4, :], fq[:, d, h, 3, :], rhs)
                pn = bv(5, 4, 128)
                p.mm(pn[:, ci % 4, :], fq[:, d, h, 0, :], fq[:, d, h, 3, :])
                p.tt(AMb[:, ci], pa.re("s (q t) -> s q t", t=128), g.MASK4[:, d], ALU.mult)
                p.tt(Xs[0][ci // 4][:, ci % 4, :], pa[:, 256:384], g.MASK4[:, d, 0, :], ALU.mult)
                if ci % 4 == 3:
                    p.tt(XTs[0][ci // 4], pn, g.MASK4[:, 1 - d, 0:1, :].bc([128, 4, 128]), ALU.mult)

        def phase2(j):
            for gq in range(4):
                p.tt(Ps[0][gq], idb, Xs[0][gq], ALU.subtract, eng="pool")
            cur = 0
            for lev in range(6):
                last = lev == 5
                for gq in range(4):
                    X, XT = Xs[cur][gq], XTs[cur][gq]
                    bs = 3 * (gq % 2)
                    for q in range(4):
                        if not last:
                            p.mm(bv(bs, 4, 128)[:, q, :], XT[:, q, :], X[:, q, :])
                        p.mm(bv(bs + 1, 4, 128)[:, q, :], X[:, q, :], XT[:, q, :])
                    if not last:
                        p.cp(Xs[1 - cur][gq], bv(bs, 4, 128), eng="act")
                    p.cp(XTs[1 - cur][gq], bv(bs + 1, 4, 128), eng="dve")
                for gq in range(4):
                    bs = 3 * (gq % 2)
                    for q in range(4):
                        p.mm(bv(bs + 2, 4, 128)[:, q, :], XTs[1 - cur][gq][:, q, :], Ps[cur][gq][:, q, :])
                    dstp = PF[j % 2][gq] if last else Ps[1 - cur][gq]
                    p.tt(dstp, bv(bs + 2, 4, 128), Ps[cur][gq], ALU.add)
                cur = 1 - cur
                yield lev

        def phase3(j):
            cd = (j, ORDER_B[j])
            fq, tq, vt, ys, AMb, Pf = FQ[j % 2], TQ[j % 2], VT[j % 2], YS[j % 2], AMs[j % 2], PF[j % 2]
            for ci in range(16):
                d, h = divmod(ci, 8)
                pr = bv(6 + ci // 8, 8, 64)[:, ci % 8, :]
                p.mm(pr, fq[:, d, h, 0, :], STb[:, ci, :], start=True, stop=False)
                p.mm(pr, AMb[:, ci, 0, :], vt[:, d, h, :], start=False, stop=True)
            p.cp(RT[:, 0:8, :], bv(6, 8, 64), eng="act")
            p.cp(RT[:, 8:16, :], bv(7, 8, 64), eng="dve")
            yield 0
            for ci in range(16):
                pr = bv(6 + ci // 8, 8, 64)[:, ci % 8, :]
                p.mm(pr, Pf[ci // 4][:, ci % 4, :], RT[:, ci, :])
            p.ts(nUT[:, 0:8, :], bv(6, 8, 64), -1.0, ALU.mult)
            p.op("act", lambda e: e.mul(out=A(nUT[:, 8:16, :]), in_=A(bv(7, 8, 64)), mul=-1.0), [B[7]], [nUT])
            yield 1
            for q4 in range(4):
                for q in range(4):
                    ci = 4 * q4 + q
                    d, h = divmod(ci, 8)
                    pv = bv(6 + q4 % 2, 4, 128, 64)[:, q, :]
                    p.mm(pv, STb[:, ci, :], fq[:, d, h, 1, :], start=True, stop=False)
                    p.mm(pv, vt[:, d, h, :], AMb[:, ci, 1, :], start=False, stop=False)
                    p.mm(pv, nUT[:, ci, :], AMb[:, ci, 3, :], start=False, stop=True)
                p.cp(ys[:, 4 * q4:4 * q4 + 4, :], bv(6 + q4 % 2, 4, 128, 64), eng=("act" if q4 % 2 else "dve"))
            for d in range(2):
                c = cd[d]
                p.dma(g.YD[d, :, c * CH:(c + 1) * CH].re("(h k) t -> k h t", k=64), ys[:, d * 8:(d + 1) * 8, :], q="pool", acc=True)
            yield 2
            for ci in range(16):
                d, h = divmod(ci, 8)
                pv = bv(6 + d, 8, 64, 64)[:, ci % 8, :]
                p.mm(pv, tq[:, d, 0, h, :], vt[:, d, h, :], start=True, stop=False)
                p.mm(pv, tq[:, d, 1, h, :], nUT[:, ci, :], start=False, stop=True)
            for d in range(2):
                wcv = WC[:, d, :, cd[d]:cd[d] + 1].bc([64, 8, 64])
                p.tt(ST[:, d * 8:(d + 1) * 8, :], ST[:, d * 8:(d + 1) * 8, :], wcv, ALU.mult, eng="pool")
                p.tt(ST[:, d * 8:(d + 1) * 8, :], ST[:, d * 8:(d + 1) * 8, :], bv(6 + d, 8, 64, 64), ALU.add)
            p.cp(STb, ST, eng="act")
            yield 3

        loads(0)
        phase1(0)
        for _ in phase2(0):
            pass
        for j in range(NCH):
            g3 = phase3(j)
            if j + 1 < NCH:
                loads(j + 1)
                phase1(j + 1)
                for lev in phase2(j + 1):
                    if lev < 4:
                        next(g3)
            for _ in g3:
                pass
    p.barrier()


def stage_mix(p, g, l, b, CO, src, last_dst):
    with contextlib.ExitStack() as st:
        wa = p.sb(st, "m_wa", [128, 4, 1024], BF16)
        wb = p.sb(st, "m_wb", [64, 8, 1024], BF16)
        wo = p.sb(st, "m_wo", [128, 8, 1024], BF16)
        G1b = []
        for kind, row in ((0, 2), (1, b)):
            t = p.sb(st, "m_g1b%d" % kind, [128, D])
            p.dma(t, g.MODR[l, row, G1:G1 + D].pb(128), q="act")
            G1b.append(t)
        p.dma(wa, g.w_a_out[l].re("(c q) n -> q c n", q=128), q="pool")
        p.dma(wb, g.w_b_out[l].re("(h k) n -> k h n", k=64), q="pool")
        p.dma(wo, g.w_o[l].re("(c q) n -> q c n", q=128), q="pool")
        y0 = p.sb(st, "m_y0", [64, 8, 512])
        y1 = p.sb(st, "m_y1", [64, 8, 512])
        aux = p.sb(st, "m_aux", [64, 8, 512])
        ybin = p.sb(st, "m_ybin", [64, 8, 512], BF16)
        sgat = [p.sb(st, "m_sga%d" % i, [128, 512]) for i in range(2)]
        sgbt = [p.sb(st, "m_sgb%d" % i, [128, 512]) for i in range(2)]
        m1 = [p.sb(st, "m_m1%d" % i, [128, 512]) for i in range(1)] * 2
        m2 = [p.sb(st, "m_m2%d" % i, [128, 512]) for i in range(1)] * 2
        mrg = p.sb(st, "m_mrg", [128, 8, 512], BF16)
        xt = [p.sb(st, "m_xt%d" % i, [128, D]) for i in range(2)]
        xo = [p.sb(st, "m_xo%d" % i, [128, D]) for i in range(2)]
        B = [p.ps(st, "m_b%d" % i, [128, 512]) for i in range(8)]
        nb = 0
        nx = 0
        for (t0, tn) in TT:
            for d, yt in ((0, y0), (1, y1)):
                p.dma(yt[:, :, :tn], g.YD[d, :, t0:t0 + tn].re("(h k) t -> k h t", k=64), q=("sp" if d == 0 else "act"))
            p.tt(y0[:, :, :tn], y0[:, :, :tn], y1[:, :, :tn], ALU.add, eng="pool")
            for h in range(8):
                pm = B[nb % 8]
                nb += 1
                p.mm(pm[0:64, :tn], g.ones64, y0[:, h, :tn])
                p.stt(y1[:, h, :tn], pm[0:64, :tn], -1.0 / 64, y0[:, h, :tn], ALU.mult, ALU.add)
            p.act(y0[:, :, :tn], y1[:, :, :tn], AF.Square)
            for h in range(8):
                pm = B[nb % 8]
                nb += 1
                p.mm(pm[0:64, :tn], g.ones64, y0[:, h, :tn])
                p.ts(y0[:, h, :tn], pm[0:64, :tn], 1.0 / 64, ALU.mult, GN_EPS, ALU.add)
            p.act(y0[:, :, :tn], y0[:, :, :tn], AF.Sqrt)
            p.recip(y0[:, :, :tn], y0[:, :, :tn])
            p.tt(y1[:, :, :tn], y1[:, :, :tn], y0[:, :, :tn], ALU.mult, eng="pool")
            for h in range(8):
                p.ts(y1[:, h, :tn], y1[:, h, :tn], g.P64T[:, l, C_LG + h:C_LG + h + 1], ALU.mult,
                     g.P64T[:, l, C_LB + h:C_LB + h + 1], ALU.add)
            p.dma(aux[:, :, :tn], g.RO[0, :, t0:t0 + tn].re("(h k) t -> k h t", k=64), q="sp")
            p.tt(y1[:, :, :tn], y1[:, :, :tn], aux[:, :, :tn], ALU.add, eng="pool")
            p.dma(aux[:, :, :tn], g.RO[1, :, t0:t0 + tn].re("(h k) t -> k h t", k=64), q="sp")
            p.tt(ybin[:, :, :tn], y1[:, :, :tn], aux[:, :, :tn], ALU.mult)
            for cc in range(8):
                sga, sgb = sgat[cc % 2], sgbt[cc % 2]
                p.dma(sga[:, :tn], g.ZF[3456 + cc * 128:3456 + (cc + 1) * 128, t0:t0 + tn], q="sp")
                p.dma(sgb[:, :tn], g.ZF[4480 + cc * 128:4480 + (cc + 1) * 128, t0:t0 + tn], q="act")
                pa = B[nb % 8]
                nb += 1
                for jj in range(4):
                    p.mm(pa[:, :tn], wa[:, jj, cc * 128:(cc + 1) * 128], CO[:, jj, t0:t0 + tn], start=(jj == 0), stop=(jj == 3))
                pb = B[nb % 8]
                nb += 1
                for h in range(8):
                    p.mm(pb[:, :tn], wb[:, h, cc * 128:(cc + 1) * 128], ybin[:, h, :tn], start=(h == 0), stop=(h == 7))
                p.tt(m1[cc % 2][:, :tn], pa[:, :tn], sga[:, :tn], ALU.mult)
                p.tt(m2[cc % 2][:, :tn], pb[:, :tn], sgb[:, :tn], ALU.mult)
                p.tt(mrg[:, cc, :tn], m1[cc % 2][:, :tn], m2[cc % 2][:, :tn], ALU.add, eng="pool")
            for sub in range(tn // 128):
                tok = t0 + sub * 128
                kind = 0 if tok < TCTX else 1
                x, o = xt[nx % 2], xo[nx % 2]
                nx += 1
                p.dma(x, src[tok:tok + 128, :], q="sp")
                for hc in range(2):
                    po = B[nb % 8]
                    nb += 1
                    for cc in range(8):
                        p.mm(po, mrg[:, cc, sub * 128:(sub + 1) * 128], wo[:, cc, hc * 512:(hc + 1) * 512],
                             start=(cc == 0), stop=(cc == 7))
                    p.tt(o[:, hc * 512:(hc + 1) * 512], po, G1b[kind][:, hc * 512:(hc + 1) * 512], ALU.mult)
                p.tt(o, o, x, ALU.add, eng="pool")
                p.dma(last_dst[tok:tok + 128, :], o, q="pool", acc=True)
    p.barrier()


def stage_router(p, g, l, hT2, GW, DEST):
    with contextlib.ExitStack() as st:
        rwf = p.sb(st, "r_wf", [128, 8, 36])
        rwb = p.sb(st, "r_wb", [128, 8, 36], BF16)
        p.dma(rwf[:, :, 0:4], g.router_g[l].re("(c q) n -> q c n", q=128))
        p.dma(rwf[:, :, 4:36], g.router_e[l].re("(c q) n -> q c n", q=128))
        p.cp(rwb, rwf)
        RB = p.sb(st, "r_rb", [128, 36])
        p.dma(RB[:, 0:4], g.router_g_b[l].pb(128))
        p.dma(RB[:, 4:36], g.router_e_b[l].pb(128))
        LG = p.sb(st, "r_lg", [128, NT, 36])
        pl = [p.ps(st, "r_pl%d" % i, [128, 36]) for i in range(2)]
        for i in range(NT):
            pp = pl[i % 2]
            for c in range(8):
                p.mm(pp, hT2[:, c, i * 128:(i + 1) * 128], rwb[:, c, :], start=(c == 0), stop=(c == 7))
            p.cp(LG[:, i, :], pp, eng=("act" if i % 2 else "dve"))
        p.tt(LG, LG, RB.re("q (o e) -> q o e", o=1).bc([128, NT, 36]), ALU.add)
        lg = LG[:, :, 0:4]
        le = LG[:, :, 4:36].re("q i (g e) -> q i g e", e=8)
        mg = p.sb(st, "r_mg", [128, NT])
        oh = p.sb(st, "r_oh", [128, NT, 4])
        eg = p.sb(st, "r_eg", [128, NT, 4])
        pg = p.sb(st, "r_pg", [128, NT])
        tmp = p.sb(st, "r_tmp", [128, NT, 4, 8])
        les = p.sb(st, "r_les", [128, NT, 8])
        les2 = p.sb(st, "r_les2", [128, NT, 8])
        m1 = p.sb(st, "r_m1", [128, NT])
        m2 = p.sb(st, "r_m2", [128, NT])
        k1 = p.sb(st, "r_k1", [128, NT, 8])
        k2 = p.sb(st, "r_k2", [128, NT, 8])
        ex = p.sb(st, "r_ex", [128, NT, 8])

        def b3(t, n):
            return t.re("q (i o) -> q i o", o=1).bc([128, NT, n])
        p.red(mg, lg, ALU.max)
        p.tt(oh, lg, b3(mg, 4), ALU.is_equal)
        p.tt(eg, lg, b3(mg, 4), ALU.subtract)
        p.act(eg, eg, AF.Exp)
        p.red(pg, eg, ALU.add)
        p.recip(pg, pg)
        p.tt(tmp, le, oh.re("q i (g o) -> q i g o", o=1).bc([128, NT, 4, 8]), ALU.mult)
        p.red(les, tmp.re("q i g e -> q i e g"), ALU.add)
        p.red(m1, les, ALU.max)
        p.tt(k1, les, b3(m1, 8), ALU.is_equal)
        p.stt(les2, k1, -1e30, les, ALU.mult, ALU.add)
        p.red(m2, les2, ALU.max)
        p.tt(k2, les2, b3(m2, 8), ALU.is_equal)
        p.tt(m2, m2, m1, ALU.subtract)
        p.act(m2, m2, AF.Exp)
        p.ts(m2, m2, 1.0, ALU.add)
        p.recip(m2, m2)
        p.tt(GW[:, :, 0], m2, pg, ALU.mult)
        p.tt(GW[:, :, 1], pg, GW[:, :, 0], ALU.subtract)
        ohb = oh.re("q i (g o) -> q i g o", o=1).bc([128, NT, 4, 8])
        M1 = p.sb(st, "r_M1", [128, NT, 4, 8])
        M2 = p.sb(st, "r_M2", [128, NT, 4, 8])
        MM = p.sb(st, "r_MM", [128, NT, 4, 8])
        p.tt(M1, ohb, k1.re("q i (o e) -> q i o e", o=1).bc([128, NT, 4, 8]), ALU.mult)
        p.tt(M2, ohb, k2.re("q i (o e) -> q i o e", o=1).bc([128, NT, 4, 8]), ALU.mult)
        p.tt(MM, M1, M2, ALU.add)
        MMf = MM.re("q i g e -> q (i g e)")
        WI = p.sb(st, "r_WI", [128, NT, 32])
        TOT = p.sb(st, "r_TOT", [128, NT, 32])
        pw = [p.ps(st, "r_pw%d" % i, [128, 512]) for i in range(4)]
        NF = NT * 32
        for (c0, cn, k) in ((0, 512, 0), (512, NF - 512, 1)):
            p.mm(pw[k][:, :cn], g.MASK4[:, 0, 0, :], MMf[:, c0:c0 + cn])
            p.cp(WI.re("q i e -> q (i e)")[:, c0:c0 + cn], pw[k][:, :cn], eng="act")
            p.mm(pw[2 + k][:, :cn], g.onesf, MMf[:, c0:c0 + cn])
            p.cp(TOT.re("q i e -> q (i e)")[:, c0:c0 + cn], pw[2 + k][:, :cn], eng="dve")
        BASE = p.sb(st, "r_BASE", [128, NT, 32])
        p.memset(BASE[:, 0, :], 0.0)
        for i in range(1, NT):
            p.tt(BASE[:, i, :], BASE[:, i - 1, :], TOT[:, i - 1, :], ALU.add)
        p.tt(WI, WI, BASE, ALU.add)
        p.ts(TOT, WI, CAP - 0.5, ALU.is_ge)
        p.tt(WI, WI, g.EOFF.re("q (o e) -> q o e", o=1).bc([128, NT, 32]), ALU.add)
        p.ts(BASE, TOT, -1.0, ALU.mult, 1.0, ALU.add)
        p.tt(WI, WI, BASE, ALU.mult)
        p.stt(WI, TOT, float(NEXP * CAP), WI, ALU.mult, ALU.add)
        DF = p.sb(st, "r_DF", [128, NT, 2])
        for k, Mk in ((0, M1), (1, M2)):
            p.tt(Mk.re("q i g e -> q i (g e)"), Mk.re("q i g e -> q i (g e)"), WI, ALU.mult)
            p.red(DF[:, :, k], Mk.re("q i g e -> q i (g e)"), ALU.add)
        p.cp(DEST, DF)
    p.barrier()


def stage_moe(p, g, l, b, GW, DEST):
    NR = NEXP * CAP
    IOA = bass.IndirectOffsetOnAxis
    with contextlib.ExitStack() as st:
        G2b = []
        for kind, row in ((0, 2), (1, b)):
            t = p.sb(st, "e_g2b%d" % kind, [128, D])
            p.dma(t, g.MODR[l, row, G2:G2 + D].pb(128), q="act")
            G2b.append(t)
        with contextlib.ExitStack() as st2:
            hbt = [p.sb(st2, "e_hbt%d" % i, [128, D], BF16) for i in range(2)]
            for i in range(NT):
                hb = hbt[i % 2]
                p.dma(hb, g.H2T[i * 128:(i + 1) * 128, :], q="sp")
                for k in range(2):
                    idx = DEST[:, i, k:k + 1]
                    p.dma(g.XE, hb, q="pool", acc=True, extra_reads=[DEST],
                          fn=lambda e, hb=hb, idx=idx: e.indirect_dma_start(
                              out=A(g.XE), out_offset=IOA(ap=A(idx), axis=0), in_=A(hb), in_offset=None))
            w1b = [p.sb(st2, "e_w1b%d" % i, [128, 8, 512], BF16) for i in range(2)]
            w3b = [p.sb(st2, "e_w3b%d" % i, [128, 8, 512], BF16) for i in range(2)]
            w2b = [p.sb(st2, "e_w2b%d" % i, [128, 4, 1024], BF16) for i in range(2)]
            xe = [p.sb(st2, "e_xe%d" % i, [128, NSL, D], BF16) for i in range(2)]
            hTe = p.sb(st2, "e_hTe", [128, 8, CAP], BF16)
            sl = [p.sb(st2, "e_sl%d" % i, [128, 512]) for i in range(2)]
            hid = p.sb(st2, "e_hid", [128, 4, CAP], BF16)
            ye = [p.sb(st2, "e_ye%d" % i, [128, NSL, D]) for i in range(2)]
            B = [p.ps(st2, "e_pb%d" % i, [128, 512]) for i in range(6)]
            pt = [p.ps(st2, "e_pt%d" % i, [128, 8, 128], BF16) for i in range(2)]
            nb = 0
            ns = 0
            nsl = 0
            npt = 0
            CT = [(0, 512), (512, CAP - 512)] if CAP > 512 else [(0, CAP)]
            def load_w(e):
                k = e % 2
                p.dma(w1b[k], g.exp_w1[l, e].re("(c q) n -> q c n", q=128), q="pool")
                p.dma(w3b[k], g.exp_w3[l, e].re("(c q) n -> q c n", q=128), q="pool")
                p.dma(w2b[k], g.exp_w2[l, e].re("(c q) n -> q c n", q=128), q="pool")
                p.dma(xe[k], g.XE[e * CAP:(e + 1) * CAP, :].re("(j q) n -> q j n", q=128), q="sp")
            load_w(0)
            for e in range(NEXP):
                k = e % 2
                if e + 1 < NEXP:
                    load_w(e + 1)
                x_ = xe[k]
                for j in range(NSL):
                    ptt = pt[npt % 2]
                    npt += 1
                    for c in range(8):
                        p.tr(ptt[:, c, :], x_[:, j, c * 128:(c + 1) * 128], g.identb)
                    p.cp(hTe[:, :, j * 128:(j + 1) * 128], ptt, eng=("act" if j % 2 else "dve"))
                for ff in range(4):
                    for (t0, tn) in CT:
                        p1 = B[nb % 6]
                        p3 = B[(nb + 1) % 6]
                        nb += 2
                        for kc in range(8):
                            p.mm(p1[:, :tn], w1b[k][:, kc, ff * 128:(ff + 1) * 128], hTe[:, kc, t0:t0 + tn],
                                 start=(kc == 0), stop=(kc == 7))
                        for kc in range(8):
                            p.mm(p3[:, :tn], w3b[k][:, kc, ff * 128:(ff + 1) * 128], hTe[:, kc, t0:t0 + tn],
                                 start=(kc == 0), stop=(kc == 7))
                        s_ = sl[nsl % 2]
                        nsl += 1
                        p.act(s_[:, :tn], p1[:, :tn], AF.Silu)
                        p.tt(hid[:, ff, t0:t0 + tn], s_[:, :tn], p3[:, :tn], ALU.mult)
                y_ = ye[k]
                for j in range(NSL):
                    for hc in range(2):
                        po = B[nb % 6]
                        nb += 1
                        for ff in range(4):
                            p.mm(po, hid[:, ff, j * 128:(j + 1) * 128], w2b[k][:, ff, hc * 512:(hc + 1) * 512],
                                 start=(ff == 0), stop=(ff == 3))
                        p.cp(y_[:, j, hc * 512:(hc + 1) * 512], po, eng=("act" if (j + hc) % 2 else "dve"))
                p.dma(g.YE[e * CAP:(e + 1) * CAP, :].re("(j q) n -> q j n", q=128), y_, q="sp", acc=True)
            p.barrier()
        ya = [p.sb(st, "e_ya%d" % i, [128, D]) for i in range(2)]
        yb = [p.sb(st, "e_yb%d" % i, [128, D]) for i in range(2)]
        xt = [p.sb(st, "e_xt%d" % i, [128, D]) for i in range(2)]
        for i in range(NT):
            tok = i * 128
            kind = 0 if i < 2 else 1
            a_, b_, x_ = ya[i % 2], yb[i % 2], xt[i % 2]
            p.dma(x_, g.XS[b][tok:tok + 128, :], q="sp")
            for k, dst in ((0, a_), (1, b_)):
                p.memset(dst, 0.0, eng="pool")
                idx = DEST[:, i, k:k + 1]
                p.dma(dst, g.YE, q="pool", extra_reads=[DEST],
                      fn=lambda e, dst=dst, idx=idx: e.indirect_dma_start(
                          out=A(dst), out_offset=None, in_=A(g.YE), in_offset=IOA(ap=A(idx), axis=0)))
            p.ts(a_, a_, GW[:, i, 0:1], ALU.mult)
            p.stt(a_, b_, GW[:, i, 1:2], a_, ALU.mult, ALU.add)
            p.tt(a_, a_, G2b[kind], ALU.mult)
            p.tt(a_, a_, x_, ALU.add)
            p.dma(g.XS[b][tok:tok + 128, :], a_, q="act", acc=True)
    p.barrier()


def stage_final(p, g, b):
    with contextlib.ExitStack() as st:
        gt = p.sb(st, "f_gt", [128, D])
        p.dma(gt, g.final_g.pb(128))
        xts = [p.sb(st, "f_xt%d" % i, [128, D]) for i in range(2)]
        sqs = [p.sb(st, "f_sq%d" % i, [128, D]) for i in range(2)]
        sss = [p.sb(st, "f_ss%d" % i, [128, 2]) for i in range(2)]
        for i in range(TLAT // 128):
            xt, sq, ss = xts[i % 2], sqs[i % 2], sss[i % 2]
            p.dma(xt, g.XS[b][TCTX + i * 128:TCTX + (i + 1) * 128, :], q=("sp" if i % 2 == 0 else "act"))
            p.act(sq, xt, AF.Square)
            p.red(ss[:, 0:1], sq, ALU.add)
            p.ts(ss[:, 1:2], ss[:, 0:1], 1.0 / D, ALU.mult, EPS, ALU.add)
            p.act(ss[:, 1:2], ss[:, 1:2], AF.Sqrt)
            p.recip(ss[:, 1:2], ss[:, 1:2])
            p.stt(sq, xt, ss[:, 1:2], gt, ALU.mult, ALU.mult)
            p.dma(g.out[b, i * 128:(i + 1) * 128, :], sq, q="pool", acc=True)
    p.barrier()
```

```python
import contextlib
import numpy as np
import concourse.bass as bass
import concourse.mybir as mybir
from concourse.bass_utils import run_bass_kernel_spmd

F32 = mybir.dt.float32
BF16 = mybir.dt.bfloat16
I32 = mybir.dt.int32
AF = mybir.ActivationFunctionType
ALU = mybir.AluOpType
AX = mybir.AxisListType

D = 1024
DEPTH = 4
NB = 2
TCTX = 256
TLAT = 2048
T = TCTX + TLAT
NT = T // 128
NIN = 5504
RW0 = 1536
NEXP = 32
DFF = 512
CAP = 640
NSL = CAP // 128
CH = 128
NCH = T // CH
EPS = 1e-6
GN_EPS = 64e-5
TT = [(0, 512), (512, 512), (1024, 512), (1536, 512), (2048, 256)]


class Tl:
    def __init__(self, ap, name=""):
        self.ap = ap
        self.name = name
        self.lw = {}
        self.rd = {}

    def __getitem__(self, k):
        return Vw(self, self.ap[k])

    def re(self, s, **kw):
        return Vw(self, self.ap.rearrange(s, **kw))

    def bc(self, shape):
        return Vw(self, self.ap.to_broadcast(list(shape)))

    def pb(self, n):
        return Vw(self, self.ap.partition_broadcast(n))


class Vw:
    def __init__(self, t, ap):
        self.t = t
        self.ap = ap

    def __getitem__(self, k):
        return Vw(self.t, self.ap[k])

    def re(self, s, **kw):
        return Vw(self.t, self.ap.rearrange(s, **kw))

    def bc(self, shape):
        return Vw(self.t, self.ap.to_broadcast(list(shape)))

    def pb(self, n):
        return Vw(self.t, self.ap.partition_broadcast(n))


def A(x):
    return x.ap if isinstance(x, (Tl, Vw)) else x


def TT_(x):
    if isinstance(x, Tl):
        return x
    if isinstance(x, Vw):
        return x.t
    return None


class P:
    KD = 8

    def __init__(self, nc):
        self.nc = nc
        self.es = contextlib.ExitStack()
        self.eng = {"pe": nc.tensor, "act": nc.scalar, "dve": nc.vector, "pool": nc.gpsimd, "sp": nc.sync}
        self.sem = {}
        self.cur = {}
        for e in ("pe", "act", "dve", "pool"):
            self.sem[e] = self.es.enter_context(nc.semaphore("s_" + e))
            self.cur[e] = 0
        self.dcount = {}
        for q in ("sp", "pool", "act"):
            self.dcount[q] = 0
            for s in range(self.KD):
                k = ("d", q, s)
                self.sem[k] = self.es.enter_context(nc.semaphore("d_%s_%d" % (q, s)))
                self.cur[k] = 0
        self.waited = {e: {} for e in self.eng}
        self.nins = 0

    def sb(self, stack, name, shape, dt=F32):
        self.uid = getattr(self, "uid", 0) + 1
        name = "%s_%d" % (name, self.uid)
        h = stack.enter_context(self.nc.sbuf_tensor(name, list(shape), dt))
        return Tl(h[:], name)

    def ps(self, stack, name, shape, dt=F32):
        self.uid = getattr(self, "uid", 0) + 1
        name = "%s_%d" % (name, self.uid)
        h = stack.enter_context(self.nc.psum_tensor(name, list(shape), dt))
        return Tl(h[:], name)

    def dram(self, name, shape, dt=F32, kind="Internal"):
        h = self.nc.dram_tensor(name, list(shape), dt, kind=kind)
        return Tl(h.ap(), name)

    def _deps(self, eng, reads, writes, acc=False):
        need = {}

        def add(tok):
            if tok is not None:
                k, v = tok
                if need.get(k, 0) < v:
                    need[k] = v
        for x in reads:
            t = TT_(x)
            if t is not None:
                for k, v in t.lw.items():
                    add((k, v))
        for x in writes:
            t = TT_(x)
            if t is not None:
                if not acc:
                    for k, v in t.lw.items():
                        add((k, v))
                for k, v in t.rd.items():
                    add((k, v))
        out = []
        w = self.waited[eng]
        for k, v in need.items():
            if k == eng and eng == "pe":
                continue
            if w.get(k, 0) >= v:
                continue
            w[k] = v
            out.append((k, v))
        return out

    def _commit(self, tok, reads, writes, acc=False):
        k, v = tok
        for x in reads:
            t = TT_(x)
            if t is not None and t.rd.get(k, 0) < v:
                t.rd[k] = v
        for x in writes:
            t = TT_(x)
            if t is not None:
                if acc:
                    if t.lw.get(k, 0) < v:
                        t.lw[k] = v
                else:
                    t.lw = {k: v}
                    t.rd = {}

    def op(self, eng, fn, reads, writes):
        e = self.eng[eng]
        for k, v in self._deps(eng, reads, writes):
            e.wait_ge(self.sem[k], v)
        ins = fn(e)
        self.cur[eng] += 1
        ins.then_inc(self.sem[eng], 1)
        self._commit((eng, self.cur[eng]), reads, writes)
        self.nins += 1

    def dma(self, out, in_, q="sp", acc=False, fn=None, extra_reads=(), **kw):
        e = self.eng[q]
        j = self.dcount[q]
        self.dcount[q] = j + 1
        slot, gen = j % self.KD, j // self.KD
        key = ("d", q, slot)
        rds = [in_] + list(extra_reads)
        deps = self._deps(q, rds, [out], acc=acc)
        if gen > 0 and self.waited[q].get(key, 0) < 16 * gen:
            self.waited[q][key] = 16 * gen
            deps.append((key, 16 * gen))
        for k, v in deps:
            e.wait_ge(self.sem[k], v)
        if fn is None:
            ins = e.dma_start(out=A(out), in_=A(in_), **kw)
        else:
            ins = fn(e)
        ins.then_inc(self.sem[key], 16)
        self.cur[key] = 16 * (gen + 1)
        self._commit((key, 16 * (gen + 1)), rds, [out], acc=acc)
        self.nins += 1

    def barrier(self, engines=None):
        for en, e in self.eng.items():
            if engines is not None and en not in engines:
                continue
            w = self.waited[en]
            for k, v in self.cur.items():
                if v == 0 or (k == en and en == "pe"):
                    continue
                if w.get(k, 0) >= v:
                    continue
                w[k] = v
                e.wait_ge(self.sem[k], v)

    def mm(self, out, lhsT, rhs, start=True, stop=True):
        self.op("pe", lambda e: e.matmul(A(out), A(lhsT), A(rhs), start=start, stop=stop), [lhsT, rhs], [out])

    def tr(self, out, in_, ident):
        self.op("pe", lambda e: e.transpose(A(out), A(in_), A(ident)), [in_, ident], [out])

    def act(self, out, in_, func, bias=None, scale=None, accum=None, extra_reads=()):
        kw = {}
        rd = [in_] + list(extra_reads)
        if bias is not None:
            kw["bias"] = A(bias)
            rd.append(bias)
        if scale is not None:
            kw["scale"] = A(scale)
            rd.append(scale)
        wr = [out]
        if accum is not None:
            kw["accum_out"] = A(accum)
            wr.append(accum)
        self.op("act", lambda e: e.activation(out=A(out), in_=A(in_), func=func, **kw), rd, wr)

    def tt(self, out, in0, in1, op, eng="dve"):
        self.op(eng, lambda e: e.tensor_tensor(out=A(out), in0=A(in0), in1=A(in1), op=op), [in0, in1], [out])

    def ts(self, out, in0, s1, op0, s2=None, op1=None, eng="dve", accum=None):
        rd = [in0, s1, s2]
        kw = {}
        if op1 is not None:
            kw["op1"] = op1
        wr = [out]
        if accum is not None:
            kw["accum_out"] = A(accum)
            wr.append(accum)
        self.op(eng, lambda e: e.tensor_scalar(out=A(out), in0=A(in0), scalar1=A(s1), scalar2=A(s2), op0=op0, **kw), rd, wr)

    def stt(self, out, in0, scalar, in1, op0, op1, eng="dve"):
        self.op(eng, lambda e: e.scalar_tensor_tensor(out=A(out), in0=A(in0), scalar=A(scalar), in1=A(in1), op0=op0, op1=op1),
                [in0, scalar, in1], [out])

    def cp(self, out, in_, eng="dve"):
        if eng == "act":
            self.op("act", lambda e: e.copy(out=A(out), in_=A(in_)), [in_], [out])
        else:
            self.op(eng, lambda e: e.tensor_copy(out=A(out), in_=A(in_)), [in_], [out])

    def recip(self, out, in_):
        self.op("dve", lambda e: e.reciprocal(out=A(out), in_=A(in_)), [in_], [out])

    def memset(self, out, val, eng="dve"):
        self.op(eng, lambda e: e.memset(A(out), val), [], [out])

    def red(self, out, in_, op, axis=AX.X, eng="dve"):
        self.op(eng, lambda e: e.tensor_reduce(out=A(out), in_=A(in_), axis=axis, op=op), [in_], [out])


class G:
    pass


SH1, SC1, G1, SH2, SC2, G2 = [i * D for i in range(6)]
C_MU, C_CONV = 0, 15
C_KK, C_KA, C_RK, C_W0, C_A0, C_LG, C_LB = 0, 8, 16, 24, 40, 56, 64
C2_KK, C2_KA, C2_RK, C2_W0, C2_A0, C2_LG, C2_LB = 27, 31, 35, 39, 47, 55, 59
NP128 = 63


def stage_params(p, g):
    nc = p.nc
    with contextlib.ExitStack() as st:
        crow = p.sb(st, "crow", [3, D])
        p.dma(crow, g.c3)
        srow = p.sb(st, "srow", [3, D])
        p.act(srow, crow, AF.Silu)
        sT = p.sb(st, "sT", [128, 8, 3], BF16)
        pst = p.ps(st, "pst", [128, 8, 4])
        for c in range(8):
            p.tr(pst[:, c, 0:3], srow[:, c * 128:(c + 1) * 128], g.ident[0:3, 0:3])
        p.cp(sT, pst[:, :, 0:3])
        wst = [p.sb(st, "wst%d" % i, [128, 8, 512], BF16) for i in range(3)]
        bm = p.sb(st, "bm", [3, 6 * D])
        mrow = p.sb(st, "mrow", [3, 6 * D])
        pm = [p.ps(st, "pm%d" % i, [3, 512]) for i in range(2)]
        k = 0
        for l in range(DEPTH):
            p.dma(bm, g.b_mod[l].pb(3))
            for cg in range(12):
                ws = wst[k % 3]
                p.dma(ws, g.w_mod[l].re("(c q) n -> q c n", q=128)[:, :, cg * 512:(cg + 1) * 512], q="pool")
                pp = pm[k % 2]
                for kc in range(8):
                    p.mm(pp, sT[:, kc, :], ws[:, kc, :], start=(kc == 0), stop=(kc == 7))
                p.tt(mrow[:, cg * 512:(cg + 1) * 512], pp, bm[:, cg * 512:(cg + 1) * 512], ALU.add)
                k += 1
            p.dma(g.MODR[l], mrow, q="sp")
        pr128 = p.sb(st, "pr128", [NP128, 128])
        pr64 = p.sb(st, "pr64", [72, 64])
        pp128 = p.ps(st, "pp128", [128, 64])
        pp64 = p.ps(st, "pp64", [64, 72])
        for l in range(DEPTH):
            p.dma(pr128[0:15, :], g.shift_mu[l].re("(c q) -> c q", q=128))
            p.dma(pr128[15:27, :], g.conv_w[l].re("j (c q) -> (j c) q", q=128))
            p.dma(pr64[C_KK:C_KK + 8, :], g.k_k[l].re("(h k) -> h k", k=64))
            p.dma(pr64[C_KA:C_KA + 8, :], g.k_a[l].re("(h k) -> h k", k=64))
            p.dma(pr64[C_RK:C_RK + 8, :], g.r_k[l])
            p.dma(pr64[C_W0:C_W0 + 16, :], g.w0[l].re("d (h k) -> (d h) k", k=64))
            p.dma(pr64[C_A0:C_A0 + 16, :], g.a0[l].re("d (h k) -> (d h) k", k=64))
            p.dma(pr64[C_LG:C_LG + 8, :], g.lnx_g[l].re("(h k) -> h k", k=64))
            p.dma(pr64[C_LB:C_LB + 8, :], g.lnx_b[l].re("(h k) -> h k", k=64))
            p.dma(pr128[C2_KK:C2_KK + 4, :], g.k_k[l].re("(c q) -> c q", q=128))
            p.dma(pr128[C2_KA:C2_KA + 4, :], g.k_a[l].re("(c q) -> c q", q=128))
            p.dma(pr128[C2_RK:C2_RK + 4, :], g.r_k[l].re("(c a) k -> c (a k)", a=2))
            p.dma(pr128[C2_W0:C2_W0 + 8, :], g.w0[l].re("d (c q) -> (d c) q", q=128))
            p.dma(pr128[C2_A0:C2_A0 + 8, :], g.a0[l].re("d (c q) -> (d c) q", q=128))
            p.dma(pr128[C2_LG:C2_LG + 4, :], g.lnx_g[l].re("(c q) -> c q", q=128))
            p.dma(pr128[C2_LB:C2_LB + 4, :], g.lnx_b[l].re("(c q) -> c q", q=128))
            p.tr(pp128[:, 0:NP128], pr128, g.ident[0:NP128, 0:NP128])
            p.cp(g.P128T[:, l, :], pp128[:, 0:NP128])
            p.tr(pp64, pr64, g.ident[0:72, 0:72])
            p.cp(g.P64T[:, l, :], pp64)
        p.ts(g.OMM, g.P128T[:, :, C_MU:C_MU + 15], -1.0, ALU.mult, 1.0, ALU.add)
        p.ts(g.HMU, g.P128T[:, :, C_MU:C_MU + 15], 0.5, ALU.mult)
        p.ts(g.OMKA, g.P128T[:, :, C2_KA:C2_KA + 4], -1.0, ALU.mult, 1.0, ALU.add)
    p.barrier()


def stage_norm(p, g, src, l, b, second, hT, hTf=None):
    SHc, SCc = (SH2, SC2) if second else (SH1, SC1)
    ng = g.norm2_g if second else g.norm1_g
    with contextlib.ExitStack() as st:
        gt = p.sb(st, "gt", [128, D])
        p.dma(gt, ng[l].pb(128))
        Ab, Bb = [], []
        for kind, row in ((0, 2), (1, b)):
            sc = p.sb(st, "scb%d" % kind, [128, D])
            p.dma(sc, g.MODR[l, row, SCc:SCc + D].pb(128))
            a = p.sb(st, "Ab%d" % kind, [128, D])
            p.stt(a, sc, 1.0, gt, ALU.add, ALU.mult)
            bb = p.sb(st, "Bb%d" % kind, [128, D])
            p.dma(bb, g.MODR[l, row, SHc:SHc + D].pb(128))
            Ab.append(a)
            Bb.append(bb)
        xall = [p.sb(st, "xa%d" % i, [128, D]) for i in range(NT)]
        sqs = [p.sb(st, "sq%d" % i, [128, D]) for i in range(2)]
        hns = [p.sb(st, "hn%d" % i, [128, D]) for i in range(2)]
        hbs = [p.sb(st, "hb%d" % i, [128, D], BF16) for i in range(2)]
        ssall = p.sb(st, "ssall", [128, NT])
        rs = p.sb(st, "rsall", [128, NT])
        ptr = [p.ps(st, "ptr%d" % i, [128, 8, 128], BF16) for i in range(2)]
        for i in range(NT):
            p.dma(xall[i], src[i * 128:(i + 1) * 128, :], q=("sp" if i % 2 == 0 else "act"))
            p.act(sqs[i % 2], xall[i], AF.Square)
            p.red(ssall[:, i:i + 1], sqs[i % 2], ALU.add)
        p.ts(rs, ssall, 1.0 / D, ALU.mult, EPS, ALU.add)
        p.act(rs, rs, AF.Sqrt)
        p.recip(rs, rs)
        for i in range(NT):
            kind = 0 if i < 2 else 1
            hn, hb, pt = hns[i % 2], hbs[i % 2], ptr[i % 2]
            p.stt(hn, xall[i], rs[:, i:i + 1], Ab[kind], ALU.mult, ALU.mult)
            p.tt(hb, hn, Bb[kind], ALU.add, eng="pool")
            if second:
                p.dma(g.H2T[i * 128:(i + 1) * 128, :], hb, q="pool", acc=True)
            for c in range(8):
                p.tr(pt[:, c, :], hb[:, c * 128:(c + 1) * 128], g.identb)
            p.cp(hT[:, :, i * 128:(i + 1) * 128], pt, eng="act")


def stage_proj(p, g, hT, l, b):
    groups = [(c0, min(512, NIN - c0)) for c0 in range(0, NIN, 512)]
    with contextlib.ExitStack() as st:
        wbf = [p.sb(st, "pwbf%d" % i, [128, 8, 512], BF16) for i in range(2)]

        def loadg(gi):
            c0, w = groups[gi]
            p.dma(wbf[gi % 2][:, :, :w], g.w_in[l].re("(c q) n -> q c n", q=128)[:, :, c0:c0 + w], q="pool")
        loadg(0)
        ot = [p.sb(st, "pot%d" % i, [128, T]) for i in range(2)]
        zt = [p.sb(st, "pzt%d" % i, [128, T]) for i in range(2)]
        pss = [p.ps(st, "pps%d" % i, [128, 512]) for i in range(4)]
        k = 0
        kk = 0
        for gi, (c0, w) in enumerate(groups):
            wb = wbf[gi % 2]
            if gi + 1 < len(groups):
                loadg(gi + 1)
            for cc in range(w // 128):
                col = c0 + cc * 128
                chunk = col // 128
                o = ot[k % 2]
                for (t0, tn) in TT:
                    ps = pss[kk % 4]
                    for kc in range(8):
                        p.mm(ps[:, :tn], wb[:, kc, cc * 128:(cc + 1) * 128], hT[:, kc, t0:t0 + tn],
                             start=(kc == 0), stop=(kc == 7))
                    if chunk >= 27:
                        p.act(o[:, t0:t0 + tn], ps[:, :tn], AF.Sigmoid)
                    elif kk % 2 == 0:
                        p.cp(o[:, t0:t0 + tn], ps[:, :tn], eng="act")
                    else:
                        p.cp(o[:, t0:t0 + tn], ps[:, :tn], eng="dve")
                    kk += 1
                if 12 <= chunk < 27:
                    j = chunk - 12
                    z = zt[k % 2]
                    om = g.OMM[:, l, j:j + 1]
                    hm = g.HMU[:, l, j:j + 1]
                    p.ts(z, o, om, ALU.mult)
                    for (a0, a1) in ((0, TCTX), (TCTX, T)):
                        p.stt(z[:, a0 + 1:a1], o[:, a0:a1 - 1], hm, z[:, a0 + 1:a1], ALU.mult, ALU.add)
                        p.stt(z[:, a0:a1 - 1], o[:, a0 + 1:a1], hm, z[:, a0:a1 - 1], ALU.mult, ALU.add)
                    o = z
                p.dma(g.ZF[col:col + 128, :], o, q="sp", acc=True)
                k += 1


WEIGHTS = [
    ("w_mod", [DEPTH, D, 6 * D]), ("b_mod", [DEPTH, 6 * D]), ("norm1_g", [DEPTH, D]), ("norm2_g", [DEPTH, D]),
    ("w_in", [DEPTH, D, NIN]), ("shift_mu", [DEPTH, 1920]), ("conv_w", [DEPTH, 3, 512]),
    ("w_up", [DEPTH, 2, 64, 512]), ("w0", [DEPTH, 2, 512]), ("a_up", [DEPTH, 2, 64, 512]), ("a0", [DEPTH, 2, 512]),
    ("g_up", [DEPTH, 128, 512]), ("k_k", [DEPTH, 512]), ("k_a", [DEPTH, 512]), ("r_k", [DEPTH, 8, 64]),
    ("lnx_g", [DEPTH, 512]), ("lnx_b", [DEPTH, 512]), ("w_a_out", [DEPTH, 512, D]), ("w_b_out", [DEPTH, 512, D]),
    ("w_o", [DEPTH, D, D]), ("router_g", [DEPTH, D, 4]), ("router_g_b", [DEPTH, 4]),
    ("router_e", [DEPTH, D, NEXP]), ("router_e_b", [DEPTH, NEXP]),
    ("exp_w1", [DEPTH, NEXP, D, DFF]), ("exp_w3", [DEPTH, NEXP, D, DFF]), ("exp_w2", [DEPTH, NEXP, DFF, D]),
    ("final_g", [D]),
]


def build(stages="all", dbg=()):
    nc = bass.Bass("TRN2", target_bir_lowering=False)
    p = P(nc)
    g = G()
    g.p = p

    def inp(name, shape):
        return Tl(nc.dram_tensor(name, list(shape), F32, kind="ExternalInput").ap(), name)
    g.xin = inp("xin", [NB, T, D])
    g.c3 = inp("c3", [3, D])
    g.idin = inp("idin", [128, 128])
    g.mskin = inp("mskin", [128, 2, 4, 128])
    g.eoffin = inp("eoffin", [128, NEXP])
    for name, shape in WEIGHTS:
        setattr(g, name, inp(name, shape))
    g.out = Tl(nc.dram_tensor("out", [NB, TLAT, D], F32, kind="ExternalOutput").ap(), "out")
    g.MODR = p.dram("MODR", [DEPTH, 3, 6 * D])
    g.ZF = p.dram("ZF", [NIN, T])
    g.XS = [p.dram("XS%d" % b, [T, D]) for b in range(NB)]
    g.SCF = p.dram("SCF", [2, 512, NCH, 4, CH], BF16)
    g.SCT = p.dram("SCT", [2, NCH, CH, 2, 512], BF16)
    g.VTD = p.dram("VTD", [NCH, CH, 512], BF16)
    g.WCD = p.dram("WCD", [512, 2, NCH])
    g.RO = p.dram("RO", [2, 512, T])
    g.YD = p.dram("YD", [2, 512, T])
    g.H2T = p.dram("H2T", [T, D], BF16)
    g.XE = p.dram("XE", [NEXP * CAP + 1, D], BF16)
    g.YE = p.dram("YE", [NEXP * CAP + 1, D])
    g.YEZ = p.dram("YEZ", [1, D])
    dbg_out = {}
    for name, shape in dbg:
        dbg_out[name] = Tl(nc.dram_tensor("dbg_" + name, list(shape), F32, kind="ExternalOutput").ap(), name)
    g.dbg = dbg_out
    top = p.es
    g.ident = p.sb(top, "ident", [128, 128])
    g.identb = p.sb(top, "identb", [128, 128], BF16)
    p.dma(g.ident, g.idin)
    p.cp(g.identb, g.ident)
    g.P128T = p.sb(top, "P128T", [128, DEPTH, NP128])
    g.P64T = p.sb(top, "P64T", [64, DEPTH, 72])
    g.OMM = p.sb(top, "OMM", [128, DEPTH, 15])
    g.HMU = p.sb(top, "HMU", [128, DEPTH, 15])
    g.OMKA = p.sb(top, "OMKA", [128, DEPTH, 4])
    g.MASK4 = p.sb(top, "MASK4", [128, 2, 4, 128])
    p.dma(g.MASK4, g.mskin)
    g.ones64 = p.sb(top, "ones64", [64, 64])
    p.memset(g.ones64, 1.0)
    g.onesf = p.sb(top, "onesf", [128, 128])
    p.memset(g.onesf, 1.0)
    g.EOFF = p.sb(top, "EOFF", [128, NEXP])
    p.dma(g.EOFF, g.eoffin)
    with contextlib.ExitStack() as stz:
        zt = p.sb(stz, "zrow", [1, D])
        p.memset(zt, 0.0)
        p.dma(g.YE[NEXP * CAP:NEXP * CAP + 1, :], zt, q="pool")
        p.barrier()
    g.ones2 = p.sb(top, "ones2", [128, 128])
    p.memset(g.ones2, 0.0)
    p.memset(g.ones2[0:64, 0:64], 1.0)
    p.memset(g.ones2[64:128, 64:128], 1.0)
    g.RM = p.sb(top, "RM", [128, TH])
    p.memset(g.RM, 1.0)
    p.memset(g.RM.re("k (c t) -> k c t", t=CH)[:, :, 0:1], 0.0)

    stage_params(p, g)
    nl = DEPTH if stages == "all" else stages[0]
    nb = NB if stages == "all" else stages[1]
    upto = "z" if stages == "all" else stages[2]
    for b in range(nb):
        for l in range(nl):
            src = g.xin[b] if l == 0 else g.XS[b]
            with contextlib.ExitStack() as st:
                hT = p.sb(st, "hT", [128, 8, T], BF16)
                stage_norm(p, g, src, l, b, False, hT)
                if upto == "A":
                    if "hT" in g.dbg:
                        with contextlib.ExitStack() as s2:
                            tmp = p.sb(s2, "dbgtmp", [128, 8, T])
                            p.cp(tmp, hT)
                            p.dma(g.dbg["hT"], tmp)
                            p.barrier()
                    p.barrier()
                    continue
                stage_proj(p, g, hT, l, b)
                p.barrier()
            if upto == "B":
                continue
            with contextlib.ExitStack() as st:
                CO = p.sb(st, "CO", [128, 4, T], BF16)
                stage_conv(p, g, l, b, CO)
                stage_prep(p, g, l, b)
                if upto == "D":
                    continue
                stage_scan(p, g, l, b)
                if upto == "E":
                    continue
                stage_mix(p, g, l, b, CO, src, g.XS[b])
            if upto == "G":
                continue
            with contextlib.ExitStack() as st:
                GW = p.sb(st, "GW", [128, NT, 2])
                DEST = p.sb(st, "DEST", [128, NT, 2], I32)
                with contextlib.ExitStack() as st2:
                    hT = p.sb(st2, "hT2", [128, 8, T], BF16)
                    stage_norm(p, g, g.XS[b], l, b, True, hT)
                    stage_router(p, g, l, hT, GW, DEST)
                stage_moe(p, g, l, b, GW, DEST)
        if upto == "z":
            stage_final(p, g, b)
    if "ZF" in g.dbg:
        p.barrier()
        with contextlib.ExitStack() as s2:
            tmp = p.sb(s2, "dbgtmp", [128, T])
            for c in range(NIN // 128):
                p.dma(tmp, g.ZF[c * 128:(c + 1) * 128, :])
                p.dma(g.dbg["ZF"][c * 128:(c + 1) * 128, :], tmp)
            p.barrier()
    for nm, t in (("YD", g.YD.re("d n t -> (d n) t")), ("RO", g.RO.re("q n t -> (q n) t")), ("XS0", g.XS[0])):
        if nm in g.dbg:
            with contextlib.ExitStack() as s2:
                n, w = t.ap.shape
                tmp = p.sb(s2, "dbgtmp3", [128, w])
                for c in range(n // 128):
                    p.dma(tmp, t[c * 128:(c + 1) * 128, :])
                    p.dma(g.dbg[nm][c * 128:(c + 1) * 128, :], tmp)
                p.barrier()
    if "MODR" in g.dbg:
        with contextlib.ExitStack() as s2:
            tmp = p.sb(s2, "dbgtmp2", [12, 6 * D])
            p.dma(tmp, g.MODR.re("l r n -> (l r) n"))
            p.dma(g.dbg["MODR"], tmp)
            p.barrier()
    p.barrier()
    p.es.close()
    return nc, p


def make_in_maps(inputs, ncores=8):
    idm = np.eye(128, dtype=np.float32)
    ii = np.arange(128)
    us = (ii[None, :] > ii[:, None]).astype(np.float32)
    ui = (ii[None, :] >= ii[:, None]).astype(np.float32)
    msk = np.ascontiguousarray(np.stack([np.stack([us, ui, us, ui], 0), np.stack([us.T, ui.T, us.T, ui.T], 0)], 0).transpose(2, 0, 1, 3))
    eoff = np.ascontiguousarray(np.broadcast_to((np.arange(NEXP, dtype=np.float32) * CAP)[None, :], (128, NEXP)))
    maps = []
    for core in range(ncores):
        b0 = core * NB
        xin = np.concatenate([inputs["ctx"][b0:b0 + NB], inputs["x"][b0:b0 + NB]], axis=1)
        c3 = np.concatenate([inputs["c"][b0:b0 + NB], inputs["c_ctx"][None, :]], axis=0)
        m = {"xin": np.ascontiguousarray(xin, dtype=np.float32), "c3": np.ascontiguousarray(c3, dtype=np.float32), "idin": idm,
             "mskin": msk, "eoffin": eoff}
        for name, _ in WEIGHTS:
            m[name] = np.ascontiguousarray(inputs[name], dtype=np.float32)
        maps.append(m)
    return maps


def kernel(**inputs):
    inputs = {k: np.asarray(v) for k, v in inputs.items()}
    nc, p = build("all")
    maps = make_in_maps(inputs, 8)
    res = run_bass_kernel_spmd(nc, maps, core_ids=list(range(8)))
    return np.concatenate([r["out"] for r in res.results], axis=0).astype(np.float32)


def stage_conv(p, g, l, b, CO):
    with contextlib.ExitStack() as st:
        bgt = [p.sb(st, "cbg%d" % i, [128, T]) for i in range(2)]
        cgt = [p.sb(st, "ccg%d" % i, [128, T]) for i in range(2)]
        hat = [p.sb(st, "cha%d" % i, [128, T]) for i in range(2)]
        ut = [p.sb(st, "cu%d" % i, [128, T]) for i in range(2)]
        ott = [p.sb(st, "co%d" % i, [128, T]) for i in range(2)]
        for j in range(4):
            bg, cg, ha, u, o = bgt[j % 2], cgt[j % 2], hat[j % 2], ut[j % 2], ott[j % 2]
            p.dma(bg, g.ZF[j * 128:(j + 1) * 128, :], q="sp")
            p.dma(cg, g.ZF[512 + j * 128:512 + (j + 1) * 128, :], q="act")
            p.dma(ha, g.ZF[1024 + j * 128:1024 + (j + 1) * 128, :], q="sp")
            w0 = g.P128T[:, l, C_CONV + 0 * 4 + j:C_CONV + 0 * 4 + j + 1]
            w1 = g.P128T[:, l, C_CONV + 1 * 4 + j:C_CONV + 1 * 4 + j + 1]
            w2 = g.P128T[:, l, C_CONV + 2 * 4 + j:C_CONV + 2 * 4 + j + 1]
            p.tt(u, cg, ha, ALU.mult, eng="pool")
            p.ts(o, u, w1, ALU.mult)
            p.stt(o[:, 1:TCTX], u[:, 0:TCTX - 1], w0, o[:, 1:TCTX], ALU.mult, ALU.add)
            p.stt(o[:, 0:TCTX - 1], u[:, 1:TCTX], w2, o[:, 0:TCTX - 1], ALU.mult, ALU.add)
            if j < 2:
                ug = u[:, TCTX:T].re("q (r w) -> q r w", w=64)
                og = o[:, TCTX:T].re("q (r w) -> q r w", w=64)
                p.stt(og[:, :, 1:64], ug[:, :, 0:63], w0, og[:, :, 1:64], ALU.mult, ALU.add)
                p.stt(og[:, :, 0:63], ug[:, :, 1:64], w2, og[:, :, 0:63], ALU.mult, ALU.add)
            else:
                p.stt(o[:, TCTX + 64:T], u[:, TCTX:T - 64], w0, o[:, TCTX + 64:T], ALU.mult, ALU.add)
                p.stt(o[:, TCTX:T - 64], u[:, TCTX + 64:T], w2, o[:, TCTX:T - 64], ALU.mult, ALU.add)
            p.tt(CO[:, j, :], o, bg, ALU.mult, eng="pool")
    p.barrier()


TH = 1152
THT = [(0, 512), (512, 512), (1024, 128)]
NCH2 = TH // CH


def stage_prep(p, g, l, b):
    with contextlib.ExitStack() as st:
        tmpf = p.sb(st, "dtmpf", [128, T])
        tzw = [p.sb(st, "tzw%d" % d, [64, T], BF16) for d in range(2)]
        zab = [p.sb(st, "zab%d" % d, [64, T], BF16) for d in range(2)]
        sgz = p.sb(st, "sgz", [128, T], BF16)
        for d in range(2):
            p.dma(tmpf[0:64, :], g.ZF[RW0 + 1536 + d * 64:RW0 + 1536 + (d + 1) * 64, :])
            p.act(tzw[d], tmpf[0:64, :], AF.Tanh)
            p.dma(tmpf[0:64, :], g.ZF[RW0 + 1664 + d * 64:RW0 + 1664 + (d + 1) * 64, :])
            p.cp(zab[d], tmpf[0:64, :], eng="act")
        p.dma(tmpf, g.ZF[RW0 + 1792:RW0 + 1920, :])
        p.act(sgz, tmpf, AF.Sigmoid)
        wupb = p.sb(st, "wupb", [64, 2, 512], BF16)
        aupb = p.sb(st, "aupb", [64, 2, 512], BF16)
        gupb = p.sb(st, "gupb", [128, 512], BF16)
        p.dma(wupb, g.w_up[l].re("d r n -> r d n"), q="pool")
        p.dma(aupb, g.a_up[l].re("d r n -> r d n"), q="pool")
        p.dma(gupb, g.g_up[l], q="pool")
        rts = [p.sb(st, "d_r%d" % i, [128, TH]) for i in range(2)]
        kts = [p.sb(st, "d_k%d" % i, [128, TH]) for i in range(2)]
        vts = [p.sb(st, "d_v%d" % i, [128, TH]) for i in range(2)]
        kks = [p.sb(st, "d_kk%d" % i, [128, TH]) for i in range(2)]
        Xss = [[p.sb(st, "d_x%d_%d" % (i, q), [128, TH]) for i in range(8)] for q in range(2)]
        ob = {n: p.sb(st, "d_ob_" + n, [128, TH], BF16) for n in ("kk", "r", "kh", "bh", "kp", "bp", "v")}
        tst = [p.sb(st, "d_tst%d" % i, [128, NCH2, 128], BF16) for i in range(3)]
        wc = p.sb(st, "d_wc", [128, NCH2])
        ps = [p.ps(st, "d_ps%d" % i, [128, 512]) for i in range(3)]
        pst = [p.ps(st, "d_pst%d" % i, [128, 16, 128], BF16) for i in range(2)]
        npst = 0
        nps = 0

        def pk(col):
            return g.P128T[:, l, col:col + 1]

        def tposed(src_b, dst_dram, q, k):
            nonlocal npst
            pt = pst[npst % 2]
            npst += 1
            stg = tst[k]
            for c in range(NCH2):
                p.tr(pt[:, c, :], src_b[:, c * CH:(c + 1) * CH], g.identb)
            p.cp(stg, pt[:, 0:NCH2, :], eng=("act" if k % 2 else "dve"))
            p.dma(dst_dram, stg, q=q, acc=True)

        for pr in range(4):
            r0 = pr * 128
            for hf in range(2):
                tb = hf * TH
                cb = hf * NCH2
                rt, kt, vt, kk, X = rts[hf], kts[hf], vts[hf], kks[hf], Xss[hf]
                p.dma(rt, g.ZF[RW0 + r0:RW0 + r0 + 128, tb:tb + TH], q="act")
                p.dma(kt, g.ZF[RW0 + 512 + r0:RW0 + 512 + r0 + 128, tb:tb + TH], q="act")
                p.dma(vt, g.ZF[RW0 + 1024 + r0:RW0 + 1024 + r0 + 128, tb:tb + TH], q="act")
                p.ts(X[0], kt, pk(C2_KK + pr), ALU.mult, eng="pool")
                p.act(X[1], X[0], AF.Square)
                for (t0, tn) in THT:
                    pp = ps[nps % 3]
                    nps += 1
                    p.mm(pp[:, :tn], g.ones2, X[1][:, t0:t0 + tn])
                    p.ts(X[2][:, t0:t0 + tn], pp[:, :tn], 1e-12, ALU.add)
                p.act(X[2], X[2], AF.Sqrt)
                p.recip(X[2], X[2])
                p.tt(kk, X[0], X[2], ALU.mult)
                p.cp(ob["v"], vt, eng="pool")
                tposed(ob["v"], g.VTD[cb:cb + NCH2, :, r0:r0 + 128].re("c t n -> t c n"), "sp", 0)
                for d in range(2):
                    lw, a, kd, bb, L = X[0], X[1], X[2], X[3], X[4]
                    for (t0, tn) in THT:
                        pp = ps[nps % 3]
                        nps += 1
                        p.mm(pp[:, :tn], wupb[:, d, r0:r0 + 128], tzw[d][:, tb + t0:tb + t0 + tn])
                        p.act(lw[:, t0:t0 + tn], pp[:, :tn], AF.Sigmoid, bias=pk(C2_W0 + d * 4 + pr))
                        pp = ps[nps % 3]
                        nps += 1
                        p.mm(pp[:, :tn], aupb[:, d, r0:r0 + 128], zab[d][:, tb + t0:tb + t0 + tn])
                        p.act(a[:, t0:t0 + tn], pp[:, :tn], AF.Sigmoid, bias=pk(C2_A0 + d * 4 + pr))
                    p.ts(lw, lw, -0.6065306597126334, ALU.mult, eng="pool")
                    p.ts(kd, a, pk(C2_KA + pr), ALU.mult, g.OMKA[:, l, pr:pr + 1], ALU.add)
                    p.tt(kd, kd, kt, ALU.mult)
                    p.tt(bb, kk, a, ALU.mult, eng="pool")
                    if d == 0:
                        p.cp(X[7], kd, eng="pool")
                    else:
                        p.tt(X[7], X[7], kd, ALU.add, eng="pool")
                    p.op("dve", lambda e: e.tensor_tensor_scan(out=A(L), data0=A(g.RM), data1=A(lw), initial=0.0,
                                                               op0=ALU.mult, op1=ALU.add), [g.RM, lw], [L])
                    if d == 1:
                        Lp = X[1]
                        p.tt(Lp, lw, L, ALU.subtract)
                        p.tt(Lp.re("k (c t) -> k c t", t=CH), Lp.re("k (c t) -> k c t", t=CH),
                             L.re("k (c t) -> k c t", t=CH)[:, :, CH - 1:CH].bc([128, NCH2, CH]), ALU.add)
                        end = 0
                    else:
                        Lp = L
                        end = CH - 1
                    E = X[5]
                    p.act(E, Lp, AF.Exp)
                    p.tt(ob["r"], rt, E, ALU.mult)
                    p.act(wc.re("k (c o) -> k c o", o=1), Lp.re("k (c t) -> k c t", t=CH)[:, :, end:end + 1], AF.Exp)
                    E2 = X[6]
                    p.act(E2, Lp, AF.Exp, scale=-1.0)
                    p.tt(kd, kd, E2, ALU.mult)
                    p.tt(bb, bb, E2, ALU.mult, eng="pool")
                    p.tt(lw, Lp, lw, ALU.subtract, eng="pool")
                    p.act(lw, lw, AF.Exp)
                    p.tt(ob["kk"], kk, lw, ALU.mult)
                    p.cp(ob["kh"], kd, eng="pool")
                    p.cp(ob["bh"], bb, eng="act")
                    wcb = wc.re("k (c o) -> k c o", o=1).bc([128, NCH2, CH])
                    p.tt(ob["kp"].re("k (c t) -> k c t", t=CH), kd.re("k (c t) -> k c t", t=CH), wcb, ALU.mult)
                    p.tt(ob["bp"].re("k (c t) -> k c t", t=CH), bb.re("k (c t) -> k c t", t=CH), wcb, ALU.mult, eng="pool")
                    for qi, n in enumerate(("kk", "r", "kh", "bh")):
                        p.dma(g.SCF[d, r0:r0 + 128, cb:cb + NCH2, qi, :], ob[n].re("k (c t) -> k c t", t=CH), q="sp", acc=True)
                    tposed(ob["kp"], g.SCT[d, cb:cb + NCH2, :, 0, r0:r0 + 128].re("c t n -> t c n"), "sp", 1)
                    tposed(ob["bp"], g.SCT[d, cb:cb + NCH2, :, 1, r0:r0 + 128].re("c t n -> t c n"), "sp", 2)
                    p.dma(g.WCD[r0:r0 + 128, d, cb:cb + NCH2], wc, q="sp", acc=True)
                p.ts(X[0], rt, pk(C2_RK + pr), ALU.mult, eng="pool")
                p.tt(X[0], X[0], X[7], ALU.mult)
                for (t0, tn) in THT:
                    pp = ps[nps % 3]
                    nps += 1
                    p.mm(pp[:, :tn], g.ones2, X[0][:, t0:t0 + tn])
                    p.tt(X[1][:, t0:t0 + tn], pp[:, :tn], vt[:, t0:t0 + tn], ALU.mult)
                    pp = ps[nps % 3]
                    nps += 1
                    p.mm(pp[:, :tn], gupb[:, r0:r0 + 128], sgz[:, tb + t0:tb + t0 + tn])
                    p.cp(X[2][:, t0:t0 + tn], pp[:, :tn], eng="act")
                p.dma(g.RO[0, r0:r0 + 128, tb:tb + TH], X[1], q="sp", acc=True)
                p.dma(g.RO[1, r0:r0 + 128, tb:tb + TH], X[2], q="sp", acc=True)
    p.barrier()


ORDER_B = [1, 0] + list(range(NCH - 1, 1, -1))


def stage_scan(p, g, l, b):
    with contextlib.ExitStack() as st:
        B = [p.ps(st, "e_b%d" % i, [128, 512]) for i in range(8)]

        def bv(i, n, w, parts=128):
            return B[i].re("s (a t) -> s a t", t=w)[0:parts, 0:n, :]
        WC = p.sb(st, "e_wc", [64, 2, 8, NCH])
        p.dma(WC, g.WCD.re("(h k) d c -> k d h c", k=64))
        ST = p.sb(st, "e_st", [64, 16, 64])
        STb = p.sb(st, "e_stb", [64, 16, 64], BF16)
        p.memset(ST, 0.0)
        p.memset(STb, 0.0, eng="pool")
        FQ = [p.sb(st, "e_fq%d" % i, [64, 2, 8, 4, 128], BF16) for i in range(2)]
        TQ = [p.sb(st, "e_tq%d" % i, [128, 2, 2, 8, 64], BF16) for i in range(2)]
        VT = [p.sb(st, "e_vt%d" % i, [128, 2, 8, 64], BF16) for i in range(2)]
        AMs = [p.sb(st, "e_am%d" % i, [128, 16, 4, 128], BF16) for i in range(2)]
        Xs = [[p.sb(st, "e_x%d_%d" % (i, q), [128, 4, 128]) for q in range(4)] for i in range(2)]
        XTs = [[p.sb(st, "e_xt%d_%d" % (i, q), [128, 4, 128]) for q in range(4)] for i in range(2)]
        Ps = [[p.sb(st, "e_p%d_%d" % (i, q), [128, 4, 128]) for q in range(4)] for i in range(2)]
        RT = p.sb(st, "e_rt", [128, 16, 64])
        PF = [[p.sb(st, "e_pf%d_%d" % (i, q), [128, 4, 128]) for q in range(4)] for i in range(2)]
        nUT = p.sb(st, "e_nut", [128, 16, 64], BF16)
        YS = [p.sb(st, "e_ys%d" % i, [64, 16, 128]) for i in range(2)]
        idb = g.ident.re("s (o t) -> s o t", o=1).bc([128, 4, 128])
        def loads(j):
            cd = (j, ORDER_B[j])
            fq, tq, vt = FQ[j % 2], TQ[j % 2], VT[j % 2]
            for d in range(2):
                c = cd[d]
                p.dma(fq[:, d], g.SCF[d, :, c].re("(h k) q t -> k h q t", k=64), q="sp")
                p.dma(tq[:, d], g.SCT[d, c].re("t q (h k) -> t q h k", k=64), q="act")
                p.dma(vt[:, d], g.VTD[c].re("t (h k) -> t h k", k=64), q="sp")

        def phase1(j):
            fq, AMb = FQ[j % 2], AMs[j % 2]
            for ci in range(16):
                d, h = divmod(ci, 8)
                pa = B[3 + ci % 2]
                rhs = fq[:, d, h, 0:2, :]
                p.mm(pa.re("s (q t) -> s q t", t=128)[:, 0:2, :], fq[:, d, h, 2, :], rhs)
                p.mm(pa.re("s (q t) -> s q t", t=128)[:, 2:4, :], fq[:, d, h, 3, :], rhs)
                pn = bv(5, 4, 128)
                p.mm(pn[:, ci % 4, :], fq[:, d, h, 0, :], fq[:, d, h, 3, :])
                p.tt(AMb[:, ci], pa.re("s (q t) -> s q t", t=128), g.MASK4[:, d], ALU.mult)
                p.tt(Xs[0][ci // 4][:, ci % 4, :], pa[:, 256:384], g.MASK4[:, d, 0, :], ALU.mult)
                if ci % 4 == 3:
                    p.tt(XTs[0][ci // 4], pn, g.MASK4[:, 1 - d, 0:1, :].bc([128, 4, 128]), ALU.mult)

        def phase2(j):
            for gq in range(4):
                p.tt(Ps[0][gq], idb, Xs[0][gq], ALU.subtract, eng="pool")
            cur = 0
            for lev in range(6):
                last = lev == 5
                if last:
                    for gq in range(4):
                        X, XT = Xs[cur][gq], XTs[cur][gq]
                        bs = 3 * (gq % 2)
                        for q in range(4):
                            p.mm(bv(bs + 1, 4, 128)[:, q, :], X[:, q, :], XT[:, q, :])
                        p.cp(XTs[1 - cur][gq], bv(bs + 1, 4, 128), eng="dve")
                else:
                    for gq in range(4):
                        X, XT = Xs[cur][gq], XTs[cur][gq]
                        bs = 3 * (gq % 2)
                        for q in range(4):
                            p.mm(bv(bs, 4, 128)[:, q, :], XT[:, q, :], X[:, q, :])
                        p.cp(Xs[1 - cur][gq], bv(bs, 4, 128), eng="act")
                    for gq in range(4):
                        bs = 3 * (gq % 2)
                        for q in range(4):
                            p.tr(bv(bs + 1, 4, 128)[:, q, :], Xs[1 - cur][gq][:, q, :], g.ident)
                        p.cp(XTs[1 - cur][gq], bv(bs + 1, 4, 128), eng="dve")
                for gq in range(4):
                    bs = 3 * (gq % 2)
                    for q in range(4):
                        p.mm(bv(bs + 2, 4, 128)[:, q, :], XTs[1 - cur][gq][:, q, :], Ps[cur][gq][:, q, :])
                    dstp = PF[j % 2][gq] if last else Ps[1 - cur][gq]
                    p.tt(dstp, bv(bs + 2, 4, 128), Ps[cur][gq], ALU.add)
                cur = 1 - cur
                yield lev

        def phase3(j):
            cd = (j, ORDER_B[j])
            fq, tq, vt, ys, AMb, Pf = FQ[j % 2], TQ[j % 2], VT[j % 2], YS[j % 2], AMs[j % 2], PF[j % 2]
            for ci in range(16):
                d, h = divmod(ci, 8)
                pr = bv(6 + ci // 8, 8, 64)[:, ci % 8, :]
                p.mm(pr, fq[:, d, h, 0, :], STb[:, ci, :], start=True, stop=False)
                p.mm(pr, AMb[:, ci, 0, :], vt[:, d, h, :], start=False, stop=True)
            p.cp(RT[:, 0:8, :], bv(6, 8, 64), eng="act")
            p.cp(RT[:, 8:16, :], bv(7, 8, 64), eng="dve")
            yield 0
            for ci in range(16):
                pr = bv(6 + ci // 8, 8, 64)[:, ci % 8, :]
                p.mm(pr, Pf[ci // 4][:, ci % 4, :], RT[:, ci, :])
            p.ts(nUT[:, 0:8, :], bv(6, 8, 64), -1.0, ALU.mult)
            p.op("act", lambda e: e.mul(out=A(nUT[:, 8:16, :]), in_=A(bv(7, 8, 64)), mul=-1.0), [B[7]], [nUT])
            yield 1
            for q4 in range(4):
                for q in range(4):
                    ci = 4 * q4 + q
                    d, h = divmod(ci, 8)
                    pv = bv(6 + q4 % 2, 4, 128, 64)[:, q, :]
                    p.mm(pv, STb[:, ci, :], fq[:, d, h, 1, :], start=True, stop=False)
                    p.mm(pv, vt[:, d, h, :], AMb[:, ci, 1, :], start=False, stop=False)
                    p.mm(pv, nUT[:, ci, :], AMb[:, ci, 3, :], start=False, stop=True)
                p.cp(ys[:, 4 * q4:4 * q4 + 4, :], bv(6 + q4 % 2, 4, 128, 64), eng=("act" if q4 % 2 else "dve"))
            for d in range(2):
                c = cd[d]
                p.dma(g.YD[d, :, c * CH:(c + 1) * CH].re("(h k) t -> k h t", k=64), ys[:, d * 8:(d + 1) * 8, :], q="sp", acc=True)
            yield 2
            for ci in range(16):
                d, h = divmod(ci, 8)
                pv = bv(6 + d, 8, 64, 64)[:, ci % 8, :]
                p.mm(pv, tq[:, d, 0, h, :], vt[:, d, h, :], start=True, stop=False)
                p.mm(pv, tq[:, d, 1, h, :], nUT[:, ci, :], start=False, stop=True)
            for d in range(2):
                wcv = WC[:, d, :, cd[d]:cd[d] + 1].bc([64, 8, 64])
                p.tt(ST[:, d * 8:(d + 1) * 8, :], ST[:, d * 8:(d + 1) * 8, :], wcv, ALU.mult, eng="pool")
                p.tt(ST[:, d * 8:(d + 1) * 8, :], ST[:, d * 8:(d + 1) * 8, :], bv(6 + d, 8, 64, 64), ALU.add)
            p.cp(STb, ST, eng="act")
            yield 3

        loads(0)
        phase1(0)
        for _ in phase2(0):
            pass
        for j in range(NCH):
            g3 = phase3(j)
            if j + 1 < NCH:
                loads(j + 1)
                phase1(j + 1)
                for lev in phase2(j + 1):
                    if lev < 4:
                        next(g3)
            for _ in g3:
                pass
    p.barrier()


def stage_mix(p, g, l, b, CO, src, last_dst):
    with contextlib.ExitStack() as st:
        wa = p.sb(st, "m_wa", [128, 4, 1024], BF16)
        wb = p.sb(st, "m_wb", [64, 8, 1024], BF16)
        wo = p.sb(st, "m_wo", [128, 8, 1024], BF16)
        G1b = []
        for kind, row in ((0, 2), (1, b)):
            t = p.sb(st, "m_g1b%d" % kind, [128, D])
            p.dma(t, g.MODR[l, row, G1:G1 + D].pb(128), q="act")
            G1b.append(t)
        p.dma(wa, g.w_a_out[l].re("(c q) n -> q c n", q=128), q="pool")
        p.dma(wb, g.w_b_out[l].re("(h k) n -> k h n", k=64), q="pool")
        p.dma(wo, g.w_o[l].re("(c q) n -> q c n", q=128), q="pool")
        y0 = p.sb(st, "m_y0", [64, 8, 512])
        y1 = p.sb(st, "m_y1", [64, 8, 512])
        aux = p.sb(st, "m_aux", [64, 8, 512])
        ybin = p.sb(st, "m_ybin", [64, 8, 512], BF16)
        sgat = [p.sb(st, "m_sga%d" % i, [128, 512]) for i in range(2)]
        sgbt = [p.sb(st, "m_sgb%d" % i, [128, 512]) for i in range(2)]
        m1 = [p.sb(st, "m_m1%d" % i, [128, 512]) for i in range(1)] * 2
        m2 = [p.sb(st, "m_m2%d" % i, [128, 512]) for i in range(1)] * 2
        mrg = p.sb(st, "m_mrg", [128, 8, 512], BF16)
        xt = [p.sb(st, "m_xt%d" % i, [128, D]) for i in range(2)]
        xo = [p.sb(st, "m_xo%d" % i, [128, D]) for i in range(2)]
        B = [p.ps(st, "m_b%d" % i, [128, 512]) for i in range(8)]
        nb = 0
        nx = 0
        for (t0, tn) in TT:
            for d, yt in ((0, y0), (1, y1)):
                p.dma(yt[:, :, :tn], g.YD[d, :, t0:t0 + tn].re("(h k) t -> k h t", k=64), q=("sp" if d == 0 else "act"))
            p.tt(y0[:, :, :tn], y0[:, :, :tn], y1[:, :, :tn], ALU.add, eng="pool")
            for h in range(8):
                pm = B[nb % 8]
                nb += 1
                p.mm(pm[0:64, :tn], g.ones64, y0[:, h, :tn])
                p.stt(y1[:, h, :tn], pm[0:64, :tn], -1.0 / 64, y0[:, h, :tn], ALU.mult, ALU.add)
            p.act(y0[:, :, :tn], y1[:, :, :tn], AF.Square)
            for h in range(8):
                pm = B[nb % 8]
                nb += 1
                p.mm(pm[0:64, :tn], g.ones64, y0[:, h, :tn])
                p.ts(y0[:, h, :tn], pm[0:64, :tn], 1.0 / 64, ALU.mult, GN_EPS, ALU.add)
            p.act(y0[:, :, :tn], y0[:, :, :tn], AF.Sqrt)
            p.recip(y0[:, :, :tn], y0[:, :, :tn])
            p.tt(y1[:, :, :tn], y1[:, :, :tn], y0[:, :, :tn], ALU.mult, eng="pool")
            for h in range(8):
                p.ts(y1[:, h, :tn], y1[:, h, :tn], g.P64T[:, l, C_LG + h:C_LG + h + 1], ALU.mult,
                     g.P64T[:, l, C_LB + h:C_LB + h + 1], ALU.add)
            p.dma(aux[:, :, :tn], g.RO[0, :, t0:t0 + tn].re("(h k) t -> k h t", k=64), q="sp")
            p.tt(y1[:, :, :tn], y1[:, :, :tn], aux[:, :, :tn], ALU.add, eng="pool")
            p.dma(aux[:, :, :tn], g.RO[1, :, t0:t0 + tn].re("(h k) t -> k h t", k=64), q="sp")
            p.tt(ybin[:, :, :tn], y1[:, :, :tn], aux[:, :, :tn], ALU.mult)
            for cc in range(8):
                sga, sgb = sgat[cc % 2], sgbt[cc % 2]
                p.dma(sga[:, :tn], g.ZF[3456 + cc * 128:3456 + (cc + 1) * 128, t0:t0 + tn], q="sp")
                p.dma(sgb[:, :tn], g.ZF[4480 + cc * 128:4480 + (cc + 1) * 128, t0:t0 + tn], q="act")
                pa = B[nb % 8]
                nb += 1
                for jj in range(4):
                    p.mm(pa[:, :tn], wa[:, jj, cc * 128:(cc + 1) * 128], CO[:, jj, t0:t0 + tn], start=(jj == 0), stop=(jj == 3))
                pb = B[nb % 8]
                nb += 1
                for h in range(8):
                    p.mm(pb[:, :tn], wb[:, h, cc * 128:(cc + 1) * 128], ybin[:, h, :tn], start=(h == 0), stop=(h == 7))
                p.tt(m1[cc % 2][:, :tn], pa[:, :tn], sga[:, :tn], ALU.mult)
                p.tt(m2[cc % 2][:, :tn], pb[:, :tn], sgb[:, :tn], ALU.mult)
                p.tt(mrg[:, cc, :tn], m1[cc % 2][:, :tn], m2[cc % 2][:, :tn], ALU.add, eng="pool")
            for sub in range(tn // 128):
                tok = t0 + sub * 128
                kind = 0 if tok < TCTX else 1
                x, o = xt[nx % 2], xo[nx % 2]
                nx += 1
                p.dma(x, src[tok:tok + 128, :], q="sp")
                for hc in range(2):
                    po = B[nb % 8]
                    nb += 1
                    for cc in range(8):
                        p.mm(po, mrg[:, cc, sub * 128:(sub + 1) * 128], wo[:, cc, hc * 512:(hc + 1) * 512],
                             start=(cc == 0), stop=(cc == 7))
                    p.tt(o[:, hc * 512:(hc + 1) * 512], po, G1b[kind][:, hc * 512:(hc + 1) * 512], ALU.mult)
                p.tt(o, o, x, ALU.add, eng="pool")
                p.dma(last_dst[tok:tok + 128, :], o, q="pool", acc=True)
    p.barrier()


def stage_router(p, g, l, hT2, GW, DEST):
    with contextlib.ExitStack() as st:
        rwf = p.sb(st, "r_wf", [128, 8, 36])
        rwb = p.sb(st, "r_wb", [128, 8, 36], BF16)
        p.dma(rwf[:, :, 0:4], g.router_g[l].re("(c q) n -> q c n", q=128))
        p.dma(rwf[:, :, 4:36], g.router_e[l].re("(c q) n -> q c n", q=128))
        p.cp(rwb, rwf)
        RB = p.sb(st, "r_rb", [128, 36])
        p.dma(RB[:, 0:4], g.router_g_b[l].pb(128))
        p.dma(RB[:, 4:36], g.router_e_b[l].pb(128))
        LG = p.sb(st, "r_lg", [128, NT, 36])
        pl = [p.ps(st, "r_pl%d" % i, [128, 36]) for i in range(2)]
        for i in range(NT):
            pp = pl[i % 2]
            for c in range(8):
                p.mm(pp, hT2[:, c, i * 128:(i + 1) * 128], rwb[:, c, :], start=(c == 0), stop=(c == 7))
            p.cp(LG[:, i, :], pp, eng=("act" if i % 2 else "dve"))
        p.tt(LG, LG, RB.re("q (o e) -> q o e", o=1).bc([128, NT, 36]), ALU.add)
        lg = LG[:, :, 0:4]
        le = LG[:, :, 4:36].re("q i (g e) -> q i g e", e=8)
        mg = p.sb(st, "r_mg", [128, NT])
        oh = p.sb(st, "r_oh", [128, NT, 4])
        eg = p.sb(st, "r_eg", [128, NT, 4])
        pg = p.sb(st, "r_pg", [128, NT])
        tmp = p.sb(st, "r_tmp", [128, NT, 4, 8])
        les = p.sb(st, "r_les", [128, NT, 8])
        les2 = p.sb(st, "r_les2", [128, NT, 8])
        m1 = p.sb(st, "r_m1", [128, NT])
        m2 = p.sb(st, "r_m2", [128, NT])
        k1 = p.sb(st, "r_k1", [128, NT, 8])
        k2 = p.sb(st, "r_k2", [128, NT, 8])
        ex = p.sb(st, "r_ex", [128, NT, 8])

        def b3(t, n):
            return t.re("q (i o) -> q i o", o=1).bc([128, NT, n])
        p.red(mg, lg, ALU.max)
        p.tt(oh, lg, b3(mg, 4), ALU.is_equal)
        p.tt(eg, lg, b3(mg, 4), ALU.subtract)
        p.act(eg, eg, AF.Exp)
        p.red(pg, eg, ALU.add)
        p.recip(pg, pg)
        p.tt(tmp, le, oh.re("q i (g o) -> q i g o", o=1).bc([128, NT, 4, 8]), ALU.mult)
        p.red(les, tmp.re("q i g e -> q i e g"), ALU.add)
        p.red(m1, les, ALU.max)
        p.tt(k1, les, b3(m1, 8), ALU.is_equal)
        p.stt(les2, k1, -1e30, les, ALU.mult, ALU.add)
        p.red(m2, les2, ALU.max)
        p.tt(k2, les2, b3(m2, 8), ALU.is_equal)
        p.tt(m2, m2, m1, ALU.subtract)
        p.act(m2, m2, AF.Exp)
        p.ts(m2, m2, 1.0, ALU.add)
        p.recip(m2, m2)
        p.tt(GW[:, :, 0], m2, pg, ALU.mult)
        p.tt(GW[:, :, 1], pg, GW[:, :, 0], ALU.subtract)
        ohb = oh.re("q i (g o) -> q i g o", o=1).bc([128, NT, 4, 8])
        M1 = p.sb(st, "r_M1", [128, NT, 4, 8])
        M2 = p.sb(st, "r_M2", [128, NT, 4, 8])
        MM = p.sb(st, "r_MM", [128, NT, 4, 8])
        p.tt(M1, ohb, k1.re("q i (o e) -> q i o e", o=1).bc([128, NT, 4, 8]), ALU.mult)
        p.tt(M2, ohb, k2.re("q i (o e) -> q i o e", o=1).bc([128, NT, 4, 8]), ALU.mult)
        p.tt(MM, M1, M2, ALU.add)
        MMf = MM.re("q i g e -> q (i g e)")
        WI = p.sb(st, "r_WI", [128, NT, 32])
        TOT = p.sb(st, "r_TOT", [128, NT, 32])
        pw = [p.ps(st, "r_pw%d" % i, [128, 512]) for i in range(4)]
        NF = NT * 32
        for (c0, cn, k) in ((0, 512, 0), (512, NF - 512, 1)):
            p.mm(pw[k][:, :cn], g.MASK4[:, 0, 0, :], MMf[:, c0:c0 + cn])
            p.cp(WI.re("q i e -> q (i e)")[:, c0:c0 + cn], pw[k][:, :cn], eng="act")
            p.mm(pw[2 + k][:, :cn], g.onesf, MMf[:, c0:c0 + cn])
            p.cp(TOT.re("q i e -> q (i e)")[:, c0:c0 + cn], pw[2 + k][:, :cn], eng="dve")
        BASE = p.sb(st, "r_BASE", [128, NT, 32])
        p.memset(BASE[:, 0, :], 0.0)
        for i in range(1, NT):
            p.tt(BASE[:, i, :], BASE[:, i - 1, :], TOT[:, i - 1, :], ALU.add)
        p.tt(WI, WI, BASE, ALU.add)
        p.ts(TOT, WI, CAP - 0.5, ALU.is_ge)
        p.tt(WI, WI, g.EOFF.re("q (o e) -> q o e", o=1).bc([128, NT, 32]), ALU.add)
        p.ts(BASE, TOT, -1.0, ALU.mult, 1.0, ALU.add)
        p.tt(WI, WI, BASE, ALU.mult)
        p.stt(WI, TOT, float(NEXP * CAP), WI, ALU.mult, ALU.add)
        DF = p.sb(st, "r_DF", [128, NT, 2])
        for k, Mk in ((0, M1), (1, M2)):
            p.tt(Mk.re("q i g e -> q i (g e)"), Mk.re("q i g e -> q i (g e)"), WI, ALU.mult)
            p.red(DF[:, :, k], Mk.re("q i g e -> q i (g e)"), ALU.add)
        p.cp(DEST, DF)
    p.barrier()


def stage_moe(p, g, l, b, GW, DEST):
    NR = NEXP * CAP
    IOA = bass.IndirectOffsetOnAxis
    with contextlib.ExitStack() as st:
        G2b = []
        for kind, row in ((0, 2), (1, b)):
            t = p.sb(st, "e_g2b%d" % kind, [128, D])
            p.dma(t, g.MODR[l, row, G2:G2 + D].pb(128), q="act")
            G2b.append(t)
        with contextlib.ExitStack() as st2:
            hbt = [p.sb(st2, "e_hbt%d" % i, [128, D], BF16) for i in range(2)]
            for i in range(NT):
                hb = hbt[i % 2]
                p.dma(hb, g.H2T[i * 128:(i + 1) * 128, :], q="sp")
                for k in range(2):
                    idx = DEST[:, i, k:k + 1]
                    p.dma(g.XE, hb, q="pool", acc=True, extra_reads=[DEST],
                          fn=lambda e, hb=hb, idx=idx: e.indirect_dma_start(
                              out=A(g.XE), out_offset=IOA(ap=A(idx), axis=0), in_=A(hb), in_offset=None))
            w1b = [p.sb(st2, "e_w1b%d" % i, [128, 8, 512], BF16) for i in range(2)]
            w3b = [p.sb(st2, "e_w3b%d" % i, [128, 8, 512], BF16) for i in range(2)]
            w2b = [p.sb(st2, "e_w2b%d" % i, [128, 4, 1024], BF16) for i in range(2)]
            xe = [p.sb(st2, "e_xe%d" % i, [128, NSL, D], BF16) for i in range(2)]
            hTe = p.sb(st2, "e_hTe", [128, 8, CAP], BF16)
            sl = [p.sb(st2, "e_sl%d" % i, [128, 512]) for i in range(2)]
            hid = p.sb(st2, "e_hid", [128, 4, CAP], BF16)
            ye = [p.sb(st2, "e_ye%d" % i, [128, NSL, D]) for i in range(2)]
            B = [p.ps(st2, "e_pb%d" % i, [128, 512]) for i in range(6)]
            pt = [p.ps(st2, "e_pt%d" % i, [128, 8, 128], BF16) for i in range(2)]
            nb = 0
            ns = 0
            nsl = 0
            npt = 0
            CT = [(0, 512), (512, CAP - 512)] if CAP > 512 else [(0, CAP)]
            def load_w(e):
                k = e % 2
                p.dma(w1b[k], g.exp_w1[l, e].re("(c q) n -> q c n", q=128), q="pool")
                p.dma(w3b[k], g.exp_w3[l, e].re("(c q) n -> q c n", q=128), q="pool")
                p.dma(w2b[k], g.exp_w2[l, e].re("(c q) n -> q c n", q=128), q="pool")
                p.dma(xe[k], g.XE[e * CAP:(e + 1) * CAP, :].re("(j q) n -> q j n", q=128), q="sp")
            load_w(0)
            for e in range(NEXP):
                k = e % 2
                if e + 1 < NEXP:
                    load_w(e + 1)
                x_ = xe[k]
                for j in range(NSL):
                    ptt = pt[npt % 2]
                    npt += 1
                    for c in range(8):
                        p.tr(ptt[:, c, :], x_[:, j, c * 128:(c + 1) * 128], g.identb)
                    p.cp(hTe[:, :, j * 128:(j + 1) * 128], ptt, eng=("act" if j % 2 else "dve"))
                for ff in range(4):
                    for (t0, tn) in CT:
                        p1 = B[nb % 6]
                        p3 = B[(nb + 1) % 6]
                        nb += 2
                        for kc in range(8):
                            p.mm(p1[:, :tn], w1b[k][:, kc, ff * 128:(ff + 1) * 128], hTe[:, kc, t0:t0 + tn],
                                 start=(kc == 0), stop=(kc == 7))
                        for kc in range(8):
                            p.mm(p3[:, :tn], w3b[k][:, kc, ff * 128:(ff + 1) * 128], hTe[:, kc, t0:t0 + tn],
                                 start=(kc == 0), stop=(kc == 7))
                        s_ = sl[nsl % 2]
                        nsl += 1
                        p.act(s_[:, :tn], p1[:, :tn], AF.Silu)
                        p.tt(hid[:, ff, t0:t0 + tn], s_[:, :tn], p3[:, :tn], ALU.mult)
                y_ = ye[k]
                for j in range(NSL):
                    for hc in range(2):
                        po = B[nb % 6]
                        nb += 1
                        for ff in range(4):
                            p.mm(po, hid[:, ff, j * 128:(j + 1) * 128], w2b[k][:, ff, hc * 512:(hc + 1) * 512],
                                 start=(ff == 0), stop=(ff == 3))
                        p.cp(y_[:, j, hc * 512:(hc + 1) * 512], po, eng=("act" if (j + hc) % 2 else "dve"))
                p.dma(g.YE[e * CAP:(e + 1) * CAP, :].re("(j q) n -> q j n", q=128), y_, q="sp", acc=True)
            p.barrier()
        ya = [p.sb(st, "e_ya%d" % i, [128, D]) for i in range(2)]
        yb = [p.sb(st, "e_yb%d" % i, [128, D]) for i in range(2)]
        xt = [p.sb(st, "e_xt%d" % i, [128, D]) for i in range(2)]
        for i in range(NT):
            tok = i * 128
            kind = 0 if i < 2 else 1
            a_, b_, x_ = ya[i % 2], yb[i % 2], xt[i % 2]
            p.dma(x_, g.XS[b][tok:tok + 128, :], q="sp")
            for k, dst in ((0, a_), (1, b_)):
                p.memset(dst, 0.0, eng="pool")
                idx = DEST[:, i, k:k + 1]
                p.dma(dst, g.YE, q="pool", extra_reads=[DEST],
                      fn=lambda e, dst=dst, idx=idx: e.indirect_dma_start(
                          out=A(dst), out_offset=None, in_=A(g.YE), in_offset=IOA(ap=A(idx), axis=0)))
            p.ts(a_, a_, GW[:, i, 0:1], ALU.mult)
            p.stt(a_, b_, GW[:, i, 1:2], a_, ALU.mult, ALU.add)
            p.tt(a_, a_, G2b[kind], ALU.mult)
            p.tt(a_, a_, x_, ALU.add)
            p.dma(g.XS[b][tok:tok + 128, :], a_, q="act", acc=True)
    p.barrier()


def stage_final(p, g, b):
    with contextlib.ExitStack() as st:
        gt = p.sb(st, "f_gt", [128, D])
        p.dma(gt, g.final_g.pb(128))
        xts = [p.sb(st, "f_xt%d" % i, [128, D]) for i in range(2)]
        sqs = [p.sb(st, "f_sq%d" % i, [128, D]) for i in range(2)]
        sss = [p.sb(st, "f_ss%d" % i, [128, 2]) for i in range(2)]
        for i in range(TLAT // 128):
            xt, sq, ss = xts[i % 2], sqs[i % 2], sss[i % 2]
            p.dma(xt, g.XS[b][TCTX + i * 128:TCTX + (i + 1) * 128, :], q=("sp" if i % 2 == 0 else "act"))
            p.act(sq, xt, AF.Square)
            p.red(ss[:, 0:1], sq, ALU.add)
            p.ts(ss[:, 1:2], ss[:, 0:1], 1.0 / D, ALU.mult, EPS, ALU.add)
            p.act(ss[:, 1:2], ss[:, 1:2], AF.Sqrt)
            p.recip(ss[:, 1:2], ss[:, 1:2])
            p.stt(sq, xt, ss[:, 1:2], gt, ALU.mult, ALU.mult)
            p.dma(g.out[b, i * 128:(i + 1) * 128, :], sq, q="pool", acc=True)
    p.barrier()
```

```python
import contextlib
import numpy as np
import concourse.bass as bass
import concourse.mybir as mybir
from concourse.bass_utils import run_bass_kernel_spmd

F32 = mybir.dt.float32
BF16 = mybir.dt.bfloat16
I32 = mybir.dt.int32
AF = mybir.ActivationFunctionType
ALU = mybir.AluOpType
AX = mybir.AxisListType

D = 1024
DEPTH = 4
NB = 2
TCTX = 256
TLAT = 2048
T = TCTX + TLAT
NT = T // 128
NIN = 5504
RW0 = 1536
NEXP = 32
DFF = 512
CAP = 640
NSL = CAP // 128
CH = 128
NCH = T // CH
EPS = 1e-6
GN_EPS = 64e-5
TT = [(0, 512), (512, 512), (1024, 512), (1536, 512), (2048, 256)]


class Tl:
    def __init__(self, ap, name=""):
        self.ap = ap
        self.name = name
        self.lw = {}
        self.rd = {}

    def __getitem__(self, k):
        return Vw(self, self.ap[k])

    def re(self, s, **kw):
        return Vw(self, self.ap.rearrange(s, **kw))

    def bc(self, shape):
        return Vw(self, self.ap.to_broadcast(list(shape)))

    def pb(self, n):
        return Vw(self, self.ap.partition_broadcast(n))


class Vw:
    def __init__(self, t, ap):
        self.t = t
        self.ap = ap

    def __getitem__(self, k):
        return Vw(self.t, self.ap[k])

    def re(self, s, **kw):
        return Vw(self.t, self.ap.rearrange(s, **kw))

    def bc(self, shape):
        return Vw(self.t, self.ap.to_broadcast(list(shape)))

    def pb(self, n):
        return Vw(self.t, self.ap.partition_broadcast(n))


def A(x):
    return x.ap if isinstance(x, (Tl, Vw)) else x


def TT_(x):
    if isinstance(x, Tl):
        return x
    if isinstance(x, Vw):
        return x.t
    return None


class P:
    KD = 8

    def __init__(self, nc):
        self.nc = nc
        self.es = contextlib.ExitStack()
        self.eng = {"pe": nc.tensor, "act": nc.scalar, "dve": nc.vector, "pool": nc.gpsimd, "sp": nc.sync}
        self.sem = {}
        self.cur = {}
        for e in ("pe", "act", "dve", "pool"):
            self.sem[e] = self.es.enter_context(nc.semaphore("s_" + e))
            self.cur[e] = 0
        self.dcount = {}
        for q in ("sp", "pool", "act"):
            self.dcount[q] = 0
            for s in range(self.KD):
                k = ("d", q, s)
                self.sem[k] = self.es.enter_context(nc.semaphore("d_%s_%d" % (q, s)))
                self.cur[k] = 0
        self.waited = {e: {} for e in self.eng}
        self.nins = 0

    def sb(self, stack, name, shape, dt=F32):
        self.uid = getattr(self, "uid", 0) + 1
        name = "%s_%d" % (name, self.uid)
        h = stack.enter_context(self.nc.sbuf_tensor(name, list(shape), dt))
        return Tl(h[:], name)

    def ps(self, stack, name, shape, dt=F32):
        self.uid = getattr(self, "uid", 0) + 1
        name = "%s_%d" % (name, self.uid)
        h = stack.enter_context(self.nc.psum_tensor(name, list(shape), dt))
        return Tl(h[:], name)

    def dram(self, name, shape, dt=F32, kind="Internal"):
        h = self.nc.dram_tensor(name, list(shape), dt, kind=kind)
        return Tl(h.ap(), name)

    def _deps(self, eng, reads, writes, acc=False):
        need = {}

        def add(tok):
            if tok is not None:
                k, v = tok
                if need.get(k, 0) < v:
                    need[k] = v
        for x in reads:
            t = TT_(x)
            if t is not None:
                for k, v in t.lw.items():
                    add((k, v))
        for x in writes:
            t = TT_(x)
            if t is not None:
                if not acc:
                    for k, v in t.lw.items():
                        add((k, v))
                for k, v in t.rd.items():
                    add((k, v))
        out = []
        w = self.waited[eng]
        for k, v in need.items():
            if k == eng and eng == "pe":
                continue
            if w.get(k, 0) >= v:
                continue
            w[k] = v
            out.append((k, v))
        return out

    def _commit(self, tok, reads, writes, acc=False):
        k, v = tok
        for x in reads:
            t = TT_(x)
            if t is not None and t.rd.get(k, 0) < v:
                t.rd[k] = v
        for x in writes:
            t = TT_(x)
            if t is not None:
                if acc:
                    if t.lw.get(k, 0) < v:
                        t.lw[k] = v
                else:
                    t.lw = {k: v}
                    t.rd = {}

    def op(self, eng, fn, reads, writes):
        e = self.eng[eng]
        for k, v in self._deps(eng, reads, writes):
            e.wait_ge(self.sem[k], v)
        ins = fn(e)
        self.cur[eng] += 1
        ins.then_inc(self.sem[eng], 1)
        self._commit((eng, self.cur[eng]), reads, writes)
        self.nins += 1

    def dma(self, out, in_, q="sp", acc=False, fn=None, extra_reads=(), **kw):
        e = self.eng[q]
        j = self.dcount[q]
        self.dcount[q] = j + 1
        slot, gen = j % self.KD, j // self.KD
        key = ("d", q, slot)
        rds = [in_] + list(extra_reads)
        deps = self._deps(q, rds, [out], acc=acc)
        if gen > 0 and self.waited[q].get(key, 0) < 16 * gen:
            self.waited[q][key] = 16 * gen
            deps.append((key, 16 * gen))
        for k, v in deps:
            e.wait_ge(self.sem[k], v)
        if fn is None:
            ins = e.dma_start(out=A(out), in_=A(in_), **kw)
        else:
            ins = fn(e)
        ins.then_inc(self.sem[key], 16)
        self.cur[key] = 16 * (gen + 1)
        self._commit((key, 16 * (gen + 1)), rds, [out], acc=acc)
        self.nins += 1

    def barrier(self, engines=None):
        for en, e in self.eng.items():
            if engines is not None and en not in engines:
                continue
            w = self.waited[en]
            for k, v in self.cur.items():
                if v == 0 or (k == en and en == "pe"):
                    continue
                if w.get(k, 0) >= v:
                    continue
                w[k] = v
                e.wait_ge(self.sem[k], v)

    def mm(self, out, lhsT, rhs, start=True, stop=True):
        self.op("pe", lambda e: e.matmul(A(out), A(lhsT), A(rhs), start=start, stop=stop), [lhsT, rhs], [out])

    def tr(self, out, in_, ident):
        self.op("pe", lambda e: e.transpose(A(out), A(in_), A(ident)), [in_, ident], [out])

    def act(self, out, in_, func, bias=None, scale=None, accum=None, extra_reads=()):
        kw = {}
        rd = [in_] + list(extra_reads)
        if bias is not None:
            kw["bias"] = A(bias)
            rd.append(bias)
        if scale is not None:
            kw["scale"] = A(scale)
            rd.append(scale)
        wr = [out]
        if accum is not None:
            kw["accum_out"] = A(accum)
            wr.append(accum)
        self.op("act", lambda e: e.activation(out=A(out), in_=A(in_), func=func, **kw), rd, wr)

    def tt(self, out, in0, in1, op, eng="dve"):
        self.op(eng, lambda e: e.tensor_tensor(out=A(out), in0=A(in0), in1=A(in1), op=op), [in0, in1], [out])

    def ts(self, out, in0, s1, op0, s2=None, op1=None, eng="dve", accum=None):
        rd = [in0, s1, s2]
        kw = {}
        if op1 is not None:
            kw["op1"] = op1
        wr = [out]
        if accum is not None:
            kw["accum_out"] = A(accum)
            wr.append(accum)
        self.op(eng, lambda e: e.tensor_scalar(out=A(out), in0=A(in0), scalar1=A(s1), scalar2=A(s2), op0=op0, **kw), rd, wr)

    def stt(self, out, in0, scalar, in1, op0, op1, eng="dve"):
        self.op(eng, lambda e: e.scalar_tensor_tensor(out=A(out), in0=A(in0), scalar=A(scalar), in1=A(in1), op0=op0, op1=op1),
                [in0, scalar, in1], [out])

    def cp(self, out, in_, eng="dve"):
        if eng == "act":
            self.op("act", lambda e: e.copy(out=A(out), in_=A(in_)), [in_], [out])
        else:
            self.op(eng, lambda e: e.tensor_copy(out=A(out), in_=A(in_)), [in_], [out])

    def recip(self, out, in_):
        self.op("dve", lambda e: e.reciprocal(out=A(out), in_=A(in_)), [in_], [out])

    def memset(self, out, val, eng="dve"):
        self.op(eng, lambda e: e.memset(A(out), val), [], [out])

    def red(self, out, in_, op, axis=AX.X, eng="dve"):
        self.op(eng, lambda e: e.tensor_reduce(out=A(out), in_=A(in_), axis=axis, op=op), [in_], [out])


class G:
    pass


SH1, SC1, G1, SH2, SC2, G2 = [i * D for i in range(6)]
C_MU, C_CONV = 0, 15
C_KK, C_KA, C_RK, C_W0, C_A0, C_LG, C_LB = 0, 8, 16, 24, 40, 56, 64
C2_KK, C2_KA, C2_RK, C2_W0, C2_A0, C2_LG, C2_LB = 27, 31, 35, 39, 47, 55, 59
NP128 = 63


def stage_params(p, g):
    nc = p.nc
    with contextlib.ExitStack() as st:
        crow = p.sb(st, "crow", [3, D])
        p.dma(crow, g.c3)
        srow = p.sb(st, "srow", [3, D])
        p.act(srow, crow, AF.Silu)
        sT = p.sb(st, "sT", [128, 8, 3], BF16)
        pst = p.ps(st, "pst", [128, 8, 4])
        for c in range(8):
            p.tr(pst[:, c, 0:3], srow[:, c * 128:(c + 1) * 128], g.ident[0:3, 0:3])
        p.cp(sT, pst[:, :, 0:3])
        wst = [p.sb(st, "wst%d" % i, [128, 8, 512], BF16) for i in range(3)]
        bm = p.sb(st, "bm", [3, 6 * D])
        mrow = p.sb(st, "mrow", [3, 6 * D])
        pm = [p.ps(st, "pm%d" % i, [3, 512]) for i in range(2)]
        k = 0
        for l in range(DEPTH):
            p.dma(bm, g.b_mod[l].pb(3))
            for cg in range(12):
                ws = wst[k % 3]
                p.dma(ws, g.w_mod[l].re("(c q) n -> q c n", q=128)[:, :, cg * 512:(cg + 1) * 512], q="pool")
                pp = pm[k % 2]
                for kc in range(8):
                    p.mm(pp, sT[:, kc, :], ws[:, kc, :], start=(kc == 0), stop=(kc == 7))
                p.tt(mrow[:, cg * 512:(cg + 1) * 512], pp, bm[:, cg * 512:(cg + 1) * 512], ALU.add)
                k += 1
            p.dma(g.MODR[l], mrow, q="sp")
        pr128 = p.sb(st, "pr128", [NP128, 128])
        pr64 = p.sb(st, "pr64", [72, 64])
        pp128 = p.ps(st, "pp128", [128, 64])
        pp64 = p.ps(st, "pp64", [64, 72])
        for l in range(DEPTH):
            p.dma(pr128[0:15, :], g.shift_mu[l].re("(c q) -> c q", q=128))
            p.dma(pr128[15:27, :], g.conv_w[l].re("j (c q) -> (j c) q", q=128))
            p.dma(pr64[C_KK:C_KK + 8, :], g.k_k[l].re("(h k) -> h k", k=64))
            p.dma(pr64[C_KA:C_KA + 8, :], g.k_a[l].re("(h k) -> h k", k=64))
            p.dma(pr64[C_RK:C_RK + 8, :], g.r_k[l])
            p.dma(pr64[C_W0:C_W0 + 16, :], g.w0[l].re("d (h k) -> (d h) k", k=64))
            p.dma(pr64[C_A0:C_A0 + 16, :], g.a0[l].re("d (h k) -> (d h) k", k=64))
            p.dma(pr64[C_LG:C_LG + 8, :], g.lnx_g[l].re("(h k) -> h k", k=64))
            p.dma(pr64[C_LB:C_LB + 8, :], g.lnx_b[l].re("(h k) -> h k", k=64))
            p.dma(pr128[C2_KK:C2_KK + 4, :], g.k_k[l].re("(c q) -> c q", q=128))
            p.dma(pr128[C2_KA:C2_KA + 4, :], g.k_a[l].re("(c q) -> c q", q=128))
            p.dma(pr128[C2_RK:C2_RK + 4, :], g.r_k[l].re("(c a) k -> c (a k)", a=2))
            p.dma(pr128[C2_W0:C2_W0 + 8, :], g.w0[l].re("d (c q) -> (d c) q", q=128))
            p.dma(pr128[C2_A0:C2_A0 + 8, :], g.a0[l].re("d (c q) -> (d c) q", q=128))
            p.dma(pr128[C2_LG:C2_LG + 4, :], g.lnx_g[l].re("(c q) -> c q", q=128))
            p.dma(pr128[C2_LB:C2_LB + 4, :], g.lnx_b[l].re("(c q) -> c q", q=128))
            p.tr(pp128[:, 0:NP128], pr128, g.ident[0:NP128, 0:NP128])
            p.cp(g.P128T[:, l, :], pp128[:, 0:NP128])
            p.tr(pp64, pr64, g.ident[0:72, 0:72])
            p.cp(g.P64T[:, l, :], pp64)
        p.ts(g.OMM, g.P128T[:, :, C_MU:C_MU + 15], -1.0, ALU.mult, 1.0, ALU.add)
        p.ts(g.HMU, g.P128T[:, :, C_MU:C_MU + 15], 0.5, ALU.mult)
        p.ts(g.OMKA, g.P128T[:, :, C2_KA:C2_KA + 4], -1.0, ALU.mult, 1.0, ALU.add)
    p.barrier()


def stage_norm(p, g, src, l, b, second, hT, hTf=None):
    SHc, SCc = (SH2, SC2) if second else (SH1, SC1)
    ng = g.norm2_g if second else g.norm1_g
    with contextlib.ExitStack() as st:
        gt = p.sb(st, "gt", [128, D])
        p.dma(gt, ng[l].pb(128))
        Ab, Bb = [], []
        for kind, row in ((0, 2), (1, b)):
            sc = p.sb(st, "scb%d" % kind, [128, D])
            p.dma(sc, g.MODR[l, row, SCc:SCc + D].pb(128))
            a = p.sb(st, "Ab%d" % kind, [128, D])
            p.stt(a, sc, 1.0, gt, ALU.add, ALU.mult)
            bb = p.sb(st, "Bb%d" % kind, [128, D])
            p.dma(bb, g.MODR[l, row, SHc:SHc + D].pb(128))
            Ab.append(a)
            Bb.append(bb)
        xall = [p.sb(st, "xa%d" % i, [128, D]) for i in range(NT)]
        sqs = [p.sb(st, "sq%d" % i, [128, D]) for i in range(2)]
        hns = [p.sb(st, "hn%d" % i, [128, D]) for i in range(2)]
        hbs = [p.sb(st, "hb%d" % i, [128, D], BF16) for i in range(2)]
        ssall = p.sb(st, "ssall", [128, NT])
        rs = p.sb(st, "rsall", [128, NT])
        ptr = [p.ps(st, "ptr%d" % i, [128, 8, 128], BF16) for i in range(2)]
        for i in range(NT):
            p.dma(xall[i], src[i * 128:(i + 1) * 128, :], q=("sp" if i % 2 == 0 else "act"))
            p.act(sqs[i % 2], xall[i], AF.Square)
            p.red(ssall[:, i:i + 1], sqs[i % 2], ALU.add)
        p.ts(rs, ssall, 1.0 / D, ALU.mult, EPS, ALU.add)
        p.act(rs, rs, AF.Sqrt)
        p.recip(rs, rs)
        for i in range(NT):
            kind = 0 if i < 2 else 1
            hn, hb, pt = hns[i % 2], hbs[i % 2], ptr[i % 2]
            p.stt(hn, xall[i], rs[:, i:i + 1], Ab[kind], ALU.mult, ALU.mult)
            p.tt(hb, hn, Bb[kind], ALU.add, eng="pool")
            if second:
                p.dma(g.H2T[i * 128:(i + 1) * 128, :], hb, q="pool", acc=True)
            for c in range(8):
                p.tr(pt[:, c, :], hb[:, c * 128:(c + 1) * 128], g.identb)
            p.cp(hT[:, :, i * 128:(i + 1) * 128], pt, eng="act")


def stage_proj(p, g, hT, l, b):
    groups = [(c0, min(512, NIN - c0)) for c0 in range(0, NIN, 512)]
    with contextlib.ExitStack() as st:
        wbf = [p.sb(st, "pwbf%d" % i, [128, 8, 512], BF16) for i in range(2)]

        def loadg(gi):
            c0, w = groups[gi]
            p.dma(wbf[gi % 2][:, :, :w], g.w_in[l].re("(c q) n -> q c n", q=128)[:, :, c0:c0 + w], q="pool")
        loadg(0)
        ot = [p.sb(st, "pot%d" % i, [128, T]) for i in range(2)]
        zt = [p.sb(st, "pzt%d" % i, [128, T]) for i in range(2)]
        pss = [p.ps(st, "pps%d" % i, [128, 512]) for i in range(4)]
        k = 0
        kk = 0
        for gi, (c0, w) in enumerate(groups):
            wb = wbf[gi % 2]
            if gi + 1 < len(groups):
                loadg(gi + 1)
            for cc in range(w // 128):
                col = c0 + cc * 128
                chunk = col // 128
                o = ot[k % 2]
                for (t0, tn) in TT:
                    ps = pss[kk % 4]
                    for kc in range(8):
                        p.mm(ps[:, :tn], wb[:, kc, cc * 128:(cc + 1) * 128], hT[:, kc, t0:t0 + tn],
                             start=(kc == 0), stop=(kc == 7))
                    if chunk >= 27:
                        p.act(o[:, t0:t0 + tn], ps[:, :tn], AF.Sigmoid)
                    elif kk % 2 == 0:
                        p.cp(o[:, t0:t0 + tn], ps[:, :tn], eng="act")
                    else:
                        p.cp(o[:, t0:t0 + tn], ps[:, :tn], eng="dve")
                    kk += 1
                if 12 <= chunk < 27:
                    j = chunk - 12
                    z = zt[k % 2]
                    om = g.OMM[:, l, j:j + 1]
                    hm = g.HMU[:, l, j:j + 1]
                    p.ts(z, o, om, ALU.mult)
                    for (a0, a1) in ((0, TCTX), (TCTX, T)):
                        p.stt(z[:, a0 + 1:a1], o[:, a0:a1 - 1], hm, z[:, a0 + 1:a1], ALU.mult, ALU.add)
                        p.stt(z[:, a0:a1 - 1], o[:, a0 + 1:a1], hm, z[:, a0:a1 - 1], ALU.mult, ALU.add)
                    o = z
                p.dma(g.ZF[col:col + 128, :], o, q="sp", acc=True)
                k += 1


WEIGHTS = [
    ("w_mod", [DEPTH, D, 6 * D]), ("b_mod", [DEPTH, 6 * D]), ("norm1_g", [DEPTH, D]), ("norm2_g", [DEPTH, D]),
    ("w_in", [DEPTH, D, NIN]), ("shift_mu", [DEPTH, 1920]), ("conv_w", [DEPTH, 3, 512]),
    ("w_up", [DEPTH, 2, 64, 512]), ("w0", [DEPTH, 2, 512]), ("a_up", [DEPTH, 2, 64, 512]), ("a0", [DEPTH, 2, 512]),
    ("g_up", [DEPTH, 128, 512]), ("k_k", [DEPTH, 512]), ("k_a", [DEPTH, 512]), ("r_k", [DEPTH, 8, 64]),
    ("lnx_g", [DEPTH, 512]), ("lnx_b", [DEPTH, 512]), ("w_a_out", [DEPTH, 512, D]), ("w_b_out", [DEPTH, 512, D]),
    ("w_o", [DEPTH, D, D]), ("router_g", [DEPTH, D, 4]), ("router_g_b", [DEPTH, 4]),
    ("router_e", [DEPTH, D, NEXP]), ("router_e_b", [DEPTH, NEXP]),
    ("exp_w1", [DEPTH, NEXP, D, DFF]), ("exp_w3", [DEPTH, NEXP, D, DFF]), ("exp_w2", [DEPTH, NEXP, DFF, D]),
    ("final_g", [D]),
]


def build(stages="all", dbg=()):
    nc = bass.Bass("TRN2", target_bir_lowering=False)
    p = P(nc)
    g = G()
    g.p = p

    def inp(name, shape):
        return Tl(nc.dram_tensor(name, list(shape), F32, kind="ExternalInput").ap(), name)
    g.xin = inp("xin", [NB, T, D])
    g.c3 = inp("c3", [3, D])
    g.idin = inp("idin", [128, 128])
    g.mskin = inp("mskin", [128, 2, 4, 128])
    g.eoffin = inp("eoffin", [128, NEXP])
    for name, shape in WEIGHTS:
        setattr(g, name, inp(name, shape))
    g.out = Tl(nc.dram_tensor("out", [NB, TLAT, D], F32, kind="ExternalOutput").ap(), "out")
    g.MODR = p.dram("MODR", [DEPTH, 3, 6 * D])
    g.ZF = p.dram("ZF", [NIN, T])
    g.XS = [p.dram("XS%d" % b, [T, D]) for b in range(NB)]
    g.SCF = p.dram("SCF", [2, 512, NCH, 4, CH], BF16)
    g.SCT = p.dram("SCT", [2, NCH, CH, 2, 512], BF16)
    g.VTD = p.dram("VTD", [NCH, CH, 512], BF16)
    g.WCD = p.dram("WCD", [512, 2, NCH])
    g.RO = p.dram("RO", [2, 512, T])
    g.YD = p.dram("YD", [2, 512, T])
    g.H2T = p.dram("H2T", [T, D], BF16)
    g.XE = p.dram("XE", [NEXP * CAP + 1, D], BF16)
    g.YE = p.dram("YE", [NEXP * CAP + 1, D])
    g.YEZ = p.dram("YEZ", [1, D])
    dbg_out = {}
    for name, shape in dbg:
        dbg_out[name] = Tl(nc.dram_tensor("dbg_" + name, list(shape), F32, kind="ExternalOutput").ap(), name)
    g.dbg = dbg_out
    top = p.es
    g.ident = p.sb(top, "ident", [128, 128])
    g.identb = p.sb(top, "identb", [128, 128], BF16)
    p.dma(g.ident, g.idin)
    p.cp(g.identb, g.ident)
    g.P128T = p.sb(top, "P128T", [128, DEPTH, NP128])
    g.P64T = p.sb(top, "P64T", [64, DEPTH, 72])
    g.OMM = p.sb(top, "OMM", [128, DEPTH, 15])
    g.HMU = p.sb(top, "HMU", [128, DEPTH, 15])
    g.OMKA = p.sb(top, "OMKA", [128, DEPTH, 4])
    g.MASK4 = p.sb(top, "MASK4", [128, 2, 4, 128])
    p.dma(g.MASK4, g.mskin)
    g.ones64 = p.sb(top, "ones64", [64, 64])
    p.memset(g.ones64, 1.0)
    g.onesf = p.sb(top, "onesf", [128, 128])
    p.memset(g.onesf, 1.0)
    g.EOFF = p.sb(top, "EOFF", [128, NEXP])
    p.dma(g.EOFF, g.eoffin)
    with contextlib.ExitStack() as stz:
        zt = p.sb(stz, "zrow", [1, D])
        p.memset(zt, 0.0)
        p.dma(g.YE[NEXP * CAP:NEXP * CAP + 1, :], zt, q="pool")
        p.barrier()
    g.ones2 = p.sb(top, "ones2", [128, 128])
    p.memset(g.ones2, 0.0)
    p.memset(g.ones2[0:64, 0:64], 1.0)
    p.memset(g.ones2[64:128, 64:128], 1.0)
    g.RM = p.sb(top, "RM", [128, TH])
    p.memset(g.RM, 1.0)
    p.memset(g.RM.re("k (c t) -> k c t", t=CH)[:, :, 0:1], 0.0)

    stage_params(p, g)
    nl = DEPTH if stages == "all" else stages[0]
    nb = NB if stages == "all" else stages[1]
    upto = "z" if stages == "all" else stages[2]
    for b in range(nb):
        for l in range(nl):
            src = g.xin[b] if l == 0 else g.XS[b]
            with contextlib.ExitStack() as st:
                hT = p.sb(st, "hT", [128, 8, T], BF16)
                stage_norm(p, g, src, l, b, False, hT)
                if upto == "A":
                    if "hT" in g.dbg:
                        with contextlib.ExitStack() as s2:
                            tmp = p.sb(s2, "dbgtmp", [128, 8, T])
                            p.cp(tmp, hT)
                            p.dma(g.dbg["hT"], tmp)
                            p.barrier()
                    p.barrier()
                    continue
                stage_proj(p, g, hT, l, b)
                p.barrier()
            if upto == "B":
                continue
            with contextlib.ExitStack() as st:
                CO = p.sb(st, "CO", [128, 4, T], BF16)
                stage_conv(p, g, l, b, CO)
                stage_prep(p, g, l, b)
                if upto == "D":
                    continue
                stage_scan(p, g, l, b)
                if upto == "E":
                    continue
                stage_mix(p, g, l, b, CO, src, g.XS[b])
            if upto == "G":
                continue
            with contextlib.ExitStack() as st:
                GW = p.sb(st, "GW", [128, NT, 2])
                DEST = p.sb(st, "DEST", [128, NT, 2], I32)
                with contextlib.ExitStack() as st2:
                    hT = p.sb(st2, "hT2", [128, 8, T], BF16)
                    stage_norm(p, g, g.XS[b], l, b, True, hT)
                    stage_router(p, g, l, hT, GW, DEST)
                stage_moe(p, g, l, b, GW, DEST)
        if upto == "z":
            stage_final(p, g, b)
    if "ZF" in g.dbg:
        p.barrier()
        with contextlib.ExitStack() as s2:
            tmp = p.sb(s2, "dbgtmp", [128, T])
            for c in range(NIN // 128):
                p.dma(tmp, g.ZF[c * 128:(c + 1) * 128, :])
                p.dma(g.dbg["ZF"][c * 128:(c + 1) * 128, :], tmp)
            p.barrier()
    for nm, t in (("YD", g.YD.re("d n t -> (d n) t")), ("RO", g.RO.re("q n t -> (q n) t")), ("XS0", g.XS[0])):
        if nm in g.dbg:
            with contextlib.ExitStack() as s2:
                n, w = t.ap.shape
                tmp = p.sb(s2, "dbgtmp3", [128, w])
                for c in range(n // 128):
                    p.dma(tmp, t[c * 128:(c + 1) * 128, :])
                    p.dma(g.dbg[nm][c * 128:(c + 1) * 128, :], tmp)
                p.barrier()
    if "MODR" in g.dbg:
        with contextlib.ExitStack() as s2:
            tmp = p.sb(s2, "dbgtmp2", [12, 6 * D])
            p.dma(tmp, g.MODR.re("l r n -> (l r) n"))
            p.dma(g.dbg["MODR"], tmp)
            p.barrier()
    p.barrier()
    p.es.close()
    return nc, p


def make_in_maps(inputs, ncores=8):
    idm = np.eye(128, dtype=np.float32)
    ii = np.arange(128)
    us = (ii[None, :] > ii[:, None]).astype(np.float32)
    ui = (ii[None, :] >= ii[:, None]).astype(np.float32)
    msk = np.ascontiguousarray(np.stack([np.stack([us, ui, us, ui], 0), np.stack([us.T, ui.T, us.T, ui.T], 0)], 0).transpose(2, 0, 1, 3))
    eoff = np.ascontiguousarray(np.broadcast_to((np.arange(NEXP, dtype=np.float32) * CAP)[None, :], (128, NEXP)))
    maps = []
    for core in range(ncores):
        b0 = core * NB
        xin = np.concatenate([inputs["ctx"][b0:b0 + NB], inputs["x"][b0:b0 + NB]], axis=1)
        c3 = np.concatenate([inputs["c"][b0:b0 + NB], inputs["c_ctx"][None, :]], axis=0)
        m = {"xin": np.ascontiguousarray(xin, dtype=np.float32), "c3": np.ascontiguousarray(c3, dtype=np.float32), "idin": idm,
             "mskin": msk, "eoffin": eoff}
        for name, _ in WEIGHTS:
            m[name] = np.ascontiguousarray(inputs[name], dtype=np.float32)
        maps.append(m)
    return maps


def kernel(**inputs):
    inputs = {k: np.asarray(v) for k, v in inputs.items()}
    nc, p = build("all")
    maps = make_in_maps(inputs, 8)
    res = run_bass_kernel_spmd(nc, maps, core_ids=list(range(8)))
    return np.concatenate([r["out"] for r in res.results], axis=0).astype(np.float32)


def stage_conv(p, g, l, b, CO):
    with contextlib.ExitStack() as st:
        bgt = [p.sb(st, "cbg%d" % i, [128, T]) for i in range(2)]
        cgt = [p.sb(st, "ccg%d" % i, [128, T]) for i in range(2)]
        hat = [p.sb(st, "cha%d" % i, [128, T]) for i in range(2)]
        ut = [p.sb(st, "cu%d" % i, [128, T]) for i in range(2)]
        ott = [p.sb(st, "co%d" % i, [128, T]) for i in range(2)]
        for j in range(4):
            bg, cg, ha, u, o = bgt[j % 2], cgt[j % 2], hat[j % 2], ut[j % 2], ott[j % 2]
            p.dma(bg, g.ZF[j * 128:(j + 1) * 128, :], q="sp")
            p.dma(cg, g.ZF[512 + j * 128:512 + (j + 1) * 128, :], q="act")
            p.dma(ha, g.ZF[1024 + j * 128:1024 + (j + 1) * 128, :], q="sp")
            w0 = g.P128T[:, l, C_CONV + 0 * 4 + j:C_CONV + 0 * 4 + j + 1]
            w1 = g.P128T[:, l, C_CONV + 1 * 4 + j:C_CONV + 1 * 4 + j + 1]
            w2 = g.P128T[:, l, C_CONV + 2 * 4 + j:C_CONV + 2 * 4 + j + 1]
            p.tt(u, cg, ha, ALU.mult, eng="pool")
            p.ts(o, u, w1, ALU.mult)
            p.stt(o[:, 1:TCTX], u[:, 0:TCTX - 1], w0, o[:, 1:TCTX], ALU.mult, ALU.add)
            p.stt(o[:, 0:TCTX - 1], u[:, 1:TCTX], w2, o[:, 0:TCTX - 1], ALU.mult, ALU.add)
            if j < 2:
                ug = u[:, TCTX:T].re("q (r w) -> q r w", w=64)
                og = o[:, TCTX:T].re("q (r w) -> q r w", w=64)
                p.stt(og[:, :, 1:64], ug[:, :, 0:63], w0, og[:, :, 1:64], ALU.mult, ALU.add)
                p.stt(og[:, :, 0:63], ug[:, :, 1:64], w2, og[:, :, 0:63], ALU.mult, ALU.add)
            else:
                p.stt(o[:, TCTX + 64:T], u[:, TCTX:T - 64], w0, o[:, TCTX + 64:T], ALU.mult, ALU.add)
                p.stt(o[:, TCTX:T - 64], u[:, TCTX + 64:T], w2, o[:, TCTX:T - 64], ALU.mult, ALU.add)
            p.tt(CO[:, j, :], o, bg, ALU.mult, eng="pool")
    p.barrier()


TH = 1152
THT = [(0, 512), (512, 512), (1024, 128)]
NCH2 = TH // CH


def stage_prep(p, g, l, b):
    with contextlib.ExitStack() as st:
        tzw = [p.sb(st, "tzw%d" % d, [64, T], BF16) for d in range(2)]
        zab = [p.sb(st, "zab%d" % d, [64, T], BF16) for d in range(2)]
        sgz = p.sb(st, "sgz", [128, T], BF16)
        with contextlib.ExitStack() as st0:
            tmpf = p.sb(st0, "dtmpf", [128, T])
            for d in range(2):
                p.dma(tmpf[0:64, :], g.ZF[RW0 + 1536 + d * 64:RW0 + 1536 + (d + 1) * 64, :])
                p.act(tzw[d], tmpf[0:64, :], AF.Tanh)
                p.dma(tmpf[0:64, :], g.ZF[RW0 + 1664 + d * 64:RW0 + 1664 + (d + 1) * 64, :])
                p.cp(zab[d], tmpf[0:64, :], eng="act")
            p.dma(tmpf, g.ZF[RW0 + 1792:RW0 + 1920, :])
            p.act(sgz, tmpf, AF.Sigmoid)
            p.barrier()
        wupb = p.sb(st, "wupb", [64, 2, 512], BF16)
        aupb = p.sb(st, "aupb", [64, 2, 512], BF16)
        gupb = p.sb(st, "gupb", [128, 512], BF16)
        p.dma(wupb, g.w_up[l].re("d r n -> r d n"), q="pool")
        p.dma(aupb, g.a_up[l].re("d r n -> r d n"), q="pool")
        p.dma(gupb, g.g_up[l], q="pool")
        rts = [p.sb(st, "d_r%d" % i, [128, TH]) for i in range(2)]
        kts = [p.sb(st, "d_k%d" % i, [128, TH]) for i in range(2)]
        vts = [p.sb(st, "d_v%d" % i, [128, TH]) for i in range(2)]
        kks = [p.sb(st, "d_kk%d" % i, [128, TH]) for i in range(2)]
        Xss = [[p.sb(st, "d_x%d_%d" % (i, q), [128, TH]) for i in range(8)] for q in range(2)]
        obs = [{n: p.sb(st, "d_ob%d_" % q + n, [128, TH], BF16) for n in ("kk", "r", "kh", "bh", "kp", "bp", "v")} for q in range(2)]
        tsts = [[p.sb(st, "d_tst%d" % i, [128, NCH2, 128], BF16) for i in range(3)]] * 2
        wcs = [p.sb(st, "d_wc%d" % q, [128, NCH2]) for q in range(2)]
        ps = [p.ps(st, "d_ps%d" % i, [128, 512]) for i in range(3)]
        pst = [p.ps(st, "d_pst%d" % i, [128, 16, 128], BF16) for i in range(2)]
        npst = 0
        nps = 0

        def pk(col):
            return g.P128T[:, l, col:col + 1]

        def tposed(src_b, dst_dram, q, k, tst):
            nonlocal npst
            pt = pst[npst % 2]
            npst += 1
            stg = tst[k]
            for c in range(NCH2):
                p.tr(pt[:, c, :], src_b[:, c * CH:(c + 1) * CH], g.identb)
            p.cp(stg, pt[:, 0:NCH2, :], eng=("act" if k % 2 else "dve"))
            p.dma(dst_dram, stg, q=q, acc=True)

        def body(pr, hf):
            nonlocal nps, npst
            r0 = pr * 128
            ob, tst, wc = obs[hf], tsts[hf], wcs[hf]
            tb = hf * TH
            cb = hf * NCH2
            rt, kt, vt, kk, X = rts[hf], kts[hf], vts[hf], kks[hf], Xss[hf]
            p.dma(rt, g.ZF[RW0 + r0:RW0 + r0 + 128, tb:tb + TH], q="act")
            yield
            p.dma(kt, g.ZF[RW0 + 512 + r0:RW0 + 512 + r0 + 128, tb:tb + TH], q="act")
            yield
            p.dma(vt, g.ZF[RW0 + 1024 + r0:RW0 + 1024 + r0 + 128, tb:tb + TH], q="act")
            yield
            p.ts(X[0], kt, pk(C2_KK + pr), ALU.mult, eng="pool")
            yield
            p.act(X[1], X[0], AF.Square)
            yield
            for (t0, tn) in THT:
                pp = ps[nps % 3]
                nps += 1
                p.mm(pp[:, :tn], g.ones2, X[1][:, t0:t0 + tn])
                yield
                p.ts(X[2][:, t0:t0 + tn], pp[:, :tn], 1e-12, ALU.add)
                yield
            p.act(X[2], X[2], AF.Sqrt)
            yield
            p.recip(X[2], X[2])
            yield
            p.tt(kk, X[0], X[2], ALU.mult)
            yield
            p.cp(ob["v"], vt, eng="pool")
            yield
            tposed(ob["v"], g.VTD[cb:cb + NCH2, :, r0:r0 + 128].re("c t n -> t c n"), "sp", 0, tst)
            yield
            for d in range(2):
                lw, a, kd, bb, L = X[0], X[1], X[2], X[3], X[4]
                for (t0, tn) in THT:
                    pp = ps[nps % 3]
                    nps += 1
                    p.mm(pp[:, :tn], wupb[:, d, r0:r0 + 128], tzw[d][:, tb + t0:tb + t0 + tn])
                    yield
                    p.act(lw[:, t0:t0 + tn], pp[:, :tn], AF.Sigmoid, bias=pk(C2_W0 + d * 4 + pr))
                    yield
                    pp = ps[nps % 3]
                    nps += 1
                    p.mm(pp[:, :tn], aupb[:, d, r0:r0 + 128], zab[d][:, tb + t0:tb + t0 + tn])
                    yield
                    p.act(a[:, t0:t0 + tn], pp[:, :tn], AF.Sigmoid, bias=pk(C2_A0 + d * 4 + pr))
                    yield
                p.ts(lw, lw, -0.6065306597126334, ALU.mult, eng="pool")
                yield
                p.ts(kd, a, pk(C2_KA + pr), ALU.mult, g.OMKA[:, l, pr:pr + 1], ALU.add)
                yield
                p.tt(kd, kd, kt, ALU.mult)
                yield
                p.tt(bb, kk, a, ALU.mult, eng="pool")
                yield
                if d == 0:
                    p.cp(X[7], kd, eng="pool")
                    yield
                else:
                    p.tt(X[7], X[7], kd, ALU.add, eng="pool")
                    yield
                p.op("dve", lambda e: e.tensor_tensor_scan(out=A(L), data0=A(g.RM), data1=A(lw), initial=0.0,
                                                           op0=ALU.mult, op1=ALU.add), [g.RM, lw], [L])
                if d == 1:
                    Lp = X[1]
                    p.tt(Lp, lw, L, ALU.subtract)
                    yield
                    p.tt(Lp.re("k (c t) -> k c t", t=CH), Lp.re("k (c t) -> k c t", t=CH),
                         L.re("k (c t) -> k c t", t=CH)[:, :, CH - 1:CH].bc([128, NCH2, CH]), ALU.add)
                    end = 0
                else:
                    Lp = L
                    end = CH - 1
                E = X[5]
                p.act(E, Lp, AF.Exp)
                yield
                p.tt(ob["r"], rt, E, ALU.mult)
                yield
                p.act(wc.re("k (c o) -> k c o", o=1), Lp.re("k (c t) -> k c t", t=CH)[:, :, end:end + 1], AF.Exp)
                yield
                E2 = X[6]
                p.act(E2, Lp, AF.Exp, scale=-1.0)
                yield
                p.tt(kd, kd, E2, ALU.mult)
                yield
                p.tt(bb, bb, E2, ALU.mult, eng="pool")
                yield
                p.tt(lw, Lp, lw, ALU.subtract, eng="pool")
                yield
                p.act(lw, lw, AF.Exp)
                yield
                p.tt(ob["kk"], kk, lw, ALU.mult)
                yield
                p.cp(ob["kh"], kd, eng="pool")
                yield
                p.cp(ob["bh"], bb, eng="act")
                yield
                wcb = wc.re("k (c o) -> k c o", o=1).bc([128, NCH2, CH])
                p.tt(ob["kp"].re("k (c t) -> k c t", t=CH), kd.re("k (c t) -> k c t", t=CH), wcb, ALU.mult)
                yield
                p.tt(ob["bp"].re("k (c t) -> k c t", t=CH), bb.re("k (c t) -> k c t", t=CH), wcb, ALU.mult, eng="pool")
                yield
                for qi, n in enumerate(("kk", "r", "kh", "bh")):
                    p.dma(g.SCF[d, r0:r0 + 128, cb:cb + NCH2, qi, :], ob[n].re("k (c t) -> k c t", t=CH), q="sp", acc=True)
                    yield
                tposed(ob["kp"], g.SCT[d, cb:cb + NCH2, :, 0, r0:r0 + 128].re("c t n -> t c n"), "sp", 1, tst)
                yield
                tposed(ob["bp"], g.SCT[d, cb:cb + NCH2, :, 1, r0:r0 + 128].re("c t n -> t c n"), "sp", 2, tst)
                yield
                p.dma(g.WCD[r0:r0 + 128, d, cb:cb + NCH2], wc, q="sp", acc=True)
                yield
            p.ts(X[0], rt, pk(C2_RK + pr), ALU.mult, eng="pool")
            yield
            p.tt(X[0], X[0], X[7], ALU.mult)
            yield
            for (t0, tn) in THT:
                pp = ps[nps % 3]
                nps += 1
                p.mm(pp[:, :tn], g.ones2, X[0][:, t0:t0 + tn])
                yield
                p.tt(X[1][:, t0:t0 + tn], pp[:, :tn], vt[:, t0:t0 + tn], ALU.mult)
                yield
                pp = ps[nps % 3]
                nps += 1
                p.mm(pp[:, :tn], gupb[:, r0:r0 + 128], sgz[:, tb + t0:tb + t0 + tn])
                yield
                p.cp(X[2][:, t0:t0 + tn], pp[:, :tn], eng="act")
                yield
            p.dma(g.RO[0, r0:r0 + 128, tb:tb + TH], X[1], q="sp", acc=True)
            yield
            p.dma(g.RO[1, r0:r0 + 128, tb:tb + TH], X[2], q="sp", acc=True)
            yield

        for pr in range(4):
            gens = [body(pr, 0), body(pr, 1)]
            live = [True, True]
            while any(live):
                for q in range(2):
                    if live[q]:
                        try:
                            next(gens[q])
                        except StopIteration:
                            live[q] = False
    p.barrier()


ORDER_B = [1, 0] + list(range(NCH - 1, 1, -1))


def stage_scan(p, g, l, b):
    with contextlib.ExitStack() as st:
        B = [p.ps(st, "e_b%d" % i, [128, 512]) for i in range(8)]

        def bv(i, n, w, parts=128):
            return B[i].re("s (a t) -> s a t", t=w)[0:parts, 0:n, :]
        WC = p.sb(st, "e_wc", [64, 2, 8, NCH])
        p.dma(WC, g.WCD.re("(h k) d c -> k d h c", k=64))
        ST = p.sb(st, "e_st", [64, 16, 64])
        STb = p.sb(st, "e_stb", [64, 16, 64], BF16)
        p.memset(ST, 0.0)
        p.memset(STb, 0.0, eng="pool")
        FQ = [p.sb(st, "e_fq%d" % i, [64, 2, 8, 4, 128], BF16) for i in range(2)]
        TQ = [p.sb(st, "e_tq%d" % i, [128, 2, 2, 8, 64], BF16) for i in range(2)]
        VT = [p.sb(st, "e_vt%d" % i, [128, 2, 8, 64], BF16) for i in range(2)]
        AMs = [p.sb(st, "e_am%d" % i, [128, 16, 4, 128], BF16) for i in range(2)]
        Xs = [[p.sb(st, "e_x%d_%d" % (i, q), [128, 4, 128]) for q in range(4)] for i in range(2)]
        XTs = [[p.sb(st, "e_xt%d_%d" % (i, q), [128, 4, 128]) for q in range(4)] for i in range(2)]
        Ps = [[p.sb(st, "e_p%d_%d" % (i, q), [128, 4, 128]) for q in range(4)] for i in range(2)]
        RT = p.sb(st, "e_rt", [128, 16, 64])
        PF = [[p.sb(st, "e_pf%d_%d" % (i, q), [128, 4, 128]) for q in range(4)] for i in range(2)]
        nUT = p.sb(st, "e_nut", [128, 16, 64], BF16)
        YS = [p.sb(st, "e_ys%d" % i, [64, 16, 128]) for i in range(2)]
        idb = g.ident.re("s (o t) -> s o t", o=1).bc([128, 4, 128])
        def loads(j):
            cd = (j, ORDER_B[j])
            fq, tq, vt = FQ[j % 2], TQ[j % 2], VT[j % 2]
            for d in range(2):
                c = cd[d]
                p.dma(fq[:, d], g.SCF[d, :, c].re("(h k) q t -> k h q t", k=64), q="sp")
                p.dma(tq[:, d], g.SCT[d, c].re("t q (h k) -> t q h k", k=64), q="act")
                p.dma(vt[:, d], g.VTD[c].re("t (h k) -> t h k", k=64), q="sp")

        def phase1(j):
            fq, AMb = FQ[j % 2], AMs[j % 2]
            for ci in range(16):
                d, h = divmod(ci, 8)
                pa = B[3 + ci % 2]
                rhs = fq[:, d, h, 0:2, :]
                p.mm(pa.re("s (q t) -> s q t", t=128)[:, 0:2, :], fq[:, d, h, 2, :], rhs)
                p.mm(pa.re("s (q t) -> s q t", t=128)[:, 2:4, :], fq[:, d, h, 3, :], rhs)
                pn = bv(5, 4, 128)
                p.mm(pn[:, ci % 4, :], fq[:, d, h, 0, :], fq[:, d, h, 3, :])
                p.tt(AMb[:, ci], pa.re("s (q t) -> s q t", t=128), g.MASK4[:, d], ALU.mult)
                p.tt(Xs[0][ci // 4][:, ci % 4, :], pa[:, 256:384], g.MASK4[:, d, 0, :], ALU.mult)
                if ci % 4 == 3:
                    p.tt(XTs[0][ci // 4], pn, g.MASK4[:, 1 - d, 0:1, :].bc([128, 4, 128]), ALU.mult)

        def phase2(j):
            for gq in range(4):
                p.tt(Ps[0][gq], idb, Xs[0][gq], ALU.subtract, eng="pool")
            cur = 0
            for lev in range(6):
                last = lev == 5
                if last:
                    for gq in range(4):
                        X, XT = Xs[cur][gq], XTs[cur][gq]
                        bs = 3 * (gq % 2)
                        for q in range(4):
                            p.mm(bv(bs + 1, 4, 128)[:, q, :], X[:, q, :], XT[:, q, :])
                        p.cp(XTs[1 - cur][gq], bv(bs + 1, 4, 128), eng="dve")
                else:
                    for gq in range(4):
                        X, XT = Xs[cur][gq], XTs[cur][gq]
                        bs = 3 * (gq % 2)
                        for q in range(4):
                            p.mm(bv(bs, 4, 128)[:, q, :], XT[:, q, :], X[:, q, :])
                        p.cp(Xs[1 - cur][gq], bv(bs, 4, 128), eng="act")
                    for gq in range(4):
                        bs = 3 * (gq % 2)
                        for q in range(4):
                            p.tr(bv(bs + 1, 4, 128)[:, q, :], Xs[1 - cur][gq][:, q, :], g.ident)
                        p.cp(XTs[1 - cur][gq], bv(bs + 1, 4, 128), eng="dve")
                for gq in range(4):
                    bs = 3 * (gq % 2)
                    for q in range(4):
                        p.mm(bv(bs + 2, 4, 128)[:, q, :], XTs[1 - cur][gq][:, q, :], Ps[cur][gq][:, q, :])
                    dstp = PF[j % 2][gq] if last else Ps[1 - cur][gq]
                    p.tt(dstp, bv(bs + 2, 4, 128), Ps[cur][gq], ALU.add)
                cur = 1 - cur
                yield lev

        def phase3(j):
            cd = (j, ORDER_B[j])
            fq, tq, vt, ys, AMb, Pf = FQ[j % 2], TQ[j % 2], VT[j % 2], YS[j % 2], AMs[j % 2], PF[j % 2]
            for ci in range(16):
                d, h = divmod(ci, 8)
                pr = bv(6 + ci // 8, 8, 64)[:, ci % 8, :]
                p.mm(pr, fq[:, d, h, 0, :], STb[:, ci, :], start=True, stop=False)
                p.mm(pr, AMb[:, ci, 0, :], vt[:, d, h, :], start=False, stop=True)
            p.cp(RT[:, 0:8, :], bv(6, 8, 64), eng="act")
            p.cp(RT[:, 8:16, :], bv(7, 8, 64), eng="dve")
            yield 0
            for ci in range(16):
                pr = bv(6 + ci // 8, 8, 64)[:, ci % 8, :]
                p.mm(pr, Pf[ci // 4][:, ci % 4, :], RT[:, ci, :])
            p.ts(nUT[:, 0:8, :], bv(6, 8, 64), -1.0, ALU.mult)
            p.op("act", lambda e: e.mul(out=A(nUT[:, 8:16, :]), in_=A(bv(7, 8, 64)), mul=-1.0), [B[7]], [nUT])
            yield 1
            for q4 in range(4):
                for q in range(4):
                    ci = 4 * q4 + q
                    d, h = divmod(ci, 8)
                    pv = bv(6 + q4 % 2, 4, 128, 64)[:, q, :]
                    p.mm(pv, STb[:, ci, :], fq[:, d, h, 1, :], start=True, stop=False)
                    p.mm(pv, vt[:, d, h, :], AMb[:, ci, 1, :], start=False, stop=False)
                    p.mm(pv, nUT[:, ci, :], AMb[:, ci, 3, :], start=False, stop=True)
                p.cp(ys[:, 4 * q4:4 * q4 + 4, :], bv(6 + q4 % 2, 4, 128, 64), eng=("act" if q4 % 2 else "dve"))
            for d in range(2):
                c = cd[d]
                p.dma(g.YD[d, :, c * CH:(c + 1) * CH].re("(h k) t -> k h t", k=64), ys[:, d * 8:(d + 1) * 8, :], q="sp", acc=True)
            yield 2
            for ci in range(16):
                d, h = divmod(ci, 8)
                pv = bv(6 + d, 8, 64, 64)[:, ci % 8, :]
                p.mm(pv, tq[:, d, 0, h, :], vt[:, d, h, :], start=True, stop=False)
                p.mm(pv, tq[:, d, 1, h, :], nUT[:, ci, :], start=False, stop=True)
            for d in range(2):
                wcv = WC[:, d, :, cd[d]:cd[d] + 1].bc([64, 8, 64])
                p.tt(ST[:, d * 8:(d + 1) * 8, :], ST[:, d * 8:(d + 1) * 8, :], wcv, ALU.mult, eng="pool")
                p.tt(ST[:, d * 8:(d + 1) * 8, :], ST[:, d * 8:(d + 1) * 8, :], bv(6 + d, 8, 64, 64), ALU.add)
            p.cp(STb, ST, eng="act")
            yield 3

        loads(0)
        phase1(0)
        for _ in phase2(0):
            pass
        for j in range(NCH):
            g3 = phase3(j)
            if j + 1 < NCH:
                loads(j + 1)
                phase1(j + 1)
                for lev in phase2(j + 1):
                    if lev < 4:
                        next(g3)
            for _ in g3:
                pass
    p.barrier()


def stage_mix(p, g, l, b, CO, src, last_dst):
    with contextlib.ExitStack() as st:
        wa = p.sb(st, "m_wa", [128, 4, 1024], BF16)
        wb = p.sb(st, "m_wb", [64, 8, 1024], BF16)
        wo = p.sb(st, "m_wo", [128, 8, 1024], BF16)
        G1b = []
        for kind, row in ((0, 2), (1, b)):
            t = p.sb(st, "m_g1b%d" % kind, [128, D])
            p.dma(t, g.MODR[l, row, G1:G1 + D].pb(128), q="act")
            G1b.append(t)
        p.dma(wa, g.w_a_out[l].re("(c q) n -> q c n", q=128), q="pool")
        p.dma(wb, g.w_b_out[l].re("(h k) n -> k h n", k=64), q="pool")
        p.dma(wo, g.w_o[l].re("(c q) n -> q c n", q=128), q="pool")
        y0 = p.sb(st, "m_y0", [64, 8, 512])
        y1 = p.sb(st, "m_y1", [64, 8, 512])
        aux = p.sb(st, "m_aux", [64, 8, 512])
        ybin = p.sb(st, "m_ybin", [64, 8, 512], BF16)
        sgat = [p.sb(st, "m_sga%d" % i, [128, 512]) for i in range(2)]
        sgbt = [p.sb(st, "m_sgb%d" % i, [128, 512]) for i in range(2)]
        m1 = [p.sb(st, "m_m1%d" % i, [128, 512]) for i in range(1)] * 2
        m2 = [p.sb(st, "m_m2%d" % i, [128, 512]) for i in range(1)] * 2
        mrg = p.sb(st, "m_mrg", [128, 8, 512], BF16)
        xt = [p.sb(st, "m_xt%d" % i, [128, D]) for i in range(2)]
        xo = [p.sb(st, "m_xo%d" % i, [128, D]) for i in range(2)]
        B = [p.ps(st, "m_b%d" % i, [128, 512]) for i in range(8)]
        nb = 0
        nx = 0
        for (t0, tn) in TT:
            for d, yt in ((0, y0), (1, y1)):
                p.dma(yt[:, :, :tn], g.YD[d, :, t0:t0 + tn].re("(h k) t -> k h t", k=64), q=("sp" if d == 0 else "act"))
            p.tt(y0[:, :, :tn], y0[:, :, :tn], y1[:, :, :tn], ALU.add, eng="pool")
            for h in range(8):
                pm = B[nb % 8]
                nb += 1
                p.mm(pm[0:64, :tn], g.ones64, y0[:, h, :tn])
                p.stt(y1[:, h, :tn], pm[0:64, :tn], -1.0 / 64, y0[:, h, :tn], ALU.mult, ALU.add)
            p.act(y0[:, :, :tn], y1[:, :, :tn], AF.Square)
            for h in range(8):
                pm = B[nb % 8]
                nb += 1
                p.mm(pm[0:64, :tn], g.ones64, y0[:, h, :tn])
                p.ts(y0[:, h, :tn], pm[0:64, :tn], 1.0 / 64, ALU.mult, GN_EPS, ALU.add)
            p.act(y0[:, :, :tn], y0[:, :, :tn], AF.Sqrt)
            p.recip(y0[:, :, :tn], y0[:, :, :tn])
            p.tt(y1[:, :, :tn], y1[:, :, :tn], y0[:, :, :tn], ALU.mult, eng="pool")
            for h in range(8):
                p.ts(y1[:, h, :tn], y1[:, h, :tn], g.P64T[:, l, C_LG + h:C_LG + h + 1], ALU.mult,
                     g.P64T[:, l, C_LB + h:C_LB + h + 1], ALU.add)
            p.dma(aux[:, :, :tn], g.RO[0, :, t0:t0 + tn].re("(h k) t -> k h t", k=64), q="sp")
            p.tt(y1[:, :, :tn], y1[:, :, :tn], aux[:, :, :tn], ALU.add, eng="pool")
            p.dma(aux[:, :, :tn], g.RO[1, :, t0:t0 + tn].re("(h k) t -> k h t", k=64), q="sp")
            p.tt(ybin[:, :, :tn], y1[:, :, :tn], aux[:, :, :tn], ALU.mult)
            for cc in range(8):
                sga, sgb = sgat[cc % 2], sgbt[cc % 2]
                p.dma(sga[:, :tn], g.ZF[3456 + cc * 128:3456 + (cc + 1) * 128, t0:t0 + tn], q="sp")
                p.dma(sgb[:, :tn], g.ZF[4480 + cc * 128:4480 + (cc + 1) * 128, t0:t0 + tn], q="act")
                pa = B[nb % 8]
                nb += 1
                for jj in range(4):
                    p.mm(pa[:, :tn], wa[:, jj, cc * 128:(cc + 1) * 128], CO[:, jj, t0:t0 + tn], start=(jj == 0), stop=(jj == 3))
                pb = B[nb % 8]
                nb += 1
                for h in range(8):
                    p.mm(pb[:, :tn], wb[:, h, cc * 128:(cc + 1) * 128], ybin[:, h, :tn], start=(h == 0), stop=(h == 7))
                p.tt(m1[cc % 2][:, :tn], pa[:, :tn], sga[:, :tn], ALU.mult)
                p.tt(m2[cc % 2][:, :tn], pb[:, :tn], sgb[:, :tn], ALU.mult)
                p.tt(mrg[:, cc, :tn], m1[cc % 2][:, :tn], m2[cc % 2][:, :tn], ALU.add, eng="pool")
            for sub in range(tn // 128):
                tok = t0 + sub * 128
                kind = 0 if tok < TCTX else 1
                x, o = xt[nx % 2], xo[nx % 2]
                nx += 1
                p.dma(x, src[tok:tok + 128, :], q="sp")
                for hc in range(2):
                    po = B[nb % 8]
                    nb += 1
                    for cc in range(8):
                        p.mm(po, mrg[:, cc, sub * 128:(sub + 1) * 128], wo[:, cc, hc * 512:(hc + 1) * 512],
                             start=(cc == 0), stop=(cc == 7))
                    p.tt(o[:, hc * 512:(hc + 1) * 512], po, G1b[kind][:, hc * 512:(hc + 1) * 512], ALU.mult)
                p.tt(o, o, x, ALU.add, eng="pool")
                p.dma(last_dst[tok:tok + 128, :], o, q="pool", acc=True)
    p.barrier()


def stage_router(p, g, l, hT2, GW, DEST):
    with contextlib.ExitStack() as st:
        rwf = p.sb(st, "r_wf", [128, 8, 36])
        rwb = p.sb(st, "r_wb", [128, 8, 36], BF16)
        p.dma(rwf[:, :, 0:4], g.router_g[l].re("(c q) n -> q c n", q=128))
        p.dma(rwf[:, :, 4:36], g.router_e[l].re("(c q) n -> q c n", q=128))
        p.cp(rwb, rwf)
        RB = p.sb(st, "r_rb", [128, 36])
        p.dma(RB[:, 0:4], g.router_g_b[l].pb(128))
        p.dma(RB[:, 4:36], g.router_e_b[l].pb(128))
        LG = p.sb(st, "r_lg", [128, NT, 36])
        pl = [p.ps(st, "r_pl%d" % i, [128, 36]) for i in range(2)]
        for i in range(NT):
            pp = pl[i % 2]
            for c in range(8):
                p.mm(pp, hT2[:, c, i * 128:(i + 1) * 128], rwb[:, c, :], start=(c == 0), stop=(c == 7))
            p.cp(LG[:, i, :], pp, eng=("act" if i % 2 else "dve"))
        p.tt(LG, LG, RB.re("q (o e) -> q o e", o=1).bc([128, NT, 36]), ALU.add)
        lg = LG[:, :, 0:4]
        le = LG[:, :, 4:36].re("q i (g e) -> q i g e", e=8)
        mg = p.sb(st, "r_mg", [128, NT])
        oh = p.sb(st, "r_oh", [128, NT, 4])
        eg = p.sb(st, "r_eg", [128, NT, 4])
        pg = p.sb(st, "r_pg", [128, NT])
        tmp = p.sb(st, "r_tmp", [128, NT, 4, 8])
        les = p.sb(st, "r_les", [128, NT, 8])
        les2 = p.sb(st, "r_les2", [128, NT, 8])
        m1 = p.sb(st, "r_m1", [128, NT])
        m2 = p.sb(st, "r_m2", [128, NT])
        k1 = p.sb(st, "r_k1", [128, NT, 8])
        k2 = p.sb(st, "r_k2", [128, NT, 8])
        ex = p.sb(st, "r_ex", [128, NT, 8])

        def b3(t, n):
            return t.re("q (i o) -> q i o", o=1).bc([128, NT, n])
        p.red(mg, lg, ALU.max)
        p.tt(oh, lg, b3(mg, 4), ALU.is_equal)
        p.tt(eg, lg, b3(mg, 4), ALU.subtract)
        p.act(eg, eg, AF.Exp)
        p.red(pg, eg, ALU.add)
        p.recip(pg, pg)
        p.tt(tmp, le, oh.re("q i (g o) -> q i g o", o=1).bc([128, NT, 4, 8]), ALU.mult)
        p.red(les, tmp.re("q i g e -> q i e g"), ALU.add)
        p.red(m1, les, ALU.max)
        p.tt(k1, les, b3(m1, 8), ALU.is_equal)
        p.stt(les2, k1, -1e30, les, ALU.mult, ALU.add)
        p.red(m2, les2, ALU.max)
        p.tt(k2, les2, b3(m2, 8), ALU.is_equal)
        p.tt(m2, m2, m1, ALU.subtract)
        p.act(m2, m2, AF.Exp)
        p.ts(m2, m2, 1.0, ALU.add)
        p.recip(m2, m2)
        p.tt(GW[:, :, 0], m2, pg, ALU.mult)
        p.tt(GW[:, :, 1], pg, GW[:, :, 0], ALU.subtract)
        ohb = oh.re("q i (g o) -> q i g o", o=1).bc([128, NT, 4, 8])
        M1 = p.sb(st, "r_M1", [128, NT, 4, 8])
        M2 = p.sb(st, "r_M2", [128, NT, 4, 8])
        MM = p.sb(st, "r_MM", [128, NT, 4, 8])
        p.tt(M1, ohb, k1.re("q i (o e) -> q i o e", o=1).bc([128, NT, 4, 8]), ALU.mult)
        p.tt(M2, ohb, k2.re("q i (o e) -> q i o e", o=1).bc([128, NT, 4, 8]), ALU.mult)
        p.tt(MM, M1, M2, ALU.add)
        MMf = MM.re("q i g e -> q (i g e)")
        WI = p.sb(st, "r_WI", [128, NT, 32])
        TOT = p.sb(st, "r_TOT", [128, NT, 32])
        pw = [p.ps(st, "r_pw%d" % i, [128, 512]) for i in range(4)]
        NF = NT * 32
        for (c0, cn, k) in ((0, 512, 0), (512, NF - 512, 1)):
            p.mm(pw[k][:, :cn], g.MASK4[:, 0, 0, :], MMf[:, c0:c0 + cn])
            p.cp(WI.re("q i e -> q (i e)")[:, c0:c0 + cn], pw[k][:, :cn], eng="act")
            p.mm(pw[2 + k][:, :cn], g.onesf, MMf[:, c0:c0 + cn])
            p.cp(TOT.re("q i e -> q (i e)")[:, c0:c0 + cn], pw[2 + k][:, :cn], eng="dve")
        BASE = p.sb(st, "r_BASE", [128, NT, 32])
        p.memset(BASE[:, 0, :], 0.0)
        for i in range(1, NT):
            p.tt(BASE[:, i, :], BASE[:, i - 1, :], TOT[:, i - 1, :], ALU.add)
        p.tt(WI, WI, BASE, ALU.add)
        p.ts(TOT, WI, CAP - 0.5, ALU.is_ge)
        p.tt(WI, WI, g.EOFF.re("q (o e) -> q o e", o=1).bc([128, NT, 32]), ALU.add)
        p.ts(BASE, TOT, -1.0, ALU.mult, 1.0, ALU.add)
        p.tt(WI, WI, BASE, ALU.mult)
        p.stt(WI, TOT, float(NEXP * CAP), WI, ALU.mult, ALU.add)
        DF = p.sb(st, "r_DF", [128, NT, 2])
        for k, Mk in ((0, M1), (1, M2)):
            p.tt(Mk.re("q i g e -> q i (g e)"), Mk.re("q i g e -> q i (g e)"), WI, ALU.mult)
            p.red(DF[:, :, k], Mk.re("q i g e -> q i (g e)"), ALU.add)
        p.cp(DEST, DF)
    p.barrier()


def stage_moe(p, g, l, b, GW, DEST):
    NR = NEXP * CAP
    IOA = bass.IndirectOffsetOnAxis
    with contextlib.ExitStack() as st:
        G2b = []
        for kind, row in ((0, 2), (1, b)):
            t = p.sb(st, "e_g2b%d" % kind, [128, D])
            p.dma(t, g.MODR[l, row, G2:G2 + D].pb(128), q="act")
            G2b.append(t)
        with contextlib.ExitStack() as st2:
            hbt = [p.sb(st2, "e_hbt%d" % i, [128, D], BF16) for i in range(2)]
            for i in range(NT):
                hb = hbt[i % 2]
                p.dma(hb, g.H2T[i * 128:(i + 1) * 128, :], q="sp")
                for k in range(2):
                    idx = DEST[:, i, k:k + 1]
                    p.dma(g.XE, hb, q="pool", acc=True, extra_reads=[DEST],
                          fn=lambda e, hb=hb, idx=idx: e.indirect_dma_start(
                              out=A(g.XE), out_offset=IOA(ap=A(idx), axis=0), in_=A(hb), in_offset=None))
            w1b = [p.sb(st2, "e_w1b%d" % i, [128, 8, 512], BF16) for i in range(2)]
            w3b = [p.sb(st2, "e_w3b%d" % i, [128, 8, 512], BF16) for i in range(2)]
            w2b = [p.sb(st2, "e_w2b%d" % i, [128, 4, 1024], BF16) for i in range(2)]
            xe = [p.sb(st2, "e_xe%d" % i, [128, NSL, D], BF16) for i in range(2)]
            hTe = p.sb(st2, "e_hTe", [128, 8, CAP], BF16)
            sl = [p.sb(st2, "e_sl%d" % i, [128, 512]) for i in range(2)]
            hid = p.sb(st2, "e_hid", [128, 4, CAP], BF16)
            ye = [p.sb(st2, "e_ye%d" % i, [128, NSL, D]) for i in range(2)]
            B = [p.ps(st2, "e_pb%d" % i, [128, 512]) for i in range(6)]
            pt = [p.ps(st2, "e_pt%d" % i, [128, 8, 128], BF16) for i in range(2)]
            nb = 0
            ns = 0
            nsl = 0
            npt = 0
            CT = [(0, 512), (512, CAP - 512)] if CAP > 512 else [(0, CAP)]
            def load_w(e):
                k = e % 2
                p.dma(w1b[k], g.exp_w1[l, e].re("(c q) n -> q c n", q=128), q="pool")
                p.dma(w3b[k], g.exp_w3[l, e].re("(c q) n -> q c n", q=128), q="pool")
                p.dma(w2b[k], g.exp_w2[l, e].re("(c q) n -> q c n", q=128), q="pool")
                p.dma(xe[k], g.XE[e * CAP:(e + 1) * CAP, :].re("(j q) n -> q j n", q=128), q="sp")
            load_w(0)
            for e in range(NEXP):
                k = e % 2
                if e + 1 < NEXP:
                    load_w(e + 1)
                x_ = xe[k]
                for j in range(NSL):
                    ptt = pt[npt % 2]
                    npt += 1
                    for c in range(8):
                        p.tr(ptt[:, c, :], x_[:, j, c * 128:(c + 1) * 128], g.identb)
                    p.cp(hTe[:, :, j * 128:(j + 1) * 128], ptt, eng=("act" if j % 2 else "dve"))
                for ff in range(4):
                    for (t0, tn) in CT:
                        p1 = B[nb % 6]
                        p3 = B[(nb + 1) % 6]
                        nb += 2
                        for kc in range(8):
                            p.mm(p1[:, :tn], w1b[k][:, kc, ff * 128:(ff + 1) * 128], hTe[:, kc, t0:t0 + tn],
                                 start=(kc == 0), stop=(kc == 7))
                        for kc in range(8):
                            p.mm(p3[:, :tn], w3b[k][:, kc, ff * 128:(ff + 1) * 128], hTe[:, kc, t0:t0 + tn],
                                 start=(kc == 0), stop=(kc == 7))
                        s_ = sl[nsl % 2]
                        nsl += 1
                        p.act(s_[:, :tn], p1[:, :tn], AF.Silu)
                        p.tt(hid[:, ff, t0:t0 + tn], s_[:, :tn], p3[:, :tn], ALU.mult)
                y_ = ye[k]
                for j in range(NSL):
                    for hc in range(2):
                        po = B[nb % 6]
                        nb += 1
                        for ff in range(4):
                            p.mm(po, hid[:, ff, j * 128:(j + 1) * 128], w2b[k][:, ff, hc * 512:(hc + 1) * 512],
                                 start=(ff == 0), stop=(ff == 3))
                        p.cp(y_[:, j, hc * 512:(hc + 1) * 512], po, eng=("act" if (j + hc) % 2 else "dve"))
                p.dma(g.YE[e * CAP:(e + 1) * CAP, :].re("(j q) n -> q j n", q=128), y_, q="sp", acc=True)
            p.barrier()
        ya = [p.sb(st, "e_ya%d" % i, [128, D]) for i in range(2)]
        yb = [p.sb(st, "e_yb%d" % i, [128, D]) for i in range(2)]
        xt = [p.sb(st, "e_xt%d" % i, [128, D]) for i in range(2)]
        for i in range(NT):
            tok = i * 128
            kind = 0 if i < 2 else 1
            a_, b_, x_ = ya[i % 2], yb[i % 2], xt[i % 2]
            p.dma(x_, g.XS[b][tok:tok + 128, :], q="sp")
            for k, dst in ((0, a_), (1, b_)):
                p.memset(dst, 0.0, eng="pool")
                idx = DEST[:, i, k:k + 1]
                p.dma(dst, g.YE, q="pool", extra_reads=[DEST],
                      fn=lambda e, dst=dst, idx=idx: e.indirect_dma_start(
                          out=A(dst), out_offset=None, in_=A(g.YE), in_offset=IOA(ap=A(idx), axis=0)))
            p.ts(a_, a_, GW[:, i, 0:1], ALU.mult)
            p.stt(a_, b_, GW[:, i, 1:2], a_, ALU.mult, ALU.add)
            p.tt(a_, a_, G2b[kind], ALU.mult)
            p.tt(a_, a_, x_, ALU.add)
            p.dma(g.XS[b][tok:tok + 128, :], a_, q="act", acc=True)
    p.barrier()


def stage_final(p, g, b):
    with contextlib.ExitStack() as st:
        gt = p.sb(st, "f_gt", [128, D])
        p.dma(gt, g.final_g.pb(128))
        xts = [p.sb(st, "f_xt%d" % i, [128, D]) for i in range(2)]
        sqs = [p.sb(st, "f_sq%d" % i, [128, D]) for i in range(2)]
        sss = [p.sb(st, "f_ss%d" % i, [128, 2]) for i in range(2)]
        for i in range(TLAT // 128):
            xt, sq, ss = xts[i % 2], sqs[i % 2], sss[i % 2]
            p.dma(xt, g.XS[b][TCTX + i * 128:TCTX + (i + 1) * 128, :], q=("sp" if i % 2 == 0 else "act"))
            p.act(sq, xt, AF.Square)
            p.red(ss[:, 0:1], sq, ALU.add)
            p.ts(ss[:, 1:2], ss[:, 0:1], 1.0 / D, ALU.mult, EPS, ALU.add)
            p.act(ss[:, 1:2], ss[:, 1:2], AF.Sqrt)
            p.recip(ss[:, 1:2], ss[:, 1:2])
            p.stt(sq, xt, ss[:, 1:2], gt, ALU.mult, ALU.mult)
            p.dma(g.out[b, i * 128:(i + 1) * 128, :], sq, q="pool", acc=True)
    p.barrier()
```

```python
import contextlib
import numpy as np
import concourse.bass as bass
import concourse.mybir as mybir
from concourse.bass_utils import run_bass_kernel_spmd

F32 = mybir.dt.float32
BF16 = mybir.dt.bfloat16
I32 = mybir.dt.int32
AF = mybir.ActivationFunctionType
ALU = mybir.AluOpType
AX = mybir.AxisListType

D = 1024
DEPTH = 4
NB = 2
TCTX = 256
TLAT = 2048
T = TCTX + TLAT
NT = T // 128
NIN = 5504
RW0 = 1536
NEXP = 32
DFF = 512
CAP = 640
NSL = CAP // 128
CH = 128
NCH = T // CH
EPS = 1e-6
GN_EPS = 64e-5
TT = [(0, 512), (512, 512), (1024, 512), (1536, 512), (2048, 256)]


class Tl:
    def __init__(self, ap, name=""):
        self.ap = ap
        self.name = name
        self.lw = {}
        self.rd = {}

    def __getitem__(self, k):
        return Vw(self, self.ap[k])

    def re(self, s, **kw):
        return Vw(self, self.ap.rearrange(s, **kw))

    def bc(self, shape):
        return Vw(self, self.ap.to_broadcast(list(shape)))

    def pb(self, n):
        return Vw(self, self.ap.partition_broadcast(n))


class Vw:
    def __init__(self, t, ap):
        self.t = t
        self.ap = ap

    def __getitem__(self, k):
        return Vw(self.t, self.ap[k])

    def re(self, s, **kw):
        return Vw(self.t, self.ap.rearrange(s, **kw))

    def bc(self, shape):
        return Vw(self.t, self.ap.to_broadcast(list(shape)))

    def pb(self, n):
        return Vw(self.t, self.ap.partition_broadcast(n))


def A(x):
    return x.ap if isinstance(x, (Tl, Vw)) else x


def TT_(x):
    if isinstance(x, Tl):
        return x
    if isinstance(x, Vw):
        return x.t
    return None


class P:
    KD = 8

    def __init__(self, nc):
        self.nc = nc
        self.es = contextlib.ExitStack()
        self.eng = {"pe": nc.tensor, "act": nc.scalar, "dve": nc.vector, "pool": nc.gpsimd, "sp": nc.sync}
        self.sem = {}
        self.cur = {}
        for e in ("pe", "act", "dve", "pool"):
            self.sem[e] = self.es.enter_context(nc.semaphore("s_" + e))
            self.cur[e] = 0
        self.dcount = {}
        for q in ("sp", "pool", "act"):
            self.dcount[q] = 0
            for s in range(self.KD):
                k = ("d", q, s)
                self.sem[k] = self.es.enter_context(nc.semaphore("d_%s_%d" % (q, s)))
                self.cur[k] = 0
        self.waited = {e: {} for e in self.eng}
        self.nins = 0

    def sb(self, stack, name, shape, dt=F32):
        self.uid = getattr(self, "uid", 0) + 1
        name = "%s_%d" % (name, self.uid)
        h = stack.enter_context(self.nc.sbuf_tensor(name, list(shape), dt))
        return Tl(h[:], name)

    def ps(self, stack, name, shape, dt=F32):
        self.uid = getattr(self, "uid", 0) + 1
        name = "%s_%d" % (name, self.uid)
        h = stack.enter_context(self.nc.psum_tensor(name, list(shape), dt))
        return Tl(h[:], name)

    def dram(self, name, shape, dt=F32, kind="Internal"):
        h = self.nc.dram_tensor(name, list(shape), dt, kind=kind)
        return Tl(h.ap(), name)

    def _deps(self, eng, reads, writes, acc=False):
        need = {}

        def add(tok):
            if tok is not None:
                k, v = tok
                if need.get(k, 0) < v:
                    need[k] = v
        for x in reads:
            t = TT_(x)
            if t is not None:
                for k, v in t.lw.items():
                    add((k, v))
        for x in writes:
            t = TT_(x)
            if t is not None:
                if not acc:
                    for k, v in t.lw.items():
                        add((k, v))
                for k, v in t.rd.items():
                    add((k, v))
        out = []
        w = self.waited[eng]
        for k, v in need.items():
            if k == eng and eng == "pe":
                continue
            if w.get(k, 0) >= v:
                continue
            w[k] = v
            out.append((k, v))
        return out

    def _commit(self, tok, reads, writes, acc=False):
        k, v = tok
        for x in reads:
            t = TT_(x)
            if t is not None and t.rd.get(k, 0) < v:
                t.rd[k] = v
        for x in writes:
            t = TT_(x)
            if t is not None:
                if acc:
                    if t.lw.get(k, 0) < v:
                        t.lw[k] = v
                else:
                    t.lw = {k: v}
                    t.rd = {}

    def op(self, eng, fn, reads, writes):
        e = self.eng[eng]
        for k, v in self._deps(eng, reads, writes):
            e.wait_ge(self.sem[k], v)
        ins = fn(e)
        self.cur[eng] += 1
        ins.then_inc(self.sem[eng], 1)
        self._commit((eng, self.cur[eng]), reads, writes)
        self.nins += 1

    def dma(self, out, in_, q="sp", acc=False, fn=None, extra_reads=(), **kw):
        e = self.eng[q]
        j = self.dcount[q]
        self.dcount[q] = j + 1
        slot, gen = j % self.KD, j // self.KD
        key = ("d", q, slot)
        rds = [in_] + list(extra_reads)
        deps = self._deps(q, rds, [out], acc=acc)
        if gen > 0 and self.waited[q].get(key, 0) < 16 * gen:
            self.waited[q][key] = 16 * gen
            deps.append((key, 16 * gen))
        for k, v in deps:
            e.wait_ge(self.sem[k], v)
        if fn is None:
            ins = e.dma_start(out=A(out), in_=A(in_), **kw)
        else:
            ins = fn(e)
        ins.then_inc(self.sem[key], 16)
        self.cur[key] = 16 * (gen + 1)
        self._commit((key, 16 * (gen + 1)), rds, [out], acc=acc)
        self.nins += 1

    def barrier(self, engines=None):
        for en, e in self.eng.items():
            if engines is not None and en not in engines:
                continue
            w = self.waited[en]
            for k, v in self.cur.items():
                if v == 0 or (k == en and en == "pe"):
                    continue
                if w.get(k, 0) >= v:
                    continue
                w[k] = v
                e.wait_ge(self.sem[k], v)

    def mm(self, out, lhsT, rhs, start=True, stop=True):
        self.op("pe", lambda e: e.matmul(A(out), A(lhsT), A(rhs), start=start, stop=stop), [lhsT, rhs], [out])

    def tr(self, out, in_, ident):
        self.op("pe", lambda e: e.transpose(A(out), A(in_), A(ident)), [in_, ident], [out])

    def act(self, out, in_, func, bias=None, scale=None, accum=None, extra_reads=()):
        kw = {}
        rd = [in_] + list(extra_reads)
        if bias is not None:
            kw["bias"] = A(bias)
            rd.append(bias)
        if scale is not None:
            kw["scale"] = A(scale)
            rd.append(scale)
        wr = [out]
        if accum is not None:
            kw["accum_out"] = A(accum)
            wr.append(accum)
        self.op("act", lambda e: e.activation(out=A(out), in_=A(in_), func=func, **kw), rd, wr)

    def tt(self, out, in0, in1, op, eng="dve"):
        self.op(eng, lambda e: e.tensor_tensor(out=A(out), in0=A(in0), in1=A(in1), op=op), [in0, in1], [out])

    def ts(self, out, in0, s1, op0, s2=None, op1=None, eng="dve", accum=None):
        rd = [in0, s1, s2]
        kw = {}
        if op1 is not None:
            kw["op1"] = op1
        wr = [out]
        if accum is not None:
            kw["accum_out"] = A(accum)
            wr.append(accum)
        self.op(eng, lambda e: e.tensor_scalar(out=A(out), in0=A(in0), scalar1=A(s1), scalar2=A(s2), op0=op0, **kw), rd, wr)

    def stt(self, out, in0, scalar, in1, op0, op1, eng="dve"):
        self.op(eng, lambda e: e.scalar_tensor_tensor(out=A(out), in0=A(in0), scalar=A(scalar), in1=A(in1), op0=op0, op1=op1),
                [in0, scalar, in1], [out])

    def cp(self, out, in_, eng="dve"):
        if eng == "act":
            self.op("act", lambda e: e.copy(out=A(out), in_=A(in_)), [in_], [out])
        else:
            self.op(eng, lambda e: e.tensor_copy(out=A(out), in_=A(in_)), [in_], [out])

    def recip(self, out, in_):
        self.op("dve", lambda e: e.reciprocal(out=A(out), in_=A(in_)), [in_], [out])

    def memset(self, out, val, eng="dve"):
        self.op(eng, lambda e: e.memset(A(out), val), [], [out])

    def red(self, out, in_, op, axis=AX.X, eng="dve"):
        self.op(eng, lambda e: e.tensor_reduce(out=A(out), in_=A(in_), axis=axis, op=op), [in_], [out])


class G:
    pass


SH1, SC1, G1, SH2, SC2, G2 = [i * D for i in range(6)]
C_MU, C_CONV = 0, 15
C_KK, C_KA, C_RK, C_W0, C_A0, C_LG, C_LB = 0, 8, 16, 24, 40, 56, 64
C2_KK, C2_KA, C2_RK, C2_W0, C2_A0, C2_LG, C2_LB = 27, 31, 35, 39, 47, 55, 59
NP128 = 63


def stage_params(p, g):
    nc = p.nc
    with contextlib.ExitStack() as st:
        crow = p.sb(st, "crow", [3, D])
        p.dma(crow, g.c3)
        srow = p.sb(st, "srow", [3, D])
        p.act(srow, crow, AF.Silu)
        sT = p.sb(st, "sT", [128, 8, 3], BF16)
        pst = p.ps(st, "pst", [128, 8, 4])
        for c in range(8):
            p.tr(pst[:, c, 0:3], srow[:, c * 128:(c + 1) * 128], g.ident[0:3, 0:3])
        p.cp(sT, pst[:, :, 0:3])
        wst = [p.sb(st, "wst%d" % i, [128, 8, 512], BF16) for i in range(3)]
        bm = p.sb(st, "bm", [3, 6 * D])
        mrow = p.sb(st, "mrow", [3, 6 * D])
        pm = [p.ps(st, "pm%d" % i, [3, 512]) for i in range(2)]
        k = 0
        for l in range(DEPTH):
            p.dma(bm, g.b_mod[l].pb(3))
            for cg in range(12):
                ws = wst[k % 3]
                p.dma(ws, g.w_mod[l].re("(c q) n -> q c n", q=128)[:, :, cg * 512:(cg + 1) * 512], q="pool")
                pp = pm[k % 2]
                for kc in range(8):
                    p.mm(pp, sT[:, kc, :], ws[:, kc, :], start=(kc == 0), stop=(kc == 7))
                p.tt(mrow[:, cg * 512:(cg + 1) * 512], pp, bm[:, cg * 512:(cg + 1) * 512], ALU.add)
                k += 1
            p.dma(g.MODR[l], mrow, q="sp")
        pr128 = p.sb(st, "pr128", [NP128, 128])
        pr64 = p.sb(st, "pr64", [72, 64])
        pp128 = p.ps(st, "pp128", [128, 64])
        pp64 = p.ps(st, "pp64", [64, 72])
        for l in range(DEPTH):
            p.dma(pr128[0:15, :], g.shift_mu[l].re("(c q) -> c q", q=128))
            p.dma(pr128[15:27, :], g.conv_w[l].re("j (c q) -> (j c) q", q=128))
            p.dma(pr64[C_KK:C_KK + 8, :], g.k_k[l].re("(h k) -> h k", k=64))
            p.dma(pr64[C_KA:C_KA + 8, :], g.k_a[l].re("(h k) -> h k", k=64))
            p.dma(pr64[C_RK:C_RK + 8, :], g.r_k[l])
            p.dma(pr64[C_W0:C_W0 + 16, :], g.w0[l].re("d (h k) -> (d h) k", k=64))
            p.dma(pr64[C_A0:C_A0 + 16, :], g.a0[l].re("d (h k) -> (d h) k", k=64))
            p.dma(pr64[C_LG:C_LG + 8, :], g.lnx_g[l].re("(h k) -> h k", k=64))
            p.dma(pr64[C_LB:C_LB + 8, :], g.lnx_b[l].re("(h k) -> h k", k=64))
            p.dma(pr128[C2_KK:C2_KK + 4, :], g.k_k[l].re("(c q) -> c q", q=128))
            p.dma(pr128[C2_KA:C2_KA + 4, :], g.k_a[l].re("(c q) -> c q", q=128))
            p.dma(pr128[C2_RK:C2_RK + 4, :], g.r_k[l].re("(c a) k -> c (a k)", a=2))
            p.dma(pr128[C2_W0:C2_W0 + 8, :], g.w0[l].re("d (c q) -> (d c) q", q=128))
            p.dma(pr128[C2_A0:C2_A0 + 8, :], g.a0[l].re("d (c q) -> (d c) q", q=128))
            p.dma(pr128[C2_LG:C2_LG + 4, :], g.lnx_g[l].re("(c q) -> c q", q=128))
            p.dma(pr128[C2_LB:C2_LB + 4, :], g.lnx_b[l].re("(c q) -> c q", q=128))
            p.tr(pp128[:, 0:NP128], pr128, g.ident[0:NP128, 0:NP128])
            p.cp(g.P128T[:, l, :], pp128[:, 0:NP128])
            p.tr(pp64, pr64, g.ident[0:72, 0:72])
            p.cp(g.P64T[:, l, :], pp64)
        p.ts(g.OMM, g.P128T[:, :, C_MU:C_MU + 15], -1.0, ALU.mult, 1.0, ALU.add)
        p.ts(g.HMU, g.P128T[:, :, C_MU:C_MU + 15], 0.5, ALU.mult)
        p.ts(g.OMKA, g.P128T[:, :, C2_KA:C2_KA + 4], -1.0, ALU.mult, 1.0, ALU.add)
    p.barrier()


def stage_norm(p, g, src, l, b, second, hT, hTf=None):
    SHc, SCc = (SH2, SC2) if second else (SH1, SC1)
    ng = g.norm2_g if second else g.norm1_g
    with contextlib.ExitStack() as st:
        gt = p.sb(st, "gt", [128, D])
        p.dma(gt, ng[l].pb(128))
        Ab, Bb = [], []
        for kind, row in ((0, 2), (1, b)):
            sc = p.sb(st, "scb%d" % kind, [128, D])
            p.dma(sc, g.MODR[l, row, SCc:SCc + D].pb(128))
            a = p.sb(st, "Ab%d" % kind, [128, D])
            p.stt(a, sc, 1.0, gt, ALU.add, ALU.mult)
            bb = p.sb(st, "Bb%d" % kind, [128, D])
            p.dma(bb, g.MODR[l, row, SHc:SHc + D].pb(128))
            Ab.append(a)
            Bb.append(bb)
        xall = [p.sb(st, "xa%d" % i, [128, D]) for i in range(NT)]
        sqs = [p.sb(st, "sq%d" % i, [128, D]) for i in range(2)]
        hns = [p.sb(st, "hn%d" % i, [128, D]) for i in range(2)]
        hbs = [p.sb(st, "hb%d" % i, [128, D], BF16) for i in range(2)]
        ssall = p.sb(st, "ssall", [128, NT])
        rs = p.sb(st, "rsall", [128, NT])
        ptr = [p.ps(st, "ptr%d" % i, [128, 8, 128], BF16) for i in range(2)]
        for i in range(NT):
            p.dma(xall[i], src[i * 128:(i + 1) * 128, :], q=("sp" if i % 2 == 0 else "act"))
            p.act(sqs[i % 2], xall[i], AF.Square)
            p.red(ssall[:, i:i + 1], sqs[i % 2], ALU.add)
        p.ts(rs, ssall, 1.0 / D, ALU.mult, EPS, ALU.add)
        p.act(rs, rs, AF.Sqrt)
        p.recip(rs, rs)
        for i in range(NT):
            kind = 0 if i < 2 else 1
            hn, hb, pt = hns[i % 2], hbs[i % 2], ptr[i % 2]
            p.stt(hn, xall[i], rs[:, i:i + 1], Ab[kind], ALU.mult, ALU.mult)
            p.tt(hb, hn, Bb[kind], ALU.add, eng="pool")
            if second:
                p.dma(g.H2T[i * 128:(i + 1) * 128, :], hb, q="pool", acc=True)
            for c in range(8):
                p.tr(pt[:, c, :], hb[:, c * 128:(c + 1) * 128], g.identb)
            p.cp(hT[:, :, i * 128:(i + 1) * 128], pt, eng="act")


def stage_proj(p, g, hT, l, b):
    groups = [(c0, min(512, NIN - c0)) for c0 in range(0, NIN, 512)]
    with contextlib.ExitStack() as st:
        wbf = [p.sb(st, "pwbf%d" % i, [128, 8, 512], BF16) for i in range(2)]

        def loadg(gi):
            c0, w = groups[gi]
            p.dma(wbf[gi % 2][:, :, :w], g.w_in[l].re("(c q) n -> q c n", q=128)[:, :, c0:c0 + w], q="pool")
        loadg(0)
        ot = [p.sb(st, "pot%d" % i, [128, T]) for i in range(2)]
        zt = [p.sb(st, "pzt%d" % i, [128, T]) for i in range(2)]
        pss = [p.ps(st, "pps%d" % i, [128, 512]) for i in range(4)]
        k = 0
        kk = 0
        for gi, (c0, w) in enumerate(groups):
            wb = wbf[gi % 2]
            if gi + 1 < len(groups):
                loadg(gi + 1)
            for cc in range(w // 128):
                col = c0 + cc * 128
                chunk = col // 128
                o = ot[k % 2]
                for (t0, tn) in TT:
                    ps = pss[kk % 4]
                    for kc in range(8):
                        p.mm(ps[:, :tn], wb[:, kc, cc * 128:(cc + 1) * 128], hT[:, kc, t0:t0 + tn],
                             start=(kc == 0), stop=(kc == 7))
                    if chunk >= 27:
                        p.act(o[:, t0:t0 + tn], ps[:, :tn], AF.Sigmoid)
                    elif kk % 2 == 0:
                        p.cp(o[:, t0:t0 + tn], ps[:, :tn], eng="act")
                    else:
                        p.cp(o[:, t0:t0 + tn], ps[:, :tn], eng="dve")
                    kk += 1
                if 12 <= chunk < 27:
                    j = chunk - 12
                    z = zt[k % 2]
                    om = g.OMM[:, l, j:j + 1]
                    hm = g.HMU[:, l, j:j + 1]
                    p.ts(z, o, om, ALU.mult)
                    for (a0, a1) in ((0, TCTX), (TCTX, T)):
                        p.stt(z[:, a0 + 1:a1], o[:, a0:a1 - 1], hm, z[:, a0 + 1:a1], ALU.mult, ALU.add)
                        p.stt(z[:, a0:a1 - 1], o[:, a0 + 1:a1], hm, z[:, a0:a1 - 1], ALU.mult, ALU.add)
                    o = z
                p.dma(g.ZF[col:col + 128, :], o, q="sp", acc=True)
                k += 1


WEIGHTS = [
    ("w_mod", [DEPTH, D, 6 * D]), ("b_mod", [DEPTH, 6 * D]), ("norm1_g", [DEPTH, D]), ("norm2_g", [DEPTH, D]),
    ("w_in", [DEPTH, D, NIN]), ("shift_mu", [DEPTH, 1920]), ("conv_w", [DEPTH, 3, 512]),
    ("w_up", [DEPTH, 2, 64, 512]), ("w0", [DEPTH, 2, 512]), ("a_up", [DEPTH, 2, 64, 512]), ("a0", [DEPTH, 2, 512]),
    ("g_up", [DEPTH, 128, 512]), ("k_k", [DEPTH, 512]), ("k_a", [DEPTH, 512]), ("r_k", [DEPTH, 8, 64]),
    ("lnx_g", [DEPTH, 512]), ("lnx_b", [DEPTH, 512]), ("w_a_out", [DEPTH, 512, D]), ("w_b_out", [DEPTH, 512, D]),
    ("w_o", [DEPTH, D, D]), ("router_g", [DEPTH, D, 4]), ("router_g_b", [DEPTH, 4]),
    ("router_e", [DEPTH, D, NEXP]), ("router_e_b", [DEPTH, NEXP]),
    ("exp_w1", [DEPTH, NEXP, D, DFF]), ("exp_w3", [DEPTH, NEXP, D, DFF]), ("exp_w2", [DEPTH, NEXP, DFF, D]),
    ("final_g", [D]),
]


def build(stages="all", dbg=()):
    nc = bass.Bass("TRN2", target_bir_lowering=False)
    p = P(nc)
    g = G()
    g.p = p

    def inp(name, shape):
        return Tl(nc.dram_tensor(name, list(shape), F32, kind="ExternalInput").ap(), name)
    g.xin = inp("xin", [NB, T, D])
    g.c3 = inp("c3", [3, D])
    g.idin = inp("idin", [128, 128])
    g.mskin = inp("mskin", [128, 2, 4, 128])
    g.eoffin = inp("eoffin", [128, NEXP])
    for name, shape in WEIGHTS:
        setattr(g, name, inp(name, shape))
    g.out = Tl(nc.dram_tensor("out", [NB, TLAT, D], F32, kind="ExternalOutput").ap(), "out")
    g.MODR = p.dram("MODR", [DEPTH, 3, 6 * D])
    g.ZF = p.dram("ZF", [NIN, T])
    g.XS = [p.dram("XS%d" % b, [T, D]) for b in range(NB)]
    g.SCF = p.dram("SCF", [2, 512, NCH, 4, CH], BF16)
    g.SCT = p.dram("SCT", [2, NCH, CH, 2, 512], BF16)
    g.VTD = p.dram("VTD", [NCH, CH, 512], BF16)
    g.WCD = p.dram("WCD", [512, 2, NCH])
    g.RO = p.dram("RO", [2, 512, T])
    g.YD = p.dram("YD", [2, 512, T])
    g.H2T = p.dram("H2T", [T, D], BF16)
    g.XE = p.dram("XE", [NEXP * CAP + 1, D], BF16)
    g.YE = p.dram("YE", [NEXP * CAP + 1, D])
    g.YEZ = p.dram("YEZ", [1, D])
    dbg_out = {}
    for name, shape in dbg:
        dbg_out[name] = Tl(nc.dram_tensor("dbg_" + name, list(shape), F32, kind="ExternalOutput").ap(), name)
    g.dbg = dbg_out
    top = p.es
    g.ident = p.sb(top, "ident", [128, 128])
    g.identb = p.sb(top, "identb", [128, 128], BF16)
    p.dma(g.ident, g.idin)
    p.cp(g.identb, g.ident)
    g.P128T = p.sb(top, "P128T", [128, DEPTH, NP128])
    g.P64T = p.sb(top, "P64T", [64, DEPTH, 72])
    g.OMM = p.sb(top, "OMM", [128, DEPTH, 15])
    g.HMU = p.sb(top, "HMU", [128, DEPTH, 15])
    g.OMKA = p.sb(top, "OMKA", [128, DEPTH, 4])
    g.MASK4 = p.sb(top, "MASK4", [128, 2, 4, 128])
    p.dma(g.MASK4, g.mskin)
    g.ones64 = p.sb(top, "ones64", [64, 64])
    p.memset(g.ones64, 1.0)
    g.onesf = p.sb(top, "onesf", [128, 128])
    p.memset(g.onesf, 1.0)
    g.EOFF = p.sb(top, "EOFF", [128, NEXP])
    p.dma(g.EOFF, g.eoffin)
    with contextlib.ExitStack() as stz:
        zt = p.sb(stz, "zrow", [1, D])
        p.memset(zt, 0.0)
        p.dma(g.YE[NEXP * CAP:NEXP * CAP + 1, :], zt, q="pool")
        p.barrier()
    g.ones2 = p.sb(top, "ones2", [128, 128])
    p.memset(g.ones2, 0.0)
    p.memset(g.ones2[0:64, 0:64], 1.0)
    p.memset(g.ones2[64:128, 64:128], 1.0)
    g.RM = p.sb(top, "RM", [128, TH])
    p.memset(g.RM, 1.0)
    p.memset(g.RM.re("k (c t) -> k c t", t=CH)[:, :, 0:1], 0.0)

    stage_params(p, g)
    nl = DEPTH if stages == "all" else stages[0]
    nb = NB if stages == "all" else stages[1]
    upto = "z" if stages == "all" else stages[2]
    for b in range(nb):
        for l in range(nl):
            src = g.xin[b] if l == 0 else g.XS[b]
            with contextlib.ExitStack() as st:
                hT = p.sb(st, "hT", [128, 8, T], BF16)
                stage_norm(p, g, src, l, b, False, hT)
                if upto == "A":
                    if "hT" in g.dbg:
                        with contextlib.ExitStack() as s2:
                            tmp = p.sb(s2, "dbgtmp", [128, 8, T])
                            p.cp(tmp, hT)
                            p.dma(g.dbg["hT"], tmp)
                            p.barrier()
                    p.barrier()
                    continue
                stage_proj(p, g, hT, l, b)
                p.barrier()
            if upto == "B":
                continue
            with contextlib.ExitStack() as st:
                CO = p.sb(st, "CO", [128, 4, T], BF16)
                stage_conv(p, g, l, b, CO)
                stage_prep(p, g, l, b)
                if upto == "D":
                    continue
                stage_scan(p, g, l, b)
                if upto == "E":
                    continue
                stage_mix(p, g, l, b, CO, src, g.XS[b])
            if upto == "G":
                continue
            with contextlib.ExitStack() as st:
                GW = p.sb(st, "GW", [128, NT, 2])
                DEST = p.sb(st, "DEST", [128, NT, 2], I32)
                with contextlib.ExitStack() as st2:
                    hT = p.sb(st2, "hT2", [128, 8, T], BF16)
                    stage_norm(p, g, g.XS[b], l, b, True, hT)
                    stage_router(p, g, l, hT, GW, DEST)
                stage_moe(p, g, l, b, GW, DEST)
        if upto == "z":
            stage_final(p, g, b)
    if "ZF" in g.dbg:
        p.barrier()
        with contextlib.ExitStack() as s2:
            tmp = p.sb(s2, "dbgtmp", [128, T])
            for c in range(NIN // 128):
                p.dma(tmp, g.ZF[c * 128:(c + 1) * 128, :])
                p.dma(g.dbg["ZF"][c * 128:(c + 1) * 128, :], tmp)
            p.barrier()
    for nm, t in (("YD", g.YD.re("d n t -> (d n) t")), ("RO", g.RO.re("q n t -> (q n) t")), ("XS0", g.XS[0])):
        if nm in g.dbg:
            with contextlib.ExitStack() as s2:
                n, w = t.ap.shape
                tmp = p.sb(s2, "dbgtmp3", [128, w])
                for c in range(n // 128):
                    p.dma(tmp, t[c * 128:(c + 1) * 128, :])
                    p.dma(g.dbg[nm][c * 128:(c + 1) * 128, :], tmp)
                p.barrier()
    if "MODR" in g.dbg:
        with contextlib.ExitStack() as s2:
            tmp = p.sb(s2, "dbgtmp2", [12, 6 * D])
            p.dma(tmp, g.MODR.re("l r n -> (l r) n"))
            p.dma(g.dbg["MODR"], tmp)
            p.barrier()
    p.barrier()
    p.es.close()
    return nc, p


def make_in_maps(inputs, ncores=8):
    idm = np.eye(128, dtype=np.float32)
    ii = np.arange(128)
    us = (ii[None, :] > ii[:, None]).astype(np.float32)
    ui = (ii[None, :] >= ii[:, None]).astype(np.float32)
    msk = np.ascontiguousarray(np.stack([np.stack([us, ui, us, ui], 0), np.stack([us.T, ui.T, us.T, ui.T], 0)], 0).transpose(2, 0, 1, 3))
    eoff = np.ascontiguousarray(np.broadcast_to((np.arange(NEXP, dtype=np.float32) * CAP)[None, :], (128, NEXP)))
    maps = []
    for core in range(ncores):
        b0 = core * NB
        xin = np.concatenate([inputs["ctx"][b0:b0 + NB], inputs["x"][b0:b0 + NB]], axis=1)
        c3 = np.concatenate([inputs["c"][b0:b0 + NB], inputs["c_ctx"][None, :]], axis=0)
        m = {"xin": np.ascontiguousarray(xin, dtype=np.float32), "c3": np.ascontiguousarray(c3, dtype=np.float32), "idin": idm,
             "mskin": msk, "eoffin": eoff}
        for name, _ in WEIGHTS:
            m[name] = np.ascontiguousarray(inputs[name], dtype=np.float32)
        maps.append(m)
    return maps


def kernel(**inputs):
    inputs = {k: np.asarray(v) for k, v in inputs.items()}
    nc, p = build("all")
    maps = make_in_maps(inputs, 8)
    res = run_bass_kernel_spmd(nc, maps, core_ids=list(range(8)))
    return np.concatenate([r["out"] for r in res.results], axis=0).astype(np.float32)


def stage_conv(p, g, l, b, CO):
    with contextlib.ExitStack() as st:
        bgt = [p.sb(st, "cbg%d" % i, [128, T]) for i in range(2)]
        cgt = [p.sb(st, "ccg%d" % i, [128, T]) for i in range(2)]
        hat = [p.sb(st, "cha%d" % i, [128, T]) for i in range(2)]
        ut = [p.sb(st, "cu%d" % i, [128, T]) for i in range(2)]
        ott = [p.sb(st, "co%d" % i, [128, T]) for i in range(2)]
        for j in range(4):
            bg, cg, ha, u, o = bgt[j % 2], cgt[j % 2], hat[j % 2], ut[j % 2], ott[j % 2]
            p.dma(bg, g.ZF[j * 128:(j + 1) * 128, :], q="sp")
            p.dma(cg, g.ZF[512 + j * 128:512 + (j + 1) * 128, :], q="act")
            p.dma(ha, g.ZF[1024 + j * 128:1024 + (j + 1) * 128, :], q="sp")
            w0 = g.P128T[:, l, C_CONV + 0 * 4 + j:C_CONV + 0 * 4 + j + 1]
            w1 = g.P128T[:, l, C_CONV + 1 * 4 + j:C_CONV + 1 * 4 + j + 1]
            w2 = g.P128T[:, l, C_CONV + 2 * 4 + j:C_CONV + 2 * 4 + j + 1]
            p.tt(u, cg, ha, ALU.mult, eng="pool")
            p.ts(o, u, w1, ALU.mult)
            p.stt(o[:, 1:TCTX], u[:, 0:TCTX - 1], w0, o[:, 1:TCTX], ALU.mult, ALU.add)
            p.stt(o[:, 0:TCTX - 1], u[:, 1:TCTX], w2, o[:, 0:TCTX - 1], ALU.mult, ALU.add)
            if j < 2:
                ug = u[:, TCTX:T].re("q (r w) -> q r w", w=64)
                og = o[:, TCTX:T].re("q (r w) -> q r w", w=64)
                p.stt(og[:, :, 1:64], ug[:, :, 0:63], w0, og[:, :, 1:64], ALU.mult, ALU.add)
                p.stt(og[:, :, 0:63], ug[:, :, 1:64], w2, og[:, :, 0:63], ALU.mult, ALU.add)
            else:
                p.stt(o[:, TCTX + 64:T], u[:, TCTX:T - 64], w0, o[:, TCTX + 64:T], ALU.mult, ALU.add)
                p.stt(o[:, TCTX:T - 64], u[:, TCTX + 64:T], w2, o[:, TCTX:T - 64], ALU.mult, ALU.add)
            p.tt(CO[:, j, :], o, bg, ALU.mult, eng="pool")
    p.barrier()


TH = 1152
THT = [(0, 512), (512, 512), (1024, 128)]
NCH2 = TH // CH


def stage_prep(p, g, l, b):
    with contextlib.ExitStack() as st:
        tzw = [p.sb(st, "tzw%d" % d, [64, T], BF16) for d in range(2)]
        zab = [p.sb(st, "zab%d" % d, [64, T], BF16) for d in range(2)]
        sgz = p.sb(st, "sgz", [128, T], BF16)
        with contextlib.ExitStack() as st0:
            tmpf = p.sb(st0, "dtmpf", [128, T])
            for d in range(2):
                p.dma(tmpf[0:64, :], g.ZF[RW0 + 1536 + d * 64:RW0 + 1536 + (d + 1) * 64, :])
                p.act(tzw[d], tmpf[0:64, :], AF.Tanh)
                p.dma(tmpf[0:64, :], g.ZF[RW0 + 1664 + d * 64:RW0 + 1664 + (d + 1) * 64, :])
                p.cp(zab[d], tmpf[0:64, :], eng="act")
            p.dma(tmpf, g.ZF[RW0 + 1792:RW0 + 1920, :])
            p.act(sgz, tmpf, AF.Sigmoid)
            p.barrier()
        wupb = p.sb(st, "wupb", [64, 2, 512], BF16)
        aupb = p.sb(st, "aupb", [64, 2, 512], BF16)
        gupb = p.sb(st, "gupb", [128, 512], BF16)
        p.dma(wupb, g.w_up[l].re("d r n -> r d n"), q="pool")
        p.dma(aupb, g.a_up[l].re("d r n -> r d n"), q="pool")
        p.dma(gupb, g.g_up[l], q="pool")
        rts = [p.sb(st, "d_r%d" % i, [128, TH]) for i in range(2)]
        kts = [p.sb(st, "d_k%d" % i, [128, TH]) for i in range(2)]
        vts = [p.sb(st, "d_v%d" % i, [128, TH]) for i in range(2)]
        kks = [p.sb(st, "d_kk%d" % i, [128, TH]) for i in range(2)]
        Xss = [[p.sb(st, "d_x%d_%d" % (i, q), [128, TH]) for i in range(8)] for q in range(2)]
        obs = [{n: p.sb(st, "d_ob%d_" % q + n, [128, TH], BF16) for n in ("kk", "r", "kh", "bh", "kp", "bp", "v")} for q in range(2)]
        tsts = [[p.sb(st, "d_tst%d" % i, [128, NCH2, 128], BF16) for i in range(3)]] * 2
        wcs = [p.sb(st, "d_wc%d" % q, [128, NCH2]) for q in range(2)]
        ps = [p.ps(st, "d_ps%d" % i, [128, 512]) for i in range(3)]
        pst = [p.ps(st, "d_pst%d" % i, [128, 16, 128], BF16) for i in range(2)]
        npst = 0
        nps = 0

        def pk(col):
            return g.P128T[:, l, col:col + 1]

        def tposed(src_b, dst_dram, q, k, tst):
            nonlocal npst
            pt = pst[npst % 2]
            npst += 1
            stg = tst[k]
            for c in range(NCH2):
                p.tr(pt[:, c, :], src_b[:, c * CH:(c + 1) * CH], g.identb)
            p.cp(stg, pt[:, 0:NCH2, :], eng=("act" if k % 2 else "dve"))
            p.dma(dst_dram, stg, q=q, acc=True)

        def body(pr, hf):
            nonlocal nps, npst
            r0 = pr * 128
            ob, tst, wc = obs[hf], tsts[hf], wcs[hf]
            tb = hf * TH
            cb = hf * NCH2
            rt, kt, vt, kk, X = rts[hf], kts[hf], vts[hf], kks[hf], Xss[hf]
            p.dma(rt, g.ZF[RW0 + r0:RW0 + r0 + 128, tb:tb + TH], q="act")
            yield
            p.dma(kt, g.ZF[RW0 + 512 + r0:RW0 + 512 + r0 + 128, tb:tb + TH], q="act")
            yield
            p.dma(vt, g.ZF[RW0 + 1024 + r0:RW0 + 1024 + r0 + 128, tb:tb + TH], q="act")
            yield
            p.ts(X[0], kt, pk(C2_KK + pr), ALU.mult)
            yield
            p.act(X[1], X[0], AF.Square)
            yield
            for (t0, tn) in THT:
                pp = ps[nps % 3]
                nps += 1
                p.mm(pp[:, :tn], g.ones2, X[1][:, t0:t0 + tn])
                yield
                p.ts(X[2][:, t0:t0 + tn], pp[:, :tn], 1e-12, ALU.add)
                yield
            p.act(X[2], X[2], AF.Sqrt)
            yield
            p.recip(X[2], X[2])
            yield
            p.tt(kk, X[0], X[2], ALU.mult)
            yield
            p.cp(ob["v"], vt)
            yield
            tposed(ob["v"], g.VTD[cb:cb + NCH2, :, r0:r0 + 128].re("c t n -> t c n"), "sp", 0, tst)
            yield
            for d in range(2):
                lw, a, kd, bb, L = X[0], X[1], X[2], X[3], X[4]
                for (t0, tn) in THT:
                    pp = ps[nps % 3]
                    nps += 1
                    p.mm(pp[:, :tn], wupb[:, d, r0:r0 + 128], tzw[d][:, tb + t0:tb + t0 + tn])
                    yield
                    p.act(lw[:, t0:t0 + tn], pp[:, :tn], AF.Sigmoid, bias=pk(C2_W0 + d * 4 + pr))
                    yield
                    pp = ps[nps % 3]
                    nps += 1
                    p.mm(pp[:, :tn], aupb[:, d, r0:r0 + 128], zab[d][:, tb + t0:tb + t0 + tn])
                    yield
                    p.act(a[:, t0:t0 + tn], pp[:, :tn], AF.Sigmoid, bias=pk(C2_A0 + d * 4 + pr))
                    yield
                p.ts(lw, lw, -0.6065306597126334, ALU.mult)
                yield
                p.ts(kd, a, pk(C2_KA + pr), ALU.mult, g.OMKA[:, l, pr:pr + 1], ALU.add)
                yield
                p.tt(kd, kd, kt, ALU.mult)
                yield
                p.tt(bb, kk, a, ALU.mult)
                yield
                if d == 0:
                    p.cp(X[7], kd)
                    yield
                else:
                    p.tt(X[7], X[7], kd, ALU.add)
                    yield
                p.op("dve", lambda e: e.tensor_tensor_scan(out=A(L), data0=A(g.RM), data1=A(lw), initial=0.0,
                                                           op0=ALU.mult, op1=ALU.add), [g.RM, lw], [L])
                if d == 1:
                    Lp = X[1]
                    p.tt(Lp, lw, L, ALU.subtract)
                    yield
                    p.tt(Lp.re("k (c t) -> k c t", t=CH), Lp.re("k (c t) -> k c t", t=CH),
                         L.re("k (c t) -> k c t", t=CH)[:, :, CH - 1:CH].bc([128, NCH2, CH]), ALU.add)
                    end = 0
                else:
                    Lp = L
                    end = CH - 1
                E = X[5]
                p.act(E, Lp, AF.Exp)
                yield
                p.tt(ob["r"], rt, E, ALU.mult)
                yield
                p.act(wc.re("k (c o) -> k c o", o=1), Lp.re("k (c t) -> k c t", t=CH)[:, :, end:end + 1], AF.Exp)
                yield
                E2 = X[6]
                p.act(E2, Lp, AF.Exp, scale=-1.0)
                yield
                p.tt(kd, kd, E2, ALU.mult)
                yield
                p.tt(bb, bb, E2, ALU.mult)
                yield
                p.tt(lw, Lp, lw, ALU.subtract)
                yield
                p.act(lw, lw, AF.Exp)
                yield
                p.tt(ob["kk"], kk, lw, ALU.mult)
                yield
                p.cp(ob["kh"], kd, eng="act")
                yield
                p.cp(ob["bh"], bb, eng="act")
                yield
                wcb = wc.re("k (c o) -> k c o", o=1).bc([128, NCH2, CH])
                p.tt(ob["kp"].re("k (c t) -> k c t", t=CH), kd.re("k (c t) -> k c t", t=CH), wcb, ALU.mult)
                yield
                p.tt(ob["bp"].re("k (c t) -> k c t", t=CH), bb.re("k (c t) -> k c t", t=CH), wcb, ALU.mult)
                yield
                for qi, n in enumerate(("kk", "r", "kh", "bh")):
                    p.dma(g.SCF[d, r0:r0 + 128, cb:cb + NCH2, qi, :], ob[n].re("k (c t) -> k c t", t=CH), q="sp", acc=True)
                    yield
                tposed(ob["kp"], g.SCT[d, cb:cb + NCH2, :, 0, r0:r0 + 128].re("c t n -> t c n"), "sp", 1, tst)
                yield
                tposed(ob["bp"], g.SCT[d, cb:cb + NCH2, :, 1, r0:r0 + 128].re("c t n -> t c n"), "sp", 2, tst)
                yield
                p.dma(g.WCD[r0:r0 + 128, d, cb:cb + NCH2], wc, q="sp", acc=True)
                yield
            p.ts(X[0], rt, pk(C2_RK + pr), ALU.mult)
            yield
            p.tt(X[0], X[0], X[7], ALU.mult)
            yield
            for (t0, tn) in THT:
                pp = ps[nps % 3]
                nps += 1
                p.mm(pp[:, :tn], g.ones2, X[0][:, t0:t0 + tn])
                yield
                p.tt(X[1][:, t0:t0 + tn], pp[:, :tn], vt[:, t0:t0 + tn], ALU.mult)
                yield
                pp = ps[nps % 3]
                nps += 1
                p.mm(pp[:, :tn], gupb[:, r0:r0 + 128], sgz[:, tb + t0:tb + t0 + tn])
                yield
                p.cp(X[2][:, t0:t0 + tn], pp[:, :tn], eng="act")
                yield
            p.dma(g.RO[0, r0:r0 + 128, tb:tb + TH], X[1], q="sp", acc=True)
            yield
            p.dma(g.RO[1, r0:r0 + 128, tb:tb + TH], X[2], q="sp", acc=True)
            yield

        for pr in range(4):
            gens = [body(pr, 0), body(pr, 1)]
            live = [True, True]
            while any(live):
                for q in range(2):
                    if live[q]:
                        try:
                            next(gens[q])
                        except StopIteration:
                            live[q] = False
    p.barrier()


ORDER_B = [1, 0] + list(range(NCH - 1, 1, -1))


def stage_scan(p, g, l, b):
    with contextlib.ExitStack() as st:
        B = [p.ps(st, "e_b%d" % i, [128, 512]) for i in range(8)]

        def bv(i, n, w, parts=128):
            return B[i].re("s (a t) -> s a t", t=w)[0:parts, 0:n, :]
        WC = p.sb(st, "e_wc", [64, 2, 8, NCH])
        p.dma(WC, g.WCD.re("(h k) d c -> k d h c", k=64))
        ST = p.sb(st, "e_st", [64, 16, 64])
        STb = p.sb(st, "e_stb", [64, 16, 64], BF16)
        p.memset(ST, 0.0)
        p.memset(STb, 0.0, eng="pool")
        FQ = [p.sb(st, "e_fq%d" % i, [64, 2, 8, 4, 128], BF16) for i in range(2)]
        TQ = [p.sb(st, "e_tq%d" % i, [128, 2, 2, 8, 64], BF16) for i in range(2)]
        VT = [p.sb(st, "e_vt%d" % i, [128, 2, 8, 64], BF16) for i in range(2)]
        AMs = [p.sb(st, "e_am%d" % i, [128, 16, 4, 128], BF16) for i in range(2)]
        Xs = [[p.sb(st, "e_x%d_%d" % (i, q), [128, 4, 128]) for q in range(4)] for i in range(2)]
        XTs = [[p.sb(st, "e_xt%d_%d" % (i, q), [128, 4, 128]) for q in range(4)] for i in range(2)]
        Ps = [[p.sb(st, "e_p%d_%d" % (i, q), [128, 4, 128]) for q in range(4)] for i in range(2)]
        RT = p.sb(st, "e_rt", [128, 16, 64])
        PF = [[p.sb(st, "e_pf%d_%d" % (i, q), [128, 4, 128]) for q in range(4)] for i in range(2)]
        nUT = p.sb(st, "e_nut", [128, 16, 64], BF16)
        YS = [p.sb(st, "e_ys%d" % i, [64, 16, 128]) for i in range(2)]
        idb = g.ident.re("s (o t) -> s o t", o=1).bc([128, 4, 128])
        def loads(j):
            cd = (j, ORDER_B[j])
            fq, tq, vt = FQ[j % 2], TQ[j % 2], VT[j % 2]
            for d in range(2):
                c = cd[d]
                p.dma(fq[:, d], g.SCF[d, :, c].re("(h k) q t -> k h q t", k=64), q="sp")
                p.dma(tq[:, d], g.SCT[d, c].re("t q (h k) -> t q h k", k=64), q="act")
                p.dma(vt[:, d], g.VTD[c].re("t (h k) -> t h k", k=64), q="sp")

        def phase1(j):
            fq, AMb = FQ[j % 2], AMs[j % 2]
            for ci in range(16):
                d, h = divmod(ci, 8)
                pa = B[3 + ci % 2]
                rhs = fq[:, d, h, 0:2, :]
                p.mm(pa.re("s (q t) -> s q t", t=128)[:, 0:2, :], fq[:, d, h, 2, :], rhs)
                p.mm(pa.re("s (q t) -> s q t", t=128)[:, 2:4, :], fq[:, d, h, 3, :], rhs)
                pn = bv(5, 4, 128)
                p.mm(pn[:, ci % 4, :], fq[:, d, h, 0, :], fq[:, d, h, 3, :])
                p.tt(AMb[:, ci], pa.re("s (q t) -> s q t", t=128), g.MASK4[:, d], ALU.mult)
                p.tt(Xs[0][ci // 4][:, ci % 4, :], pa[:, 256:384], g.MASK4[:, d, 0, :], ALU.mult)
                if ci % 4 == 3:
                    p.tt(XTs[0][ci // 4], pn, g.MASK4[:, 1 - d, 0:1, :].bc([128, 4, 128]), ALU.mult)

        def phase2(j):
            for gq in range(4):
                p.tt(Ps[0][gq], idb, Xs[0][gq], ALU.subtract, eng="pool")
            cur = 0
            for lev in range(6):
                last = lev == 5
                if last:
                    for gq in range(4):
                        X, XT = Xs[cur][gq], XTs[cur][gq]
                        bs = 3 * (gq % 2)
                        for q in range(4):
                            p.mm(bv(bs + 1, 4, 128)[:, q, :], X[:, q, :], XT[:, q, :])
                        p.cp(XTs[1 - cur][gq], bv(bs + 1, 4, 128), eng="dve")
                else:
                    for gq in range(4):
                        X, XT = Xs[cur][gq], XTs[cur][gq]
                        bs = 3 * (gq % 2)
                        for q in range(4):
                            p.mm(bv(bs, 4, 128)[:, q, :], XT[:, q, :], X[:, q, :])
                        p.cp(Xs[1 - cur][gq], bv(bs, 4, 128), eng="act")
                    for gq in range(4):
                        bs = 3 * (gq % 2)
                        for q in range(4):
                            p.tr(bv(bs + 1, 4, 128)[:, q, :], Xs[1 - cur][gq][:, q, :], g.ident)
                        p.cp(XTs[1 - cur][gq], bv(bs + 1, 4, 128), eng="dve")
                for gq in range(4):
                    bs = 3 * (gq % 2)
                    for q in range(4):
                        p.mm(bv(bs + 2, 4, 128)[:, q, :], XTs[1 - cur][gq][:, q, :], Ps[cur][gq][:, q, :])
                    dstp = PF[j % 2][gq] if last else Ps[1 - cur][gq]
                    p.tt(dstp, bv(bs + 2, 4, 128), Ps[cur][gq], ALU.add)
                cur = 1 - cur
                yield lev

        def phase3(j):
            cd = (j, ORDER_B[j])
            fq, tq, vt, ys, AMb, Pf = FQ[j % 2], TQ[j % 2], VT[j % 2], YS[j % 2], AMs[j % 2], PF[j % 2]
            for ci in range(16):
                d, h = divmod(ci, 8)
                pr = bv(6 + ci // 8, 8, 64)[:, ci % 8, :]
                p.mm(pr, fq[:, d, h, 0, :], STb[:, ci, :], start=True, stop=False)
                p.mm(pr, AMb[:, ci, 0, :], vt[:, d, h, :], start=False, stop=True)
            p.cp(RT[:, 0:8, :], bv(6, 8, 64), eng="act")
            p.cp(RT[:, 8:16, :], bv(7, 8, 64), eng="dve")
            yield 0
            for ci in range(16):
                pr = bv(6 + ci // 8, 8, 64)[:, ci % 8, :]
                p.mm(pr, Pf[ci // 4][:, ci % 4, :], RT[:, ci, :])
            p.ts(nUT[:, 0:8, :], bv(6, 8, 64), -1.0, ALU.mult)
            p.op("act", lambda e: e.mul(out=A(nUT[:, 8:16, :]), in_=A(bv(7, 8, 64)), mul=-1.0), [B[7]], [nUT])
            yield 1
            for q4 in range(4):
                for q in range(4):
                    ci = 4 * q4 + q
                    d, h = divmod(ci, 8)
                    pv = bv(6 + q4 % 2, 4, 128, 64)[:, q, :]
                    p.mm(pv, STb[:, ci, :], fq[:, d, h, 1, :], start=True, stop=False)
                    p.mm(pv, vt[:, d, h, :], AMb[:, ci, 1, :], start=False, stop=False)
                    p.mm(pv, nUT[:, ci, :], AMb[:, ci, 3, :], start=False, stop=True)
                p.cp(ys[:, 4 * q4:4 * q4 + 4, :], bv(6 + q4 % 2, 4, 128, 64), eng=("act" if q4 % 2 else "dve"))
            for d in range(2):
                c = cd[d]
                p.dma(g.YD[d, :, c * CH:(c + 1) * CH].re("(h k) t -> k h t", k=64), ys[:, d * 8:(d + 1) * 8, :], q="sp", acc=True)
            yield 2
            for ci in range(16):
                d, h = divmod(ci, 8)
                pv = bv(6 + d, 8, 64, 64)[:, ci % 8, :]
                p.mm(pv, tq[:, d, 0, h, :], vt[:, d, h, :], start=True, stop=False)
                p.mm(pv, tq[:, d, 1, h, :], nUT[:, ci, :], start=False, stop=True)
            for d in range(2):
                wcv = WC[:, d, :, cd[d]:cd[d] + 1].bc([64, 8, 64])
                p.tt(ST[:, d * 8:(d + 1) * 8, :], ST[:, d * 8:(d + 1) * 8, :], wcv, ALU.mult, eng="pool")
                p.tt(ST[:, d * 8:(d + 1) * 8, :], ST[:, d * 8:(d + 1) * 8, :], bv(6 + d, 8, 64, 64), ALU.add)
            p.cp(STb, ST, eng="act")
            yield 3

        loads(0)
        phase1(0)
        for _ in phase2(0):
            pass
        for j in range(NCH):
            g3 = phase3(j)
            if j + 1 < NCH:
                loads(j + 1)
                phase1(j + 1)
                for lev in phase2(j + 1):
                    if lev < 4:
                        next(g3)
            for _ in g3:
                pass
    p.barrier()


def stage_mix(p, g, l, b, CO, src, last_dst):
    with contextlib.ExitStack() as st:
        wa = p.sb(st, "m_wa", [128, 4, 1024], BF16)
        wb = p.sb(st, "m_wb", [64, 8, 1024], BF16)
        wo = p.sb(st, "m_wo", [128, 8, 1024], BF16)
        G1b = []
        for kind, row in ((0, 2), (1, b)):
            t = p.sb(st, "m_g1b%d" % kind, [128, D])
            p.dma(t, g.MODR[l, row, G1:G1 + D].pb(128), q="act")
            G1b.append(t)
        p.dma(wa, g.w_a_out[l].re("(c q) n -> q c n", q=128), q="pool")
        p.dma(wb, g.w_b_out[l].re("(h k) n -> k h n", k=64), q="pool")
        p.dma(wo, g.w_o[l].re("(c q) n -> q c n", q=128), q="pool")
        y0 = p.sb(st, "m_y0", [64, 8, 512])
        y1 = p.sb(st, "m_y1", [64, 8, 512])
        aux = p.sb(st, "m_aux", [64, 8, 512])
        ybin = p.sb(st, "m_ybin", [64, 8, 512], BF16)
        sgat = [p.sb(st, "m_sga%d" % i, [128, 512]) for i in range(2)]
        sgbt = [p.sb(st, "m_sgb%d" % i, [128, 512]) for i in range(2)]
        m1 = [p.sb(st, "m_m1%d" % i, [128, 512]) for i in range(1)] * 2
        m2 = [p.sb(st, "m_m2%d" % i, [128, 512]) for i in range(1)] * 2
        mrg = p.sb(st, "m_mrg", [128, 8, 512], BF16)
        xt = [p.sb(st, "m_xt%d" % i, [128, D]) for i in range(2)]
        xo = [p.sb(st, "m_xo%d" % i, [128, D]) for i in range(2)]
        B = [p.ps(st, "m_b%d" % i, [128, 512]) for i in range(8)]
        nb = 0
        nx = 0
        for (t0, tn) in TT:
            for d, yt in ((0, y0), (1, y1)):
                p.dma(yt[:, :, :tn], g.YD[d, :, t0:t0 + tn].re("(h k) t -> k h t", k=64), q=("sp" if d == 0 else "act"))
            p.tt(y0[:, :, :tn], y0[:, :, :tn], y1[:, :, :tn], ALU.add)
            for h in range(8):
                pm = B[nb % 8]
                nb += 1
                p.mm(pm[0:64, :tn], g.ones64, y0[:, h, :tn])
                p.stt(y1[:, h, :tn], pm[0:64, :tn], -1.0 / 64, y0[:, h, :tn], ALU.mult, ALU.add)
            p.act(y0[:, :, :tn], y1[:, :, :tn], AF.Square)
            for h in range(8):
                pm = B[nb % 8]
                nb += 1
                p.mm(pm[0:64, :tn], g.ones64, y0[:, h, :tn])
                p.ts(y0[:, h, :tn], pm[0:64, :tn], 1.0 / 64, ALU.mult, GN_EPS, ALU.add)
            p.act(y0[:, :, :tn], y0[:, :, :tn], AF.Sqrt)
            p.recip(y0[:, :, :tn], y0[:, :, :tn])
            p.tt(y1[:, :, :tn], y1[:, :, :tn], y0[:, :, :tn], ALU.mult)
            for h in range(8):
                p.ts(y1[:, h, :tn], y1[:, h, :tn], g.P64T[:, l, C_LG + h:C_LG + h + 1], ALU.mult,
                     g.P64T[:, l, C_LB + h:C_LB + h + 1], ALU.add)
            p.dma(aux[:, :, :tn], g.RO[0, :, t0:t0 + tn].re("(h k) t -> k h t", k=64), q="sp")
            p.tt(y1[:, :, :tn], y1[:, :, :tn], aux[:, :, :tn], ALU.add)
            p.dma(aux[:, :, :tn], g.RO[1, :, t0:t0 + tn].re("(h k) t -> k h t", k=64), q="sp")
            p.tt(ybin[:, :, :tn], y1[:, :, :tn], aux[:, :, :tn], ALU.mult)
            for cc in range(8):
                sga, sgb = sgat[cc % 2], sgbt[cc % 2]
                p.dma(sga[:, :tn], g.ZF[3456 + cc * 128:3456 + (cc + 1) * 128, t0:t0 + tn], q="sp")
                p.dma(sgb[:, :tn], g.ZF[4480 + cc * 128:4480 + (cc + 1) * 128, t0:t0 + tn], q="act")
                pa = B[nb % 8]
                nb += 1
                for jj in range(4):
                    p.mm(pa[:, :tn], wa[:, jj, cc * 128:(cc + 1) * 128], CO[:, jj, t0:t0 + tn], start=(jj == 0), stop=(jj == 3))
                pb = B[nb % 8]
                nb += 1
                for h in range(8):
                    p.mm(pb[:, :tn], wb[:, h, cc * 128:(cc + 1) * 128], ybin[:, h, :tn], start=(h == 0), stop=(h == 7))
                p.tt(m1[cc % 2][:, :tn], pa[:, :tn], sga[:, :tn], ALU.mult)
                p.tt(m2[cc % 2][:, :tn], pb[:, :tn], sgb[:, :tn], ALU.mult)
                p.tt(mrg[:, cc, :tn], m1[cc % 2][:, :tn], m2[cc % 2][:, :tn], ALU.add)
            for sub in range(tn // 128):
                tok = t0 + sub * 128
                kind = 0 if tok < TCTX else 1
                x, o = xt[nx % 2], xo[nx % 2]
                nx += 1
                p.dma(x, src[tok:tok + 128, :], q="sp")
                for hc in range(2):
                    po = B[nb % 8]
                    nb += 1
                    for cc in range(8):
                        p.mm(po, mrg[:, cc, sub * 128:(sub + 1) * 128], wo[:, cc, hc * 512:(hc + 1) * 512],
                             start=(cc == 0), stop=(cc == 7))
                    p.tt(o[:, hc * 512:(hc + 1) * 512], po, G1b[kind][:, hc * 512:(hc + 1) * 512], ALU.mult)
                p.tt(o, o, x, ALU.add)
                p.dma(last_dst[tok:tok + 128, :], o, q="pool", acc=True)
    p.barrier()


def stage_router(p, g, l, hT2, GW, DEST):
    with contextlib.ExitStack() as st:
        rwf = p.sb(st, "r_wf", [128, 8, 36])
        rwb = p.sb(st, "r_wb", [128, 8, 36], BF16)
        p.dma(rwf[:, :, 0:4], g.router_g[l].re("(c q) n -> q c n", q=128))
        p.dma(rwf[:, :, 4:36], g.router_e[l].re("(c q) n -> q c n", q=128))
        p.cp(rwb, rwf)
        RB = p.sb(st, "r_rb", [128, 36])
        p.dma(RB[:, 0:4], g.router_g_b[l].pb(128))
        p.dma(RB[:, 4:36], g.router_e_b[l].pb(128))
        LG = p.sb(st, "r_lg", [128, NT, 36])
        pl = [p.ps(st, "r_pl%d" % i, [128, 36]) for i in range(2)]
        for i in range(NT):
            pp = pl[i % 2]
            for c in range(8):
                p.mm(pp, hT2[:, c, i * 128:(i + 1) * 128], rwb[:, c, :], start=(c == 0), stop=(c == 7))
            p.cp(LG[:, i, :], pp, eng=("act" if i % 2 else "dve"))
        p.tt(LG, LG, RB.re("q (o e) -> q o e", o=1).bc([128, NT, 36]), ALU.add)
        lg = LG[:, :, 0:4]
        le = LG[:, :, 4:36].re("q i (g e) -> q i g e", e=8)
        mg = p.sb(st, "r_mg", [128, NT])
        oh = p.sb(st, "r_oh", [128, NT, 4])
        eg = p.sb(st, "r_eg", [128, NT, 4])
        pg = p.sb(st, "r_pg", [128, NT])
        tmp = p.sb(st, "r_tmp", [128, NT, 4, 8])
        les = p.sb(st, "r_les", [128, NT, 8])
        les2 = p.sb(st, "r_les2", [128, NT, 8])
        m1 = p.sb(st, "r_m1", [128, NT])
        m2 = p.sb(st, "r_m2", [128, NT])
        k1 = p.sb(st, "r_k1", [128, NT, 8])
        k2 = p.sb(st, "r_k2", [128, NT, 8])
        ex = p.sb(st, "r_ex", [128, NT, 8])

        def b3(t, n):
            return t.re("q (i o) -> q i o", o=1).bc([128, NT, n])
        p.red(mg, lg, ALU.max)
        p.tt(oh, lg, b3(mg, 4), ALU.is_equal)
        p.tt(eg, lg, b3(mg, 4), ALU.subtract)
        p.act(eg, eg, AF.Exp)
        p.red(pg, eg, ALU.add)
        p.recip(pg, pg)
        p.tt(tmp, le, oh.re("q i (g o) -> q i g o", o=1).bc([128, NT, 4, 8]), ALU.mult)
        p.red(les, tmp.re("q i g e -> q i e g"), ALU.add)
        p.red(m1, les, ALU.max)
        p.tt(k1, les, b3(m1, 8), ALU.is_equal)
        p.stt(les2, k1, -1e30, les, ALU.mult, ALU.add)
        p.red(m2, les2, ALU.max)
        p.tt(k2, les2, b3(m2, 8), ALU.is_equal)
        p.tt(m2, m2, m1, ALU.subtract)
        p.act(m2, m2, AF.Exp)
        p.ts(m2, m2, 1.0, ALU.add)
        p.recip(m2, m2)
        p.tt(GW[:, :, 0], m2, pg, ALU.mult)
        p.tt(GW[:, :, 1], pg, GW[:, :, 0], ALU.subtract)
        ohb = oh.re("q i (g o) -> q i g o", o=1).bc([128, NT, 4, 8])
        M1 = p.sb(st, "r_M1", [128, NT, 4, 8])
        M2 = p.sb(st, "r_M2", [128, NT, 4, 8])
        MM = p.sb(st, "r_MM", [128, NT, 4, 8])
        p.tt(M1, ohb, k1.re("q i (o e) -> q i o e", o=1).bc([128, NT, 4, 8]), ALU.mult)
        p.tt(M2, ohb, k2.re("q i (o e) -> q i o e", o=1).bc([128, NT, 4, 8]), ALU.mult)
        p.tt(MM, M1, M2, ALU.add)
        MMf = MM.re("q i g e -> q (i g e)")
        WI = p.sb(st, "r_WI", [128, NT, 32])
        TOT = p.sb(st, "r_TOT", [128, NT, 32])
        pw = [p.ps(st, "r_pw%d" % i, [128, 512]) for i in range(4)]
        NF = NT * 32
        for (c0, cn, k) in ((0, 512, 0), (512, NF - 512, 1)):
            p.mm(pw[k][:, :cn], g.MASK4[:, 0, 0, :], MMf[:, c0:c0 + cn])
            p.cp(WI.re("q i e -> q (i e)")[:, c0:c0 + cn], pw[k][:, :cn], eng="act")
            p.mm(pw[2 + k][:, :cn], g.onesf, MMf[:, c0:c0 + cn])
            p.cp(TOT.re("q i e -> q (i e)")[:, c0:c0 + cn], pw[2 + k][:, :cn], eng="dve")
        BASE = p.sb(st, "r_BASE", [128, NT, 32])
        p.memset(BASE[:, 0, :], 0.0)
        for i in range(1, NT):
            p.tt(BASE[:, i, :], BASE[:, i - 1, :], TOT[:, i - 1, :], ALU.add)
        p.tt(WI, WI, BASE, ALU.add)
        p.ts(TOT, WI, CAP - 0.5, ALU.is_ge)
        p.tt(WI, WI, g.EOFF.re("q (o e) -> q o e", o=1).bc([128, NT, 32]), ALU.add)
        p.ts(BASE, TOT, -1.0, ALU.mult, 1.0, ALU.add)
        p.tt(WI, WI, BASE, ALU.mult)
        p.stt(WI, TOT, float(NEXP * CAP), WI, ALU.mult, ALU.add)
        DF = p.sb(st, "r_DF", [128, NT, 2])
        for k, Mk in ((0, M1), (1, M2)):
            p.tt(Mk.re("q i g e -> q i (g e)"), Mk.re("q i g e -> q i (g e)"), WI, ALU.mult)
            p.red(DF[:, :, k], Mk.re("q i g e -> q i (g e)"), ALU.add)
        p.cp(DEST, DF)
    p.barrier()


def stage_moe(p, g, l, b, GW, DEST):
    NR = NEXP * CAP
    IOA = bass.IndirectOffsetOnAxis
    with contextlib.ExitStack() as st:
        G2b = []
        for kind, row in ((0, 2), (1, b)):
            t = p.sb(st, "e_g2b%d" % kind, [128, D])
            p.dma(t, g.MODR[l, row, G2:G2 + D].pb(128), q="act")
            G2b.append(t)
        with contextlib.ExitStack() as st2:
            hbt = [p.sb(st2, "e_hbt%d" % i, [128, D], BF16) for i in range(2)]
            for i in range(NT):
                hb = hbt[i % 2]
                p.dma(hb, g.H2T[i * 128:(i + 1) * 128, :], q="sp")
                for k in range(2):
                    idx = DEST[:, i, k:k + 1]
                    p.dma(g.XE, hb, q="pool", acc=True, extra_reads=[DEST],
                          fn=lambda e, hb=hb, idx=idx: e.indirect_dma_start(
                              out=A(g.XE), out_offset=IOA(ap=A(idx), axis=0), in_=A(hb), in_offset=None))
            w1b = [p.sb(st2, "e_w1b%d" % i, [128, 8, 512], BF16) for i in range(2)]
            w3b = [p.sb(st2, "e_w3b%d" % i, [128, 8, 512], BF16) for i in range(2)]
            w2b = [p.sb(st2, "e_w2b%d" % i, [128, 4, 1024], BF16) for i in range(2)]
            xe = [p.sb(st2, "e_xe%d" % i, [128, NSL, D], BF16) for i in range(2)]
            hTe = p.sb(st2, "e_hTe", [128, 8, CAP], BF16)
            sl = [p.sb(st2, "e_sl%d" % i, [128, 512]) for i in range(2)]
            hid = p.sb(st2, "e_hid", [128, 4, CAP], BF16)
            ye = [p.sb(st2, "e_ye%d" % i, [128, NSL, D]) for i in range(2)]
            B = [p.ps(st2, "e_pb%d" % i, [128, 512]) for i in range(6)]
            pt = [p.ps(st2, "e_pt%d" % i, [128, 8, 128], BF16) for i in range(2)]
            nb = 0
            ns = 0
            nsl = 0
            npt = 0
            CT = [(0, 512), (512, CAP - 512)] if CAP > 512 else [(0, CAP)]
            def load_w(e):
                k = e % 2
                p.dma(w1b[k], g.exp_w1[l, e].re("(c q) n -> q c n", q=128), q="pool")
                p.dma(w3b[k], g.exp_w3[l, e].re("(c q) n -> q c n", q=128), q="pool")
                p.dma(w2b[k], g.exp_w2[l, e].re("(c q) n -> q c n", q=128), q="pool")
                p.dma(xe[k], g.XE[e * CAP:(e + 1) * CAP, :].re("(j q) n -> q j n", q=128), q="sp")
            load_w(0)
            for e in range(NEXP):
                k = e % 2
                if e + 1 < NEXP:
                    load_w(e + 1)
                x_ = xe[k]
                for j in range(NSL):
                    ptt = pt[npt % 2]
                    npt += 1
                    for c in range(8):
                        p.tr(ptt[:, c, :], x_[:, j, c * 128:(c + 1) * 128], g.identb)
                    p.cp(hTe[:, :, j * 128:(j + 1) * 128], ptt, eng=("act" if j % 2 else "dve"))
                for ff in range(4):
                    for (t0, tn) in CT:
                        p1 = B[nb % 6]
                        p3 = B[(nb + 1) % 6]
                        nb += 2
                        for kc in range(8):
                            p.mm(p1[:, :tn], w1b[k][:, kc, ff * 128:(ff + 1) * 128], hTe[:, kc, t0:t0 + tn],
                                 start=(kc == 0), stop=(kc == 7))
                        for kc in range(8):
                            p.mm(p3[:, :tn], w3b[k][:, kc, ff * 128:(ff + 1) * 128], hTe[:, kc, t0:t0 + tn],
                                 start=(kc == 0), stop=(kc == 7))
                        s_ = sl[nsl % 2]
                        nsl += 1
                        p.act(s_[:, :tn], p1[:, :tn], AF.Silu)
                        p.tt(hid[:, ff, t0:t0 + tn], s_[:, :tn], p3[:, :tn], ALU.mult)
                y_ = ye[k]
                for j in range(NSL):
                    for hc in range(2):
                        po = B[nb % 6]
                        nb += 1
                        for ff in range(4):
                            p.mm(po, hid[:, ff, j * 128:(j + 1) * 128], w2b[k][:, ff, hc * 512:(hc + 1) * 512],
                                 start=(ff == 0), stop=(ff == 3))
                        p.cp(y_[:, j, hc * 512:(hc + 1) * 512], po, eng=("act" if (j + hc) % 2 else "dve"))
                p.dma(g.YE[e * CAP:(e + 1) * CAP, :].re("(j q) n -> q j n", q=128), y_, q="sp", acc=True)
            p.barrier()
        ya = [p.sb(st, "e_ya%d" % i, [128, D]) for i in range(2)]
        yb = [p.sb(st, "e_yb%d" % i, [128, D]) for i in range(2)]
        xt = [p.sb(st, "e_xt%d" % i, [128, D]) for i in range(2)]
        for i in range(NT):
            tok = i * 128
            kind = 0 if i < 2 else 1
            a_, b_, x_ = ya[i % 2], yb[i % 2], xt[i % 2]
            p.dma(x_, g.XS[b][tok:tok + 128, :], q="sp")
            for k, dst in ((0, a_), (1, b_)):
                p.memset(dst, 0.0)
                idx = DEST[:, i, k:k + 1]
                p.dma(dst, g.YE, q="pool", extra_reads=[DEST],
                      fn=lambda e, dst=dst, idx=idx: e.indirect_dma_start(
                          out=A(dst), out_offset=None, in_=A(g.YE), in_offset=IOA(ap=A(idx), axis=0)))
            p.ts(a_, a_, GW[:, i, 0:1], ALU.mult)
            p.stt(a_, b_, GW[:, i, 1:2], a_, ALU.mult, ALU.add)
            p.tt(a_, a_, G2b[kind], ALU.mult)
            p.tt(a_, a_, x_, ALU.add)
            p.dma(g.XS[b][tok:tok + 128, :], a_, q="act", acc=True)
    p.barrier()


def stage_final(p, g, b):
    with contextlib.ExitStack() as st:
        gt = p.sb(st, "f_gt", [128, D])
        p.dma(gt, g.final_g.pb(128))
        xts = [p.sb(st, "f_xt%d" % i, [128, D]) for i in range(2)]
        sqs = [p.sb(st, "f_sq%d" % i, [128, D]) for i in range(2)]
        sss = [p.sb(st, "f_ss%d" % i, [128, 2]) for i in range(2)]
        for i in range(TLAT // 128):
            xt, sq, ss = xts[i % 2], sqs[i % 2], sss[i % 2]
            p.dma(xt, g.XS[b][TCTX + i * 128:TCTX + (i + 1) * 128, :], q=("sp" if i % 2 == 0 else "act"))
            p.act(sq, xt, AF.Square)
            p.red(ss[:, 0:1], sq, ALU.add)
            p.ts(ss[:, 1:2], ss[:, 0:1], 1.0 / D, ALU.mult, EPS, ALU.add)
            p.act(ss[:, 1:2], ss[:, 1:2], AF.Sqrt)
            p.recip(ss[:, 1:2], ss[:, 1:2])
            p.stt(sq, xt, ss[:, 1:2], gt, ALU.mult, ALU.mult)
            p.dma(g.out[b, i * 128:(i + 1) * 128, :], sq, q="pool", acc=True)
    p.barrier()
```

```python
import contextlib
import numpy as np
import concourse.bass as bass
import concourse.mybir as mybir
from concourse.bass_utils import run_bass_kernel_spmd

F32 = mybir.dt.float32
BF16 = mybir.dt.bfloat16
I32 = mybir.dt.int32
AF = mybir.ActivationFunctionType
ALU = mybir.AluOpType
AX = mybir.AxisListType

D = 1024
DEPTH = 4
NB = 2
TCTX = 256
TLAT = 2048
T = TCTX + TLAT
NT = T // 128
NIN = 5504
RW0 = 1536
NEXP = 32
DFF = 512
CAP = 640
NSL = CAP // 128
CH = 128
NCH = T // CH
EPS = 1e-6
GN_EPS = 64e-5
TT = [(0, 512), (512, 512), (1024, 512), (1536, 512), (2048, 256)]


class Tl:
    def __init__(self, ap, name=""):
        self.ap = ap
        self.name = name
        self.lw = {}
        self.rd = {}

    def __getitem__(self, k):
        return Vw(self, self.ap[k])

    def re(self, s, **kw):
        return Vw(self, self.ap.rearrange(s, **kw))

    def bc(self, shape):
        return Vw(self, self.ap.to_broadcast(list(shape)))

    def pb(self, n):
        return Vw(self, self.ap.partition_broadcast(n))


class Vw:
    def __init__(self, t, ap):
        self.t = t
        self.ap = ap

    def __getitem__(self, k):
        return Vw(self.t, self.ap[k])

    def re(self, s, **kw):
        return Vw(self.t, self.ap.rearrange(s, **kw))

    def bc(self, shape):
        return Vw(self.t, self.ap.to_broadcast(list(shape)))

    def pb(self, n):
        return Vw(self.t, self.ap.partition_broadcast(n))


def A(x):
    return x.ap if isinstance(x, (Tl, Vw)) else x


def TT_(x):
    if isinstance(x, Tl):
        return x
    if isinstance(x, Vw):
        return x.t
    return None


class P:
    KD = 8

    def __init__(self, nc):
        self.nc = nc
        self.es = contextlib.ExitStack()
        self.eng = {"pe": nc.tensor, "act": nc.scalar, "dve": nc.vector, "pool": nc.gpsimd, "sp": nc.sync}
        self.sem = {}
        self.cur = {}
        for e in ("pe", "act", "dve", "pool"):
            self.sem[e] = self.es.enter_context(nc.semaphore("s_" + e))
            self.cur[e] = 0
        self.dcount = {}
        for q in ("sp", "pool", "act"):
            self.dcount[q] = 0
            for s in range(self.KD):
                k = ("d", q, s)
                self.sem[k] = self.es.enter_context(nc.semaphore("d_%s_%d" % (q, s)))
                self.cur[k] = 0
        self.waited = {e: {} for e in self.eng}
        self.nins = 0

    def sb(self, stack, name, shape, dt=F32):
        self.uid = getattr(self, "uid", 0) + 1
        name = "%s_%d" % (name, self.uid)
        h = stack.enter_context(self.nc.sbuf_tensor(name, list(shape), dt))
        return Tl(h[:], name)

    def ps(self, stack, name, shape, dt=F32):
        self.uid = getattr(self, "uid", 0) + 1
        name = "%s_%d" % (name, self.uid)
        h = stack.enter_context(self.nc.psum_tensor(name, list(shape), dt))
        return Tl(h[:], name)

    def dram(self, name, shape, dt=F32, kind="Internal"):
        h = self.nc.dram_tensor(name, list(shape), dt, kind=kind)
        return Tl(h.ap(), name)

    def _deps(self, eng, reads, writes, acc=False):
        need = {}

        def add(tok):
            if tok is not None:
                k, v = tok
                if need.get(k, 0) < v:
                    need[k] = v
        for x in reads:
            t = TT_(x)
            if t is not None:
                for k, v in t.lw.items():
                    add((k, v))
        for x in writes:
            t = TT_(x)
            if t is not None:
                if not acc:
                    for k, v in t.lw.items():
                        add((k, v))
                for k, v in t.rd.items():
                    add((k, v))
        out = []
        w = self.waited[eng]
        for k, v in need.items():
            if k == eng and eng == "pe":
                continue
            if w.get(k, 0) >= v:
                continue
            w[k] = v
            out.append((k, v))
        return out

    def _commit(self, tok, reads, writes, acc=False):
        k, v = tok
        for x in reads:
            t = TT_(x)
            if t is not None and t.rd.get(k, 0) < v:
                t.rd[k] = v
        for x in writes:
            t = TT_(x)
            if t is not None:
                if acc:
                    if t.lw.get(k, 0) < v:
                        t.lw[k] = v
                else:
                    t.lw = {k: v}
                    t.rd = {}

    def op(self, eng, fn, reads, writes):
        e = self.eng[eng]
        for k, v in self._deps(eng, reads, writes):
            e.wait_ge(self.sem[k], v)
        ins = fn(e)
        self.cur[eng] += 1
        ins.then_inc(self.sem[eng], 1)
        self._commit((eng, self.cur[eng]), reads, writes)
        self.nins += 1

    def dma(self, out, in_, q="sp", acc=False, fn=None, extra_reads=(), **kw):
        e = self.eng[q]
        j = self.dcount[q]
        self.dcount[q] = j + 1
        slot, gen = j % self.KD, j // self.KD
        key = ("d", q, slot)
        rds = [in_] + list(extra_reads)
        deps = self._deps(q, rds, [out], acc=acc)
        if gen > 0 and self.waited[q].get(key, 0) < 16 * gen:
            self.waited[q][key] = 16 * gen
            deps.append((key, 16 * gen))
        for k, v in deps:
            e.wait_ge(self.sem[k], v)
        if fn is None:
            ins = e.dma_start(out=A(out), in_=A(in_), **kw)
        else:
            ins = fn(e)
        ins.then_inc(self.sem[key], 16)
        self.cur[key] = 16 * (gen + 1)
        self._commit((key, 16 * (gen + 1)), rds, [out], acc=acc)
        self.nins += 1

    def barrier(self, engines=None):
        for en, e in self.eng.items():
            if engines is not None and en not in engines:
                continue
            w = self.waited[en]
            for k, v in self.cur.items():
                if v == 0 or (k == en and en == "pe"):
                    continue
                if w.get(k, 0) >= v:
                    continue
                w[k] = v
                e.wait_ge(self.sem[k], v)

    def mm(self, out, lhsT, rhs, start=True, stop=True):
        self.op("pe", lambda e: e.matmul(A(out), A(lhsT), A(rhs), start=start, stop=stop), [lhsT, rhs], [out])

    def tr(self, out, in_, ident):
        self.op("pe", lambda e: e.transpose(A(out), A(in_), A(ident)), [in_, ident], [out])

    def act(self, out, in_, func, bias=None, scale=None, accum=None, extra_reads=()):
        kw = {}
        rd = [in_] + list(extra_reads)
        if bias is not None:
            kw["bias"] = A(bias)
            rd.append(bias)
        if scale is not None:
            kw["scale"] = A(scale)
            rd.append(scale)
        wr = [out]
        if accum is not None:
            kw["accum_out"] = A(accum)
            wr.append(accum)
        self.op("act", lambda e: e.activation(out=A(out), in_=A(in_), func=func, **kw), rd, wr)

    def tt(self, out, in0, in1, op, eng="dve"):
        self.op(eng, lambda e: e.tensor_tensor(out=A(out), in0=A(in0), in1=A(in1), op=op), [in0, in1], [out])

    def ts(self, out, in0, s1, op0, s2=None, op1=None, eng="dve", accum=None):
        rd = [in0, s1, s2]
        kw = {}
        if op1 is not None:
            kw["op1"] = op1
        wr = [out]
        if accum is not None:
            kw["accum_out"] = A(accum)
            wr.append(accum)
        self.op(eng, lambda e: e.tensor_scalar(out=A(out), in0=A(in0), scalar1=A(s1), scalar2=A(s2), op0=op0, **kw), rd, wr)

    def stt(self, out, in0, scalar, in1, op0, op1, eng="dve"):
        self.op(eng, lambda e: e.scalar_tensor_tensor(out=A(out), in0=A(in0), scalar=A(scalar), in1=A(in1), op0=op0, op1=op1),
                [in0, scalar, in1], [out])

    def cp(self, out, in_, eng="dve"):
        if eng == "act":
            self.op("act", lambda e: e.copy(out=A(out), in_=A(in_)), [in_], [out])
        else:
            self.op(eng, lambda e: e.tensor_copy(out=A(out), in_=A(in_)), [in_], [out])

    def recip(self, out, in_):
        self.op("dve", lambda e: e.reciprocal(out=A(out), in_=A(in_)), [in_], [out])

    def memset(self, out, val, eng="dve"):
        self.op(eng, lambda e: e.memset(A(out), val), [], [out])

    def red(self, out, in_, op, axis=AX.X, eng="dve"):
        self.op(eng, lambda e: e.tensor_reduce(out=A(out), in_=A(in_), axis=axis, op=op), [in_], [out])


class G:
    pass


SH1, SC1, G1, SH2, SC2, G2 = [i * D for i in range(6)]
C_MU, C_CONV = 0, 15
C_KK, C_KA, C_RK, C_W0, C_A0, C_LG, C_LB = 0, 8, 16, 24, 40, 56, 64
C2_KK, C2_KA, C2_RK, C2_W0, C2_A0, C2_LG, C2_LB = 27, 31, 35, 39, 47, 55, 59
NP128 = 63


def stage_params(p, g):
    nc = p.nc
    with contextlib.ExitStack() as st:
        crow = p.sb(st, "crow", [3, D])
        p.dma(crow, g.c3)
        srow = p.sb(st, "srow", [3, D])
        p.act(srow, crow, AF.Silu)
        sT = p.sb(st, "sT", [128, 8, 3], BF16)
        pst = p.ps(st, "pst", [128, 8, 4])
        for c in range(8):
            p.tr(pst[:, c, 0:3], srow[:, c * 128:(c + 1) * 128], g.ident[0:3, 0:3])
        p.cp(sT, pst[:, :, 0:3])
        wst = [p.sb(st, "wst%d" % i, [128, 8, 512], BF16) for i in range(3)]
        bm = p.sb(st, "bm", [3, 6 * D])
        mrow = p.sb(st, "mrow", [3, 6 * D])
        pm = [p.ps(st, "pm%d" % i, [3, 512]) for i in range(2)]
        k = 0
        for l in range(DEPTH):
            p.dma(bm, g.b_mod[l].pb(3))
            for cg in range(12):
                ws = wst[k % 3]
                p.dma(ws, g.w_mod[l].re("(c q) n -> q c n", q=128)[:, :, cg * 512:(cg + 1) * 512], q="pool")
                pp = pm[k % 2]
                for kc in range(8):
                    p.mm(pp, sT[:, kc, :], ws[:, kc, :], start=(kc == 0), stop=(kc == 7))
                p.tt(mrow[:, cg * 512:(cg + 1) * 512], pp, bm[:, cg * 512:(cg + 1) * 512], ALU.add)
                k += 1
            p.dma(g.MODR[l], mrow, q="sp")
        pr128 = p.sb(st, "pr128", [NP128, 128])
        pr64 = p.sb(st, "pr64", [72, 64])
        pp128 = p.ps(st, "pp128", [128, 64])
        pp64 = p.ps(st, "pp64", [64, 72])
        for l in range(DEPTH):
            p.dma(pr128[0:15, :], g.shift_mu[l].re("(c q) -> c q", q=128))
            p.dma(pr128[15:27, :], g.conv_w[l].re("j (c q) -> (j c) q", q=128))
            p.dma(pr64[C_KK:C_KK + 8, :], g.k_k[l].re("(h k) -> h k", k=64))
            p.dma(pr64[C_KA:C_KA + 8, :], g.k_a[l].re("(h k) -> h k", k=64))
            p.dma(pr64[C_RK:C_RK + 8, :], g.r_k[l])
            p.dma(pr64[C_W0:C_W0 + 16, :], g.w0[l].re("d (h k) -> (d h) k", k=64))
            p.dma(pr64[C_A0:C_A0 + 16, :], g.a0[l].re("d (h k) -> (d h) k", k=64))
            p.dma(pr64[C_LG:C_LG + 8, :], g.lnx_g[l].re("(h k) -> h k", k=64))
            p.dma(pr64[C_LB:C_LB + 8, :], g.lnx_b[l].re("(h k) -> h k", k=64))
            p.dma(pr128[C2_KK:C2_KK + 4, :], g.k_k[l].re("(c q) -> c q", q=128))
            p.dma(pr128[C2_KA:C2_KA + 4, :], g.k_a[l].re("(c q) -> c q", q=128))
            p.dma(pr128[C2_RK:C2_RK + 4, :], g.r_k[l].re("(c a) k -> c (a k)", a=2))
            p.dma(pr128[C2_W0:C2_W0 + 8, :], g.w0[l].re("d (c q) -> (d c) q", q=128))
            p.dma(pr128[C2_A0:C2_A0 + 8, :], g.a0[l].re("d (c q) -> (d c) q", q=128))
            p.dma(pr128[C2_LG:C2_LG + 4, :], g.lnx_g[l].re("(c q) -> c q", q=128))
            p.dma(pr128[C2_LB:C2_LB + 4, :], g.lnx_b[l].re("(c q) -> c q", q=128))
            p.tr(pp128[:, 0:NP128], pr128, g.ident[0:NP128, 0:NP128])
            p.cp(g.P128T[:, l, :], pp128[:, 0:NP128])
            p.tr(pp64, pr64, g.ident[0:72, 0:72])
            p.cp(g.P64T[:, l, :], pp64)
        p.ts(g.OMM, g.P128T[:, :, C_MU:C_MU + 15], -1.0, ALU.mult, 1.0, ALU.add)
        p.ts(g.HMU, g.P128T[:, :, C_MU:C_MU + 15], 0.5, ALU.mult)
        p.ts(g.OMKA, g.P128T[:, :, C2_KA:C2_KA + 4], -1.0, ALU.mult, 1.0, ALU.add)
    p.barrier()


def stage_norm(p, g, src, l, b, second, hT, hTf=None):
    SHc, SCc = (SH2, SC2) if second else (SH1, SC1)
    ng = g.norm2_g if second else g.norm1_g
    with contextlib.ExitStack() as st:
        gt = p.sb(st, "gt", [128, D])
        p.dma(gt, ng[l].pb(128))
        Ab, Bb = [], []
        for kind, row in ((0, 2), (1, b)):
            sc = p.sb(st, "scb%d" % kind, [128, D])
            p.dma(sc, g.MODR[l, row, SCc:SCc + D].pb(128))
            a = p.sb(st, "Ab%d" % kind, [128, D])
            p.stt(a, sc, 1.0, gt, ALU.add, ALU.mult)
            bb = p.sb(st, "Bb%d" % kind, [128, D])
            p.dma(bb, g.MODR[l, row, SHc:SHc + D].pb(128))
            Ab.append(a)
            Bb.append(bb)
        xall = [p.sb(st, "xa%d" % i, [128, D]) for i in range(NT)]
        sqs = [p.sb(st, "sq%d" % i, [128, D]) for i in range(2)]
        hns = [p.sb(st, "hn%d" % i, [128, D]) for i in range(2)]
        hbs = [p.sb(st, "hb%d" % i, [128, D], BF16) for i in range(2)]
        ssall = p.sb(st, "ssall", [128, NT])
        rs = p.sb(st, "rsall", [128, NT])
        ptr = [p.ps(st, "ptr%d" % i, [128, 8, 128], BF16) for i in range(2)]
        for i in range(NT):
            p.dma(xall[i], src[i * 128:(i + 1) * 128, :], q=("sp" if i % 2 == 0 else "act"))
            p.act(sqs[i % 2], xall[i], AF.Square)
            p.red(ssall[:, i:i + 1], sqs[i % 2], ALU.add)
        p.ts(rs, ssall, 1.0 / D, ALU.mult, EPS, ALU.add)
        p.act(rs, rs, AF.Sqrt)
        p.recip(rs, rs)
        for i in range(NT):
            kind = 0 if i < 2 else 1
            hn, hb, pt = hns[i % 2], hbs[i % 2], ptr[i % 2]
            p.stt(hn, xall[i], rs[:, i:i + 1], Ab[kind], ALU.mult, ALU.mult)
            p.tt(hb, hn, Bb[kind], ALU.add, eng="pool")
            if second:
                p.dma(g.H2T[i * 128:(i + 1) * 128, :], hb, q="pool", acc=True)
            for c in range(8):
                p.tr(pt[:, c, :], hb[:, c * 128:(c + 1) * 128], g.identb)
            p.cp(hT[:, :, i * 128:(i + 1) * 128], pt, eng="act")


def stage_proj(p, g, hT, l, b):
    groups = [(c0, min(512, NIN - c0)) for c0 in range(0, NIN, 512)]
    with contextlib.ExitStack() as st:
        wbf = [p.sb(st, "pwbf%d" % i, [128, 8, 512], BF16) for i in range(2)]

        def loadg(gi):
            c0, w = groups[gi]
            p.dma(wbf[gi % 2][:, :, :w], g.w_in[l].re("(c q) n -> q c n", q=128)[:, :, c0:c0 + w], q="pool")
        loadg(0)
        ot = [p.sb(st, "pot%d" % i, [128, T]) for i in range(2)]
        zt = [p.sb(st, "pzt%d" % i, [128, T]) for i in range(2)]
        pss = [p.ps(st, "pps%d" % i, [128, 512]) for i in range(4)]
        k = 0
        kk = 0
        for gi, (c0, w) in enumerate(groups):
            wb = wbf[gi % 2]
            if gi + 1 < len(groups):
                loadg(gi + 1)
            for cc in range(w // 128):
                col = c0 + cc * 128
                chunk = col // 128
                o = ot[k % 2]
                for (t0, tn) in TT:
                    ps = pss[kk % 4]
                    for kc in range(8):
                        p.mm(ps[:, :tn], wb[:, kc, cc * 128:(cc + 1) * 128], hT[:, kc, t0:t0 + tn],
                             start=(kc == 0), stop=(kc == 7))
                    if chunk >= 27:
                        p.act(o[:, t0:t0 + tn], ps[:, :tn], AF.Sigmoid)
                    elif kk % 2 == 0:
                        p.cp(o[:, t0:t0 + tn], ps[:, :tn], eng="act")
                    else:
                        p.cp(o[:, t0:t0 + tn], ps[:, :tn], eng="dve")
                    kk += 1
                if 12 <= chunk < 27:
                    j = chunk - 12
                    z = zt[k % 2]
                    om = g.OMM[:, l, j:j + 1]
                    hm = g.HMU[:, l, j:j + 1]
                    p.ts(z, o, om, ALU.mult)
                    for (a0, a1) in ((0, TCTX), (TCTX, T)):
                        p.stt(z[:, a0 + 1:a1], o[:, a0:a1 - 1], hm, z[:, a0 + 1:a1], ALU.mult, ALU.add)
                        p.stt(z[:, a0:a1 - 1], o[:, a0 + 1:a1], hm, z[:, a0:a1 - 1], ALU.mult, ALU.add)
                    o = z
                p.dma(g.ZF[col:col + 128, :], o, q="sp", acc=True)
                k += 1


WEIGHTS = [
    ("w_mod", [DEPTH, D, 6 * D]), ("b_mod", [DEPTH, 6 * D]), ("norm1_g", [DEPTH, D]), ("norm2_g", [DEPTH, D]),
    ("w_in", [DEPTH, D, NIN]), ("shift_mu", [DEPTH, 1920]), ("conv_w", [DEPTH, 3, 512]),
    ("w_up", [DEPTH, 2, 64, 512]), ("w0", [DEPTH, 2, 512]), ("a_up", [DEPTH, 2, 64, 512]), ("a0", [DEPTH, 2, 512]),
    ("g_up", [DEPTH, 128, 512]), ("k_k", [DEPTH, 512]), ("k_a", [DEPTH, 512]), ("r_k", [DEPTH, 8, 64]),
    ("lnx_g", [DEPTH, 512]), ("lnx_b", [DEPTH, 512]), ("w_a_out", [DEPTH, 512, D]), ("w_b_out", [DEPTH, 512, D]),
    ("w_o", [DEPTH, D, D]), ("router_g", [DEPTH, D, 4]), ("router_g_b", [DEPTH, 4]),
    ("router_e", [DEPTH, D, NEXP]), ("router_e_b", [DEPTH, NEXP]),
    ("exp_w1", [DEPTH, NEXP, D, DFF]), ("exp_w3", [DEPTH, NEXP, D, DFF]), ("exp_w2", [DEPTH, NEXP, DFF, D]),
    ("final_g", [D]),
]


def build(stages="all", dbg=()):
    nc = bass.Bass("TRN2", target_bir_lowering=False)
    p = P(nc)
    g = G()
    g.p = p

    def inp(name, shape):
        return Tl(nc.dram_tensor(name, list(shape), F32, kind="ExternalInput").ap(), name)
    g.xin = inp("xin", [NB, T, D])
    g.c3 = inp("c3", [3, D])
    g.idin = inp("idin", [128, 128])
    g.mskin = inp("mskin", [128, 2, 4, 128])
    g.eoffin = inp("eoffin", [128, NEXP])
    for name, shape in WEIGHTS:
        setattr(g, name, inp(name, shape))
    g.out = Tl(nc.dram_tensor("out", [NB, TLAT, D], F32, kind="ExternalOutput").ap(), "out")
    g.MODR = p.dram("MODR", [DEPTH, 3, 6 * D])
    g.ZF = p.dram("ZF", [NIN, T])
    g.XS = [p.dram("XS%d" % b, [T, D]) for b in range(NB)]
    g.SCF = p.dram("SCF", [2, 512, NCH, 4, CH], BF16)
    g.SCT = p.dram("SCT", [2, NCH, CH, 2, 512], BF16)
    g.VTD = p.dram("VTD", [NCH, CH, 512], BF16)
    g.WCD = p.dram("WCD", [512, 2, NCH])
    g.RO = p.dram("RO", [2, 512, T])
    g.YD = p.dram("YD", [2, 512, T])
    g.H2T = p.dram("H2T", [T, D], BF16)
    g.XE = p.dram("XE", [NEXP * CAP + 1, D], BF16)
    g.YE = p.dram("YE", [NEXP * CAP + 1, D])
    g.YEZ = p.dram("YEZ", [1, D])
    dbg_out = {}
    for name, shape in dbg:
        dbg_out[name] = Tl(nc.dram_tensor("dbg_" + name, list(shape), F32, kind="ExternalOutput").ap(), name)
    g.dbg = dbg_out
    top = p.es
    g.ident = p.sb(top, "ident", [128, 128])
    g.identb = p.sb(top, "identb", [128, 128], BF16)
    p.dma(g.ident, g.idin)
    p.cp(g.identb, g.ident)
    g.P128T = p.sb(top, "P128T", [128, DEPTH, NP128])
    g.P64T = p.sb(top, "P64T", [64, DEPTH, 72])
    g.OMM = p.sb(top, "OMM", [128, DEPTH, 15])
    g.HMU = p.sb(top, "HMU", [128, DEPTH, 15])
    g.OMKA = p.sb(top, "OMKA", [128, DEPTH, 4])
    g.MASK4 = p.sb(top, "MASK4", [128, 2, 4, 128])
    p.dma(g.MASK4, g.mskin)
    g.ones64 = p.sb(top, "ones64", [64, 64])
    p.memset(g.ones64, 1.0)
    g.onesf = p.sb(top, "onesf", [128, 128])
    p.memset(g.onesf, 1.0)
    g.EOFF = p.sb(top, "EOFF", [128, NEXP])
    p.dma(g.EOFF, g.eoffin)
    with contextlib.ExitStack() as stz:
        zt = p.sb(stz, "zrow", [1, D])
        p.memset(zt, 0.0)
        p.dma(g.YE[NEXP * CAP:NEXP * CAP + 1, :], zt, q="pool")
        p.barrier()
    g.ones2 = p.sb(top, "ones2", [128, 128])
    p.memset(g.ones2, 0.0)
    p.memset(g.ones2[0:64, 0:64], 1.0)
    p.memset(g.ones2[64:128, 64:128], 1.0)
    g.RM = p.sb(top, "RM", [128, TH])
    p.memset(g.RM, 1.0)
    p.memset(g.RM.re("k (c t) -> k c t", t=CH)[:, :, 0:1], 0.0)

    stage_params(p, g)
    nl = DEPTH if stages == "all" else stages[0]
    nb = NB if stages == "all" else stages[1]
    upto = "z" if stages == "all" else stages[2]
    for b in range(nb):
        for l in range(nl):
            src = g.xin[b] if l == 0 else g.XS[b]
            with contextlib.ExitStack() as st:
                hT = p.sb(st, "hT", [128, 8, T], BF16)
                stage_norm(p, g, src, l, b, False, hT)
                if upto == "A":
                    if "hT" in g.dbg:
                        with contextlib.ExitStack() as s2:
                            tmp = p.sb(s2, "dbgtmp", [128, 8, T])
                            p.cp(tmp, hT)
                            p.dma(g.dbg["hT"], tmp)
                            p.barrier()
                    p.barrier()
                    continue
                stage_proj(p, g, hT, l, b)
                p.barrier()
            if upto == "B":
                continue
            with contextlib.ExitStack() as st:
                CO = p.sb(st, "CO", [128, 4, T], BF16)
                stage_conv(p, g, l, b, CO)
                stage_prep(p, g, l, b)
                if upto == "D":
                    continue
                stage_scan(p, g, l, b)
                if upto == "E":
                    continue
                stage_mix(p, g, l, b, CO, src, g.XS[b])
            if upto == "G":
                continue
            with contextlib.ExitStack() as st:
                GW = p.sb(st, "GW", [128, NT, 2])
                DEST = p.sb(st, "DEST", [128, NT, 2], I32)
                with contextlib.ExitStack() as st2:
                    hT = p.sb(st2, "hT2", [128, 8, T], BF16)
                    stage_norm(p, g, g.XS[b], l, b, True, hT)
                    stage_router(p, g, l, hT, GW, DEST)
                stage_moe(p, g, l, b, GW, DEST)
        if upto == "z":
            stage_final(p, g, b)
    if "ZF" in g.dbg:
        p.barrier()
        with contextlib.ExitStack() as s2:
            tmp = p.sb(s2, "dbgtmp", [128, T])
            for c in range(NIN // 128):
                p.dma(tmp, g.ZF[c * 128:(c + 1) * 128, :])
                p.dma(g.dbg["ZF"][c * 128:(c + 1) * 128, :], tmp)
            p.barrier()
    for nm, t in (("YD", g.YD.re("d n t -> (d n) t")), ("RO", g.RO.re("q n t -> (q n) t")), ("XS0", g.XS[0])):
        if nm in g.dbg:
            with contextlib.ExitStack() as s2:
                n, w = t.ap.shape
                tmp = p.sb(s2, "dbgtmp3", [128, w])
                for c in range(n // 128):
                    p.dma(tmp, t[c * 128:(c + 1) * 128, :])
                    p.dma(g.dbg[nm][c * 128:(c + 1) * 128, :], tmp)
                p.barrier()
    if "MODR" in g.dbg:
        with contextlib.ExitStack() as s2:
            tmp = p.sb(s2, "dbgtmp2", [12, 6 * D])
            p.dma(tmp, g.MODR.re("l r n -> (l r) n"))
            p.dma(g.dbg["MODR"], tmp)
            p.barrier()
    p.barrier()
    p.es.close()
    return nc, p


def make_in_maps(inputs, ncores=8):
    idm = np.eye(128, dtype=np.float32)
    ii = np.arange(128)
    us = (ii[None, :] > ii[:, None]).astype(np.float32)
    ui = (ii[None, :] >= ii[:, None]).astype(np.float32)
    msk = np.ascontiguousarray(np.stack([np.stack([us, ui, us, ui], 0), np.stack([us.T, ui.T, us.T, ui.T], 0)], 0).transpose(2, 0, 1, 3))
    eoff = np.ascontiguousarray(np.broadcast_to((np.arange(NEXP, dtype=np.float32) * CAP)[None, :], (128, NEXP)))
    maps = []
    for core in range(ncores):
        b0 = core * NB
        xin = np.concatenate([inputs["ctx"][b0:b0 + NB], inputs["x"][b0:b0 + NB]], axis=1)
        c3 = np.concatenate([inputs["c"][b0:b0 + NB], inputs["c_ctx"][None, :]], axis=0)
        m = {"xin": np.ascontiguousarray(xin, dtype=np.float32), "c3": np.ascontiguousarray(c3, dtype=np.float32), "idin": idm,
             "mskin": msk, "eoffin": eoff}
        for name, _ in WEIGHTS:
            m[name] = np.ascontiguousarray(inputs[name], dtype=np.float32)
        maps.append(m)
    return maps


def kernel(**inputs):
    inputs = {k: np.asarray(v) for k, v in inputs.items()}
    nc, p = build("all")
    maps = make_in_maps(inputs, 8)
    res = run_bass_kernel_spmd(nc, maps, core_ids=list(range(8)))
    return np.concatenate([r["out"] for r in res.results], axis=0).astype(np.float32)


def stage_conv(p, g, l, b, CO):
    with contextlib.ExitStack() as st:
        bgt = [p.sb(st, "cbg%d" % i, [128, T]) for i in range(2)]
        cgt = [p.sb(st, "ccg%d" % i, [128, T]) for i in range(2)]
        hat = [p.sb(st, "cha%d" % i, [128, T]) for i in range(2)]
        ut = [p.sb(st, "cu%d" % i, [128, T]) for i in range(2)]
        ott = [p.sb(st, "co%d" % i, [128, T]) for i in range(2)]
        for j in range(4):
            bg, cg, ha, u, o = bgt[j % 2], cgt[j % 2], hat[j % 2], ut[j % 2], ott[j % 2]
            p.dma(bg, g.ZF[j * 128:(j + 1) * 128, :], q="sp")
            p.dma(cg, g.ZF[512 + j * 128:512 + (j + 1) * 128, :], q="act")
            p.dma(ha, g.ZF[1024 + j * 128:1024 + (j + 1) * 128, :], q="sp")
            w0 = g.P128T[:, l, C_CONV + 0 * 4 + j:C_CONV + 0 * 4 + j + 1]
            w1 = g.P128T[:, l, C_CONV + 1 * 4 + j:C_CONV + 1 * 4 + j + 1]
            w2 = g.P128T[:, l, C_CONV + 2 * 4 + j:C_CONV + 2 * 4 + j + 1]
            p.tt(u, cg, ha, ALU.mult)
            p.ts(o, u, w1, ALU.mult)
            p.stt(o[:, 1:TCTX], u[:, 0:TCTX - 1], w0, o[:, 1:TCTX], ALU.mult, ALU.add)
            p.stt(o[:, 0:TCTX - 1], u[:, 1:TCTX], w2, o[:, 0:TCTX - 1], ALU.mult, ALU.add)
            if j < 2:
                ug = u[:, TCTX:T].re("q (r w) -> q r w", w=64)
                og = o[:, TCTX:T].re("q (r w) -> q r w", w=64)
                p.stt(og[:, :, 1:64], ug[:, :, 0:63], w0, og[:, :, 1:64], ALU.mult, ALU.add)
                p.stt(og[:, :, 0:63], ug[:, :, 1:64], w2, og[:, :, 0:63], ALU.mult, ALU.add)
            else:
                p.stt(o[:, TCTX + 64:T], u[:, TCTX:T - 64], w0, o[:, TCTX + 64:T], ALU.mult, ALU.add)
                p.stt(o[:, TCTX:T - 64], u[:, TCTX + 64:T], w2, o[:, TCTX:T - 64], ALU.mult, ALU.add)
            p.tt(CO[:, j, :], o, bg, ALU.mult)
    p.barrier()


TH = 1152
THT = [(0, 512), (512, 512), (1024, 128)]
NCH2 = TH // CH


def stage_prep(p, g, l, b):
    with contextlib.ExitStack() as st:
        tzw = [p.sb(st, "tzw%d" % d, [64, T], BF16) for d in range(2)]
        zab = [p.sb(st, "zab%d" % d, [64, T], BF16) for d in range(2)]
        sgz = p.sb(st, "sgz", [128, T], BF16)
        with contextlib.ExitStack() as st0:
            tmpf = p.sb(st0, "dtmpf", [128, T])
            for d in range(2):
                p.dma(tmpf[0:64, :], g.ZF[RW0 + 1536 + d * 64:RW0 + 1536 + (d + 1) * 64, :])
                p.act(tzw[d], tmpf[0:64, :], AF.Tanh)
                p.dma(tmpf[0:64, :], g.ZF[RW0 + 1664 + d * 64:RW0 + 1664 + (d + 1) * 64, :])
                p.cp(zab[d], tmpf[0:64, :], eng="act")
            p.dma(tmpf, g.ZF[RW0 + 1792:RW0 + 1920, :])
            p.act(sgz, tmpf, AF.Sigmoid)
            p.barrier()
        wupb = p.sb(st, "wupb", [64, 2, 512], BF16)
        aupb = p.sb(st, "aupb", [64, 2, 512], BF16)
        gupb = p.sb(st, "gupb", [128, 512], BF16)
        p.dma(wupb, g.w_up[l].re("d r n -> r d n"), q="pool")
        p.dma(aupb, g.a_up[l].re("d r n -> r d n"), q="pool")
        p.dma(gupb, g.g_up[l], q="pool")
        rts = [p.sb(st, "d_r%d" % i, [128, TH]) for i in range(2)]
        kts = [p.sb(st, "d_k%d" % i, [128, TH]) for i in range(2)]
        vts = [p.sb(st, "d_v%d" % i, [128, TH]) for i in range(2)]
        kks = [p.sb(st, "d_kk%d" % i, [128, TH]) for i in range(2)]
        Xss = [[p.sb(st, "d_x%d_%d" % (i, q), [128, TH]) for i in range(8)] for q in range(2)]
        obs = [{n: p.sb(st, "d_ob%d_" % q + n, [128, TH], BF16) for n in ("kk", "r", "kh", "bh", "kp", "bp", "v")} for q in range(2)]
        tsts = [[p.sb(st, "d_tst%d" % i, [128, NCH2, 128], BF16) for i in range(3)]] * 2
        wcs = [p.sb(st, "d_wc%d" % q, [128, NCH2]) for q in range(2)]
        ps = [p.ps(st, "d_ps%d" % i, [128, 512]) for i in range(3)]
        pst = [p.ps(st, "d_pst%d" % i, [128, 16, 128], BF16) for i in range(2)]
        npst = 0
        nps = 0

        def pk(col):
            return g.P128T[:, l, col:col + 1]

        def tposed(src_b, dst_dram, q, k, tst):
            nonlocal npst
            pt = pst[npst % 2]
            npst += 1
            stg = tst[k]
            for c in range(NCH2):
                p.tr(pt[:, c, :], src_b[:, c * CH:(c + 1) * CH], g.identb)
            p.cp(stg, pt[:, 0:NCH2, :], eng=("act" if k % 2 else "dve"))
            p.dma(dst_dram, stg, q=q, acc=True)

        def body(pr, hf):
            nonlocal nps, npst
            r0 = pr * 128
            ob, tst, wc = obs[hf], tsts[hf], wcs[hf]
            tb = hf * TH
            cb = hf * NCH2
            rt, kt, vt, kk, X = rts[hf], kts[hf], vts[hf], kks[hf], Xss[hf]
            p.dma(rt, g.ZF[RW0 + r0:RW0 + r0 + 128, tb:tb + TH], q="act")
            yield
            p.dma(kt, g.ZF[RW0 + 512 + r0:RW0 + 512 + r0 + 128, tb:tb + TH], q="act")
            yield
            p.dma(vt, g.ZF[RW0 + 1024 + r0:RW0 + 1024 + r0 + 128, tb:tb + TH], q="act")
            yield
            p.ts(X[0], kt, pk(C2_KK + pr), ALU.mult)
            yield
            p.act(X[1], X[0], AF.Square)
            yield
            for (t0, tn) in THT:
                pp = ps[nps % 3]
                nps += 1
                p.mm(pp[:, :tn], g.ones2, X[1][:, t0:t0 + tn])
                yield
                p.ts(X[2][:, t0:t0 + tn], pp[:, :tn], 1e-12, ALU.add)
                yield
            p.act(X[2], X[2], AF.Sqrt)
            yield
            p.recip(X[2], X[2])
            yield
            p.tt(kk, X[0], X[2], ALU.mult)
            yield
            p.cp(ob["v"], vt)
            yield
            tposed(ob["v"], g.VTD[cb:cb + NCH2, :, r0:r0 + 128].re("c t n -> t c n"), "sp", 0, tst)
            yield
            for d in range(2):
                lw, a, kd, bb, L = X[0], X[1], X[2], X[3], X[4]
                for (t0, tn) in THT:
                    pp = ps[nps % 3]
                    nps += 1
                    p.mm(pp[:, :tn], wupb[:, d, r0:r0 + 128], tzw[d][:, tb + t0:tb + t0 + tn])
                    yield
                    p.act(lw[:, t0:t0 + tn], pp[:, :tn], AF.Sigmoid, bias=pk(C2_W0 + d * 4 + pr))
                    yield
                    pp = ps[nps % 3]
                    nps += 1
                    p.mm(pp[:, :tn], aupb[:, d, r0:r0 + 128], zab[d][:, tb + t0:tb + t0 + tn])
                    yield
                    p.act(a[:, t0:t0 + tn], pp[:, :tn], AF.Sigmoid, bias=pk(C2_A0 + d * 4 + pr))
                    yield
                p.ts(lw, lw, -0.6065306597126334, ALU.mult)
                yield
                p.ts(kd, a, pk(C2_KA + pr), ALU.mult, g.OMKA[:, l, pr:pr + 1], ALU.add)
                yield
                p.tt(kd, kd, kt, ALU.mult)
                yield
                p.tt(bb, kk, a, ALU.mult)
                yield
                if d == 0:
                    p.cp(X[7], kd)
                    yield
                else:
                    p.tt(X[7], X[7], kd, ALU.add)
                    yield
                p.op("dve", lambda e: e.tensor_tensor_scan(out=A(L), data0=A(g.RM), data1=A(lw), initial=0.0,
                                                           op0=ALU.mult, op1=ALU.add), [g.RM, lw], [L])
                if d == 1:
                    Lp = X[1]
                    p.tt(Lp, lw, L, ALU.subtract)
                    yield
                    p.tt(Lp.re("k (c t) -> k c t", t=CH), Lp.re("k (c t) -> k c t", t=CH),
                         L.re("k (c t) -> k c t", t=CH)[:, :, CH - 1:CH].bc([128, NCH2, CH]), ALU.add)
                    end = 0
                else:
                    Lp = L
                    end = CH - 1
                E = X[5]
                p.act(E, Lp, AF.Exp)
                yield
                p.tt(ob["r"], rt, E, ALU.mult)
                yield
                p.act(wc.re("k (c o) -> k c o", o=1), Lp.re("k (c t) -> k c t", t=CH)[:, :, end:end + 1], AF.Exp)
                yield
                E2 = X[6]
                p.act(E2, Lp, AF.Exp, scale=-1.0)
                yield
                p.tt(kd, kd, E2, ALU.mult)
                yield
                p.tt(bb, bb, E2, ALU.mult)
                yield
                p.tt(lw, Lp, lw, ALU.subtract)
                yield
                p.act(lw, lw, AF.Exp)
                yield
                p.tt(ob["kk"], kk, lw, ALU.mult)
                yield
                p.cp(ob["kh"], kd, eng="act")
                yield
                p.cp(ob["bh"], bb, eng="act")
                yield
                wcb = wc.re("k (c o) -> k c o", o=1).bc([128, NCH2, CH])
                p.tt(ob["kp"].re("k (c t) -> k c t", t=CH), kd.re("k (c t) -> k c t", t=CH), wcb, ALU.mult)
                yield
                p.tt(ob["bp"].re("k (c t) -> k c t", t=CH), bb.re("k (c t) -> k c t", t=CH), wcb, ALU.mult)
                yield
                for qi, n in enumerate(("kk", "r", "kh", "bh")):
                    p.dma(g.SCF[d, r0:r0 + 128, cb:cb + NCH2, qi, :], ob[n].re("k (c t) -> k c t", t=CH), q="sp", acc=True)
                    yield
                tposed(ob["kp"], g.SCT[d, cb:cb + NCH2, :, 0, r0:r0 + 128].re("c t n -> t c n"), "sp", 1, tst)
                yield
                tposed(ob["bp"], g.SCT[d, cb:cb + NCH2, :, 1, r0:r0 + 128].re("c t n -> t c n"), "sp", 2, tst)
                yield
                p.dma(g.WCD[r0:r0 + 128, d, cb:cb + NCH2], wc, q="sp", acc=True)
                yield
            p.ts(X[0], rt, pk(C2_RK + pr), ALU.mult)
            yield
            p.tt(X[0], X[0], X[7], ALU.mult)
            yield
            for (t0, tn) in THT:
                pp = ps[nps % 3]
                nps += 1
                p.mm(pp[:, :tn], g.ones2, X[0][:, t0:t0 + tn])
                yield
                p.tt(X[1][:, t0:t0 + tn], pp[:, :tn], vt[:, t0:t0 + tn], ALU.mult)
                yield
                pp = ps[nps % 3]
                nps += 1
                p.mm(pp[:, :tn], gupb[:, r0:r0 + 128], sgz[:, tb + t0:tb + t0 + tn])
                yield
                p.cp(X[2][:, t0:t0 + tn], pp[:, :tn], eng="act")
                yield
            p.dma(g.RO[0, r0:r0 + 128, tb:tb + TH], X[1], q="sp", acc=True)
            yield
            p.dma(g.RO[1, r0:r0 + 128, tb:tb + TH], X[2], q="sp", acc=True)
            yield

        for pr in range(4):
            gens = [body(pr, 0), body(pr, 1)]
            live = [True, True]
            while any(live):
                for q in range(2):
                    if live[q]:
                        try:
                            next(gens[q])
                        except StopIteration:
                            live[q] = False
    p.barrier()


ORDER_B = [1, 0] + list(range(NCH - 1, 1, -1))


def stage_scan(p, g, l, b):
    with contextlib.ExitStack() as st:
        B = [p.ps(st, "e_b%d" % i, [128, 512]) for i in range(8)]

        def bv(i, n, w, parts=128):
            return B[i].re("s (a t) -> s a t", t=w)[0:parts, 0:n, :]
        WC = p.sb(st, "e_wc", [64, 2, 8, NCH])
        p.dma(WC, g.WCD.re("(h k) d c -> k d h c", k=64))
        ST = p.sb(st, "e_st", [64, 16, 64])
        STb = p.sb(st, "e_stb", [64, 16, 64], BF16)
        p.memset(ST, 0.0)
        p.memset(STb, 0.0, eng="pool")
        FQ = [p.sb(st, "e_fq%d" % i, [64, 2, 8, 4, 128], BF16) for i in range(2)]
        TQ = [p.sb(st, "e_tq%d" % i, [128, 2, 2, 8, 64], BF16) for i in range(2)]
        VT = [p.sb(st, "e_vt%d" % i, [128, 2, 8, 64], BF16) for i in range(2)]
        AMs = [p.sb(st, "e_am%d" % i, [128, 16, 4, 128], BF16) for i in range(2)]
        Xs = [[p.sb(st, "e_x%d_%d" % (i, q), [128, 4, 128]) for q in range(4)] for i in range(2)]
        XTs = [[p.sb(st, "e_xt%d_%d" % (i, q), [128, 4, 128]) for q in range(4)] for i in range(2)]
        Ps = [[p.sb(st, "e_p%d_%d" % (i, q), [128, 4, 128]) for q in range(4)] for i in range(2)]
        RT = p.sb(st, "e_rt", [128, 16, 64])
        PF = [[p.sb(st, "e_pf%d_%d" % (i, q), [128, 4, 128]) for q in range(4)] for i in range(2)]
        nUT = p.sb(st, "e_nut", [128, 16, 64], BF16)
        YS = [p.sb(st, "e_ys%d" % i, [64, 16, 128]) for i in range(2)]
        idb = g.ident.re("s (o t) -> s o t", o=1).bc([128, 4, 128])
        def loads(j):
            cd = (j, ORDER_B[j])
            fq, tq, vt = FQ[j % 2], TQ[j % 2], VT[j % 2]
            for d in range(2):
                c = cd[d]
                p.dma(fq[:, d], g.SCF[d, :, c].re("(h k) q t -> k h q t", k=64), q="sp")
                p.dma(tq[:, d], g.SCT[d, c].re("t q (h k) -> t q h k", k=64), q="act")
                p.dma(vt[:, d], g.VTD[c].re("t (h k) -> t h k", k=64), q="sp")

        def phase1(j):
            fq, AMb = FQ[j % 2], AMs[j % 2]
            for ci in range(16):
                d, h = divmod(ci, 8)
                pa = B[3 + ci % 2]
                rhs = fq[:, d, h, 0:2, :]
                p.mm(pa.re("s (q t) -> s q t", t=128)[:, 0:2, :], fq[:, d, h, 2, :], rhs)
                p.mm(pa.re("s (q t) -> s q t", t=128)[:, 2:4, :], fq[:, d, h, 3, :], rhs)
                pn = bv(5, 4, 128)
                p.mm(pn[:, ci % 4, :], fq[:, d, h, 0, :], fq[:, d, h, 3, :])
                p.tt(AMb[:, ci], pa.re("s (q t) -> s q t", t=128), g.MASK4[:, d], ALU.mult)
                p.tt(Xs[0][ci // 4][:, ci % 4, :], pa[:, 256:384], g.MASK4[:, d, 0, :], ALU.mult)
                if ci % 4 == 3:
                    p.tt(XTs[0][ci // 4], pn, g.MASK4[:, 1 - d, 0:1, :].bc([128, 4, 128]), ALU.mult)

        def phase2(j):
            for gq in range(4):
                p.tt(Ps[0][gq], idb, Xs[0][gq], ALU.subtract, eng="pool")
            cur = 0
            for lev in range(6):
                last = lev == 5
                if last:
                    for gq in range(4):
                        X, XT = Xs[cur][gq], XTs[cur][gq]
                        bs = 3 * (gq % 2)
                        for q in range(4):
                            p.mm(bv(bs + 1, 4, 128)[:, q, :], X[:, q, :], XT[:, q, :])
                        p.cp(XTs[1 - cur][gq], bv(bs + 1, 4, 128), eng="dve")
                else:
                    for gq in range(4):
                        X, XT = Xs[cur][gq], XTs[cur][gq]
                        bs = 3 * (gq % 2)
                        for q in range(4):
                            p.mm(bv(bs, 4, 128)[:, q, :], XT[:, q, :], X[:, q, :])
                        p.cp(Xs[1 - cur][gq], bv(bs, 4, 128), eng="act")
                    for gq in range(4):
                        bs = 3 * (gq % 2)
                        for q in range(4):
                            p.tr(bv(bs + 1, 4, 128)[:, q, :], Xs[1 - cur][gq][:, q, :], g.ident)
                        p.cp(XTs[1 - cur][gq], bv(bs + 1, 4, 128), eng="dve")
                for gq in range(4):
                    bs = 3 * (gq % 2)
                    for q in range(4):
                        p.mm(bv(bs + 2, 4, 128)[:, q, :], XTs[1 - cur][gq][:, q, :], Ps[cur][gq][:, q, :])
                    dstp = PF[j % 2][gq] if last else Ps[1 - cur][gq]
                    p.tt(dstp, bv(bs + 2, 4, 128), Ps[cur][gq], ALU.add)
                cur = 1 - cur
                yield lev

        def phase3(j):
            cd = (j, ORDER_B[j])
            fq, tq, vt, ys, AMb, Pf = FQ[j % 2], TQ[j % 2], VT[j % 2], YS[j % 2], AMs[j % 2], PF[j % 2]
            for ci in range(16):
                d, h = divmod(ci, 8)
                pr = bv(6 + ci // 8, 8, 64)[:, ci % 8, :]
                p.mm(pr, fq[:, d, h, 0, :], STb[:, ci, :], start=True, stop=False)
                p.mm(pr, AMb[:, ci, 0, :], vt[:, d, h, :], start=False, stop=True)
            p.cp(RT[:, 0:8, :], bv(6, 8, 64), eng="act")
            p.cp(RT[:, 8:16, :], bv(7, 8, 64), eng="dve")
            yield 0
            for ci in range(16):
                pr = bv(6 + ci // 8, 8, 64)[:, ci % 8, :]
                p.mm(pr, Pf[ci // 4][:, ci % 4, :], RT[:, ci, :])
            p.ts(nUT[:, 0:8, :], bv(6, 8, 64), -1.0, ALU.mult)
            p.op("act", lambda e: e.mul(out=A(nUT[:, 8:16, :]), in_=A(bv(7, 8, 64)), mul=-1.0), [B[7]], [nUT])
            yield 1
            for q4 in range(4):
                for q in range(4):
                    ci = 4 * q4 + q
                    d, h = divmod(ci, 8)
                    pv = bv(6 + q4 % 2, 4, 128, 64)[:, q, :]
                    p.mm(pv, STb[:, ci, :], fq[:, d, h, 1, :], start=True, stop=False)
                    p.mm(pv, vt[:, d, h, :], AMb[:, ci, 1, :], start=False, stop=False)
                    p.mm(pv, nUT[:, ci, :], AMb[:, ci, 3, :], start=False, stop=True)
                p.cp(ys[:, 4 * q4:4 * q4 + 4, :], bv(6 + q4 % 2, 4, 128, 64), eng=("act" if q4 % 2 else "dve"))
            for d in range(2):
                c = cd[d]
                p.dma(g.YD[d, :, c * CH:(c + 1) * CH].re("(h k) t -> k h t", k=64), ys[:, d * 8:(d + 1) * 8, :], q="sp", acc=True)
            yield 2
            for ci in range(16):
                d, h = divmod(ci, 8)
                pv = bv(6 + d, 8, 64, 64)[:, ci % 8, :]
                p.mm(pv, tq[:, d, 0, h, :], vt[:, d, h, :], start=True, stop=False)
                p.mm(pv, tq[:, d, 1, h, :], nUT[:, ci, :], start=False, stop=True)
            for d in range(2):
                wcv = WC[:, d, :, cd[d]:cd[d] + 1].bc([64, 8, 64])
                p.tt(ST[:, d * 8:(d + 1) * 8, :], ST[:, d * 8:(d + 1) * 8, :], wcv, ALU.mult, eng="pool")
                p.tt(ST[:, d * 8:(d + 1) * 8, :], ST[:, d * 8:(d + 1) * 8, :], bv(6 + d, 8, 64, 64), ALU.add)
            p.cp(STb, ST, eng="act")
            yield 3

        loads(0)
        phase1(0)
        for _ in phase2(0):
            pass
        for j in range(NCH):
            g3 = phase3(j)
            if j + 1 < NCH:
                loads(j + 1)
                phase1(j + 1)
                for lev in phase2(j + 1):
                    if lev < 4:
                        next(g3)
            for _ in g3:
                pass
    p.barrier()


def stage_mix(p, g, l, b, CO, src, last_dst):
    with contextlib.ExitStack() as st:
        wa = p.sb(st, "m_wa", [128, 4, 1024], BF16)
        wb = p.sb(st, "m_wb", [64, 8, 1024], BF16)
        wo = p.sb(st, "m_wo", [128, 8, 1024], BF16)
        G1b = []
        for kind, row in ((0, 2), (1, b)):
            t = p.sb(st, "m_g1b%d" % kind, [128, D])
            p.dma(t, g.MODR[l, row, G1:G1 + D].pb(128), q="act")
            G1b.append(t)
        p.dma(wa, g.w_a_out[l].re("(c q) n -> q c n", q=128), q="pool")
        p.dma(wb, g.w_b_out[l].re("(h k) n -> k h n", k=64), q="pool")
        p.dma(wo, g.w_o[l].re("(c q) n -> q c n", q=128), q="pool")
        y0 = p.sb(st, "m_y0", [64, 8, 512])
        y1 = p.sb(st, "m_y1", [64, 8, 512])
        aux = p.sb(st, "m_aux", [64, 8, 512])
        ybin = p.sb(st, "m_ybin", [64, 8, 512], BF16)
        sgat = [p.sb(st, "m_sga%d" % i, [128, 512]) for i in range(2)]
        sgbt = [p.sb(st, "m_sgb%d" % i, [128, 512]) for i in range(2)]
        m1 = [p.sb(st, "m_m1%d" % i, [128, 512]) for i in range(1)] * 2
        m2 = [p.sb(st, "m_m2%d" % i, [128, 512]) for i in range(1)] * 2
        mrg = p.sb(st, "m_mrg", [128, 8, 512], BF16)
        xt = [p.sb(st, "m_xt%d" % i, [128, D]) for i in range(2)]
        xo = [p.sb(st, "m_xo%d" % i, [128, D]) for i in range(2)]
        B = [p.ps(st, "m_b%d" % i, [128, 512]) for i in range(8)]
        nb = 0
        nx = 0
        for (t0, tn) in TT:
            for d, yt in ((0, y0), (1, y1)):
                p.dma(yt[:, :, :tn], g.YD[d, :, t0:t0 + tn].re("(h k) t -> k h t", k=64), q=("sp" if d == 0 else "act"))
            p.tt(y0[:, :, :tn], y0[:, :, :tn], y1[:, :, :tn], ALU.add)
            for h in range(8):
                pm = B[nb % 8]
                nb += 1
                p.mm(pm[0:64, :tn], g.ones64, y0[:, h, :tn])
                p.stt(y1[:, h, :tn], pm[0:64, :tn], -1.0 / 64, y0[:, h, :tn], ALU.mult, ALU.add)
            p.act(y0[:, :, :tn], y1[:, :, :tn], AF.Square)
            for h in range(8):
                pm = B[nb % 8]
                nb += 1
                p.mm(pm[0:64, :tn], g.ones64, y0[:, h, :tn])
                p.ts(y0[:, h, :tn], pm[0:64, :tn], 1.0 / 64, ALU.mult, GN_EPS, ALU.add)
            p.act(y0[:, :, :tn], y0[:, :, :tn], AF.Sqrt)
            p.recip(y0[:, :, :tn], y0[:, :, :tn])
            p.tt(y1[:, :, :tn], y1[:, :, :tn], y0[:, :, :tn], ALU.mult)
            for h in range(8):
                p.ts(y1[:, h, :tn], y1[:, h, :tn], g.P64T[:, l, C_LG + h:C_LG + h + 1], ALU.mult,
                     g.P64T[:, l, C_LB + h:C_LB + h + 1], ALU.add)
            p.dma(aux[:, :, :tn], g.RO[0, :, t0:t0 + tn].re("(h k) t -> k h t", k=64), q="sp")
            p.tt(y1[:, :, :tn], y1[:, :, :tn], aux[:, :, :tn], ALU.add)
            p.dma(aux[:, :, :tn], g.RO[1, :, t0:t0 + tn].re("(h k) t -> k h t", k=64), q="sp")
            p.tt(ybin[:, :, :tn], y1[:, :, :tn], aux[:, :, :tn], ALU.mult)
            for cc in range(8):
                sga, sgb = sgat[cc % 2], sgbt[cc % 2]
                p.dma(sga[:, :tn], g.ZF[3456 + cc * 128:3456 + (cc + 1) * 128, t0:t0 + tn], q="sp")
                p.dma(sgb[:, :tn], g.ZF[4480 + cc * 128:4480 + (cc + 1) * 128, t0:t0 + tn], q="act")
                pa = B[nb % 8]
                nb += 1
                for jj in range(4):
                    p.mm(pa[:, :tn], wa[:, jj, cc * 128:(cc + 1) * 128], CO[:, jj, t0:t0 + tn], start=(jj == 0), stop=(jj == 3))
                pb = B[nb % 8]
                nb += 1
                for h in range(8):
                    p.mm(pb[:, :tn], wb[:, h, cc * 128:(cc + 1) * 128], ybin[:, h, :tn], start=(h == 0), stop=(h == 7))
                p.tt(m1[cc % 2][:, :tn], pa[:, :tn], sga[:, :tn], ALU.mult)
                p.tt(m2[cc % 2][:, :tn], pb[:, :tn], sgb[:, :tn], ALU.mult)
                p.tt(mrg[:, cc, :tn], m1[cc % 2][:, :tn], m2[cc % 2][:, :tn], ALU.add)
            for sub in range(tn // 128):
                tok = t0 + sub * 128
                kind = 0 if tok < TCTX else 1
                x, o = xt[nx % 2], xo[nx % 2]
                nx += 1
                p.dma(x, src[tok:tok + 128, :], q="sp")
                for hc in range(2):
                    po = B[nb % 8]
                    nb += 1
                    for cc in range(8):
                        p.mm(po, mrg[:, cc, sub * 128:(sub + 1) * 128], wo[:, cc, hc * 512:(hc + 1) * 512],
                             start=(cc == 0), stop=(cc == 7))
                    p.tt(o[:, hc * 512:(hc + 1) * 512], po, G1b[kind][:, hc * 512:(hc + 1) * 512], ALU.mult)
                p.tt(o, o, x, ALU.add)
                p.dma(last_dst[tok:tok + 128, :], o, q="pool", acc=True)
    p.barrier()


def stage_router(p, g, l, hT2, GW, DEST):
    with contextlib.ExitStack() as st:
        rwf = p.sb(st, "r_wf", [128, 8, 36])
        rwb = p.sb(st, "r_wb", [128, 8, 36], BF16)
        p.dma(rwf[:, :, 0:4], g.router_g[l].re("(c q) n -> q c n", q=128))
        p.dma(rwf[:, :, 4:36], g.router_e[l].re("(c q) n -> q c n", q=128))
        p.cp(rwb, rwf)
        RB = p.sb(st, "r_rb", [128, 36])
        p.dma(RB[:, 0:4], g.router_g_b[l].pb(128))
        p.dma(RB[:, 4:36], g.router_e_b[l].pb(128))
        LG = p.sb(st, "r_lg", [128, NT, 36])
        pl = [p.ps(st, "r_pl%d" % i, [128, 36]) for i in range(2)]
        for i in range(NT):
            pp = pl[i % 2]
            for c in range(8):
                p.mm(pp, hT2[:, c, i * 128:(i + 1) * 128], rwb[:, c, :], start=(c == 0), stop=(c == 7))
            p.cp(LG[:, i, :], pp, eng=("act" if i % 2 else "dve"))
        p.tt(LG, LG, RB.re("q (o e) -> q o e", o=1).bc([128, NT, 36]), ALU.add)
        lg = LG[:, :, 0:4]
        le = LG[:, :, 4:36].re("q i (g e) -> q i g e", e=8)
        mg = p.sb(st, "r_mg", [128, NT])
        oh = p.sb(st, "r_oh", [128, NT, 4])
        eg = p.sb(st, "r_eg", [128, NT, 4])
        pg = p.sb(st, "r_pg", [128, NT])
        tmp = p.sb(st, "r_tmp", [128, NT, 4, 8])
        les = p.sb(st, "r_les", [128, NT, 8])
        les2 = p.sb(st, "r_les2", [128, NT, 8])
        m1 = p.sb(st, "r_m1", [128, NT])
        m2 = p.sb(st, "r_m2", [128, NT])
        k1 = p.sb(st, "r_k1", [128, NT, 8])
        k2 = p.sb(st, "r_k2", [128, NT, 8])
        ex = p.sb(st, "r_ex", [128, NT, 8])

        def b3(t, n):
            return t.re("q (i o) -> q i o", o=1).bc([128, NT, n])
        p.red(mg, lg, ALU.max)
        p.tt(oh, lg, b3(mg, 4), ALU.is_equal)
        p.tt(eg, lg, b3(mg, 4), ALU.subtract)
        p.act(eg, eg, AF.Exp)
        p.red(pg, eg, ALU.add)
        p.recip(pg, pg)
        p.tt(tmp, le, oh.re("q i (g o) -> q i g o", o=1).bc([128, NT, 4, 8]), ALU.mult)
        p.red(les, tmp.re("q i g e -> q i e g"), ALU.add)
        p.red(m1, les, ALU.max)
        p.tt(k1, les, b3(m1, 8), ALU.is_equal)
        p.stt(les2, k1, -1e30, les, ALU.mult, ALU.add)
        p.red(m2, les2, ALU.max)
        p.tt(k2, les2, b3(m2, 8), ALU.is_equal)
        p.tt(m2, m2, m1, ALU.subtract)
        p.act(m2, m2, AF.Exp)
        p.ts(m2, m2, 1.0, ALU.add)
        p.recip(m2, m2)
        p.tt(GW[:, :, 0], m2, pg, ALU.mult)
        p.tt(GW[:, :, 1], pg, GW[:, :, 0], ALU.subtract)
        ohb = oh.re("q i (g o) -> q i g o", o=1).bc([128, NT, 4, 8])
        M1 = p.sb(st, "r_M1", [128, NT, 4, 8])
        M2 = p.sb(st, "r_M2", [128, NT, 4, 8])
        MM = p.sb(st, "r_MM", [128, NT, 4, 8])
        p.tt(M1, ohb, k1.re("q i (o e) -> q i o e", o=1).bc([128, NT, 4, 8]), ALU.mult)
        p.tt(M2, ohb, k2.re("q i (o e) -> q i o e", o=1).bc([128, NT, 4, 8]), ALU.mult)
        p.tt(MM, M1, M2, ALU.add)
        MMf = MM.re("q i g e -> q (i g e)")
        WI = p.sb(st, "r_WI", [128, NT, 32])
        TOT = p.sb(st, "r_TOT", [128, NT, 32])
        pw = [p.ps(st, "r_pw%d" % i, [128, 512]) for i in range(4)]
        NF = NT * 32
        for (c0, cn, k) in ((0, 512, 0), (512, NF - 512, 1)):
            p.mm(pw[k][:, :cn], g.MASK4[:, 0, 0, :], MMf[:, c0:c0 + cn])
            p.cp(WI.re("q i e -> q (i e)")[:, c0:c0 + cn], pw[k][:, :cn], eng="act")
            p.mm(pw[2 + k][:, :cn], g.onesf, MMf[:, c0:c0 + cn])
            p.cp(TOT.re("q i e -> q (i e)")[:, c0:c0 + cn], pw[2 + k][:, :cn], eng="dve")
        BASE = p.sb(st, "r_BASE", [128, NT, 32])
        p.memset(BASE[:, 0, :], 0.0)
        for i in range(1, NT):
            p.tt(BASE[:, i, :], BASE[:, i - 1, :], TOT[:, i - 1, :], ALU.add)
        p.tt(WI, WI, BASE, ALU.add)
        p.ts(TOT, WI, CAP - 0.5, ALU.is_ge)
        p.tt(WI, WI, g.EOFF.re("q (o e) -> q o e", o=1).bc([128, NT, 32]), ALU.add)
        p.ts(BASE, TOT, -1.0, ALU.mult, 1.0, ALU.add)
        p.tt(WI, WI, BASE, ALU.mult)
        p.stt(WI, TOT, float(NEXP * CAP), WI, ALU.mult, ALU.add)
        DF = p.sb(st, "r_DF", [128, NT, 2])
        for k, Mk in ((0, M1), (1, M2)):
            p.tt(Mk.re("q i g e -> q i (g e)"), Mk.re("q i g e -> q i (g e)"), WI, ALU.mult)
            p.red(DF[:, :, k], Mk.re("q i g e -> q i (g e)"), ALU.add)
        p.cp(DEST, DF)
    p.barrier()


def stage_moe(p, g, l, b, GW, DEST):
    NR = NEXP * CAP
    IOA = bass.IndirectOffsetOnAxis
    with contextlib.ExitStack() as st:
        G2b = []
        for kind, row in ((0, 2), (1, b)):
            t = p.sb(st, "e_g2b%d" % kind, [128, D])
            p.dma(t, g.MODR[l, row, G2:G2 + D].pb(128), q="act")
            G2b.append(t)
        with contextlib.ExitStack() as st2:
            hbt = [p.sb(st2, "e_hbt%d" % i, [128, D], BF16) for i in range(2)]
            for i in range(NT):
                hb = hbt[i % 2]
                p.dma(hb, g.H2T[i * 128:(i + 1) * 128, :], q="sp")
                for k in range(2):
                    idx = DEST[:, i, k:k + 1]
                    p.dma(g.XE, hb, q="pool", acc=True, extra_reads=[DEST],
                          fn=lambda e, hb=hb, idx=idx: e.indirect_dma_start(
                              out=A(g.XE), out_offset=IOA(ap=A(idx), axis=0), in_=A(hb), in_offset=None))
            w1b = [p.sb(st2, "e_w1b%d" % i, [128, 8, 512], BF16) for i in range(2)]
            w3b = [p.sb(st2, "e_w3b%d" % i, [128, 8, 512], BF16) for i in range(2)]
            w2b = [p.sb(st2, "e_w2b%d" % i, [128, 4, 1024], BF16) for i in range(2)]
            xe = [p.sb(st2, "e_xe%d" % i, [128, NSL, D], BF16) for i in range(2)]
            hTe = p.sb(st2, "e_hTe", [128, 8, CAP], BF16)
            sl = [p.sb(st2, "e_sl%d" % i, [128, 512]) for i in range(2)]
            hid = p.sb(st2, "e_hid", [128, 4, CAP], BF16)
            ye = [p.sb(st2, "e_ye%d" % i, [128, NSL, D]) for i in range(2)]
            B = [p.ps(st2, "e_pb%d" % i, [128, 512]) for i in range(6)]
            pt = [p.ps(st2, "e_pt%d" % i, [128, 8, 128], BF16) for i in range(2)]
            nb = 0
            ns = 0
            nsl = 0
            npt = 0
            CT = [(0, 512), (512, CAP - 512)] if CAP > 512 else [(0, CAP)]
            def load_w(e):
                k = e % 2
                p.dma(w1b[k], g.exp_w1[l, e].re("(c q) n -> q c n", q=128), q="pool")
                p.dma(w3b[k], g.exp_w3[l, e].re("(c q) n -> q c n", q=128), q="pool")
                p.dma(w2b[k], g.exp_w2[l, e].re("(c q) n -> q c n", q=128), q="pool")
                p.dma(xe[k], g.XE[e * CAP:(e + 1) * CAP, :].re("(j q) n -> q j n", q=128), q="sp")
            load_w(0)
            for e in range(NEXP):
                k = e % 2
                if e + 1 < NEXP:
                    load_w(e + 1)
                x_ = xe[k]
                for j in range(NSL):
                    ptt = pt[npt % 2]
                    npt += 1
                    for c in range(8):
                        p.tr(ptt[:, c, :], x_[:, j, c * 128:(c + 1) * 128], g.identb)
                    p.cp(hTe[:, :, j * 128:(j + 1) * 128], ptt, eng=("act" if j % 2 else "dve"))
                for ff in range(4):
                    for (t0, tn) in CT:
                        p1 = B[nb % 6]
                        p3 = B[(nb + 1) % 6]
                        nb += 2
                        for kc in range(8):
                            p.mm(p1[:, :tn], w1b[k][:, kc, ff * 128:(ff + 1) * 128], hTe[:, kc, t0:t0 + tn],
                                 start=(kc == 0), stop=(kc == 7))
                        for kc in range(8):
                            p.mm(p3[:, :tn], w3b[k][:, kc, ff * 128:(ff + 1) * 128], hTe[:, kc, t0:t0 + tn],
                                 start=(kc == 0), stop=(kc == 7))
                        s_ = sl[nsl % 2]
                        nsl += 1
                        p.act(s_[:, :tn], p1[:, :tn], AF.Silu)
                        p.tt(hid[:, ff, t0:t0 + tn], s_[:, :tn], p3[:, :tn], ALU.mult)
                y_ = ye[k]
                for j in range(NSL):
                    for hc in range(2):
                        po = B[nb % 6]
                        nb += 1
                        for ff in range(4):
                            p.mm(po, hid[:, ff, j * 128:(j + 1) * 128], w2b[k][:, ff, hc * 512:(hc + 1) * 512],
                                 start=(ff == 0), stop=(ff == 3))
                        p.cp(y_[:, j, hc * 512:(hc + 1) * 512], po, eng=("act" if (j + hc) % 2 else "dve"))
                p.dma(g.YE[e * CAP:(e + 1) * CAP, :].re("(j q) n -> q j n", q=128), y_, q="sp", acc=True)
            p.barrier()
        ya = [p.sb(st, "e_ya%d" % i, [128, D]) for i in range(2)]
        yb = [p.sb(st, "e_yb%d" % i, [128, D]) for i in range(2)]
        xt = [p.sb(st, "e_xt%d" % i, [128, D]) for i in range(2)]
        for i in range(NT):
            tok = i * 128
            kind = 0 if i < 2 else 1
            a_, b_, x_ = ya[i % 2], yb[i % 2], xt[i % 2]
            p.dma(x_, g.XS[b][tok:tok + 128, :], q="sp")
            for k, dst in ((0, a_), (1, b_)):
                p.memset(dst, 0.0)
                idx = DEST[:, i, k:k + 1]
                p.dma(dst, g.YE, q="pool", extra_reads=[DEST],
                      fn=lambda e, dst=dst, idx=idx: e.indirect_dma_start(
                          out=A(dst), out_offset=None, in_=A(g.YE), in_offset=IOA(ap=A(idx), axis=0)))
            p.ts(a_, a_, GW[:, i, 0:1], ALU.mult)
            p.stt(a_, b_, GW[:, i, 1:2], a_, ALU.mult, ALU.add)
            p.tt(a_, a_, G2b[kind], ALU.mult)
            p.tt(a_, a_, x_, ALU.add)
            p.dma(g.XS[b][tok:tok + 128, :], a_, q="act", acc=True)
    p.barrier()


def stage_final(p, g, b):
    with contextlib.ExitStack() as st:
        gt = p.sb(st, "f_gt", [128, D])
        p.dma(gt, g.final_g.pb(128))
        xts = [p.sb(st, "f_xt%d" % i, [128, D]) for i in range(2)]
        sqs = [p.sb(st, "f_sq%d" % i, [128, D]) for i in range(2)]
        sss = [p.sb(st, "f_ss%d" % i, [128, 2]) for i in range(2)]
        for i in range(TLAT // 128):
            xt, sq, ss = xts[i % 2], sqs[i % 2], sss[i % 2]
            p.dma(xt, g.XS[b][TCTX + i * 128:TCTX + (i + 1) * 128, :], q=("sp" if i % 2 == 0 else "act"))
            p.act(sq, xt, AF.Square)
            p.red(ss[:, 0:1], sq, ALU.add)
            p.ts(ss[:, 1:2], ss[:, 0:1], 1.0 / D, ALU.mult, EPS, ALU.add)
            p.act(ss[:, 1:2], ss[:, 1:2], AF.Sqrt)
            p.recip(ss[:, 1:2], ss[:, 1:2])
            p.stt(sq, xt, ss[:, 1:2], gt, ALU.mult, ALU.mult)
            p.dma(g.out[b, i * 128:(i + 1) * 128, :], sq, q="pool", acc=True)
    p.barrier()
```
